# Optimizing a Trainium2 kernel written in Bass

```python
import math
import jax, jax.numpy as jnp
from jax import lax
import numpy as np


D_MODEL = 2048
BATCH = 8
SEQ = 2048
DEPTH = 1

A_HEADS = 8
A_HEAD_DIM = 128
A_WIDTH = A_HEADS * A_HEAD_DIM
MOBA_BLOCK = 256
MOBA_TOPK = 3
MOBA_Q_CHUNK = 16
REL_BUCKETS = 32
REL_MAX_DIST = 128
B_HEADS = 8
B_KEY_DIM = 128
B_VAL_DIM = 128
B_KW = B_HEADS * B_KEY_DIM
B_VW = B_HEADS * B_VAL_DIM
CONV_WIDTH = 4
DELTA_CHUNK = 64
PEER_HEADS = 8
PEER_NKEYS = 128
PEER_EXPERTS = PEER_NKEYS * PEER_NKEYS
PEER_QDIM = 256
PEER_TOPK = 16
PEER_TOK_CHUNK = 128
EPS = 1e-6
IN_SIZES = (A_WIDTH, A_WIDTH, A_WIDTH, 2 * B_KW + B_VW, B_VW, B_HEADS, B_HEADS, D_MODEL, D_MODEL)
IN_TOTAL = 3 * A_WIDTH + 2 * B_KW + 2 * B_VW + 2 * B_HEADS + 2 * D_MODEL

kernel_name = 'hybrid_moba_gdn_peer_block'


def rms_norm(x, gain):
    xf = x.astype(jnp.float32)
    y = xf * lax.rsqrt(jnp.mean(xf * xf, axis=-1, keepdims=True) + EPS)
    return (y * gain.astype(jnp.float32)).astype(x.dtype)


def l2_norm(x):
    xf = x.astype(jnp.float32)
    return (xf * lax.rsqrt(jnp.sum(xf * xf, axis=-1, keepdims=True) + EPS)).astype(x.dtype)


def t5_bucket(rel):
    n = jnp.maximum(-rel, 0)
    max_exact = REL_BUCKETS // 2
    nf = jnp.maximum(n, 1).astype(jnp.float32)
    large = max_exact + (jnp.log(nf / max_exact) / math.log(REL_MAX_DIST / max_exact)
                         * (REL_BUCKETS - max_exact)).astype(jnp.int32)
    large = jnp.minimum(large, REL_BUCKETS - 1)
    return jnp.where(n < max_exact, n, large)


def moba_attention(q, k, v, rel_bias):
    B, H, S, dh = q.shape
    nb = -(-S // MOBA_BLOCK)
    pad = nb * MOBA_BLOCK - S
    kb = jnp.pad(k, ((0, 0), (0, 0), (0, pad), (0, 0))).reshape(B, H, nb, MOBA_BLOCK, dh)
    vb = jnp.pad(v, ((0, 0), (0, 0), (0, pad), (0, 0))).reshape(B, H, nb, MOBA_BLOCK, dh)
    kmean = jnp.mean(kb.astype(jnp.float32), axis=3)
    qblk = jnp.arange(S) // MOBA_BLOCK
    gate = jnp.einsum('bhsd,bhnd->bhsn', q.astype(jnp.float32), kmean)
    past = jnp.arange(nb)[None, :] < qblk[:, None]
    gate = jnp.where(past, gate, -jnp.inf)
    topk = min(MOBA_TOPK, nb)
    gsel, sel = lax.top_k(gate, topk)
    valid = jnp.isfinite(gsel)
    bi = jnp.arange(B)[:, None, None, None]
    hi = jnp.arange(H)[None, :, None, None]
    offs = jnp.arange(MOBA_BLOCK)
    scale = dh ** -0.5
    bias_hb = rel_bias.T.astype(jnp.float32)
    QC = MOBA_Q_CHUNK

    def one_chunk(c):
        start = c * QC
        qc = lax.dynamic_slice_in_dim(q, start, QC, axis=2)
        selc = lax.dynamic_slice_in_dim(sel, start, QC, axis=2)
        validc = lax.dynamic_slice_in_dim(valid, start, QC, axis=2)
        qpos = start + jnp.arange(QC)
        own = start // MOBA_BLOCK
        k_sel = kb[bi, hi, selc]
        v_sel = vb[bi, hi, selc]
        s_sel = jnp.einsum('bhqd,bhqkjd->bhqkj', qc, k_sel).astype(jnp.float32) * scale
        kpos_sel = selc[..., None] * MOBA_BLOCK + offs
        bias_sel = bias_hb[hi[..., None], t5_bucket(kpos_sel - qpos[None, None, :, None, None])]
        s_sel = jnp.where(validc[..., None], s_sel + bias_sel, -jnp.inf).reshape(B, H, QC, topk * MOBA_BLOCK)
        k_own = lax.dynamic_index_in_dim(kb, own, axis=2, keepdims=False)
        v_own = lax.dynamic_index_in_dim(vb, own, axis=2, keepdims=False)
        rel_own = (own * MOBA_BLOCK + offs)[None, :] - qpos[:, None]
        s_own = (jnp.einsum('bhqd,bhjd->bhqj', qc, k_own).astype(jnp.float32) * scale
                 + bias_hb[:, t5_bucket(rel_own)][None])
        s_own = jnp.where(rel_own <= 0, s_own, -jnp.inf)
        p = jax.nn.softmax(jnp.concatenate([s_sel, s_own], axis=-1), axis=-1)
        p_sel = p[..., :topk * MOBA_BLOCK].reshape(B, H, QC, topk, MOBA_BLOCK).astype(v.dtype)
        p_own = p[..., topk * MOBA_BLOCK:].astype(v.dtype)
        return (jnp.einsum('bhqkj,bhqkjd->bhqd', p_sel, v_sel)
                + jnp.einsum('bhqj,bhjd->bhqd', p_own, v_own))

    out = lax.map(one_chunk, jnp.arange(S // QC))
    return out.transpose(1, 2, 0, 3, 4).reshape(B, H, S, dh)


def causal_depthwise_conv(x, w):
    C = x.shape[-1]
    return lax.conv_general_dilated(x, w[:, None, :].astype(x.dtype), window_strides=(1,),
                                    padding=[(CONV_WIDTH - 1, 0)],
                                    dimension_numbers=('NWC', 'WIO', 'NWC'),
                                    feature_group_count=C)


def gated_delta_rule(q, k, v, g, beta):
    B, H, S, dk = q.shape
    dv = v.shape[-1]
    C = DELTA_CHUNK
    N = S // C
    f32 = jnp.float32
    qc = (q.astype(f32) * dk ** -0.5).reshape(B, H, N, C, dk)
    kc = k.astype(f32).reshape(B, H, N, C, dk)
    vc = v.astype(f32).reshape(B, H, N, C, dv)
    bc = beta.astype(f32).reshape(B, H, N, C)
    gcum = jnp.cumsum(g.astype(f32).reshape(B, H, N, C), axis=-1)
    idx = jnp.arange(C)
    causal = idx[:, None] >= idx[None, :]
    strict = idx[:, None] > idx[None, :]
    decay = jnp.exp(jnp.where(causal, gcum[..., :, None] - gcum[..., None, :], -jnp.inf))
    kbeta = kc * bc[..., None]
    vbeta = vc * bc[..., None]
    L = jnp.where(strict, jnp.einsum('bhnid,bhnjd->bhnij', kbeta, kc) * decay, 0.0)
    eye = jnp.eye(C, dtype=f32)
    T = lax.linalg.triangular_solve(eye + L, jnp.broadcast_to(eye, L.shape), left_side=True,
                                    lower=True, unit_diagonal=True)
    u = T @ vbeta
    w = T @ (kbeta * jnp.exp(gcum)[..., None])
    attn = jnp.einsum('bhnid,bhnjd->bhnij', qc, kc) * decay
    q_dec = qc * jnp.exp(gcum)[..., None]
    g_last = gcum[..., -1]
    k_dec = kc * jnp.exp(g_last[..., None] - gcum)[..., None]

    def step(state, inp):
        u_n, w_n, attn_n, qd_n, kd_n, gl_n = inp
        v_new = u_n - w_n @ state
        o = qd_n @ state + attn_n @ v_new
        state = state * jnp.exp(gl_n)[..., None, None] + jnp.swapaxes(kd_n, -1, -2) @ v_new
        return state, o

    xs = tuple(jnp.moveaxis(a, 2, 0) for a in (u, w, attn, q_dec, k_dec, g_last))
    s0 = jnp.zeros((B, H, dk, dv), f32)
    _, o = lax.scan(step, s0, xs)
    return o.transpose(1, 2, 0, 3, 4).reshape(B, H, S, dv)


def token_mixers(h, rel_bias, w_in, conv_w, a_log, dt_bias, q_norm_gain, k_norm_gain,
                 gdn_norm_gain, w_up_a, w_up_b, w_out):
    B, S, _ = h.shape
    proj = h @ w_in
    offsets = np.cumsum(IN_SIZES)[:-1].tolist()
    qa, ka, va, qkv_b, z_b, beta_raw, alpha_raw, gate_a, gate_b = jnp.split(proj, offsets, axis=-1)
    qa = rms_norm(qa.reshape(B, S, A_HEADS, A_HEAD_DIM), q_norm_gain).transpose(0, 2, 1, 3)
    ka = rms_norm(ka.reshape(B, S, A_HEADS, A_HEAD_DIM), k_norm_gain).transpose(0, 2, 1, 3)
    va = va.reshape(B, S, A_HEADS, A_HEAD_DIM).transpose(0, 2, 1, 3)
    o_a = moba_attention(qa, ka, va, rel_bias).transpose(0, 2, 1, 3).reshape(B, S, A_WIDTH)
    qkv_b = jax.nn.silu(causal_depthwise_conv(qkv_b, conv_w))
    qb, kb, vb = jnp.split(qkv_b, [B_KW, 2 * B_KW], axis=-1)
    qb = l2_norm(qb.reshape(B, S, B_HEADS, B_KEY_DIM)).transpose(0, 2, 1, 3)
    kb = l2_norm(kb.reshape(B, S, B_HEADS, B_KEY_DIM)).transpose(0, 2, 1, 3)
    vb = vb.reshape(B, S, B_HEADS, B_VAL_DIM).transpose(0, 2, 1, 3)
    beta = jax.nn.sigmoid(beta_raw.astype(jnp.float32)).transpose(0, 2, 1)
    g = (-jnp.exp(a_log.astype(jnp.float32))
         * jax.nn.softplus(alpha_raw.astype(jnp.float32) + dt_bias.astype(jnp.float32))).transpose(0, 2, 1)
    o_b = gated_delta_rule(qb, kb, vb, g, beta).astype(h.dtype).transpose(0, 2, 1, 3)
    o_b = rms_norm(o_b, gdn_norm_gain) * jax.nn.silu(z_b.reshape(B, S, B_HEADS, B_VAL_DIM))
    o_b = o_b.reshape(B, S, B_VW)
    merged = jax.nn.sigmoid(gate_a) * (o_a @ w_up_a) + jax.nn.sigmoid(gate_b) * (o_b @ w_up_b)
    return merged @ w_out


def peer_ffn(h, w_query, sub_keys, expert_down, expert_up):
    B, S, D = h.shape
    T = B * S
    K = PEER_TOPK
    ht = h.reshape(T, D)
    qry = (ht @ w_query).reshape(T, PEER_HEADS, 2, PEER_QDIM // 2)
    scores = jnp.einsum('thpd,hpkd->thpk', qry, sub_keys).astype(jnp.float32)
    s_top, i_top = lax.top_k(scores, K)
    cand = (s_top[:, :, 0, :, None] + s_top[:, :, 1, None, :]).reshape(T, PEER_HEADS, K * K)
    cand_idx = (i_top[:, :, 0, :, None] * PEER_NKEYS + i_top[:, :, 1, None, :]).reshape(T, PEER_HEADS, K * K)
    best, pos = lax.top_k(cand, K)
    expert_idx = jnp.take_along_axis(cand_idx, pos, axis=-1)
    gate = jax.nn.softmax(best, axis=-1)
    TC = PEER_TOK_CHUNK

    def one_chunk(c):
        start = c * TC
        hc = lax.dynamic_slice_in_dim(ht, start, TC, axis=0)
        ec = lax.dynamic_slice_in_dim(expert_idx, start, TC, axis=0)
        gc = lax.dynamic_slice_in_dim(gate, start, TC, axis=0)
        u = expert_down[ec]
        vv = expert_up[ec]
        act = jax.nn.gelu(jnp.einsum('td,thkd->thk', hc, u).astype(jnp.float32), approximate=False)
        return jnp.einsum('thk,thkd->td', (gc * act).astype(vv.dtype), vv)

    out = lax.map(one_chunk, jnp.arange(T // TC))
    return out.reshape(B, S, D)


def setup_inputs(seed: int = 0) -> dict:
    key = jax.random.key(seed)
    ks = jax.random.split(key, 20)
    f32 = jnp.float32

    def nrm(k, shape, scale):
        return jax.random.normal(k, shape, f32) * scale

    def gain(k, n):
        return 1.0 + 0.05 * jax.random.normal(k, (DEPTH, n), f32)

    dt = jnp.exp(jax.random.uniform(ks[6], (DEPTH, B_HEADS), f32, math.log(1e-3), math.log(1e-1)))
    return {
        'x': nrm(ks[0], (BATCH, SEQ, D_MODEL), 1.0),
        'rel_bias': nrm(ks[1], (REL_BUCKETS, A_HEADS), 0.5),
        'norm1_gain': gain(ks[2], D_MODEL),
        'w_in': nrm(ks[3], (DEPTH, D_MODEL, IN_TOTAL), D_MODEL ** -0.5),
        'conv_w': nrm(ks[4], (DEPTH, CONV_WIDTH, 2 * B_KW + B_VW), CONV_WIDTH ** -0.5),
        'a_log': jnp.log(jax.random.uniform(ks[5], (DEPTH, B_HEADS), f32, 1.0, 16.0)),
        'dt_bias': jnp.log(jnp.expm1(dt)),
        'q_norm_gain': gain(ks[7], A_HEAD_DIM),
        'k_norm_gain': gain(ks[8], A_HEAD_DIM),
        'gdn_norm_gain': gain(ks[9], B_VAL_DIM),
        'w_up_a': nrm(ks[10], (DEPTH, A_WIDTH, D_MODEL), A_WIDTH ** -0.5),
        'w_up_b': nrm(ks[11], (DEPTH, B_VW, D_MODEL), B_VW ** -0.5),
        'w_out': nrm(ks[12], (DEPTH, D_MODEL, D_MODEL), D_MODEL ** -0.5),
        'norm2_gain': gain(ks[13], D_MODEL),
        'peer_w_query': nrm(ks[14], (DEPTH, D_MODEL, PEER_HEADS * PEER_QDIM), D_MODEL ** -0.5),
        'peer_sub_keys': nrm(ks[15], (DEPTH, PEER_HEADS, 2, PEER_NKEYS, PEER_QDIM // 2), (PEER_QDIM // 2) ** -0.5),
        'peer_u': nrm(ks[16], (DEPTH, PEER_EXPERTS, D_MODEL), D_MODEL ** -0.5),
        'peer_v': nrm(ks[17], (DEPTH, PEER_EXPERTS, D_MODEL), PEER_HEADS ** -0.5),
    }


def reference(x, rel_bias, norm1_gain, w_in, conv_w, a_log, dt_bias, q_norm_gain, k_norm_gain,
              gdn_norm_gain, w_up_a, w_up_b, w_out, norm2_gain, peer_w_query, peer_sub_keys,
              peer_u, peer_v):
    for l in range(DEPTH):
        h = rms_norm(x, norm1_gain[l])
        x = x + token_mixers(h, rel_bias, w_in[l], conv_w[l], a_log[l], dt_bias[l], q_norm_gain[l],
                             k_norm_gain[l], gdn_norm_gain[l], w_up_a[l], w_up_b[l], w_out[l])
        h2 = rms_norm(x, norm2_gain[l])
        x = x + peer_ffn(h2, peer_w_query[l], peer_sub_keys[l], peer_u[l], peer_v[l])
    return x
```

```python
import math
from contextlib import ExitStack

import numpy as np
import concourse.bass as bass
import concourse.mybir as mybir
from concourse.bass_utils import run_bass_kernel_spmd

F32 = mybir.dt.float32
F32R = mybir.dt.float32r
U32 = mybir.dt.uint32
I32 = mybir.dt.int32
AF = mybir.ActivationFunctionType
ALU = mybir.AluOpType
AX = mybir.AxisListType

D = 2048
S = 2048
NT = S // 128
IN_TOTAL = 11280
EPS = 1e-6
NEG = -30000.0

COMPUTE = ("tensor", "vector", "scalar", "gpsimd")
ALLENG = ("tensor", "vector", "scalar", "gpsimd", "sync")


import re as _re
_PSUM_KEY = _re.compile(r"^(n\d_tp|gp|aux|scp|op|dp|pp|pa|pb|po|pq)\d+$")


class Op:
    __slots__ = ("eng", "fn", "deps", "needed", "is_dma", "slot", "use", "tok", "idx")


class Prog:
    def __init__(self, nc, es, ndma=8):
        self.nc = nc
        self.ops = {e: [] for e in ALLENG}
        self.last_w = {}
        self.rd_comp = {}
        self.rd_dma = {}
        self.sem = {e: es.enter_context(nc.semaphore("s_" + e)) for e in COMPUTE}
        self.dsem = {}
        self.dlast = {}
        self.dnext = {}
        self.duse = {}
        for q in ("sync", "gpsimd", "scalar"):
            self.dsem[q] = [es.enter_context(nc.semaphore("d_%s%d" % (q, i))) for i in range(ndma)]
            self.dlast[q] = [None] * ndma
            self.duse[q] = [0] * ndma
            self.dnext[q] = 0
        self.pending_barrier = {e: None for e in ALLENG}
        self.all_dma = []

    def _add(self, eng, fn, r, w, is_dma):
        o = Op()
        o.eng = eng
        o.fn = fn
        o.needed = False
        o.is_dma = is_dma
        o.slot = None
        o.use = 0
        deps = set()
        for k in r:
            lw = self.last_w.get(k)
            if lw is not None:
                deps.add(lw)
            if isinstance(k, str) and _PSUM_KEY.match(k):
                for en, ro in self.rd_comp.get(k, {}).items():
                    if en != eng:
                        deps.add(ro)
        for k in w:
            lw = self.last_w.get(k)
            if lw is not None:
                deps.add(lw)
            for ro in self.rd_comp.get(k, {}).values():
                deps.add(ro)
            for ro in self.rd_dma.get(k, ()):
                deps.add(ro)
        if is_dma:
            q = eng
            sl = self.dnext[q]
            self.dnext[q] = (sl + 1) % len(self.dsem[q])
            prev = self.dlast[q][sl]
            if prev is not None:
                deps.add(prev)
            self.duse[q][sl] += 1
            o.slot = sl
            o.use = self.duse[q][sl]
            self.dlast[q][sl] = o
            self.all_dma.append(o)
        pb = self.pending_barrier[eng]
        if pb is not None:
            deps |= pb
            self.pending_barrier[eng] = None
        if eng == "tensor":
            deps = {d for d in deps if not (d.eng == "tensor" and not d.is_dma)}
        o.deps = deps
        for d in deps:
            d.needed = True
        for k in w:
            self.last_w[k] = o
            self.rd_comp[k] = {}
            self.rd_dma[k] = []
        for k in r:
            if is_dma:
                self.rd_dma.setdefault(k, []).append(o)
            else:
                self.rd_comp.setdefault(k, {})[eng] = o
        o.idx = len(self.ops[eng])
        self.ops[eng].append(o)
        return o

    def op(self, eng, fn, r=(), w=()):
        return self._add(eng, fn, tuple(r), tuple(w), False)

    def dma(self, q, out, in_, r=(), w=(), **kw):
        return self._add(q, lambda e: e.dma_start(out=out, in_=in_, **kw), tuple(r), tuple(w), True)

    def dma_fn(self, q, fn, r=(), w=()):
        return self._add(q, fn, tuple(r), tuple(w), True)

    def barrier(self):
        deps = set()
        for e in ALLENG:
            if self.ops[e]:
                last = [o for o in self.ops[e] if not o.is_dma]
                if last:
                    deps.add(last[-1])
        for o in self.all_dma:
            deps.add(o)
        self.all_dma = []
        for e in ALLENG:
            pb = self.pending_barrier[e]
            self.pending_barrier[e] = set(deps) | (pb or set())
        self.last_w = {}
        self.rd_comp = {}
        self.rd_dma = {}

    def finish(self, block):
        self.barrier()
        nc = self.nc
        for e in ALLENG:
            self._add(e, None, (), (), False)
        for e in COMPUTE:
            c = 0
            for o in self.ops[e]:
                if o.is_dma:
                    o.tok = (self.dsem[e][o.slot], 16 * o.use)
                elif o.needed:
                    c += 1
                    o.tok = (self.sem[e], c)
                else:
                    o.tok = None
        for o in self.ops["sync"]:
            if o.is_dma:
                o.tok = (self.dsem["sync"][o.slot], 16 * o.use)
            else:
                o.tok = None

        def emit(engname):
            def body(eng):
                waited = {}
                for o in self.ops[engname]:
                    for d in o.deps:
                        sem, val = d.tok
                        key = id(sem)
                        if waited.get(key, 0) < val:
                            eng.wait_ge(sem, val)
                            waited[key] = val
                    if o.fn is None:
                        continue
                    ins = o.fn(eng)
                    if o.is_dma:
                        ins.then_inc(o.tok[0], 16)
                    elif o.needed:
                        ins.then_inc(o.tok[0], 1)
            return body

        block.tensor(emit("tensor"))
        block.vector(emit("vector"))
        block.scalar(emit("scalar"))
        block.gpsimd(emit("gpsimd"))
        block.sync(emit("sync"))


def r32(ap):
    return ap.bitcast(F32R)


class Ctx:
    pass


_uid = [0]


def _un(name):
    _uid[0] += 1
    return "%s_%d" % (name, _uid[0])


def sb(c, name, shape, dt=F32):
    return c.es.enter_context(c.nc.sbuf_tensor(_un(name), list(shape), dt))


def ps(c, name, shape, dt=F32):
    return c.es.enter_context(c.nc.psum_tensor(_un(name), list(shape), dt))


FM_SRC = [(0, 1024), (1024, 1024), (3072, 3072), (7184, 2048), (9232, 2048)]
TM_SRC = [(2048, 1024), (6144, 1024), (7168, 16)]
N_FM = 9216
N_TM = 2064


def phase_norm_T(c, p, x_ap, gain_ap, hT, pfx, half, h_out=None):
    nc = c.nc
    with ExitStack() as es:
        c2 = Ctx(); c2.nc = nc; c2.es = es
        gbc = sb(c2, pfx + "gbc", [128, D])
        xt = [sb(c2, pfx + "xt%d" % i, [128, D]) for i in range(2)]
        ht = [sb(c2, pfx + "ht%d" % i, [128, D]) for i in range(2)]
        sq = sb(c2, pfx + "sq", [128, D])
        st = [sb(c2, pfx + "st%d" % i, [128, 4]) for i in range(2)]
        tp = [ps(c2, pfx + "tp%d" % i, [128, 512]) for i in range(4)]
        p.dma("sync", gbc[:], gain_ap.partition_broadcast(128), w=[pfx + "gbc"])
        for ti in range(8):
            t = half * 8 + ti
            b = ti % 2
            p.dma("sync", xt[b][:], x_ap[t * 128:(t + 1) * 128, :], w=[pfx + "xt%d" % b])
            p.op("scalar", lambda e, b=b: e.activation(out=sq[:], in_=xt[b][:], func=AF.Square,
                                                       accum_out=st[b][:, 0:1]),
                 r=[pfx + "xt%d" % b], w=[pfx + "sq", pfx + "st%d" % b])
            p.op("scalar", lambda e, b=b: e.activation(out=st[b][:, 1:2], in_=st[b][:, 0:1], func=AF.Sqrt,
                                                       scale=1.0 / D, bias=c.eps_t[:, 0:1]),
                 r=[pfx + "st%d" % b], w=[pfx + "st%d" % b])
            p.op("vector", lambda e, b=b: e.reciprocal(out=st[b][:, 2:3], in_=st[b][:, 1:2]),
                 r=[pfx + "st%d" % b], w=[pfx + "st%d" % b])
            p.op("vector", lambda e, b=b: e.scalar_tensor_tensor(out=ht[b][:], in0=xt[b][:], scalar=st[b][:, 2:3],
                                                                 in1=gbc[:], op0=ALU.mult, op1=ALU.mult),
                 r=[pfx + "xt%d" % b, pfx + "st%d" % b, pfx + "gbc"], w=[pfx + "ht%d" % b])
            if h_out is not None:
                p.dma("sync", h_out[t * 128:(t + 1) * 128, :], ht[b][:], r=[pfx + "ht%d" % b], w=[(pfx + "hout", t)])
            for g4 in range(4):
                pb = tp[g4]
                for j in range(4):
                    kc = g4 * 4 + j
                    p.op("tensor", lambda e, b=b, kc=kc, j=j, pb=pb: e.transpose(
                        out=pb[:, j * 128:(j + 1) * 128], in_=ht[b][:, kc * 128:(kc + 1) * 128], identity=c.ident[:]),
                        r=[pfx + "ht%d" % b], w=[pfx + "tp%d" % g4])
                eng = "scalar" if g4 % 2 == 0 else "vector"
                dst = hT[:, g4 * 4:(g4 + 1) * 4, ti * 128:(ti + 1) * 128]
                src = pb[:].rearrange("p (j t) -> p j t", j=4)
                if eng == "scalar":
                    p.op("scalar", lambda e, dst=dst, src=src: e.copy(out=dst, in_=src),
                         r=[pfx + "tp%d" % g4], w=["hT"])
                else:
                    p.op("vector", lambda e, dst=dst, src=src: e.tensor_copy(out=dst, in_=src),
                         r=[pfx + "tp%d" % g4], w=["hT"])
        p.barrier()


def phase_inproj(c, p):
    nc = c.nc
    for half in range(2):
        with ExitStack() as es:
            c2 = Ctx(); c2.nc = nc; c2.es = es
            hT = sb(c2, "hT", [128, 16, 1024], F32R)
            phase_norm_T(c, p, c.x, c.norm1_gain, hT, "n1_", half)
            with ExitStack() as es2:
                c3 = Ctx(); c3.nc = nc; c3.es = es2
                wb = [sb(c3, "wb%d" % i, [128, 16, 256], F32R) for i in range(2)]
                ob = [sb(c3, "ob%d" % i, [128, 1024]) for i in range(2)]
                pp = [ps(c3, "pp%d" % i, [128, 512]) for i in range(4)]
                gi = 0
                oi = 0
                pi = 0
                t0 = half * 1024
                row = 0
                for (c0, n) in FM_SRC:
                    for g in range(n // 256):
                        col = c0 + g * 256
                        b = gi % 2
                        gi += 1
                        p.dma("sync", wb[b][:], r32(c.w_in[:, col:col + 256]).rearrange("(kc p) n -> p kc n", p=128),
                              w=["wb%d" % b])
                        for cc in range(2):
                            o = oi % 2
                            oi += 1
                            for tg in range(2):
                                pb = pi % 4
                                pi += 1
                                for kc in range(16):
                                    p.op("tensor", lambda e, b=b, cc=cc, tg=tg, kc=kc, pb=pb: e.matmul(
                                        pp[pb][:], lhsT=wb[b][:, kc, cc * 128:(cc + 1) * 128],
                                        rhs=hT[:, kc, tg * 512:(tg + 1) * 512],
                                        start=(kc == 0), stop=(kc == 15)),
                                        r=["wb%d" % b, "hT"], w=["pp%d" % pb])
                                if tg == 0:
                                    p.op("scalar", lambda e, o=o, pb=pb: e.copy(out=ob[o][:, 0:512], in_=pp[pb][:]),
                                         r=["pp%d" % pb], w=["ob%d" % o])
                                else:
                                    p.op("vector", lambda e, o=o, pb=pb: e.tensor_copy(out=ob[o][:, 512:1024], in_=pp[pb][:]),
                                         r=["pp%d" % pb], w=["ob%d" % o])
                            rr = row + g * 256 + cc * 128
                            p.dma("sync", c.sc_fm[rr:rr + 128, t0:t0 + 1024], ob[o][:],
                                  r=["ob%d" % o], w=[("sc_fm", rr // 128)])
                    row += n
                colo = 0
                for (c0, n) in TM_SRC:
                    w = min(n, 256)
                    for g in range(max(1, n // 256)):
                        col = c0 + g * 256
                        b = gi % 2
                        gi += 1
                        p.dma("sync", wb[b][:, :, 0:w], r32(c.w_in[:, col:col + w]).rearrange("(kc p) n -> p kc n", p=128),
                              w=["wb%d" % b])
                        for tq in range(2):
                            o = oi % 2
                            oi += 1
                            for tt in range(4):
                                tl = tq * 4 + tt
                                pb = pi % 4
                                pi += 1
                                for kc in range(16):
                                    p.op("tensor", lambda e, b=b, tl=tl, kc=kc, pb=pb, w=w: e.matmul(
                                        pp[pb][:, 0:w], lhsT=hT[:, kc, tl * 128:(tl + 1) * 128],
                                        rhs=wb[b][:, kc, 0:w],
                                        start=(kc == 0), stop=(kc == 15)),
                                        r=["wb%d" % b, "hT"], w=["pp%d" % pb])
                                if tt % 2 == 0:
                                    p.op("scalar", lambda e, o=o, pb=pb, tt=tt, w=w: e.copy(
                                        out=ob[o][:, tt * 256:tt * 256 + w], in_=pp[pb][:, 0:w]),
                                        r=["pp%d" % pb], w=["ob%d" % o])
                                else:
                                    p.op("vector", lambda e, o=o, pb=pb, tt=tt, w=w: e.tensor_copy(
                                        out=ob[o][:, tt * 256:tt * 256 + w], in_=pp[pb][:, 0:w]),
                                        r=["pp%d" % pb], w=["ob%d" % o])
                            tb = t0 + tq * 512
                            cw = colo + g * 256
                            p.dma("sync",
                                  c.sc_tm[tb:tb + 512, cw:cw + w].rearrange("(tt p) n -> p tt n", p=128),
                                  ob[o][:].rearrange("p (tt n) -> p tt n", n=256)[:, :, 0:w],
                                  r=["ob%d" % o], w=[("sc_tm", tb // 128, cw)])
                    colo += n
                p.barrier()


def t5_bucket_np(n):
    n = np.maximum(n, 0)
    nf = np.maximum(n, 1).astype(np.float32)
    large = 16 + (np.log(nf / np.float32(16)) / np.float32(math.log(8.0)) * np.float32(16)).astype(np.int32)
    large = np.minimum(large, 31)
    return np.where(n < 16, n, large)


def moba_consts(rel_bias):
    k = np.arange(128)[:, None]
    q = np.arange(128)[None, :]
    out = np.zeros((8, 2, 128, 128), np.float32)
    b0 = t5_bucket_np(q - k)
    b1 = t5_bucket_np(q - k + 128)
    for h in range(8):
        out[h, 0] = np.where(q >= k, rel_bias[b0, h], np.float32(NEG))
        out[h, 1] = rel_bias[b1, h]
    cm = np.zeros((128, 16, 8), np.float32)
    notown = np.ones((128, 16, 8), np.float32)
    for t in range(16):
        for n in range(8):
            if n >= t // 2:
                cm[:, t, n] = -1e30
            if n == t // 2:
                notown[:, t, n] = 0.0
    esel = np.zeros((8, 8, 128), np.float32)
    for n in range(8):
        esel[n, n, :] = 1.0
    return out, cm, notown, esel


def phase_moba(c, p):
    nc = c.nc
    with ExitStack() as es:
        c2 = Ctx(); c2.nc = nc; c2.es = es
        cm = sb(c2, "cm", [128, 16, 8])
        notown = sb(c2, "notown", [128, 16, 8])
        esel = sb(c2, "esel", [8, 8, 128], F32R)
        cb = sb(c2, "cb", [128, 8])
        gq = sb(c2, "gq", [128, 2])
        gk = sb(c2, "gk", [128, 1])
        d01 = sb(c2, "d01", [128, 2, 128], F32R)
        d01r = sb(c2, "d01r", [128, 2, 128])
        qr = sb(c2, "qr", [128, S])
        kr = sb(c2, "kr", [128, S])
        sq = sb(c2, "sq", [128, S], F32R)
        qn = sb(c2, "qn", [128, S], F32R)
        kn = sb(c2, "kn", [128, S], F32R)
        vt = sb(c2, "vt", [128, 16, 128], F32R)
        rs = sb(c2, "rs", [128, 512])
        km = sb(c2, "km", [128, 8], F32R)
        kmf = sb(c2, "kmf", [128, 8])
        gm = sb(c2, "gm", [128, 16, 8])
        cmp_ = sb(c2, "cmp", [128, 16, 8, 8])
        rank = sb(c2, "rank", [128, 16, 8])
        nmk = sb(c2, "nmk", [128, 16, 8])
        negT = sb(c2, "negT", [8, S], F32R)
        pT = [sb(c2, "pT%d" % i, [128, 512], F32R) for i in range(2)]
        rden = sb(c2, "rden", [128, 512])
        oo = [sb(c2, "oo%d" % i, [128, 512]) for i in range(2)]
        aux = [ps(c2, "aux%d" % i, [128, 512]) for i in range(2)]
        scp = [ps(c2, "scp%d" % i, [128, 512]) for i in range(2)]
        op_ = [ps(c2, "op%d" % i, [128, 512]) for i in range(2)]
        dp = [ps(c2, "dp%d" % i, [128, 512]) for i in range(2)]

        p.dma("sync", cm[:], c.cm_d[:, :, :], w=["cm"])
        p.dma("sync", notown[:], c.notown_d[:, :, :], w=["notown"])
        p.dma("sync", esel[:], r32(c.esel_d[:, :, :]), w=["esel"])
        p.dma("sync", cb[:], c.rel_bias[31:32, :].partition_broadcast(128), w=["cb"])
        p.dma("sync", gq[:, 0:1], c.q_norm_gain.rearrange("o d -> d o"), w=["gq"])
        p.dma("sync", gk[:, 0:1], c.k_norm_gain.rearrange("o d -> d o"), w=["gk"])
        p.op("vector", lambda e: e.tensor_scalar(out=gq[:, 1:2], in0=gq[:, 0:1], scalar1=128.0 ** -0.5, scalar2=None,
                                                 op0=ALU.mult), r=["gq"], w=["gq"])
        auxi = 0
        sci = 0
        gi = 0
        for h in range(8):
            p.dma("sync", qr[:], c.sc_fm[h * 128:(h + 1) * 128, :], w=["qr"])
            p.dma("sync", kr[:], c.sc_fm[1024 + h * 128:1024 + (h + 1) * 128, :], w=["kr"])
            p.dma("sync", vt[:], r32(c.sc_tm[:, h * 128:(h + 1) * 128]).rearrange("(t p) d -> p t d", p=128), w=["vt"])
            p.dma("sync", d01r[:], c.d01_d[h].rearrange("a k q -> k a q"), w=["d01r"])
            p.op("vector", lambda e, h=h: e.tensor_scalar(out=d01[:], in0=d01r[:], scalar1=cb[:, h:h + 1], scalar2=None,
                                                          op0=ALU.subtract), r=["d01r", "cb"], w=["d01"])
            for (raw, dst, gcol, rk, wk) in ((qr, qn, gq[:, 1:2], "qr", "qn"), (kr, kn, gk[:, 0:1], "kr", "kn")):
                p.op("scalar", lambda e, raw=raw: e.activation(out=sq[:], in_=raw[:], func=AF.Square), r=[rk], w=["sq"])
                for tg in range(4):
                    a = auxi % 2
                    auxi += 1
                    sl = slice(tg * 512, (tg + 1) * 512)
                    p.op("tensor", lambda e, a=a, sl=sl: e.matmul(aux[a][:], lhsT=c.ones_r[:], rhs=sq[:, sl], start=True, stop=True),
                         r=["sq"], w=["aux%d" % a])
                    p.op("scalar", lambda e, a=a: e.activation(out=rs[:], in_=aux[a][:], func=AF.Sqrt, scale=1.0 / 128,
                                                               bias=c.eps_t[:, 0:1]), r=["aux%d" % a], w=["rs"])
                    p.op("vector", lambda e: e.reciprocal(out=rs[:], in_=rs[:]), r=["rs"], w=["rs"])
                    p.op("vector", lambda e, raw=raw, dst=dst, gcol=gcol, sl=sl: e.scalar_tensor_tensor(
                        out=dst[:, sl], in0=raw[:, sl], scalar=gcol, in1=rs[:], op0=ALU.mult, op1=ALU.mult),
                        r=[rk, "rs", "gq", "gk"], w=[wk])
            p.op("vector", lambda e: e.tensor_reduce(out=kmf[:], in_=kn[:].bitcast(F32).rearrange("p (n j) -> p n j", j=256),
                                                     axis=AX.X, op=ALU.add), r=["kn"], w=["kmf"])
            p.op("vector", lambda e: e.tensor_copy(out=km[:], in_=kmf[:]), r=["kmf"], w=["km"])
            a = auxi % 2
            auxi += 1
            for t in range(16):
                p.op("tensor", lambda e, a=a, t=t: e.matmul(aux[a][:, t * 8:(t + 1) * 8], lhsT=qn[:, t * 128:(t + 1) * 128],
                                                            rhs=km[:], start=True, stop=True),
                     r=["qn", "km"], w=["aux%d" % a])
            p.op("vector", lambda e, a=a: e.tensor_tensor(out=gm[:], in0=aux[a][:, 0:128].rearrange("p (t n) -> p t n", n=8),
                                                          in1=cm[:], op=ALU.add), r=["aux%d" % a, "cm"], w=["gm"])
            p.op("vector", lambda e: e.tensor_tensor(out=cmp_[:], in0=gm[:].unsqueeze(2).to_broadcast([128, 16, 8, 8]),
                                                     in1=gm[:].unsqueeze(3).to_broadcast([128, 16, 8, 8]), op=ALU.is_gt),
                 r=["gm"], w=["cmp"])
            p.op("vector", lambda e: e.tensor_reduce(out=rank[:], in_=cmp_[:], axis=AX.X, op=ALU.add), r=["cmp"], w=["rank"])
            p.op("vector", lambda e: e.tensor_scalar(out=rank[:], in0=rank[:], scalar1=3.0, scalar2=NEG, op0=ALU.is_ge,
                                                     op1=ALU.mult), r=["rank"], w=["rank"])
            p.op("vector", lambda e: e.tensor_tensor(out=nmk[:], in0=rank[:], in1=notown[:], op=ALU.mult),
                 r=["rank", "notown"], w=["nmk"])
            for tg in range(4):
                a = auxi % 2
                auxi += 1
                for j in range(4):
                    t = tg * 4 + j
                    p.op("tensor", lambda e, a=a, t=t, j=j: e.transpose(out=aux[a][0:8, j * 128:(j + 1) * 128], in_=nmk[:, t, :],
                                                                        identity=c.ident[:]),
                         r=["nmk"], w=["aux%d" % a])
                p.op("scalar", lambda e, a=a, tg=tg: e.copy(out=negT[:, tg * 512:(tg + 1) * 512], in_=aux[a][0:8, :]),
                     r=["aux%d" % a], w=["negT"])
            for g in range(4):
                gb = gi % 2
                gi += 1
                nk = 4 * g + 4
                for kt in range(nk):
                    s_ = sci % 2
                    sci += 1
                    jj0 = max(0, kt - 4 * g)
                    c0 = jj0 * 128
                    qs = slice(g * 512 + c0, (g + 1) * 512)
                    cs = slice(c0, 512)
                    n = kt // 2
                    extra = []
                    if kt >= 4 * g:
                        extra.append((jj0, 0))
                        if jj0 + 1 < 4:
                            extra.append((jj0 + 1, 1))
                    elif kt == 4 * g - 1:
                        extra.append((0, 1))
                    p.op("tensor", lambda e, s_=s_, kt=kt, qs=qs, cs=cs: e.matmul(
                        scp[s_][:, cs], lhsT=kn[:, kt * 128:(kt + 1) * 128], rhs=qn[:, qs], start=True, stop=False),
                        r=["kn", "qn"], w=["scp%d" % s_])
                    p.op("tensor", lambda e, s_=s_, n=n, qs=qs, cs=cs, last=(not extra): e.matmul(
                        scp[s_][:, cs], lhsT=esel[:, n, :], rhs=negT[:, qs], start=False, stop=last),
                        r=["esel", "negT"], w=["scp%d" % s_])
                    for ei, (jj, which) in enumerate(extra):
                        p.op("tensor", lambda e, s_=s_, jj=jj, which=which, last=(ei == len(extra) - 1): e.matmul(
                            scp[s_][:, jj * 128:(jj + 1) * 128], lhsT=c.ident_r[:], rhs=d01[:, which, :], start=False, stop=last),
                            r=["d01"], w=["scp%d" % s_])
                    p.op("scalar", lambda e, s_=s_, cs=cs, h=h: e.activation(out=pT[s_][:, cs], in_=scp[s_][:, cs], func=AF.Exp,
                                                                             bias=cb[:, h:h + 1]),
                         r=["scp%d" % s_, "cb"], w=["pT%d" % s_])
                    p.op("tensor", lambda e, s_=s_, kt=kt, cs=cs, gb=gb, nk=nk: e.matmul(
                        op_[gb][:, cs], lhsT=vt[:, kt, :], rhs=pT[s_][:, cs], start=(kt == 0), stop=(kt == nk - 1)),
                        r=["vt", "pT%d" % s_], w=["op%d" % gb])
                    p.op("tensor", lambda e, s_=s_, cs=cs, gb=gb, kt=kt, nk=nk: e.matmul(
                        dp[gb][:, cs], lhsT=c.ones_r[:], rhs=pT[s_][:, cs], start=(kt == 0), stop=(kt == nk - 1)),
                        r=["pT%d" % s_], w=["dp%d" % gb])
                p.op("vector", lambda e, gb=gb: e.reciprocal(out=rden[:], in_=dp[gb][:]), r=["dp%d" % gb], w=["rden"])
                p.op("vector", lambda e, gb=gb: e.tensor_tensor(out=oo[gb][:], in0=op_[gb][:], in1=rden[:], op=ALU.mult),
                     r=["op%d" % gb, "rden"], w=["oo%d" % gb])
                p.dma("sync", c.sc_oa[h * 128:(h + 1) * 128, g * 512:(g + 1) * 512], oo[gb][:],
                      r=["oo%d" % gb], w=[("sc_oa", h, g)])
        p.barrier()


def gdn_consts():
    k = np.arange(128)[:, None]
    i = np.arange(128)[None, :]
    same = (k // 64) == (i // 64)
    tri = ((k <= i) & same).astype(np.float32)
    blk = same.astype(np.float32)
    half0 = np.broadcast_to((k < 64), (128, 128)).astype(np.float32)
    half1 = np.broadcast_to((k >= 64), (128, 128)).astype(np.float32)
    ustr = ((k > i) & same).astype(np.float32)
    ii = np.arange(128)[:, None]
    jj = np.arange(128)[None, :]
    same2 = (ii // 64) == (jj // 64)
    negm_strict = np.where((ii > jj) & same2, 0.0, NEG).astype(np.float32)
    negm_inclT = np.where((jj >= ii) & same2, 0.0, NEG).astype(np.float32)
    return np.stack([tri, blk, half0, half1, ustr, negm_strict, negm_inclT], 0)


class _Stop(Exception):
    pass


def _chk(n):
    import os
    return int(os.environ.get("GDN_STOP", "99")) == n


def phase_gdn(c, p):
    _phase_gdn(c, p)
    p.barrier()


def _phase_gdn(c, p):
    nc = c.nc
    with ExitStack() as es:
        c2 = Ctx(); c2.nc = nc; c2.es = es
        G = sb(c2, "gc", [128, 7, 128])
        TRI, BLK, H0, H1, USTR, NMS, NMIT = [G[:, i, :] for i in range(7)]
        ba = sb(c2, "ba", [128, 16, 16])
        dtb = sb(c2, "dtb", [128, 8])
        Aex = sb(c2, "Aex", [128, 8])
        gng = sb(c2, "gng", [128, 128])
        beta = sb(c2, "beta", [128, 16, 8])
        nbeta = sb(c2, "nbeta", [128, 16, 8])
        gg = sb(c2, "gg", [128, 16, 8])
        egs = sb(c2, "egs", [128, 16, 8])
        kds = sb(c2, "kds", [128, 16, 8])
        kbs = sb(c2, "kbs", [128, 16, 8])
        egl = sb(c2, "egl", [128, 2, 16, 8])
        tmp8 = sb(c2, "tmp8", [128, 16, 8])
        cw = sb(c2, "cw", [128, 3, 4])
        xp = [sb(c2, "xp%d" % i, [128, 3 + S]) for i in range(3)]
        cv = [sb(c2, "cv%d" % i, [128, S]) for i in range(3)]
        sq = sb(c2, "gsq", [128, S], F32R)
        rs = sb(c2, "grs", [128, 512])
        kbg = sb(c2, "kbg", [128, 128])
        vbe = sb(c2, "vbe", [128, 128])
        Ag = sb(c2, "Ag", [128, 128])
        Dec = sb(c2, "Dec", [128, 128])
        DecT = sb(c2, "DecT", [128, 128])
        Am = [sb(c2, "Am%d" % i, [128, 128]) for i in range(2)]
        At = [sb(c2, "At%d" % i, [128, 128]) for i in range(2)]
        Rt = [sb(c2, "Rt%d" % i, [128, 128]) for i in range(2)]
        u_all = sb(c2, "u_all", [128, 16, 128])
        wT_all = sb(c2, "wT_all", [128, 16, 128])
        aT_all = sb(c2, "aT_all", [128, 16, 128])
        kd_all = sb(c2, "kd_all", [128, 2, 16, 128])
        kds2 = sb(c2, "kds2", [128, 2, 16, 8])
        St = sb(c2, "St", [128, 128])
        vnew = sb(c2, "vnew", [128, 128])
        otmp = sb(c2, "otmp", [128, 128])
        ob = sb(c2, "ob", [128, 16, 128])
        zt = sb(c2, "zt", [128, 16, 128])
        ssq = sb(c2, "ssq", [128, 16])
        junk = sb(c2, "junk", [128, 128])
        obT = sb(c2, "obT", [128, S])
        pool = [ps(c2, "gp%d" % i, [128, 512]) for i in range(8)]
        pc = [0]

        def nxt():
            i = pc[0] % 8
            pc[0] += 1
            return i

        p.dma("sync", G[:], c.gdnc_d.rearrange("a k i -> k a i"), w=["G"])
        p.dma("sync", ba[:], c.sc_tm[:, 2048:2064].rearrange("(t p) n -> p t n", p=128), w=["ba"])
        p.dma("sync", dtb[:], c.dt_bias.partition_broadcast(128), w=["dtb"])
        p.dma("sync", Aex[:], c.a_log.partition_broadcast(128), w=["Aex"])
        p.dma("sync", gng[:], c.gdn_norm_gain.partition_broadcast(128), w=["gng"])
        p.op("scalar", lambda e: e.activation(out=Aex[:], in_=Aex[:], func=AF.Exp), r=["Aex"], w=["Aex"])
        p.op("scalar", lambda e: e.activation(out=beta[:], in_=ba[:, :, 0:8], func=AF.Exp, scale=-1.0), r=["ba"], w=["beta"])
        p.op("vector", lambda e: e.tensor_scalar(out=beta[:], in0=beta[:], scalar1=1.0, scalar2=None, op0=ALU.add), r=["beta"], w=["beta"])
        p.op("vector", lambda e: e.reciprocal(out=beta[:], in_=beta[:]), r=["beta"], w=["beta"])
        p.op("vector", lambda e: e.tensor_scalar(out=nbeta[:], in0=beta[:], scalar1=-1.0, scalar2=None, op0=ALU.mult), r=["beta"], w=["nbeta"])
        p.op("vector", lambda e: e.tensor_tensor(out=gg[:], in0=ba[:, :, 8:16], in1=dtb[:].unsqueeze(1).to_broadcast([128, 16, 8]),
                                                 op=ALU.add), r=["ba", "dtb"], w=["gg"])
        p.op("scalar", lambda e: e.activation(out=gg[:], in_=gg[:], func=AF.Exp), r=["gg"], w=["gg"])
        p.op("scalar", lambda e: e.activation(out=gg[:], in_=gg[:], func=AF.Ln, bias=c.one_t[:, 0:1]), r=["gg"], w=["gg"])
        p.op("vector", lambda e: e.scalar_tensor_tensor(out=gg[:], in0=gg[:], scalar=-1.0, in1=Aex[:].unsqueeze(1).to_broadcast([128, 16, 8]),
                                                        op0=ALU.mult, op1=ALU.mult), r=["gg", "Aex"], w=["gg"])
        ggf = gg[:].rearrange("p t h -> p (t h)")
        i0 = nxt(); i1 = nxt(); i2 = nxt(); i3 = nxt()
        p.op("tensor", lambda e: e.matmul(pool[i0][:, 0:128], lhsT=TRI, rhs=ggf, start=True, stop=True), r=["G", "gg"], w=["gp%d" % i0])
        p.op("tensor", lambda e: e.matmul(pool[i1][:, 0:128], lhsT=BLK, rhs=ggf, start=True, stop=True), r=["G", "gg"], w=["gp%d" % i1])
        p.op("tensor", lambda e: e.matmul(pool[i2][:, 0:128], lhsT=H0, rhs=ggf, start=True, stop=True), r=["G", "gg"], w=["gp%d" % i2])
        p.op("tensor", lambda e: e.matmul(pool[i3][:, 0:128], lhsT=H1, rhs=ggf, start=True, stop=True), r=["G", "gg"], w=["gp%d" % i3])
        v3 = lambda t_: t_.rearrange("p (t h) -> p t h", h=8)
        p.op("scalar", lambda e: e.activation(out=egs[:], in_=v3(pool[i0][:, 0:128]), func=AF.Exp), r=["gp%d" % i0], w=["egs"])
        p.op("vector", lambda e: e.tensor_tensor(out=kbs[:], in0=egs[:], in1=beta[:], op=ALU.mult), r=["egs", "beta"], w=["kbs"])
        p.op("vector", lambda e: e.tensor_copy(out=tmp8[:], in_=v3(pool[i0][:, 0:128])), r=["gp%d" % i0], w=["tmp8"])
        p.op("vector", lambda e: e.tensor_tensor(out=tmp8[:], in0=v3(pool[i1][:, 0:128]), in1=tmp8[:], op=ALU.subtract),
             r=["gp%d" % i1, "tmp8"], w=["tmp8"])
        p.op("scalar", lambda e: e.activation(out=kds[:], in_=tmp8[:], func=AF.Exp), r=["tmp8"], w=["kds"])
        p.op("scalar", lambda e: e.activation(out=egl[:, 0], in_=v3(pool[i2][:, 0:128]), func=AF.Exp), r=["gp%d" % i2], w=["egl"])
        p.op("scalar", lambda e: e.activation(out=egl[:, 1], in_=v3(pool[i3][:, 0:128]), func=AF.Exp), r=["gp%d" % i3], w=["egl"])
        p.op("vector", lambda e: e.tensor_scalar(out=egs[:], in0=egs[:], scalar1=128.0 ** -0.5, scalar2=None, op0=ALU.mult),
             r=["egs", "kbs"], w=["egs"])
        p.op("vector", lambda e: e.tensor_scalar(out=kds2[:, 0], in0=kds[:], scalar1=H0[:, 0:1], scalar2=None, op0=ALU.mult),
             r=["kds", "G"], w=["kds2"])
        p.op("vector", lambda e: e.tensor_scalar(out=kds2[:, 1], in0=kds[:], scalar1=H1[:, 0:1], scalar2=None, op0=ALU.mult),
             r=["kds", "G"], w=["kds2"])
        for i in range(3):
            p.op("vector", lambda e, i=i: e.memset(xp[i][:, 0:3], 0.0), w=["xp%d" % i])
        p.op("vector", lambda e: e.memset(vnew[:], 0.0), w=["vnew"])

        if _chk(0):
            return
        for h in range(8):
            for i in range(3):
                row = 2048 + i * 1024 + h * 128
                p.dma("sync", xp[i][:, 3:3 + S], c.sc_fm[row:row + 128, :], w=["xp%d" % i])
                p.dma("sync", cw[:, i, :], c.conv_wT[i * 1024 + h * 128:i * 1024 + (h + 1) * 128, :], w=["cw"])
                p.op("vector", lambda e, i=i: e.tensor_scalar(out=cv[i][:], in0=xp[i][:, 0:S], scalar1=cw[:, i, 0:1], scalar2=None,
                                                              op0=ALU.mult), r=["xp%d" % i, "cw"], w=["cv%d" % i])
                for tap in range(1, 4):
                    p.op("vector", lambda e, i=i, tap=tap: e.scalar_tensor_tensor(
                        out=cv[i][:], in0=xp[i][:, tap:tap + S], scalar=cw[:, i, tap:tap + 1], in1=cv[i][:],
                        op0=ALU.mult, op1=ALU.add), r=["xp%d" % i, "cw", "cv%d" % i], w=["cv%d" % i])
                p.op("scalar", lambda e, i=i: e.activation(out=cv[i][:], in_=cv[i][:], func=AF.Silu), r=["cv%d" % i], w=["cv%d" % i])
                if i < 2:
                    p.op("scalar", lambda e, i=i: e.activation(out=sq[:], in_=cv[i][:], func=AF.Square), r=["cv%d" % i], w=["gsq"])
                    for tg in range(4):
                        a = nxt()
                        sl = slice(tg * 512, (tg + 1) * 512)
                        p.op("tensor", lambda e, a=a, sl=sl: e.matmul(pool[a][:], lhsT=c.ones_r[:], rhs=sq[:, sl], start=True, stop=True),
                             r=["gsq"], w=["gp%d" % a])
                        p.op("scalar", lambda e, a=a: e.activation(out=rs[:], in_=pool[a][:], func=AF.Sqrt, bias=c.eps_t[:, 0:1]),
                             r=["gp%d" % a], w=["grs"])
                        p.op("vector", lambda e: e.reciprocal(out=rs[:], in_=rs[:]), r=["grs"], w=["grs"])
                        p.op("vector", lambda e, i=i, sl=sl: e.tensor_tensor(out=cv[i][:, sl], in0=cv[i][:, sl], in1=rs[:], op=ALU.mult),
                             r=["cv%d" % i, "grs"], w=["cv%d" % i])
            qn, kn, vn = cv
            if _chk(1):
                return
            p.dma("sync", zt[:], c.sc_tm[:, 1024 + h * 128:1024 + (h + 1) * 128].rearrange("(t p) d -> p t d", p=128), w=["zt"])
            for t in range(16):
                ts = slice(t * 128, (t + 1) * 128)
                a_k = nxt()
                p.op("tensor", lambda e, a=a_k, ts=ts: e.transpose(out=pool[a][:, 0:128], in_=kn[:, ts], identity=c.ident[:]),
                     r=["cv1"], w=["gp%d" % a_k])
                p.op("vector", lambda e, a=a_k, t=t, h=h: e.tensor_scalar(out=kbg[:], in0=pool[a][:, 0:128], scalar1=kbs[:, t, h:h + 1],
                                                                          scalar2=None, op0=ALU.mult),
                     r=["gp%d" % a_k, "kbs"], w=["kbg"])
                for hf_ in range(2):
                    p.op("scalar", lambda e, a=a_k, t=t, h=h, hf_=hf_: e.activation(out=kd_all[:, hf_, t, :], in_=pool[a][:, 0:128],
                                                                                    func=AF.Identity, scale=kds2[:, hf_, t, h:h + 1]),
                         r=["gp%d" % a_k, "kds2"], w=["kd_all"])
                a_v = nxt()
                p.op("tensor", lambda e, a=a_v, ts=ts: e.transpose(out=pool[a][:, 0:128], in_=vn[:, ts], identity=c.ident[:]),
                     r=["cv2"], w=["gp%d" % a_v])
                p.op("vector", lambda e, a=a_v, t=t, h=h: e.tensor_scalar(out=vbe[:], in0=pool[a][:, 0:128], scalar1=beta[:, t, h:h + 1],
                                                                          scalar2=None, op0=ALU.mult),
                     r=["gp%d" % a_v, "beta"], w=["vbe"])
                if _chk(20):
                    return
                p.op("vector", lambda e, t=t, h=h: e.tensor_scalar(out=Ag[:], in0=USTR, scalar1=gg[:, t, h:h + 1], scalar2=None,
                                                                   op0=ALU.mult), r=["G", "gg"], w=["Ag"])
                a_kk = nxt()
                p.op("tensor", lambda e, a=a_kk, ts=ts: e.matmul(pool[a][:, 0:128], lhsT=kn[:, ts], rhs=kn[:, ts], start=True, stop=True),
                     r=["cv1"], w=["gp%d" % a_kk])
                a_gd = nxt()
                p.op("tensor", lambda e, a=a_gd: e.matmul(pool[a][:, 0:128], lhsT=TRI, rhs=Ag[:], start=True, stop=False),
                     r=["G", "Ag"], w=["gp%d" % a_gd])
                p.op("tensor", lambda e, a=a_gd: e.matmul(pool[a][:, 0:128], lhsT=c.ident[:], rhs=NMS, start=False, stop=True),
                     r=["G"], w=["gp%d" % a_gd])
                p.op("scalar", lambda e, a=a_gd: e.activation(out=Dec[:], in_=pool[a][:, 0:128], func=AF.Exp), r=["gp%d" % a_gd], w=["Dec"])
                p.op("vector", lambda e, a=a_kk, t=t, h=h: e.scalar_tensor_tensor(out=Am[0][:], in0=pool[a][:, 0:128],
                                                                                  scalar=nbeta[:, t, h:h + 1], in1=Dec[:],
                                                                                  op0=ALU.mult, op1=ALU.mult),
                     r=["gp%d" % a_kk, "nbeta", "Dec"], w=["Am0"])
                if _chk(21):
                    return
                a_qk = nxt()
                p.op("tensor", lambda e, a=a_qk, ts=ts: e.matmul(pool[a][:, 0:128], lhsT=kn[:, ts], rhs=qn[:, ts], start=True, stop=True),
                     r=["cv1", "cv0"], w=["gp%d" % a_qk])
                a_gt = nxt()
                p.op("tensor", lambda e, a=a_gt: e.matmul(pool[a][:, 0:128], lhsT=Ag[:], rhs=TRI, start=True, stop=False),
                     r=["G", "Ag"], w=["gp%d" % a_gt])
                p.op("tensor", lambda e, a=a_gt: e.matmul(pool[a][:, 0:128], lhsT=c.ident[:], rhs=NMIT, start=False, stop=True),
                     r=["G"], w=["gp%d" % a_gt])
                p.op("scalar", lambda e, a=a_gt: e.activation(out=DecT[:], in_=pool[a][:, 0:128], func=AF.Exp), r=["gp%d" % a_gt], w=["DecT"])
                p.op("vector", lambda e, a=a_qk, t=t: e.scalar_tensor_tensor(out=aT_all[:, t, :], in0=pool[a][:, 0:128],
                                                                             scalar=128.0 ** -0.5, in1=DecT[:],
                                                                             op0=ALU.mult, op1=ALU.mult),
                     r=["gp%d" % a_qk, "DecT"], w=["aT_all"])
                if _chk(22):
                    return
                a_mt = nxt()
                p.op("tensor", lambda e, a=a_mt: e.transpose(out=pool[a][:, 0:128], in_=Am[0][:], identity=c.ident[:]),
                     r=["Am0"], w=["gp%d" % a_mt])
                p.op("scalar", lambda e, a=a_mt: e.copy(out=At[0][:], in_=pool[a][:, 0:128]), r=["gp%d" % a_mt], w=["At0"])
                p.op("vector", lambda e: e.tensor_tensor(out=Rt[0][:], in0=At[0][:], in1=c.ident[:], op=ALU.add),
                     r=["At0"], w=["Rt0"])
                if _chk(23):
                    return
                cur = 0
                for m in range(1, 6):
                    nx = 1 - cur
                    a1 = nxt()
                    p.op("tensor", lambda e, a=a1, cur=cur: e.matmul(pool[a][:, 0:128], lhsT=At[cur][:], rhs=Am[cur][:], start=True, stop=True),
                         r=["At%d" % cur, "Am%d" % cur], w=["gp%d" % a1])
                    p.op("scalar", lambda e, a=a1, nx=nx: e.copy(out=Am[nx][:], in_=pool[a][:, 0:128]), r=["gp%d" % a1], w=["Am%d" % nx])
                    if m < 5:
                        a2 = nxt()
                        p.op("tensor", lambda e, a=a2, cur=cur: e.matmul(pool[a][:, 0:128], lhsT=Am[cur][:], rhs=At[cur][:], start=True, stop=True),
                             r=["At%d" % cur, "Am%d" % cur], w=["gp%d" % a2])
                        p.op("vector", lambda e, a=a2, nx=nx: e.tensor_copy(out=At[nx][:], in_=pool[a][:, 0:128]),
                             r=["gp%d" % a2], w=["At%d" % nx])
                    a3 = nxt()
                    p.op("tensor", lambda e, a=a3, cur=cur, nx=nx: e.matmul(pool[a][:, 0:128], lhsT=Am[nx][:], rhs=Rt[cur][:], start=True, stop=True),
                         r=["Am%d" % nx, "Rt%d" % cur], w=["gp%d" % a3])
                    p.op("vector", lambda e, a=a3, cur=cur, nx=nx: e.tensor_tensor(out=Rt[nx][:], in0=pool[a][:, 0:128], in1=Rt[cur][:], op=ALU.add),
                         r=["gp%d" % a3, "Rt%d" % cur], w=["Rt%d" % nx])
                    cur = nx
                RtF = Rt[cur]
                if _chk(24):
                    return
                a_u = nxt()
                p.op("tensor", lambda e, a=a_u, RtF=RtF: e.matmul(pool[a][:, 0:128], lhsT=RtF[:], rhs=vbe[:], start=True, stop=True),
                     r=["Rt%d" % cur, "vbe"], w=["gp%d" % a_u])
                p.op("scalar", lambda e, a=a_u, t=t: e.copy(out=u_all[:, t, :], in_=pool[a][:, 0:128]), r=["gp%d" % a_u], w=["u_all"])
                a_w = nxt()
                p.op("tensor", lambda e, a=a_w, RtF=RtF: e.matmul(pool[a][:, 0:128], lhsT=kbg[:], rhs=RtF[:], start=True, stop=True),
                     r=["Rt%d" % cur, "kbg"], w=["gp%d" % a_w])
                p.op("vector", lambda e, a=a_w, t=t: e.tensor_copy(out=wT_all[:, t, :], in_=pool[a][:, 0:128]), r=["gp%d" % a_w], w=["wT_all"])
                if _chk(2):
                    return
            if _chk(3):
                return
            p.op("vector", lambda e: e.memset(St[:], 0.0), w=["St"])
            for ch in range(32):
                t = ch // 2
                hf = ch % 2
                rows = slice(hf * 64, hf * 64 + 64)
                ts = slice(t * 128, (t + 1) * 128)
                a1 = nxt()
                p.op("tensor", lambda e, a=a1, t=t: e.matmul(pool[a][:, 0:128], lhsT=wT_all[:, t, :], rhs=St[:], start=True, stop=True),
                     r=["wT_all", "St"], w=["gp%d" % a1])
                p.op("vector", lambda e, a=a1, t=t, rows=rows: e.tensor_tensor(out=vnew[rows, :], in0=u_all[rows, t, :],
                                                                               in1=pool[a][rows, 0:128], op=ALU.subtract),
                     r=["gp%d" % a1, "u_all"], w=["vnew"])
                aA = nxt()
                p.op("tensor", lambda e, a=aA, ts=ts: e.matmul(pool[a][:, 0:128], lhsT=qn[:, ts], rhs=St[:], start=True, stop=True),
                     r=["cv0", "St"], w=["gp%d" % aA])
                aB = nxt()
                p.op("tensor", lambda e, a=aB, t=t: e.matmul(pool[a][:, 0:128], lhsT=aT_all[:, t, :], rhs=vnew[:], start=True, stop=True),
                     r=["aT_all", "vnew"], w=["gp%d" % aB])
                aS = nxt()
                p.op("tensor", lambda e, a=aS, t=t, hf=hf: e.matmul(pool[a][:, 0:128], lhsT=kd_all[:, hf, t, :], rhs=vnew[:],
                                                                    start=True, stop=True),
                     r=["kd_all", "vnew"], w=["gp%d" % aS])
                p.op("scalar", lambda e, a=aA, t=t, h=h, rows=rows: e.activation(out=otmp[rows, :], in_=pool[a][rows, 0:128], func=AF.Identity,
                                                                                 scale=egs[rows, t, h:h + 1]),
                     r=["gp%d" % aA, "egs"], w=["otmp"])
                p.op("vector", lambda e, a=aB, t=t, rows=rows: e.tensor_tensor(out=ob[rows, t, :], in0=otmp[rows, :], in1=pool[a][rows, 0:128],
                                                                               op=ALU.add),
                     r=["gp%d" % aB, "otmp"], w=["ob"])
                p.op("vector", lambda e, a=aS, t=t, hf=hf, h=h: e.scalar_tensor_tensor(out=St[:], in0=St[:], scalar=egl[:, hf, t, h:h + 1],
                                                                                       in1=pool[a][:, 0:128], op0=ALU.mult, op1=ALU.add),
                     r=["gp%d" % aS, "St", "egl"], w=["St"])
            if _chk(4):
                return
            for t in range(16):
                p.op("scalar", lambda e, t=t: e.activation(out=junk[:], in_=ob[:, t, :], func=AF.Square, accum_out=ssq[:, t:t + 1]),
                     r=["ob"], w=["junk", "ssq"])
            p.op("scalar", lambda e: e.activation(out=ssq[:], in_=ssq[:], func=AF.Sqrt, scale=1.0 / 128, bias=c.eps_t[:, 0:1]),
                 r=["ssq"], w=["ssq"])
            p.op("vector", lambda e: e.reciprocal(out=ssq[:], in_=ssq[:]), r=["ssq"], w=["ssq"])
            p.op("scalar", lambda e: e.activation(out=zt[:], in_=zt[:], func=AF.Silu), r=["zt"], w=["zt"])
            p.op("vector", lambda e: e.tensor_tensor(out=ob[:], in0=ob[:], in1=ssq[:].unsqueeze(2).to_broadcast([128, 16, 128]), op=ALU.mult),
                 r=["ob", "ssq"], w=["ob"])
            p.op("vector", lambda e: e.tensor_tensor(out=ob[:], in0=ob[:], in1=gng[:].unsqueeze(1).to_broadcast([128, 16, 128]), op=ALU.mult),
                 r=["ob", "gng"], w=["ob"])
            p.op("vector", lambda e: e.tensor_tensor(out=ob[:], in0=ob[:], in1=zt[:], op=ALU.mult), r=["ob", "zt"], w=["ob"])
            for t in range(16):
                a = nxt()
                p.op("tensor", lambda e, a=a, t=t: e.transpose(out=pool[a][:, 0:128], in_=ob[:, t, :], identity=c.ident[:]),
                     r=["ob"], w=["gp%d" % a])
                p.op("scalar", lambda e, a=a, t=t: e.copy(out=obT[:, t * 128:(t + 1) * 128], in_=pool[a][:, 0:128]),
                     r=["gp%d" % a], w=["obT"])
            p.dma("sync", c.sc_ob[h * 128:(h + 1) * 128, :], obT[:], r=["obT"], w=[("sc_ob", h)])
        p.barrier()


def phase_merge(c, p):
    nc = c.nc
    with ExitStack() as es:
        c2 = Ctx(); c2.nc = nc; c2.es = es
        oa = sb(c2, "oa", [128, 8, 512], F32R)
        obb = sb(c2, "obb", [128, 8, 512], F32R)
        wa = [sb(c2, "wa%d" % i, [128, 8, 128], F32R) for i in range(2)]
        wbb = [sb(c2, "wbb%d" % i, [128, 8, 128], F32R) for i in range(2)]
        ga = [sb(c2, "ga%d" % i, [128, 512]) for i in range(2)]
        gb_ = [sb(c2, "gb%d" % i, [128, 512]) for i in range(2)]
        m1 = sb(c2, "m1", [128, 512])
        mT = sb(c2, "mT", [128, 16, 512], F32R)
        wo = [sb(c2, "wo%d" % i, [128, 16, 512], F32R) for i in range(2)]
        xt = [sb(c2, "xt%d" % i, [128, 512]) for i in range(2)]
        pa = [ps(c2, "pa%d" % i, [128, 512]) for i in range(2)]
        pb = [ps(c2, "pb%d" % i, [128, 512]) for i in range(2)]
        po = [ps(c2, "po%d" % i, [128, 512]) for i in range(4)]
        ci = 0
        wi = 0
        oi = 0
        for tg in range(4):
            tsl = slice(tg * 512, (tg + 1) * 512)
            p.dma("sync", oa[:], r32(c.sc_oa[:, tsl]).rearrange("(kc p) t -> p kc t", p=128), w=["oa"])
            p.dma("sync", obb[:], r32(c.sc_ob[:, tsl]).rearrange("(kc p) t -> p kc t", p=128), w=["obb"])
            for cc in range(16):
                b = ci % 2
                ci += 1
                csl = slice(cc * 128, (cc + 1) * 128)
                p.dma("sync", wa[b][:], r32(c.w_up_a[:, csl]).rearrange("(kc p) n -> p kc n", p=128), w=["wa%d" % b])
                p.dma("sync", wbb[b][:], r32(c.w_up_b[:, csl]).rearrange("(kc p) n -> p kc n", p=128), w=["wbb%d" % b])
                p.dma("sync", ga[b][:], c.sc_fm[5120 + cc * 128:5120 + (cc + 1) * 128, tsl], w=["ga%d" % b])
                p.dma("sync", gb_[b][:], c.sc_fm[7168 + cc * 128:7168 + (cc + 1) * 128, tsl], w=["gb%d" % b])
                for kc in range(8):
                    p.op("tensor", lambda e, b=b, kc=kc: e.matmul(pa[b][:], lhsT=wa[b][:, kc, :], rhs=oa[:, kc, :],
                                                                  start=(kc == 0), stop=(kc == 7)),
                         r=["wa%d" % b, "oa"], w=["pa%d" % b])
                for kc in range(8):
                    p.op("tensor", lambda e, b=b, kc=kc: e.matmul(pb[b][:], lhsT=wbb[b][:, kc, :], rhs=obb[:, kc, :],
                                                                  start=(kc == 0), stop=(kc == 7)),
                         r=["wbb%d" % b, "obb"], w=["pb%d" % b])
                p.op("scalar", lambda e, b=b: e.activation(out=ga[b][:], in_=ga[b][:], func=AF.Sigmoid), r=["ga%d" % b], w=["ga%d" % b])
                p.op("scalar", lambda e, b=b: e.activation(out=gb_[b][:], in_=gb_[b][:], func=AF.Sigmoid), r=["gb%d" % b], w=["gb%d" % b])
                p.op("vector", lambda e, b=b: e.tensor_tensor(out=m1[:], in0=pa[b][:], in1=ga[b][:], op=ALU.mult),
                     r=["pa%d" % b, "ga%d" % b], w=["m1"])
                p.op("vector", lambda e, b=b: e.tensor_tensor(out=gb_[b][:], in0=pb[b][:], in1=gb_[b][:], op=ALU.mult),
                     r=["pb%d" % b, "gb%d" % b], w=["gb%d" % b])
                p.op("vector", lambda e, b=b, cc=cc: e.tensor_tensor(out=mT[:, cc, :], in0=m1[:], in1=gb_[b][:], op=ALU.add),
                     r=["m1", "gb%d" % b], w=["mT"])
            for dg in range(4):
                wb_ = wi % 2
                wi += 1
                dsl = slice(dg * 512, (dg + 1) * 512)
                p.dma("sync", wo[wb_][:], r32(c.w_out[:, dsl]).rearrange("(kc p) n -> p kc n", p=128), w=["wo%d" % wb_])
                for tt in range(4):
                    o = oi % 4
                    oi += 1
                    x_ = oi % 2
                    t0 = tg * 512 + tt * 128
                    p.dma("sync", xt[x_][:], c.x[t0:t0 + 128, dsl], w=["xt%d" % x_])
                    for cc in range(16):
                        p.op("tensor", lambda e, o=o, cc=cc, tt=tt, wb_=wb_: e.matmul(
                            po[o][:], lhsT=mT[:, cc, tt * 128:(tt + 1) * 128], rhs=wo[wb_][:, cc, :],
                            start=(cc == 0), stop=(cc == 15)), r=["mT", "wo%d" % wb_], w=["po%d" % o])
                    p.op("vector", lambda e, o=o, x_=x_: e.tensor_tensor(out=xt[x_][:], in0=po[o][:], in1=xt[x_][:], op=ALU.add),
                         r=["po%d" % o, "xt%d" % x_], w=["xt%d" % x_])
                    p.dma("sync", c.sc_x1[t0:t0 + 128, dsl], xt[x_][:], r=["xt%d" % x_], w=[("sc_x1", t0, dg)])
        p.barrier()


def phase_peer(c, p):
    nc = c.nc
    for half in range(2):
        with ExitStack() as es:
            c2 = Ctx(); c2.nc = nc; c2.es = es
            hT = sb(c2, "h2T", [128, 16, 1024], F32R)
            phase_norm_T(c, p, c.sc_x1, c.norm2_gain, hT, "n2_", half, h_out=c.sc_h2)
            with ExitStack() as es2:
                c3 = Ctx(); c3.nc = nc; c3.es = es2
                wq = [sb(c3, "wq%d" % i, [128, 16, 128], F32R) for i in range(2)]
                skT = sb(c3, "skT", [128, 16, 128], F32R)
                qT = [sb(c3, "qT%d" % i, [128, 1024], F32R) for i in range(2)]
                pq = [ps(c3, "pq%d" % i, [128, 512]) for i in range(4)]
                so_t = [sb(c3, "so_t%d" % i, [128, 512]) for i in range(2)]
                pqi = 0
                p.dma("sync", skT[:], r32(c.skT_d[:, :, :]), w=["skT"])
                for ch in range(16):
                    b = ch % 2
                    p.dma("sync", wq[b][:], r32(c.w_query[:, ch * 128:(ch + 1) * 128]).rearrange("(kc p) n -> p kc n", p=128),
                          w=["wq%d" % b])
                    for tg in range(2):
                        a = pqi % 4
                        pqi += 1
                        for kc in range(16):
                            p.op("tensor", lambda e, a=a, b=b, kc=kc, tg=tg: e.matmul(
                                pq[a][:], lhsT=wq[b][:, kc, :], rhs=hT[:, kc, tg * 512:(tg + 1) * 512],
                                start=(kc == 0), stop=(kc == 15)), r=["wq%d" % b, "hT"], w=["pq%d" % a])
                        p.op("scalar", lambda e, a=a, b=b, tg=tg: e.copy(out=qT[b][:, tg * 512:(tg + 1) * 512], in_=pq[a][:]),
                             r=["pq%d" % a], w=["qT%d" % b])
                    for tq in range(2):
                        a = pqi % 4
                        pqi += 1
                        for tt in range(4):
                            tl = tq * 4 + tt
                            p.op("tensor", lambda e, a=a, b=b, tl=tl, tt=tt, ch=ch: e.matmul(
                                pq[a][:, tt * 128:(tt + 1) * 128], lhsT=qT[b][:, tl * 128:(tl + 1) * 128], rhs=skT[:, ch, :],
                                start=True, stop=True), r=["qT%d" % b, "skT"], w=["pq%d" % a])
                        so = "so%d" % (pqi % 2)
                        sot = so_t[pqi % 2]
                        p.op("vector", lambda e, a=a, sot=sot: e.tensor_copy(out=sot[:], in_=pq[a][:]), r=["pq%d" % a], w=[so])
                        tb = half * 1024 + tq * 512
                        p.dma("sync", c.sc_sc[tb:tb + 512, ch * 128:(ch + 1) * 128].rearrange("(tt p) k -> p tt k", p=128),
                              sot[:].rearrange("p (tt k) -> p tt k", k=128), r=[so], w=[("sc_sc", tb, ch)])
                p.barrier()
    with ExitStack() as es:
        c2 = Ctx(); c2.nc = nc; c2.es = es
        sc = sb(c2, "sc", [128, 16, 128])
        wk = sb(c2, "wk", [128, 128])
        stop_ = sb(c2, "stop", [128, 16, 16])
        itop = sb(c2, "itop", [128, 16, 16], U32)
        itf = sb(c2, "itf", [128, 16, 16])
        cand = sb(c2, "cand", [128, 8, 16, 16])
        cidx = sb(c2, "cidx", [128, 8, 16, 16])
        wk2 = sb(c2, "wk2", [128, 256])
        best = sb(c2, "best", [128, 8, 16])
        pos = sb(c2, "pos", [128, 8, 16], U32)
        posf = sb(c2, "posf", [128, 8, 16])
        iota = sb(c2, "iota", [128, 256])
        junk2 = sb(c2, "junk2", [128, 256])
        eidf = sb(c2, "eidf", [128, 128])
        eid = sb(c2, "eid", [128, 128], U32)
        nmx = sb(c2, "nmx", [128, 8])
        gsum = sb(c2, "gsum", [128, 8])
        gate = sb(c2, "gate", [128, 8, 16])
        dots = sb(c2, "dots", [128, 128])
        gact = sb(c2, "gact", [128, 128])
        h2 = sb(c2, "h2", [128, D])
        NB = 6
        gu = [sb(c2, "gu%d" % i, [128, D], F32R) for i in range(NB)]
        diag = [sb(c2, "diag%d" % i, [128, 128], F32R) for i in range(2)]
        po = [ps(c2, "po%d" % i, [128, 512]) for i in range(4)]
        junk = sb(c2, "junkp", [128, D])
        acc = sb(c2, "acc", [128, D])
        x1 = sb(c2, "x1", [128, D])
        p.dma("sync", iota[:], c.iota_d[:, :], w=["iota"])
        gi = 0
        for t in range(16):
            t0 = t * 128
            p.dma("sync", sc[:], c.sc_sc[t0:t0 + 128, :].rearrange("p (c k) -> p c k", k=128), w=["sc"])
            p.dma("sync", h2[:], c.sc_h2[t0:t0 + 128, :], w=["h2"])
            p.dma("sync", x1[:], c.sc_x1[t0:t0 + 128, :], w=["x1"])
            for ch in range(16):
                p.op("vector", lambda e, ch=ch: e.max(out=stop_[:, ch, 0:8], in_=sc[:, ch, :]), r=["sc"], w=["stop"])
                p.op("vector", lambda e, ch=ch: e.max_index(out=itop[:, ch, 0:8], in_max=stop_[:, ch, 0:8], in_values=sc[:, ch, :]),
                     r=["sc", "stop"], w=["itop"])
                p.op("vector", lambda e, ch=ch: e.match_replace(out=wk[:], in_to_replace=stop_[:, ch, 0:8], in_values=sc[:, ch, :],
                                                                imm_value=-1e30), r=["sc", "stop"], w=["wk"])
                p.op("vector", lambda e, ch=ch: e.max(out=stop_[:, ch, 8:16], in_=wk[:]), r=["wk"], w=["stop"])
                p.op("vector", lambda e, ch=ch: e.max_index(out=itop[:, ch, 8:16], in_max=stop_[:, ch, 8:16], in_values=wk[:]),
                     r=["wk", "stop"], w=["itop"])
            p.op("vector", lambda e: e.tensor_copy(out=itf[:], in_=itop[:]), r=["itop"], w=["itf"])
            s4 = stop_[:].rearrange("p (h two) k -> p h two k", two=2)
            i4 = itf[:].rearrange("p (h two) k -> p h two k", two=2)
            p.op("vector", lambda e, s4=s4: e.tensor_tensor(out=cand[:], in0=s4[:, :, 0, :].unsqueeze(3).to_broadcast([128, 8, 16, 16]),
                                                            in1=s4[:, :, 1, :].unsqueeze(2).to_broadcast([128, 8, 16, 16]), op=ALU.add),
                 r=["stop"], w=["cand"])
            for hh in range(8):
                p.op("vector", lambda e, i4=i4, hh=hh: e.scalar_tensor_tensor(
                    out=cidx[:, hh], in0=i4[:, hh, 0, :].unsqueeze(2).to_broadcast([128, 16, 16]), scalar=128.0,
                    in1=i4[:, hh, 1, :].unsqueeze(1).to_broadcast([128, 16, 16]), op0=ALU.mult, op1=ALU.add),
                    r=["itf"], w=["cidx"])
            for hh in range(8):
                cv_ = cand[:, hh].rearrange("p a b -> p (a b)")
                p.op("vector", lambda e, hh=hh, cv_=cv_: e.max(out=best[:, hh, 0:8], in_=cv_), r=["cand"], w=["best"])
                p.op("vector", lambda e, hh=hh, cv_=cv_: e.max_index(out=pos[:, hh, 0:8], in_max=best[:, hh, 0:8], in_values=cv_),
                     r=["cand", "best"], w=["pos"])
                p.op("vector", lambda e, hh=hh, cv_=cv_: e.match_replace(out=wk2[:], in_to_replace=best[:, hh, 0:8], in_values=cv_,
                                                                         imm_value=-1e30), r=["cand", "best"], w=["wk2"])
                p.op("vector", lambda e, hh=hh: e.max(out=best[:, hh, 8:16], in_=wk2[:]), r=["wk2"], w=["best"])
                p.op("vector", lambda e, hh=hh: e.max_index(out=pos[:, hh, 8:16], in_max=best[:, hh, 8:16], in_values=wk2[:]),
                     r=["wk2", "best"], w=["pos"])
            p.op("vector", lambda e: e.tensor_copy(out=posf[:], in_=pos[:]), r=["pos"], w=["posf"])
            for hh in range(8):
                ci_ = cidx[:, hh].rearrange("p a b -> p (a b)")
                for m in range(16):
                    p.op("vector", lambda e, hh=hh, m=m, ci_=ci_: e.scalar_tensor_tensor(
                        out=junk2[:], in0=iota[:], scalar=posf[:, hh, m:m + 1], in1=ci_, op0=ALU.is_equal, op1=ALU.mult,
                        accum_out=eidf[:, hh * 16 + m:hh * 16 + m + 1]), r=["iota", "posf", "cidx"], w=["junk2", "eidf"])
            p.op("vector", lambda e: e.tensor_copy(out=eid[:], in_=eidf[:]), r=["eidf"], w=["eid"])
            p.op("vector", lambda e: e.tensor_scalar(out=nmx[:], in0=best[:, :, 0], scalar1=-1.0, scalar2=None, op0=ALU.mult),
                 r=["best"], w=["nmx"])
            for hh in range(8):
                p.op("scalar", lambda e, hh=hh: e.activation(out=gate[:, hh, :], in_=best[:, hh, :], func=AF.Exp, bias=nmx[:, hh:hh + 1],
                                                             accum_out=gsum[:, hh:hh + 1]), r=["best", "nmx"], w=["gate", "gsum"])
            p.op("vector", lambda e: e.reciprocal(out=gsum[:], in_=gsum[:]), r=["gsum"], w=["gsum"])
            p.op("vector", lambda e: e.tensor_tensor(out=gate[:], in0=gate[:], in1=gsum[:].unsqueeze(2).to_broadcast([128, 8, 16]), op=ALU.mult),
                 r=["gate", "gsum"], w=["gate"])
            for s_ in range(128):
                b = gi % NB
                gi += 1
                p.dma_fn("gpsimd", lambda e, b=b, s_=s_: e.indirect_dma_start(
                    out=gu[b][:], out_offset=None, in_=r32(c.peer_u[:, :]),
                    in_offset=bass.IndirectOffsetOnAxis(ap=eid[:, s_:s_ + 1], axis=0)), r=["eid"], w=["gu%d" % b])
                p.op("vector", lambda e, b=b, s_=s_: e.scalar_tensor_tensor(out=junk[:], in0=gu[b][:].bitcast(F32), scalar=1.0, in1=h2[:],
                                                                            op0=ALU.mult, op1=ALU.mult, accum_out=dots[:, s_:s_ + 1]),
                     r=["gu%d" % b, "h2"], w=["junkp", "dots"])
            p.op("scalar", lambda e: e.activation(out=gact[:], in_=dots[:], func=AF.Gelu), r=["dots"], w=["gact"])
            p.op("vector", lambda e: e.tensor_tensor(out=gact[:], in0=gact[:], in1=gate[:].rearrange("p h k -> p (h k)"), op=ALU.mult),
                 r=["gact", "gate"], w=["gact"])
            for s_ in range(128):
                b = gi % NB
                gi += 1
                db = s_ % 2
                p.dma_fn("gpsimd", lambda e, b=b, s_=s_: e.indirect_dma_start(
                    out=gu[b][:], out_offset=None, in_=r32(c.peer_v[:, :]),
                    in_offset=bass.IndirectOffsetOnAxis(ap=eid[:, s_:s_ + 1], axis=0)), r=["eid"], w=["gu%d" % b])
                p.op("vector", lambda e, db=db, s_=s_: e.tensor_scalar(out=diag[db][:], in0=c.ident[:], scalar1=gact[:, s_:s_ + 1],
                                                                       scalar2=None, op0=ALU.mult),
                     r=["gact"], w=["diag%d" % db])
                for dg in range(4):
                    p.op("tensor", lambda e, b=b, db=db, dg=dg, s_=s_: e.matmul(
                        po[dg][:], lhsT=diag[db][:], rhs=gu[b][:, dg * 512:(dg + 1) * 512], start=(s_ == 0), stop=(s_ == 127)),
                        r=["diag%d" % db, "gu%d" % b], w=["po%d" % dg])
            for dg in range(4):
                dsl = slice(dg * 512, (dg + 1) * 512)
                p.op("vector", lambda e, dg=dg, dsl=dsl: e.tensor_tensor(out=acc[:, dsl], in0=po[dg][:], in1=x1[:, dsl], op=ALU.add),
                     r=["po%d" % dg, "x1"], w=["acc"])
            p.dma("sync", c.y[t0:t0 + 128, :], acc[:], r=["acc"], w=[("y", t)])
        p.barrier()


ALL_PHASES = ("inproj", "moba", "gdn", "merge", "peer")


def build_nc(debug=False, phases=ALL_PHASES):
    nc = bass.Bass("TRN2", target_bir_lowering=False)
    nc.dge_precook = False
    c = Ctx()
    c.nc = nc
    kind_s = "ExternalOutput" if debug else "Internal"

    def din(name, shape, dt=F32):
        return nc.dram_tensor(name, list(shape), dt, kind="ExternalInput").ap()

    def dsc(name, shape):
        return nc.dram_tensor(name, list(shape), F32, kind=kind_s).ap()

    c.x = din("x", [S, D])
    c.norm1_gain = din("norm1_gain", [1, D])
    c.w_in = din("w_in", [D, IN_TOTAL])
    c.ident_d = din("ident", [128, 128])
    c.ones_d = din("ones", [128, 128])
    c.rel_bias = din("rel_bias", [32, 8])
    c.q_norm_gain = din("q_norm_gain", [1, 128])
    c.k_norm_gain = din("k_norm_gain", [1, 128])
    c.d01_d = din("d01", [8, 2, 128, 128])
    c.cm_d = din("cm", [128, 16, 8])
    c.notown_d = din("notown", [128, 16, 8])
    c.esel_d = din("esel", [8, 8, 128])
    c.gdnc_d = din("gdnc", [7, 128, 128])
    c.conv_wT = din("conv_wT", [3072, 4])
    c.a_log = din("a_log", [1, 8])
    c.dt_bias = din("dt_bias", [1, 8])
    c.gdn_norm_gain = din("gdn_norm_gain", [1, 128])
    c.w_up_a = din("w_up_a", [1024, D])
    c.w_up_b = din("w_up_b", [1024, D])
    c.w_out = din("w_out", [D, D])
    if "peer" in phases:
        c.norm2_gain = din("norm2_gain", [1, D])
        c.w_query = din("w_query", [D, D])
        c.skT_d = din("skT", [128, 16, 128])
        c.peer_u = din("peer_u", [16384, D])
        c.peer_v = din("peer_v", [16384, D])
        c.iota_d = din("iota", [128, 256])
        c.sc_h2 = dsc("sc_h2", [S, D])
        c.sc_sc = dsc("sc_sc", [S, D])
    c.sc_fm = dsc("sc_fm", [N_FM, S])
    c.sc_tm = dsc("sc_tm", [S, N_TM])
    c.sc_oa = dsc("sc_oa", [1024, S])
    c.sc_ob = dsc("sc_ob", [1024, S])
    if "merge" in phases or "peer" not in phases:
        c.sc_x1 = dsc("sc_x1", [S, D])
    else:
        c.sc_x1 = din("sc_x1", [S, D])
    c.y = nc.dram_tensor("y", [S, D], F32, kind="ExternalOutput").ap()

    with ExitStack() as es:
        c.es = es
        p = Prog(nc, es)
        block = es.enter_context(nc.Block())
        c.ident = sb(c, "ident_s", [128, 128])
        c.ident_r = sb(c, "ident_r", [128, 128], F32R)
        c.ones_r = sb(c, "ones_r", [128, 128], F32R)
        c.eps_t = sb(c, "eps_t", [128, 1])
        c.one_t = sb(c, "one_t", [128, 1])
        p.dma("sync", c.ident[:], c.ident_d[:, :], w=["ident"])
        p.dma("sync", c.ident_r[:], r32(c.ident_d[:, :]), w=["ident_r"])
        p.dma("sync", c.ones_r[:], r32(c.ones_d[:, :]), w=["ones_r"])
        p.op("vector", lambda e: e.memset(c.eps_t[:], EPS), w=["eps"])
        p.op("vector", lambda e: e.memset(c.one_t[:], 1.0), w=["one"])
        p.barrier()
        if "inproj" in phases:
            phase_inproj(c, p)
        if "moba" in phases:
            phase_moba(c, p)
        if "gdn" in phases:
            phase_gdn(c, p)
        if "merge" in phases:
            phase_merge(c, p)
        if "peer" in phases:
            phase_peer(c, p)
        p.finish(block)
    return nc


def make_in_maps(inputs, phases=ALL_PHASES):
    f = lambda a: np.ascontiguousarray(np.asarray(a, dtype=np.float32))
    rel_bias = f(inputs["rel_bias"])
    d01, cm, notown, esel = moba_consts(rel_bias)
    shared = {
        "norm1_gain": f(inputs["norm1_gain"]), "w_in": f(inputs["w_in"][0]),
        "ident": np.eye(128, dtype=np.float32), "ones": np.ones((128, 128), np.float32),
        "rel_bias": rel_bias, "q_norm_gain": f(inputs["q_norm_gain"]), "k_norm_gain": f(inputs["k_norm_gain"]),
        "d01": d01, "cm": cm, "notown": notown, "esel": esel, "gdnc": gdn_consts(),
        "conv_wT": f(np.asarray(inputs["conv_w"][0]).T), "a_log": f(inputs["a_log"]), "dt_bias": f(inputs["dt_bias"]),
        "gdn_norm_gain": f(inputs["gdn_norm_gain"]), "w_up_a": f(inputs["w_up_a"][0]), "w_up_b": f(inputs["w_up_b"][0]),
        "w_out": f(inputs["w_out"][0]),
    }
    if "peer" in phases:
        sk = np.asarray(inputs["peer_sub_keys"][0], dtype=np.float32).reshape(16, 128, 128)
        shared.update({
            "norm2_gain": f(inputs["norm2_gain"]), "w_query": f(inputs["peer_w_query"][0]),
            "skT": f(sk.transpose(2, 0, 1)), "peer_u": f(inputs["peer_u"][0]), "peer_v": f(inputs["peer_v"][0]),
            "iota": np.broadcast_to(np.arange(256, dtype=np.float32), (128, 256)).copy(),
        })
    maps = []
    for b in range(8):
        m = dict(shared)
        m["x"] = f(inputs["x"][b])
        maps.append(m)
    return maps


_NC_CACHE = {}


def kernel(**inputs):
    if "nc" not in _NC_CACHE:
        _NC_CACHE["nc"] = build_nc()
    nc = _NC_CACHE["nc"]
    maps = make_in_maps(inputs)
    res = run_bass_kernel_spmd(nc, maps, core_ids=list(range(8)))
    return np.stack([np.asarray(r["y"], dtype=np.float32) for r in res.results], axis=0)
```

```python
import math
from contextlib import ExitStack

import numpy as np
import concourse.bass as bass
import concourse.mybir as mybir
from concourse.bass_utils import run_bass_kernel_spmd

F32 = mybir.dt.float32
F32R = mybir.dt.float32r
U32 = mybir.dt.uint32
I32 = mybir.dt.int32
AF = mybir.ActivationFunctionType
ALU = mybir.AluOpType
AX = mybir.AxisListType

D = 2048
S = 2048
NT = S // 128
IN_TOTAL = 11280
EPS = 1e-6
NEG = -30000.0

COMPUTE = ("tensor", "vector", "scalar", "gpsimd")
ALLENG = ("tensor", "vector", "scalar", "gpsimd", "sync")


import re as _re
_PSUM_KEY = _re.compile(r"^(n\d_tp|gp|aux|scp|op|dp|pp|pa|pb|po|pq)\d+$")


class Op:
    __slots__ = ("eng", "fn", "deps", "needed", "is_dma", "slot", "use", "tok", "idx")


class Prog:
    def __init__(self, nc, es, ndma=8):
        self.nc = nc
        self.ops = {e: [] for e in ALLENG}
        self.last_w = {}
        self.rd_comp = {}
        self.rd_dma = {}
        self.sem = {e: es.enter_context(nc.semaphore("s_" + e)) for e in COMPUTE}
        self.dsem = {}
        self.dlast = {}
        self.dnext = {}
        self.duse = {}
        for q in ("sync", "gpsimd", "scalar"):
            self.dsem[q] = [es.enter_context(nc.semaphore("d_%s%d" % (q, i))) for i in range(ndma)]
            self.dlast[q] = [None] * ndma
            self.duse[q] = [0] * ndma
            self.dnext[q] = 0
        self.pending_barrier = {e: None for e in ALLENG}
        self.all_dma = []

    def _add(self, eng, fn, r, w, is_dma):
        o = Op()
        o.eng = eng
        o.fn = fn
        o.needed = False
        o.is_dma = is_dma
        o.slot = None
        o.use = 0
        deps = set()
        for k in r:
            lw = self.last_w.get(k)
            if lw is not None:
                deps.add(lw)
            if isinstance(k, str) and _PSUM_KEY.match(k):
                for en, ro in self.rd_comp.get(k, {}).items():
                    if en != eng:
                        deps.add(ro)
        for k in w:
            lw = self.last_w.get(k)
            if lw is not None:
                deps.add(lw)
            for ro in self.rd_comp.get(k, {}).values():
                deps.add(ro)
            for ro in self.rd_dma.get(k, ()):
                deps.add(ro)
        if is_dma:
            q = eng
            sl = self.dnext[q]
            self.dnext[q] = (sl + 1) % len(self.dsem[q])
            prev = self.dlast[q][sl]
            if prev is not None:
                deps.add(prev)
            self.duse[q][sl] += 1
            o.slot = sl
            o.use = self.duse[q][sl]
            self.dlast[q][sl] = o
            self.all_dma.append(o)
        pb = self.pending_barrier[eng]
        if pb is not None:
            deps |= pb
            self.pending_barrier[eng] = None
        if eng == "tensor":
            deps = {d for d in deps if not (d.eng == "tensor" and not d.is_dma)}
        o.deps = deps
        for d in deps:
            d.needed = True
        for k in w:
            self.last_w[k] = o
            self.rd_comp[k] = {}
            self.rd_dma[k] = []
        for k in r:
            if is_dma:
                self.rd_dma.setdefault(k, []).append(o)
            else:
                self.rd_comp.setdefault(k, {})[eng] = o
        o.idx = len(self.ops[eng])
        self.ops[eng].append(o)
        return o

    def op(self, eng, fn, r=(), w=()):
        return self._add(eng, fn, tuple(r), tuple(w), False)

    def dma(self, q, out, in_, r=(), w=(), **kw):
        return self._add(q, lambda e: e.dma_start(out=out, in_=in_, **kw), tuple(r), tuple(w), True)

    def dma_fn(self, q, fn, r=(), w=()):
        return self._add(q, fn, tuple(r), tuple(w), True)

    def barrier(self):
        deps = set()
        for e in ALLENG:
            if self.ops[e]:
                last = [o for o in self.ops[e] if not o.is_dma]
                if last:
                    deps.add(last[-1])
        for o in self.all_dma:
            deps.add(o)
        self.all_dma = []
        for e in ALLENG:
            pb = self.pending_barrier[e]
            self.pending_barrier[e] = set(deps) | (pb or set())
        self.last_w = {}
        self.rd_comp = {}
        self.rd_dma = {}

    def finish(self, block):
        self.barrier()
        nc = self.nc
        for e in ALLENG:
            self._add(e, None, (), (), False)
        for e in COMPUTE:
            c = 0
            for o in self.ops[e]:
                if o.is_dma:
                    o.tok = (self.dsem[e][o.slot], 16 * o.use)
                elif o.needed:
                    c += 1
                    o.tok = (self.sem[e], c)
                else:
                    o.tok = None
        for o in self.ops["sync"]:
            if o.is_dma:
                o.tok = (self.dsem["sync"][o.slot], 16 * o.use)
            else:
                o.tok = None

        def emit(engname):
            def body(eng):
                waited = {}
                for o in self.ops[engname]:
                    for d in o.deps:
                        sem, val = d.tok
                        key = id(sem)
                        if waited.get(key, 0) < val:
                            eng.wait_ge(sem, val)
                            waited[key] = val
                    if o.fn is None:
                        continue
                    ins = o.fn(eng)
                    if o.is_dma:
                        ins.then_inc(o.tok[0], 16)
                    elif o.needed:
                        ins.then_inc(o.tok[0], 1)
            return body

        block.tensor(emit("tensor"))
        block.vector(emit("vector"))
        block.scalar(emit("scalar"))
        block.gpsimd(emit("gpsimd"))
        block.sync(emit("sync"))


def r32(ap):
    return ap.bitcast(F32R)


class Ctx:
    pass


_uid = [0]


def _un(name):
    _uid[0] += 1
    return "%s_%d" % (name, _uid[0])


def sb(c, name, shape, dt=F32):
    return c.es.enter_context(c.nc.sbuf_tensor(_un(name), list(shape), dt))


def ps(c, name, shape, dt=F32):
    return c.es.enter_context(c.nc.psum_tensor(_un(name), list(shape), dt))


FM_SRC = [(0, 1024), (1024, 1024), (3072, 3072), (7184, 2048), (9232, 2048)]
TM_SRC = [(2048, 1024), (6144, 1024), (7168, 16)]
N_FM = 9216
N_TM = 2064


def phase_norm_T(c, p, x_ap, gain_ap, hT, pfx, half, h_out=None):
    nc = c.nc
    with ExitStack() as es:
        c2 = Ctx(); c2.nc = nc; c2.es = es
        gbc = sb(c2, pfx + "gbc", [128, D])
        xt = [sb(c2, pfx + "xt%d" % i, [128, D]) for i in range(2)]
        ht = [sb(c2, pfx + "ht%d" % i, [128, D]) for i in range(2)]
        sq = sb(c2, pfx + "sq", [128, D])
        st = [sb(c2, pfx + "st%d" % i, [128, 4]) for i in range(2)]
        tp = [ps(c2, pfx + "tp%d" % i, [128, 512]) for i in range(4)]
        p.dma("sync", gbc[:], gain_ap.partition_broadcast(128), w=[pfx + "gbc"])
        for ti in range(8):
            t = half * 8 + ti
            b = ti % 2
            p.dma("sync", xt[b][:], x_ap[t * 128:(t + 1) * 128, :], w=[pfx + "xt%d" % b])
            p.op("scalar", lambda e, b=b: e.activation(out=sq[:], in_=xt[b][:], func=AF.Square,
                                                       accum_out=st[b][:, 0:1]),
                 r=[pfx + "xt%d" % b], w=[pfx + "sq", pfx + "st%d" % b])
            p.op("scalar", lambda e, b=b: e.activation(out=st[b][:, 1:2], in_=st[b][:, 0:1], func=AF.Sqrt,
                                                       scale=1.0 / D, bias=c.eps_t[:, 0:1]),
                 r=[pfx + "st%d" % b], w=[pfx + "st%d" % b])
            p.op("vector", lambda e, b=b: e.reciprocal(out=st[b][:, 2:3], in_=st[b][:, 1:2]),
                 r=[pfx + "st%d" % b], w=[pfx + "st%d" % b])
            p.op("vector", lambda e, b=b: e.scalar_tensor_tensor(out=ht[b][:], in0=xt[b][:], scalar=st[b][:, 2:3],
                                                                 in1=gbc[:], op0=ALU.mult, op1=ALU.mult),
                 r=[pfx + "xt%d" % b, pfx + "st%d" % b, pfx + "gbc"], w=[pfx + "ht%d" % b])
            if h_out is not None:
                p.dma("sync", h_out[t * 128:(t + 1) * 128, :], ht[b][:], r=[pfx + "ht%d" % b], w=[(pfx + "hout", t)])
            for g4 in range(4):
                pb = tp[g4]
                for j in range(4):
                    kc = g4 * 4 + j
                    p.op("tensor", lambda e, b=b, kc=kc, j=j, pb=pb: e.transpose(
                        out=pb[:, j * 128:(j + 1) * 128], in_=ht[b][:, kc * 128:(kc + 1) * 128], identity=c.ident[:]),
                        r=[pfx + "ht%d" % b], w=[pfx + "tp%d" % g4])
                eng = "scalar" if g4 % 2 == 0 else "vector"
                dst = hT[:, g4 * 4:(g4 + 1) * 4, ti * 128:(ti + 1) * 128]
                src = pb[:].rearrange("p (j t) -> p j t", j=4)
                if eng == "scalar":
                    p.op("scalar", lambda e, dst=dst, src=src: e.copy(out=dst, in_=src),
                         r=[pfx + "tp%d" % g4], w=["hT"])
                else:
                    p.op("vector", lambda e, dst=dst, src=src: e.tensor_copy(out=dst, in_=src),
                         r=[pfx + "tp%d" % g4], w=["hT"])
        p.barrier()


def phase_inproj(c, p):
    nc = c.nc
    for half in range(2):
        with ExitStack() as es:
            c2 = Ctx(); c2.nc = nc; c2.es = es
            hT = sb(c2, "hT", [128, 16, 1024], F32R)
            phase_norm_T(c, p, c.x, c.norm1_gain, hT, "n1_", half)
            with ExitStack() as es2:
                c3 = Ctx(); c3.nc = nc; c3.es = es2
                wb = [sb(c3, "wb%d" % i, [128, 16, 256], F32R) for i in range(2)]
                ob = [sb(c3, "ob%d" % i, [128, 1024]) for i in range(2)]
                pp = [ps(c3, "pp%d" % i, [128, 512]) for i in range(4)]
                gi = 0
                oi = 0
                pi = 0
                t0 = half * 1024
                row = 0
                for (c0, n) in FM_SRC:
                    for g in range(n // 256):
                        col = c0 + g * 256
                        b = gi % 2
                        gi += 1
                        p.dma("sync", wb[b][:], r32(c.w_in[:, col:col + 256]).rearrange("(kc p) n -> p kc n", p=128),
                              w=["wb%d" % b])
                        for cc in range(2):
                            o = oi % 2
                            oi += 1
                            for tg in range(2):
                                pb = pi % 4
                                pi += 1
                                for kc in range(16):
                                    p.op("tensor", lambda e, b=b, cc=cc, tg=tg, kc=kc, pb=pb: e.matmul(
                                        pp[pb][:], lhsT=wb[b][:, kc, cc * 128:(cc + 1) * 128],
                                        rhs=hT[:, kc, tg * 512:(tg + 1) * 512],
                                        start=(kc == 0), stop=(kc == 15)),
                                        r=["wb%d" % b, "hT"], w=["pp%d" % pb])
                                if tg == 0:
                                    p.op("scalar", lambda e, o=o, pb=pb: e.copy(out=ob[o][:, 0:512], in_=pp[pb][:]),
                                         r=["pp%d" % pb], w=["ob%d" % o])
                                else:
                                    p.op("vector", lambda e, o=o, pb=pb: e.tensor_copy(out=ob[o][:, 512:1024], in_=pp[pb][:]),
                                         r=["pp%d" % pb], w=["ob%d" % o])
                            rr = row + g * 256 + cc * 128
                            p.dma("sync", c.sc_fm[rr:rr + 128, t0:t0 + 1024], ob[o][:],
                                  r=["ob%d" % o], w=[("sc_fm", rr // 128)])
                    row += n
                colo = 0
                for (c0, n) in TM_SRC:
                    w = min(n, 256)
                    for g in range(max(1, n // 256)):
                        col = c0 + g * 256
                        b = gi % 2
                        gi += 1
                        p.dma("sync", wb[b][:, :, 0:w], r32(c.w_in[:, col:col + w]).rearrange("(kc p) n -> p kc n", p=128),
                              w=["wb%d" % b])
                        for tq in range(2):
                            o = oi % 2
                            oi += 1
                            for tt in range(4):
                                tl = tq * 4 + tt
                                pb = pi % 4
                                pi += 1
                                for kc in range(16):
                                    p.op("tensor", lambda e, b=b, tl=tl, kc=kc, pb=pb, w=w: e.matmul(
                                        pp[pb][:, 0:w], lhsT=hT[:, kc, tl * 128:(tl + 1) * 128],
                                        rhs=wb[b][:, kc, 0:w],
                                        start=(kc == 0), stop=(kc == 15)),
                                        r=["wb%d" % b, "hT"], w=["pp%d" % pb])
                                if tt % 2 == 0:
                                    p.op("scalar", lambda e, o=o, pb=pb, tt=tt, w=w: e.copy(
                                        out=ob[o][:, tt * 256:tt * 256 + w], in_=pp[pb][:, 0:w]),
                                        r=["pp%d" % pb], w=["ob%d" % o])
                                else:
                                    p.op("vector", lambda e, o=o, pb=pb, tt=tt, w=w: e.tensor_copy(
                                        out=ob[o][:, tt * 256:tt * 256 + w], in_=pp[pb][:, 0:w]),
                                        r=["pp%d" % pb], w=["ob%d" % o])
                            tb = t0 + tq * 512
                            cw = colo + g * 256
                            p.dma("sync",
                                  c.sc_tm[tb:tb + 512, cw:cw + w].rearrange("(tt p) n -> p tt n", p=128),
                                  ob[o][:].rearrange("p (tt n) -> p tt n", n=256)[:, :, 0:w],
                                  r=["ob%d" % o], w=[("sc_tm", tb // 128, cw)])
                    colo += n
                p.barrier()


def t5_bucket_np(n):
    n = np.maximum(n, 0)
    nf = np.maximum(n, 1).astype(np.float32)
    large = 16 + (np.log(nf / np.float32(16)) / np.float32(math.log(8.0)) * np.float32(16)).astype(np.int32)
    large = np.minimum(large, 31)
    return np.where(n < 16, n, large)


def moba_consts(rel_bias):
    k = np.arange(128)[:, None]
    q = np.arange(128)[None, :]
    out = np.zeros((8, 2, 128, 128), np.float32)
    b0 = t5_bucket_np(q - k)
    b1 = t5_bucket_np(q - k + 128)
    for h in range(8):
        out[h, 0] = np.where(q >= k, rel_bias[b0, h], np.float32(NEG))
        out[h, 1] = rel_bias[b1, h]
    cm = np.zeros((128, 16, 8), np.float32)
    notown = np.ones((128, 16, 8), np.float32)
    for t in range(16):
        for n in range(8):
            if n >= t // 2:
                cm[:, t, n] = -1e30
            if n == t // 2:
                notown[:, t, n] = 0.0
    esel = np.zeros((8, 8, 128), np.float32)
    for n in range(8):
        esel[n, n, :] = 1.0
    return out, cm, notown, esel


def phase_moba(c, p):
    nc = c.nc
    with ExitStack() as es:
        c2 = Ctx(); c2.nc = nc; c2.es = es
        cm = sb(c2, "cm", [128, 16, 8])
        notown = sb(c2, "notown", [128, 16, 8])
        esel = sb(c2, "esel", [8, 8, 128], F32R)
        cb = sb(c2, "cb", [128, 8])
        gq = sb(c2, "gq", [128, 2])
        gk = sb(c2, "gk", [128, 1])
        d01 = sb(c2, "d01", [128, 2, 128], F32R)
        d01r = sb(c2, "d01r", [128, 2, 128])
        qr = sb(c2, "qr", [128, S])
        kr = sb(c2, "kr", [128, S])
        sq = sb(c2, "sq", [128, S], F32R)
        qn = sb(c2, "qn", [128, S], F32R)
        kn = sb(c2, "kn", [128, S], F32R)
        vt = sb(c2, "vt", [128, 16, 128], F32R)
        rs = sb(c2, "rs", [128, 512])
        km = sb(c2, "km", [128, 8], F32R)
        kmf = sb(c2, "kmf", [128, 8])
        gm = sb(c2, "gm", [128, 16, 8])
        cmp_ = sb(c2, "cmp", [128, 16, 8, 8])
        rank = sb(c2, "rank", [128, 16, 8])
        nmk = sb(c2, "nmk", [128, 16, 8])
        negT = sb(c2, "negT", [8, S], F32R)
        pT = [sb(c2, "pT%d" % i, [128, 512], F32R) for i in range(2)]
        rden = sb(c2, "rden", [128, 512])
        oo = [sb(c2, "oo%d" % i, [128, 512]) for i in range(2)]
        aux = [ps(c2, "aux%d" % i, [128, 512]) for i in range(2)]
        scp = [ps(c2, "scp%d" % i, [128, 512]) for i in range(2)]
        op_ = [ps(c2, "op%d" % i, [128, 512]) for i in range(2)]
        dp = [ps(c2, "dp%d" % i, [128, 512]) for i in range(2)]

        p.dma("sync", cm[:], c.cm_d[:, :, :], w=["cm"])
        p.dma("sync", notown[:], c.notown_d[:, :, :], w=["notown"])
        p.dma("sync", esel[:], r32(c.esel_d[:, :, :]), w=["esel"])
        p.dma("sync", cb[:], c.rel_bias[31:32, :].partition_broadcast(128), w=["cb"])
        p.dma("sync", gq[:, 0:1], c.q_norm_gain.rearrange("o d -> d o"), w=["gq"])
        p.dma("sync", gk[:, 0:1], c.k_norm_gain.rearrange("o d -> d o"), w=["gk"])
        p.op("vector", lambda e: e.tensor_scalar(out=gq[:, 1:2], in0=gq[:, 0:1], scalar1=128.0 ** -0.5, scalar2=None,
                                                 op0=ALU.mult), r=["gq"], w=["gq"])
        auxi = 0
        sci = 0
        gi = 0
        for h in range(8):
            p.dma("sync", qr[:], c.sc_fm[h * 128:(h + 1) * 128, :], w=["qr"])
            p.dma("sync", kr[:], c.sc_fm[1024 + h * 128:1024 + (h + 1) * 128, :], w=["kr"])
            p.dma("sync", vt[:], r32(c.sc_tm[:, h * 128:(h + 1) * 128]).rearrange("(t p) d -> p t d", p=128), w=["vt"])
            p.dma("sync", d01r[:], c.d01_d[h].rearrange("a k q -> k a q"), w=["d01r"])
            p.op("vector", lambda e, h=h: e.tensor_scalar(out=d01[:], in0=d01r[:], scalar1=cb[:, h:h + 1], scalar2=None,
                                                          op0=ALU.subtract), r=["d01r", "cb"], w=["d01"])
            for (raw, dst, gcol, rk, wk) in ((qr, qn, gq[:, 1:2], "qr", "qn"), (kr, kn, gk[:, 0:1], "kr", "kn")):
                p.op("scalar", lambda e, raw=raw: e.activation(out=sq[:], in_=raw[:], func=AF.Square), r=[rk], w=["sq"])
                for tg in range(4):
                    a = auxi % 2
                    auxi += 1
                    sl = slice(tg * 512, (tg + 1) * 512)
                    p.op("tensor", lambda e, a=a, sl=sl: e.matmul(aux[a][:], lhsT=c.ones_r[:], rhs=sq[:, sl], start=True, stop=True),
                         r=["sq"], w=["aux%d" % a])
                    p.op("scalar", lambda e, a=a: e.activation(out=rs[:], in_=aux[a][:], func=AF.Sqrt, scale=1.0 / 128,
                                                               bias=c.eps_t[:, 0:1]), r=["aux%d" % a], w=["rs"])
                    p.op("vector", lambda e: e.reciprocal(out=rs[:], in_=rs[:]), r=["rs"], w=["rs"])
                    p.op("vector", lambda e, raw=raw, dst=dst, gcol=gcol, sl=sl: e.scalar_tensor_tensor(
                        out=dst[:, sl], in0=raw[:, sl], scalar=gcol, in1=rs[:], op0=ALU.mult, op1=ALU.mult),
                        r=[rk, "rs", "gq", "gk"], w=[wk])
            p.op("vector", lambda e: e.tensor_reduce(out=kmf[:], in_=kn[:].bitcast(F32).rearrange("p (n j) -> p n j", j=256),
                                                     axis=AX.X, op=ALU.add), r=["kn"], w=["kmf"])
            p.op("vector", lambda e: e.tensor_copy(out=km[:], in_=kmf[:]), r=["kmf"], w=["km"])
            a = auxi % 2
            auxi += 1
            for t in range(16):
                p.op("tensor", lambda e, a=a, t=t: e.matmul(aux[a][:, t * 8:(t + 1) * 8], lhsT=qn[:, t * 128:(t + 1) * 128],
                                                            rhs=km[:], start=True, stop=True),
                     r=["qn", "km"], w=["aux%d" % a])
            p.op("vector", lambda e, a=a: e.tensor_tensor(out=gm[:], in0=aux[a][:, 0:128].rearrange("p (t n) -> p t n", n=8),
                                                          in1=cm[:], op=ALU.add), r=["aux%d" % a, "cm"], w=["gm"])
            p.op("vector", lambda e: e.tensor_tensor(out=cmp_[:], in0=gm[:].unsqueeze(2).to_broadcast([128, 16, 8, 8]),
                                                     in1=gm[:].unsqueeze(3).to_broadcast([128, 16, 8, 8]), op=ALU.is_gt),
                 r=["gm"], w=["cmp"])
            p.op("vector", lambda e: e.tensor_reduce(out=rank[:], in_=cmp_[:], axis=AX.X, op=ALU.add), r=["cmp"], w=["rank"])
            p.op("vector", lambda e: e.tensor_scalar(out=rank[:], in0=rank[:], scalar1=3.0, scalar2=NEG, op0=ALU.is_ge,
                                                     op1=ALU.mult), r=["rank"], w=["rank"])
            p.op("vector", lambda e: e.tensor_tensor(out=nmk[:], in0=rank[:], in1=notown[:], op=ALU.mult),
                 r=["rank", "notown"], w=["nmk"])
            for tg in range(4):
                a = auxi % 2
                auxi += 1
                for j in range(4):
                    t = tg * 4 + j
                    p.op("tensor", lambda e, a=a, t=t, j=j: e.transpose(out=aux[a][0:8, j * 128:(j + 1) * 128], in_=nmk[:, t, :],
                                                                        identity=c.ident[:]),
                         r=["nmk"], w=["aux%d" % a])
                p.op("scalar", lambda e, a=a, tg=tg: e.copy(out=negT[:, tg * 512:(tg + 1) * 512], in_=aux[a][0:8, :]),
                     r=["aux%d" % a], w=["negT"])
            for g in range(4):
                gb = gi % 2
                gi += 1
                nk = 4 * g + 4
                for kt in range(nk):
                    s_ = sci % 2
                    sci += 1
                    jj0 = max(0, kt - 4 * g)
                    c0 = jj0 * 128
                    qs = slice(g * 512 + c0, (g + 1) * 512)
                    cs = slice(c0, 512)
                    n = kt // 2
                    extra = []
                    if kt >= 4 * g:
                        extra.append((jj0, 0))
                        if jj0 + 1 < 4:
                            extra.append((jj0 + 1, 1))
                    elif kt == 4 * g - 1:
                        extra.append((0, 1))
                    p.op("tensor", lambda e, s_=s_, kt=kt, qs=qs, cs=cs: e.matmul(
                        scp[s_][:, cs], lhsT=kn[:, kt * 128:(kt + 1) * 128], rhs=qn[:, qs], start=True, stop=False),
                        r=["kn", "qn"], w=["scp%d" % s_])
                    p.op("tensor", lambda e, s_=s_, n=n, qs=qs, cs=cs, last=(not extra): e.matmul(
                        scp[s_][:, cs], lhsT=esel[:, n, :], rhs=negT[:, qs], start=False, stop=last),
                        r=["esel", "negT"], w=["scp%d" % s_])
                    for ei, (jj, which) in enumerate(extra):
                        p.op("tensor", lambda e, s_=s_, jj=jj, which=which, last=(ei == len(extra) - 1): e.matmul(
                            scp[s_][:, jj * 128:(jj + 1) * 128], lhsT=c.ident_r[:], rhs=d01[:, which, :], start=False, stop=last),
                            r=["d01"], w=["scp%d" % s_])
                    p.op("scalar", lambda e, s_=s_, cs=cs, h=h: e.activation(out=pT[s_][:, cs], in_=scp[s_][:, cs], func=AF.Exp,
                                                                             bias=cb[:, h:h + 1]),
                         r=["scp%d" % s_, "cb"], w=["pT%d" % s_])
                    p.op("tensor", lambda e, s_=s_, kt=kt, cs=cs, gb=gb, nk=nk: e.matmul(
                        op_[gb][:, cs], lhsT=vt[:, kt, :], rhs=pT[s_][:, cs], start=(kt == 0), stop=(kt == nk - 1)),
                        r=["vt", "pT%d" % s_], w=["op%d" % gb])
                    p.op("tensor", lambda e, s_=s_, cs=cs, gb=gb, kt=kt, nk=nk: e.matmul(
                        dp[gb][:, cs], lhsT=c.ones_r[:], rhs=pT[s_][:, cs], start=(kt == 0), stop=(kt == nk - 1)),
                        r=["pT%d" % s_], w=["dp%d" % gb])
                p.op("vector", lambda e, gb=gb: e.reciprocal(out=rden[:], in_=dp[gb][:]), r=["dp%d" % gb], w=["rden"])
                p.op("vector", lambda e, gb=gb: e.tensor_tensor(out=oo[gb][:], in0=op_[gb][:], in1=rden[:], op=ALU.mult),
                     r=["op%d" % gb, "rden"], w=["oo%d" % gb])
                p.dma("sync", c.sc_oa[h * 128:(h + 1) * 128, g * 512:(g + 1) * 512], oo[gb][:],
                      r=["oo%d" % gb], w=[("sc_oa", h, g)])
        p.barrier()


def gdn_consts():
    k = np.arange(128)[:, None]
    i = np.arange(128)[None, :]
    same = (k // 64) == (i // 64)
    tri = ((k <= i) & same).astype(np.float32)
    blk = same.astype(np.float32)
    half0 = np.broadcast_to((k < 64), (128, 128)).astype(np.float32)
    half1 = np.broadcast_to((k >= 64), (128, 128)).astype(np.float32)
    ustr = ((k > i) & same).astype(np.float32)
    ii = np.arange(128)[:, None]
    jj = np.arange(128)[None, :]
    same2 = (ii // 64) == (jj // 64)
    negm_strict = np.where((ii > jj) & same2, 0.0, NEG).astype(np.float32)
    negm_inclT = np.where((jj >= ii) & same2, 0.0, NEG).astype(np.float32)
    return np.stack([tri, blk, half0, half1, ustr, negm_strict, negm_inclT], 0)


class _Stop(Exception):
    pass


def _chk(n):
    import os
    return int(os.environ.get("GDN_STOP", "99")) == n


def phase_gdn(c, p):
    _phase_gdn(c, p)
    p.barrier()


def _phase_gdn(c, p):
    nc = c.nc
    with ExitStack() as es:
        c2 = Ctx(); c2.nc = nc; c2.es = es
        G = sb(c2, "gc", [128, 7, 128])
        TRI, BLK, H0, H1, USTR, NMS, NMIT = [G[:, i, :] for i in range(7)]
        ba = sb(c2, "ba", [128, 16, 16])
        dtb = sb(c2, "dtb", [128, 8])
        Aex = sb(c2, "Aex", [128, 8])
        gng = sb(c2, "gng", [128, 128])
        beta = sb(c2, "beta", [128, 16, 8])
        nbeta = sb(c2, "nbeta", [128, 16, 8])
        gg = sb(c2, "gg", [128, 16, 8])
        egs = sb(c2, "egs", [128, 16, 8])
        kds = sb(c2, "kds", [128, 16, 8])
        kbs = sb(c2, "kbs", [128, 16, 8])
        egl = sb(c2, "egl", [128, 2, 16, 8])
        tmp8 = sb(c2, "tmp8", [128, 16, 8])
        cw = sb(c2, "cw", [128, 3, 4])
        xp = [sb(c2, "xp%d" % i, [128, 3 + S]) for i in range(3)]
        cv = [sb(c2, "cv%d" % i, [128, S]) for i in range(3)]
        sq = sb(c2, "gsq", [128, S], F32R)
        rs = sb(c2, "grs", [128, 512])
        NGB = 4
        kbg = [sb(c2, "kbg%d" % j, [128, 128]) for j in range(NGB)]
        vbe = [sb(c2, "vbe%d" % j, [128, 128]) for j in range(NGB)]
        Ag = [sb(c2, "Ag%d" % j, [128, 128]) for j in range(NGB)]
        Dec = [sb(c2, "Dec%d" % j, [128, 128]) for j in range(NGB)]
        DecT = [sb(c2, "DecT%d" % j, [128, 128]) for j in range(NGB)]
        Am = [[sb(c2, "Am%d_%d" % (j, i), [128, 128]) for i in range(2)] for j in range(NGB)]
        At = [[sb(c2, "At%d_%d" % (j, i), [128, 128]) for i in range(2)] for j in range(NGB)]
        Rt = [[sb(c2, "Rt%d_%d" % (j, i), [128, 128]) for i in range(2)] for j in range(NGB)]
        u_all = sb(c2, "u_all", [128, 16, 128])
        wT_all = sb(c2, "wT_all", [128, 16, 128])
        aT_all = sb(c2, "aT_all", [128, 16, 128])
        kd_all = sb(c2, "kd_all", [128, 2, 16, 128])
        kds2 = sb(c2, "kds2", [128, 2, 16, 8])
        St = sb(c2, "St", [128, 128])
        vnew = sb(c2, "vnew", [128, 128])
        otmp = sb(c2, "otmp", [128, 128])
        ob = sb(c2, "ob", [128, 16, 128])
        zt = sb(c2, "zt", [128, 16, 128])
        ssq = sb(c2, "ssq", [128, 16])
        junk = sb(c2, "junk", [128, 128])
        obT = sb(c2, "obT", [128, S])
        pool = [ps(c2, "gp%d" % i, [128, 512]) for i in range(8)]
        pc = [0]

        def nxt():
            i = pc[0] % 8
            pc[0] += 1
            return i

        p.dma("sync", G[:], c.gdnc_d.rearrange("a k i -> k a i"), w=["G"])
        p.dma("sync", ba[:], c.sc_tm[:, 2048:2064].rearrange("(t p) n -> p t n", p=128), w=["ba"])
        p.dma("sync", dtb[:], c.dt_bias.partition_broadcast(128), w=["dtb"])
        p.dma("sync", Aex[:], c.a_log.partition_broadcast(128), w=["Aex"])
        p.dma("sync", gng[:], c.gdn_norm_gain.partition_broadcast(128), w=["gng"])
        p.op("scalar", lambda e: e.activation(out=Aex[:], in_=Aex[:], func=AF.Exp), r=["Aex"], w=["Aex"])
        p.op("scalar", lambda e: e.activation(out=beta[:], in_=ba[:, :, 0:8], func=AF.Exp, scale=-1.0), r=["ba"], w=["beta"])
        p.op("vector", lambda e: e.tensor_scalar(out=beta[:], in0=beta[:], scalar1=1.0, scalar2=None, op0=ALU.add), r=["beta"], w=["beta"])
        p.op("vector", lambda e: e.reciprocal(out=beta[:], in_=beta[:]), r=["beta"], w=["beta"])
        p.op("vector", lambda e: e.tensor_scalar(out=nbeta[:], in0=beta[:], scalar1=-1.0, scalar2=None, op0=ALU.mult), r=["beta"], w=["nbeta"])
        p.op("vector", lambda e: e.tensor_tensor(out=gg[:], in0=ba[:, :, 8:16], in1=dtb[:].unsqueeze(1).to_broadcast([128, 16, 8]),
                                                 op=ALU.add), r=["ba", "dtb"], w=["gg"])
        p.op("scalar", lambda e: e.activation(out=gg[:], in_=gg[:], func=AF.Exp), r=["gg"], w=["gg"])
        p.op("scalar", lambda e: e.activation(out=gg[:], in_=gg[:], func=AF.Ln, bias=c.one_t[:, 0:1]), r=["gg"], w=["gg"])
        p.op("vector", lambda e: e.scalar_tensor_tensor(out=gg[:], in0=gg[:], scalar=-1.0, in1=Aex[:].unsqueeze(1).to_broadcast([128, 16, 8]),
                                                        op0=ALU.mult, op1=ALU.mult), r=["gg", "Aex"], w=["gg"])
        ggf = gg[:].rearrange("p t h -> p (t h)")
        i0 = nxt(); i1 = nxt(); i2 = nxt(); i3 = nxt()
        p.op("tensor", lambda e: e.matmul(pool[i0][:, 0:128], lhsT=TRI, rhs=ggf, start=True, stop=True), r=["G", "gg"], w=["gp%d" % i0])
        p.op("tensor", lambda e: e.matmul(pool[i1][:, 0:128], lhsT=BLK, rhs=ggf, start=True, stop=True), r=["G", "gg"], w=["gp%d" % i1])
        p.op("tensor", lambda e: e.matmul(pool[i2][:, 0:128], lhsT=H0, rhs=ggf, start=True, stop=True), r=["G", "gg"], w=["gp%d" % i2])
        p.op("tensor", lambda e: e.matmul(pool[i3][:, 0:128], lhsT=H1, rhs=ggf, start=True, stop=True), r=["G", "gg"], w=["gp%d" % i3])
        v3 = lambda t_: t_.rearrange("p (t h) -> p t h", h=8)
        p.op("scalar", lambda e: e.activation(out=egs[:], in_=v3(pool[i0][:, 0:128]), func=AF.Exp), r=["gp%d" % i0], w=["egs"])
        p.op("vector", lambda e: e.tensor_tensor(out=kbs[:], in0=egs[:], in1=beta[:], op=ALU.mult), r=["egs", "beta"], w=["kbs"])
        p.op("vector", lambda e: e.tensor_copy(out=tmp8[:], in_=v3(pool[i0][:, 0:128])), r=["gp%d" % i0], w=["tmp8"])
        p.op("vector", lambda e: e.tensor_tensor(out=tmp8[:], in0=v3(pool[i1][:, 0:128]), in1=tmp8[:], op=ALU.subtract),
             r=["gp%d" % i1, "tmp8"], w=["tmp8"])
        p.op("scalar", lambda e: e.activation(out=kds[:], in_=tmp8[:], func=AF.Exp), r=["tmp8"], w=["kds"])
        p.op("scalar", lambda e: e.activation(out=egl[:, 0], in_=v3(pool[i2][:, 0:128]), func=AF.Exp), r=["gp%d" % i2], w=["egl"])
        p.op("scalar", lambda e: e.activation(out=egl[:, 1], in_=v3(pool[i3][:, 0:128]), func=AF.Exp), r=["gp%d" % i3], w=["egl"])
        p.op("vector", lambda e: e.tensor_scalar(out=egs[:], in0=egs[:], scalar1=128.0 ** -0.5, scalar2=None, op0=ALU.mult),
             r=["egs", "kbs"], w=["egs"])
        p.op("vector", lambda e: e.tensor_scalar(out=kds2[:, 0], in0=kds[:], scalar1=H0[:, 0:1], scalar2=None, op0=ALU.mult),
             r=["kds", "G"], w=["kds2"])
        p.op("vector", lambda e: e.tensor_scalar(out=kds2[:, 1], in0=kds[:], scalar1=H1[:, 0:1], scalar2=None, op0=ALU.mult),
             r=["kds", "G"], w=["kds2"])
        for i in range(3):
            p.op("vector", lambda e, i=i: e.memset(xp[i][:, 0:3], 0.0), w=["xp%d" % i])
        p.op("vector", lambda e: e.memset(vnew[:], 0.0), w=["vnew"])

        if _chk(0):
            return
        for h in range(8):
            for i in range(3):
                row = 2048 + i * 1024 + h * 128
                p.dma("sync", xp[i][:, 3:3 + S], c.sc_fm[row:row + 128, :], w=["xp%d" % i])
                p.dma("sync", cw[:, i, :], c.conv_wT[i * 1024 + h * 128:i * 1024 + (h + 1) * 128, :], w=["cw"])
                p.op("vector", lambda e, i=i: e.tensor_scalar(out=cv[i][:], in0=xp[i][:, 0:S], scalar1=cw[:, i, 0:1], scalar2=None,
                                                              op0=ALU.mult), r=["xp%d" % i, "cw"], w=["cv%d" % i])
                for tap in range(1, 4):
                    p.op("vector", lambda e, i=i, tap=tap: e.scalar_tensor_tensor(
                        out=cv[i][:], in0=xp[i][:, tap:tap + S], scalar=cw[:, i, tap:tap + 1], in1=cv[i][:],
                        op0=ALU.mult, op1=ALU.add), r=["xp%d" % i, "cw", "cv%d" % i], w=["cv%d" % i])
                p.op("scalar", lambda e, i=i: e.activation(out=cv[i][:], in_=cv[i][:], func=AF.Silu), r=["cv%d" % i], w=["cv%d" % i])
                if i < 2:
                    p.op("scalar", lambda e, i=i: e.activation(out=sq[:], in_=cv[i][:], func=AF.Square), r=["cv%d" % i], w=["gsq"])
                    for tg in range(4):
                        a = nxt()
                        sl = slice(tg * 512, (tg + 1) * 512)
                        p.op("tensor", lambda e, a=a, sl=sl: e.matmul(pool[a][:], lhsT=c.ones_r[:], rhs=sq[:, sl], start=True, stop=True),
                             r=["gsq"], w=["gp%d" % a])
                        p.op("scalar", lambda e, a=a: e.activation(out=rs[:], in_=pool[a][:], func=AF.Sqrt, bias=c.eps_t[:, 0:1]),
                             r=["gp%d" % a], w=["grs"])
                        p.op("vector", lambda e: e.reciprocal(out=rs[:], in_=rs[:]), r=["grs"], w=["grs"])
                        p.op("vector", lambda e, i=i, sl=sl: e.tensor_tensor(out=cv[i][:, sl], in0=cv[i][:, sl], in1=rs[:], op=ALU.mult),
                             r=["cv%d" % i, "grs"], w=["cv%d" % i])
            qn, kn, vn = cv
            if _chk(1):
                return
            p.dma("sync", zt[:], c.sc_tm[:, 1024 + h * 128:1024 + (h + 1) * 128].rearrange("(t p) d -> p t d", p=128), w=["zt"])
            NG = 4
            for t0_ in range(0, 16, NG):
                tl = list(range(t0_, t0_ + NG))
                ak = {}; av = {}; akk = {}; agd = {}; aqk = {}; agt = {}; amt = {}
                for j, t in enumerate(tl):
                    ts = slice(t * 128, (t + 1) * 128)
                    ak[j] = nxt()
                    p.op("tensor", lambda e, a=ak[j], ts=ts: e.transpose(out=pool[a][:, 0:128], in_=kn[:, ts], identity=c.ident[:]),
                         r=["cv1"], w=["gp%d" % ak[j]])
                    p.op("vector", lambda e, a=ak[j], t=t, h=h, j=j: e.tensor_scalar(out=kbg[j][:], in0=pool[a][:, 0:128],
                                                                                   scalar1=kbs[:, t, h:h + 1], scalar2=None, op0=ALU.mult),
                         r=["gp%d" % ak[j], "kbs"], w=["kbg%d" % j])
                    for hf_ in range(2):
                        p.op("scalar", lambda e, a=ak[j], t=t, h=h, hf_=hf_: e.activation(out=kd_all[:, hf_, t, :], in_=pool[a][:, 0:128],
                                                                                        func=AF.Identity, scale=kds2[:, hf_, t, h:h + 1]),
                             r=["gp%d" % ak[j], "kds2"], w=["kd_all"])
                    av[j] = nxt()
                    p.op("tensor", lambda e, a=av[j], ts=ts: e.transpose(out=pool[a][:, 0:128], in_=vn[:, ts], identity=c.ident[:]),
                         r=["cv2"], w=["gp%d" % av[j]])
                    p.op("vector", lambda e, a=av[j], t=t, h=h, j=j: e.tensor_scalar(out=vbe[j][:], in0=pool[a][:, 0:128],
                                                                                   scalar1=beta[:, t, h:h + 1], scalar2=None, op0=ALU.mult),
                         r=["gp%d" % av[j], "beta"], w=["vbe%d" % j])
                    p.op("gpsimd", lambda e, t=t, h=h, j=j: e.tensor_scalar(out=Ag[j][:], in0=USTR, scalar1=gg[:, t, h:h + 1], scalar2=None,
                                                                            op0=ALU.mult), r=["G", "gg"], w=["Ag%d" % j])
                for j, t in enumerate(tl):
                    ts = slice(t * 128, (t + 1) * 128)
                    akk[j] = nxt()
                    p.op("tensor", lambda e, a=akk[j], ts=ts: e.matmul(pool[a][:, 0:128], lhsT=kn[:, ts], rhs=kn[:, ts], start=True, stop=True),
                         r=["cv1"], w=["gp%d" % akk[j]])
                    agd[j] = nxt()
                    p.op("tensor", lambda e, a=agd[j], j=j: e.matmul(pool[a][:, 0:128], lhsT=TRI, rhs=Ag[j][:], start=True, stop=False),
                         r=["G", "Ag%d" % j], w=["gp%d" % agd[j]])
                    p.op("tensor", lambda e, a=agd[j]: e.matmul(pool[a][:, 0:128], lhsT=c.ident[:], rhs=NMS, start=False, stop=True),
                         r=["G"], w=["gp%d" % agd[j]])
                    p.op("scalar", lambda e, a=agd[j], j=j: e.activation(out=Dec[j][:], in_=pool[a][:, 0:128], func=AF.Exp),
                         r=["gp%d" % agd[j]], w=["Dec%d" % j])
                    p.op("vector", lambda e, a=akk[j], t=t, h=h, j=j: e.scalar_tensor_tensor(out=Am[j][0][:], in0=pool[a][:, 0:128],
                                                                                           scalar=nbeta[:, t, h:h + 1], in1=Dec[j][:],
                                                                                           op0=ALU.mult, op1=ALU.mult),
                         r=["gp%d" % akk[j], "nbeta", "Dec%d" % j], w=["Am%d_0" % j])
                for j, t in enumerate(tl):
                    ts = slice(t * 128, (t + 1) * 128)
                    aqk[j] = nxt()
                    p.op("tensor", lambda e, a=aqk[j], ts=ts: e.matmul(pool[a][:, 0:128], lhsT=kn[:, ts], rhs=qn[:, ts], start=True, stop=True),
                         r=["cv1", "cv0"], w=["gp%d" % aqk[j]])
                    agt[j] = nxt()
                    p.op("tensor", lambda e, a=agt[j], j=j: e.matmul(pool[a][:, 0:128], lhsT=Ag[j][:], rhs=TRI, start=True, stop=False),
                         r=["G", "Ag%d" % j], w=["gp%d" % agt[j]])
                    p.op("tensor", lambda e, a=agt[j]: e.matmul(pool[a][:, 0:128], lhsT=c.ident[:], rhs=NMIT, start=False, stop=True),
                         r=["G"], w=["gp%d" % agt[j]])
                    p.op("scalar", lambda e, a=agt[j], j=j: e.activation(out=DecT[j][:], in_=pool[a][:, 0:128], func=AF.Exp),
                         r=["gp%d" % agt[j]], w=["DecT%d" % j])
                    p.op("vector", lambda e, a=aqk[j], t=t, j=j: e.scalar_tensor_tensor(out=aT_all[:, t, :], in0=pool[a][:, 0:128],
                                                                                      scalar=128.0 ** -0.5, in1=DecT[j][:],
                                                                                      op0=ALU.mult, op1=ALU.mult),
                         r=["gp%d" % aqk[j], "DecT%d" % j], w=["aT_all"])
                for j, t in enumerate(tl):
                    amt[j] = nxt()
                    p.op("tensor", lambda e, a=amt[j], j=j: e.transpose(out=pool[a][:, 0:128], in_=Am[j][0][:], identity=c.ident[:]),
                         r=["Am%d_0" % j], w=["gp%d" % amt[j]])
                    p.op("scalar", lambda e, a=amt[j], j=j: e.copy(out=At[j][0][:], in_=pool[a][:, 0:128]),
                         r=["gp%d" % amt[j]], w=["At%d_0" % j])
                    p.op("gpsimd", lambda e, j=j: e.tensor_tensor(out=Rt[j][0][:], in0=At[j][0][:], in1=c.ident[:], op=ALU.add),
                         r=["At%d_0" % j], w=["Rt%d_0" % j])
                cur = 0
                for m in range(1, 6):
                    nx = 1 - cur
                    for j, t in enumerate(tl):
                        a1 = nxt()
                        p.op("tensor", lambda e, a=a1, cur=cur, j=j: e.matmul(pool[a][:, 0:128], lhsT=At[j][cur][:], rhs=Am[j][cur][:],
                                                                              start=True, stop=True),
                             r=["At%d_%d" % (j, cur), "Am%d_%d" % (j, cur)], w=["gp%d" % a1])
                        p.op("scalar", lambda e, a=a1, nx=nx, j=j: e.copy(out=Am[j][nx][:], in_=pool[a][:, 0:128]),
                             r=["gp%d" % a1], w=["Am%d_%d" % (j, nx)])
                        if m < 5:
                            a2 = nxt()
                            p.op("tensor", lambda e, a=a2, cur=cur, j=j: e.matmul(pool[a][:, 0:128], lhsT=Am[j][cur][:], rhs=At[j][cur][:],
                                                                                  start=True, stop=True),
                                 r=["At%d_%d" % (j, cur), "Am%d_%d" % (j, cur)], w=["gp%d" % a2])
                            p.op("vector", lambda e, a=a2, nx=nx, j=j: e.tensor_copy(out=At[j][nx][:], in_=pool[a][:, 0:128]),
                                 r=["gp%d" % a2], w=["At%d_%d" % (j, nx)])
                    for j, t in enumerate(tl):
                        a3 = nxt()
                        p.op("tensor", lambda e, a=a3, cur=cur, nx=nx, j=j: e.matmul(pool[a][:, 0:128], lhsT=Am[j][nx][:], rhs=Rt[j][cur][:],
                                                                                     start=True, stop=True),
                             r=["Am%d_%d" % (j, nx), "Rt%d_%d" % (j, cur)], w=["gp%d" % a3])
                        p.op("vector", lambda e, a=a3, cur=cur, nx=nx, j=j: e.tensor_tensor(out=Rt[j][nx][:], in0=pool[a][:, 0:128],
                                                                                            in1=Rt[j][cur][:], op=ALU.add),
                             r=["gp%d" % a3, "Rt%d_%d" % (j, cur)], w=["Rt%d_%d" % (j, nx)])
                    cur = nx
                for j, t in enumerate(tl):
                    RtF = Rt[j][cur]
                    a_u = nxt()
                    p.op("tensor", lambda e, a=a_u, RtF=RtF, j=j: e.matmul(pool[a][:, 0:128], lhsT=RtF[:], rhs=vbe[j][:], start=True, stop=True),
                         r=["Rt%d_%d" % (j, cur), "vbe%d" % j], w=["gp%d" % a_u])
                    p.op("scalar", lambda e, a=a_u, t=t: e.copy(out=u_all[:, t, :], in_=pool[a][:, 0:128]), r=["gp%d" % a_u], w=["u_all"])
                    a_w = nxt()
                    p.op("tensor", lambda e, a=a_w, RtF=RtF, j=j: e.matmul(pool[a][:, 0:128], lhsT=kbg[j][:], rhs=RtF[:], start=True, stop=True),
                         r=["Rt%d_%d" % (j, cur), "kbg%d" % j], w=["gp%d" % a_w])
                    p.op("vector", lambda e, a=a_w, t=t: e.tensor_copy(out=wT_all[:, t, :], in_=pool[a][:, 0:128]),
                         r=["gp%d" % a_w], w=["wT_all"])
            if _chk(3):
                return
            p.op("vector", lambda e: e.memset(St[:], 0.0), w=["St"])
            for ch in range(32):
                t = ch // 2
                hf = ch % 2
                rows = slice(hf * 64, hf * 64 + 64)
                ts = slice(t * 128, (t + 1) * 128)
                a1 = nxt()
                p.op("tensor", lambda e, a=a1, t=t: e.matmul(pool[a][:, 0:128], lhsT=wT_all[:, t, :], rhs=St[:], start=True, stop=True),
                     r=["wT_all", "St"], w=["gp%d" % a1])
                p.op("vector", lambda e, a=a1, t=t, rows=rows: e.tensor_tensor(out=vnew[rows, :], in0=u_all[rows, t, :],
                                                                               in1=pool[a][rows, 0:128], op=ALU.subtract),
                     r=["gp%d" % a1, "u_all"], w=["vnew"])
                aA = nxt()
                p.op("tensor", lambda e, a=aA, ts=ts: e.matmul(pool[a][:, 0:128], lhsT=qn[:, ts], rhs=St[:], start=True, stop=True),
                     r=["cv0", "St"], w=["gp%d" % aA])
                aB = nxt()
                p.op("tensor", lambda e, a=aB, t=t: e.matmul(pool[a][:, 0:128], lhsT=aT_all[:, t, :], rhs=vnew[:], start=True, stop=True),
                     r=["aT_all", "vnew"], w=["gp%d" % aB])
                aS = nxt()
                p.op("tensor", lambda e, a=aS, t=t, hf=hf: e.matmul(pool[a][:, 0:128], lhsT=kd_all[:, hf, t, :], rhs=vnew[:],
                                                                    start=True, stop=True),
                     r=["kd_all", "vnew"], w=["gp%d" % aS])
                p.op("scalar", lambda e, a=aA, t=t, h=h, rows=rows: e.activation(out=otmp[rows, :], in_=pool[a][rows, 0:128], func=AF.Identity,
                                                                                 scale=egs[rows, t, h:h + 1]),
                     r=["gp%d" % aA, "egs"], w=["otmp"])
                p.op("vector", lambda e, a=aB, t=t, rows=rows: e.tensor_tensor(out=ob[rows, t, :], in0=otmp[rows, :], in1=pool[a][rows, 0:128],
                                                                               op=ALU.add),
                     r=["gp%d" % aB, "otmp"], w=["ob"])
                p.op("vector", lambda e, a=aS, t=t, hf=hf, h=h: e.scalar_tensor_tensor(out=St[:], in0=St[:], scalar=egl[:, hf, t, h:h + 1],
                                                                                       in1=pool[a][:, 0:128], op0=ALU.mult, op1=ALU.add),
                     r=["gp%d" % aS, "St", "egl"], w=["St"])
            if _chk(4):
                return
            for t in range(16):
                p.op("scalar", lambda e, t=t: e.activation(out=junk[:], in_=ob[:, t, :], func=AF.Square, accum_out=ssq[:, t:t + 1]),
                     r=["ob"], w=["junk", "ssq"])
            p.op("scalar", lambda e: e.activation(out=ssq[:], in_=ssq[:], func=AF.Sqrt, scale=1.0 / 128, bias=c.eps_t[:, 0:1]),
                 r=["ssq"], w=["ssq"])
            p.op("vector", lambda e: e.reciprocal(out=ssq[:], in_=ssq[:]), r=["ssq"], w=["ssq"])
            p.op("scalar", lambda e: e.activation(out=zt[:], in_=zt[:], func=AF.Silu), r=["zt"], w=["zt"])
            p.op("vector", lambda e: e.tensor_tensor(out=ob[:], in0=ob[:], in1=ssq[:].unsqueeze(2).to_broadcast([128, 16, 128]), op=ALU.mult),
                 r=["ob", "ssq"], w=["ob"])
            p.op("vector", lambda e: e.tensor_tensor(out=ob[:], in0=ob[:], in1=gng[:].unsqueeze(1).to_broadcast([128, 16, 128]), op=ALU.mult),
                 r=["ob", "gng"], w=["ob"])
            p.op("vector", lambda e: e.tensor_tensor(out=ob[:], in0=ob[:], in1=zt[:], op=ALU.mult), r=["ob", "zt"], w=["ob"])
            for t in range(16):
                a = nxt()
                p.op("tensor", lambda e, a=a, t=t: e.transpose(out=pool[a][:, 0:128], in_=ob[:, t, :], identity=c.ident[:]),
                     r=["ob"], w=["gp%d" % a])
                p.op("scalar", lambda e, a=a, t=t: e.copy(out=obT[:, t * 128:(t + 1) * 128], in_=pool[a][:, 0:128]),
                     r=["gp%d" % a], w=["obT"])
            p.dma("sync", c.sc_ob[h * 128:(h + 1) * 128, :], obT[:], r=["obT"], w=[("sc_ob", h)])
        p.barrier()


def phase_merge(c, p):
    nc = c.nc
    with ExitStack() as es:
        c2 = Ctx(); c2.nc = nc; c2.es = es
        oa = sb(c2, "oa", [128, 8, 512], F32R)
        obb = sb(c2, "obb", [128, 8, 512], F32R)
        wa = [sb(c2, "wa%d" % i, [128, 8, 128], F32R) for i in range(2)]
        wbb = [sb(c2, "wbb%d" % i, [128, 8, 128], F32R) for i in range(2)]
        ga = [sb(c2, "ga%d" % i, [128, 512]) for i in range(2)]
        gb_ = [sb(c2, "gb%d" % i, [128, 512]) for i in range(2)]
        m1 = sb(c2, "m1", [128, 512])
        mT = sb(c2, "mT", [128, 16, 512], F32R)
        wo = [sb(c2, "wo%d" % i, [128, 16, 512], F32R) for i in range(2)]
        xt = [sb(c2, "xt%d" % i, [128, 512]) for i in range(2)]
        pa = [ps(c2, "pa%d" % i, [128, 512]) for i in range(2)]
        pb = [ps(c2, "pb%d" % i, [128, 512]) for i in range(2)]
        po = [ps(c2, "po%d" % i, [128, 512]) for i in range(4)]
        ci = 0
        wi = 0
        oi = 0
        for tg in range(4):
            tsl = slice(tg * 512, (tg + 1) * 512)
            p.dma("sync", oa[:], r32(c.sc_oa[:, tsl]).rearrange("(kc p) t -> p kc t", p=128), w=["oa"])
            p.dma("sync", obb[:], r32(c.sc_ob[:, tsl]).rearrange("(kc p) t -> p kc t", p=128), w=["obb"])
            for cc in range(16):
                b = ci % 2
                ci += 1
                csl = slice(cc * 128, (cc + 1) * 128)
                p.dma("sync", wa[b][:], r32(c.w_up_a[:, csl]).rearrange("(kc p) n -> p kc n", p=128), w=["wa%d" % b])
                p.dma("sync", wbb[b][:], r32(c.w_up_b[:, csl]).rearrange("(kc p) n -> p kc n", p=128), w=["wbb%d" % b])
                p.dma("sync", ga[b][:], c.sc_fm[5120 + cc * 128:5120 + (cc + 1) * 128, tsl], w=["ga%d" % b])
                p.dma("sync", gb_[b][:], c.sc_fm[7168 + cc * 128:7168 + (cc + 1) * 128, tsl], w=["gb%d" % b])
                for kc in range(8):
                    p.op("tensor", lambda e, b=b, kc=kc: e.matmul(pa[b][:], lhsT=wa[b][:, kc, :], rhs=oa[:, kc, :],
                                                                  start=(kc == 0), stop=(kc == 7)),
                         r=["wa%d" % b, "oa"], w=["pa%d" % b])
                for kc in range(8):
                    p.op("tensor", lambda e, b=b, kc=kc: e.matmul(pb[b][:], lhsT=wbb[b][:, kc, :], rhs=obb[:, kc, :],
                                                                  start=(kc == 0), stop=(kc == 7)),
                         r=["wbb%d" % b, "obb"], w=["pb%d" % b])
                p.op("scalar", lambda e, b=b: e.activation(out=ga[b][:], in_=ga[b][:], func=AF.Sigmoid), r=["ga%d" % b], w=["ga%d" % b])
                p.op("scalar", lambda e, b=b: e.activation(out=gb_[b][:], in_=gb_[b][:], func=AF.Sigmoid), r=["gb%d" % b], w=["gb%d" % b])
                p.op("vector", lambda e, b=b: e.tensor_tensor(out=m1[:], in0=pa[b][:], in1=ga[b][:], op=ALU.mult),
                     r=["pa%d" % b, "ga%d" % b], w=["m1"])
                p.op("vector", lambda e, b=b: e.tensor_tensor(out=gb_[b][:], in0=pb[b][:], in1=gb_[b][:], op=ALU.mult),
                     r=["pb%d" % b, "gb%d" % b], w=["gb%d" % b])
                p.op("vector", lambda e, b=b, cc=cc: e.tensor_tensor(out=mT[:, cc, :], in0=m1[:], in1=gb_[b][:], op=ALU.add),
                     r=["m1", "gb%d" % b], w=["mT"])
            for dg in range(4):
                wb_ = wi % 2
                wi += 1
                dsl = slice(dg * 512, (dg + 1) * 512)
                p.dma("sync", wo[wb_][:], r32(c.w_out[:, dsl]).rearrange("(kc p) n -> p kc n", p=128), w=["wo%d" % wb_])
                for tt in range(4):
                    o = oi % 4
                    oi += 1
                    x_ = oi % 2
                    t0 = tg * 512 + tt * 128
                    p.dma("sync", xt[x_][:], c.x[t0:t0 + 128, dsl], w=["xt%d" % x_])
                    for cc in range(16):
                        p.op("tensor", lambda e, o=o, cc=cc, tt=tt, wb_=wb_: e.matmul(
                            po[o][:], lhsT=mT[:, cc, tt * 128:(tt + 1) * 128], rhs=wo[wb_][:, cc, :],
                            start=(cc == 0), stop=(cc == 15)), r=["mT", "wo%d" % wb_], w=["po%d" % o])
                    p.op("vector", lambda e, o=o, x_=x_: e.tensor_tensor(out=xt[x_][:], in0=po[o][:], in1=xt[x_][:], op=ALU.add),
                         r=["po%d" % o, "xt%d" % x_], w=["xt%d" % x_])
                    p.dma("sync", c.sc_x1[t0:t0 + 128, dsl], xt[x_][:], r=["xt%d" % x_], w=[("sc_x1", t0, dg)])
        p.barrier()


def phase_peer(c, p):
    nc = c.nc
    for half in range(2):
        with ExitStack() as es:
            c2 = Ctx(); c2.nc = nc; c2.es = es
            hT = sb(c2, "h2T", [128, 16, 1024], F32R)
            phase_norm_T(c, p, c.sc_x1, c.norm2_gain, hT, "n2_", half, h_out=c.sc_h2)
            with ExitStack() as es2:
                c3 = Ctx(); c3.nc = nc; c3.es = es2
                wq = [sb(c3, "wq%d" % i, [128, 16, 128], F32R) for i in range(2)]
                skT = sb(c3, "skT", [128, 16, 128], F32R)
                qT = [sb(c3, "qT%d" % i, [128, 1024], F32R) for i in range(2)]
                pq = [ps(c3, "pq%d" % i, [128, 512]) for i in range(4)]
                so_t = [sb(c3, "so_t%d" % i, [128, 512]) for i in range(2)]
                pqi = 0
                p.dma("sync", skT[:], r32(c.skT_d[:, :, :]), w=["skT"])
                for ch in range(16):
                    b = ch % 2
                    p.dma("sync", wq[b][:], r32(c.w_query[:, ch * 128:(ch + 1) * 128]).rearrange("(kc p) n -> p kc n", p=128),
                          w=["wq%d" % b])
                    for tg in range(2):
                        a = pqi % 4
                        pqi += 1
                        for kc in range(16):
                            p.op("tensor", lambda e, a=a, b=b, kc=kc, tg=tg: e.matmul(
                                pq[a][:], lhsT=wq[b][:, kc, :], rhs=hT[:, kc, tg * 512:(tg + 1) * 512],
                                start=(kc == 0), stop=(kc == 15)), r=["wq%d" % b, "hT"], w=["pq%d" % a])
                        p.op("scalar", lambda e, a=a, b=b, tg=tg: e.copy(out=qT[b][:, tg * 512:(tg + 1) * 512], in_=pq[a][:]),
                             r=["pq%d" % a], w=["qT%d" % b])
                    for tq in range(2):
                        a = pqi % 4
                        pqi += 1
                        for tt in range(4):
                            tl = tq * 4 + tt
                            p.op("tensor", lambda e, a=a, b=b, tl=tl, tt=tt, ch=ch: e.matmul(
                                pq[a][:, tt * 128:(tt + 1) * 128], lhsT=qT[b][:, tl * 128:(tl + 1) * 128], rhs=skT[:, ch, :],
                                start=True, stop=True), r=["qT%d" % b, "skT"], w=["pq%d" % a])
                        so = "so%d" % (pqi % 2)
                        sot = so_t[pqi % 2]
                        p.op("vector", lambda e, a=a, sot=sot: e.tensor_copy(out=sot[:], in_=pq[a][:]), r=["pq%d" % a], w=[so])
                        tb = half * 1024 + tq * 512
                        p.dma("sync", c.sc_sc[tb:tb + 512, ch * 128:(ch + 1) * 128].rearrange("(tt p) k -> p tt k", p=128),
                              sot[:].rearrange("p (tt k) -> p tt k", k=128), r=[so], w=[("sc_sc", tb, ch)])
                p.barrier()
    with ExitStack() as es:
        c2 = Ctx(); c2.nc = nc; c2.es = es
        sc = sb(c2, "sc", [128, 16, 128])
        wk = sb(c2, "wk", [128, 128])
        stop_ = sb(c2, "stop", [128, 16, 16])
        itop = sb(c2, "itop", [128, 16, 16], U32)
        itf = sb(c2, "itf", [128, 16, 16])
        cand = sb(c2, "cand", [128, 8, 16, 16])
        cidx = sb(c2, "cidx", [128, 8, 16, 16])
        wk2 = sb(c2, "wk2", [128, 256])
        best = sb(c2, "best", [128, 8, 16])
        pos = sb(c2, "pos", [128, 8, 16], U32)
        posf = sb(c2, "posf", [128, 8, 16])
        iota = sb(c2, "iota", [128, 256])
        junk2 = sb(c2, "junk2", [128, 256])
        eidf = sb(c2, "eidf", [128, 128])
        eid = sb(c2, "eid", [128, 128], U32)
        nmx = sb(c2, "nmx", [128, 8])
        gsum = sb(c2, "gsum", [128, 8])
        gate = sb(c2, "gate", [128, 8, 16])
        dots = sb(c2, "dots", [128, 128])
        gact = sb(c2, "gact", [128, 128])
        h2 = sb(c2, "h2", [128, D])
        NB = 6
        gu = [sb(c2, "gu%d" % i, [128, D], F32R) for i in range(NB)]
        diag = [sb(c2, "diag%d" % i, [128, 128], F32R) for i in range(2)]
        po = [ps(c2, "po%d" % i, [128, 512]) for i in range(4)]
        junk = sb(c2, "junkp", [128, D])
        acc = sb(c2, "acc", [128, D])
        x1 = sb(c2, "x1", [128, D])
        p.dma("sync", iota[:], c.iota_d[:, :], w=["iota"])
        gi = 0
        for t in range(16):
            t0 = t * 128
            p.dma("sync", sc[:], c.sc_sc[t0:t0 + 128, :].rearrange("p (c k) -> p c k", k=128), w=["sc"])
            p.dma("sync", h2[:], c.sc_h2[t0:t0 + 128, :], w=["h2"])
            p.dma("sync", x1[:], c.sc_x1[t0:t0 + 128, :], w=["x1"])
            for ch in range(16):
                p.op("vector", lambda e, ch=ch: e.max(out=stop_[:, ch, 0:8], in_=sc[:, ch, :]), r=["sc"], w=["stop"])
                p.op("vector", lambda e, ch=ch: e.max_index(out=itop[:, ch, 0:8], in_max=stop_[:, ch, 0:8], in_values=sc[:, ch, :]),
                     r=["sc", "stop"], w=["itop"])
                p.op("vector", lambda e, ch=ch: e.match_replace(out=wk[:], in_to_replace=stop_[:, ch, 0:8], in_values=sc[:, ch, :],
                                                                imm_value=-1e30), r=["sc", "stop"], w=["wk"])
                p.op("vector", lambda e, ch=ch: e.max(out=stop_[:, ch, 8:16], in_=wk[:]), r=["wk"], w=["stop"])
                p.op("vector", lambda e, ch=ch: e.max_index(out=itop[:, ch, 8:16], in_max=stop_[:, ch, 8:16], in_values=wk[:]),
                     r=["wk", "stop"], w=["itop"])
            p.op("vector", lambda e: e.tensor_copy(out=itf[:], in_=itop[:]), r=["itop"], w=["itf"])
            s4 = stop_[:].rearrange("p (h two) k -> p h two k", two=2)
            i4 = itf[:].rearrange("p (h two) k -> p h two k", two=2)
            p.op("vector", lambda e, s4=s4: e.tensor_tensor(out=cand[:], in0=s4[:, :, 0, :].unsqueeze(3).to_broadcast([128, 8, 16, 16]),
                                                            in1=s4[:, :, 1, :].unsqueeze(2).to_broadcast([128, 8, 16, 16]), op=ALU.add),
                 r=["stop"], w=["cand"])
            for hh in range(8):
                p.op("vector", lambda e, i4=i4, hh=hh: e.scalar_tensor_tensor(
                    out=cidx[:, hh], in0=i4[:, hh, 0, :].unsqueeze(2).to_broadcast([128, 16, 16]), scalar=128.0,
                    in1=i4[:, hh, 1, :].unsqueeze(1).to_broadcast([128, 16, 16]), op0=ALU.mult, op1=ALU.add),
                    r=["itf"], w=["cidx"])
            for hh in range(8):
                cv_ = cand[:, hh].rearrange("p a b -> p (a b)")
                p.op("vector", lambda e, hh=hh, cv_=cv_: e.max(out=best[:, hh, 0:8], in_=cv_), r=["cand"], w=["best"])
                p.op("vector", lambda e, hh=hh, cv_=cv_: e.max_index(out=pos[:, hh, 0:8], in_max=best[:, hh, 0:8], in_values=cv_),
                     r=["cand", "best"], w=["pos"])
                p.op("vector", lambda e, hh=hh, cv_=cv_: e.match_replace(out=wk2[:], in_to_replace=best[:, hh, 0:8], in_values=cv_,
                                                                         imm_value=-1e30), r=["cand", "best"], w=["wk2"])
                p.op("vector", lambda e, hh=hh: e.max(out=best[:, hh, 8:16], in_=wk2[:]), r=["wk2"], w=["best"])
                p.op("vector", lambda e, hh=hh: e.max_index(out=pos[:, hh, 8:16], in_max=best[:, hh, 8:16], in_values=wk2[:]),
                     r=["wk2", "best"], w=["pos"])
            p.op("vector", lambda e: e.tensor_copy(out=posf[:], in_=pos[:]), r=["pos"], w=["posf"])
            for hh in range(8):
                ci_ = cidx[:, hh].rearrange("p a b -> p (a b)")
                for m in range(16):
                    p.op("vector", lambda e, hh=hh, m=m, ci_=ci_: e.scalar_tensor_tensor(
                        out=junk2[:], in0=iota[:], scalar=posf[:, hh, m:m + 1], in1=ci_, op0=ALU.is_equal, op1=ALU.mult,
                        accum_out=eidf[:, hh * 16 + m:hh * 16 + m + 1]), r=["iota", "posf", "cidx"], w=["junk2", "eidf"])
            p.op("vector", lambda e: e.tensor_copy(out=eid[:], in_=eidf[:]), r=["eidf"], w=["eid"])
            p.op("vector", lambda e: e.tensor_scalar(out=nmx[:], in0=best[:, :, 0], scalar1=-1.0, scalar2=None, op0=ALU.mult),
                 r=["best"], w=["nmx"])
            for hh in range(8):
                p.op("scalar", lambda e, hh=hh: e.activation(out=gate[:, hh, :], in_=best[:, hh, :], func=AF.Exp, bias=nmx[:, hh:hh + 1],
                                                             accum_out=gsum[:, hh:hh + 1]), r=["best", "nmx"], w=["gate", "gsum"])
            p.op("vector", lambda e: e.reciprocal(out=gsum[:], in_=gsum[:]), r=["gsum"], w=["gsum"])
            p.op("vector", lambda e: e.tensor_tensor(out=gate[:], in0=gate[:], in1=gsum[:].unsqueeze(2).to_broadcast([128, 8, 16]), op=ALU.mult),
                 r=["gate", "gsum"], w=["gate"])
            for s_ in range(128):
                b = gi % NB
                gi += 1
                p.dma_fn("gpsimd", lambda e, b=b, s_=s_: e.indirect_dma_start(
                    out=gu[b][:], out_offset=None, in_=r32(c.peer_u[:, :]),
                    in_offset=bass.IndirectOffsetOnAxis(ap=eid[:, s_:s_ + 1], axis=0)), r=["eid"], w=["gu%d" % b])
                p.op("vector", lambda e, b=b, s_=s_: e.scalar_tensor_tensor(out=junk[:], in0=gu[b][:].bitcast(F32), scalar=1.0, in1=h2[:],
                                                                            op0=ALU.mult, op1=ALU.mult, accum_out=dots[:, s_:s_ + 1]),
                     r=["gu%d" % b, "h2"], w=["junkp", "dots"])
            p.op("scalar", lambda e: e.activation(out=gact[:], in_=dots[:], func=AF.Gelu), r=["dots"], w=["gact"])
            p.op("vector", lambda e: e.tensor_tensor(out=gact[:], in0=gact[:], in1=gate[:].rearrange("p h k -> p (h k)"), op=ALU.mult),
                 r=["gact", "gate"], w=["gact"])
            for s_ in range(128):
                b = gi % NB
                gi += 1
                db = s_ % 2
                p.dma_fn("gpsimd", lambda e, b=b, s_=s_: e.indirect_dma_start(
                    out=gu[b][:], out_offset=None, in_=r32(c.peer_v[:, :]),
                    in_offset=bass.IndirectOffsetOnAxis(ap=eid[:, s_:s_ + 1], axis=0)), r=["eid"], w=["gu%d" % b])
                p.op("vector", lambda e, db=db, s_=s_: e.tensor_scalar(out=diag[db][:], in0=c.ident[:], scalar1=gact[:, s_:s_ + 1],
                                                                       scalar2=None, op0=ALU.mult),
                     r=["gact"], w=["diag%d" % db])
                for dg in range(4):
                    p.op("tensor", lambda e, b=b, db=db, dg=dg, s_=s_: e.matmul(
                        po[dg][:], lhsT=diag[db][:], rhs=gu[b][:, dg * 512:(dg + 1) * 512], start=(s_ == 0), stop=(s_ == 127)),
                        r=["diag%d" % db, "gu%d" % b], w=["po%d" % dg])
            for dg in range(4):
                dsl = slice(dg * 512, (dg + 1) * 512)
                p.op("vector", lambda e, dg=dg, dsl=dsl: e.tensor_tensor(out=acc[:, dsl], in0=po[dg][:], in1=x1[:, dsl], op=ALU.add),
                     r=["po%d" % dg, "x1"], w=["acc"])
            p.dma("sync", c.y[t0:t0 + 128, :], acc[:], r=["acc"], w=[("y", t)])
        p.barrier()


ALL_PHASES = ("inproj", "moba", "gdn", "merge", "peer")


def build_nc(debug=False, phases=ALL_PHASES):
    nc = bass.Bass("TRN2", target_bir_lowering=False)
    nc.dge_precook = False
    c = Ctx()
    c.nc = nc
    kind_s = "ExternalOutput" if debug else "Internal"

    def din(name, shape, dt=F32):
        return nc.dram_tensor(name, list(shape), dt, kind="ExternalInput").ap()

    def dsc(name, shape):
        return nc.dram_tensor(name, list(shape), F32, kind=kind_s).ap()

    c.x = din("x", [S, D])
    c.norm1_gain = din("norm1_gain", [1, D])
    c.w_in = din("w_in", [D, IN_TOTAL])
    c.ident_d = din("ident", [128, 128])
    c.ones_d = din("ones", [128, 128])
    c.rel_bias = din("rel_bias", [32, 8])
    c.q_norm_gain = din("q_norm_gain", [1, 128])
    c.k_norm_gain = din("k_norm_gain", [1, 128])
    c.d01_d = din("d01", [8, 2, 128, 128])
    c.cm_d = din("cm", [128, 16, 8])
    c.notown_d = din("notown", [128, 16, 8])
    c.esel_d = din("esel", [8, 8, 128])
    c.gdnc_d = din("gdnc", [7, 128, 128])
    c.conv_wT = din("conv_wT", [3072, 4])
    c.a_log = din("a_log", [1, 8])
    c.dt_bias = din("dt_bias", [1, 8])
    c.gdn_norm_gain = din("gdn_norm_gain", [1, 128])
    c.w_up_a = din("w_up_a", [1024, D])
    c.w_up_b = din("w_up_b", [1024, D])
    c.w_out = din("w_out", [D, D])
    if "peer" in phases:
        c.norm2_gain = din("norm2_gain", [1, D])
        c.w_query = din("w_query", [D, D])
        c.skT_d = din("skT", [128, 16, 128])
        c.peer_u = din("peer_u", [16384, D])
        c.peer_v = din("peer_v", [16384, D])
        c.iota_d = din("iota", [128, 256])
        c.sc_h2 = dsc("sc_h2", [S, D])
        c.sc_sc = dsc("sc_sc", [S, D])
    c.sc_fm = dsc("sc_fm", [N_FM, S])
    c.sc_tm = dsc("sc_tm", [S, N_TM])
    c.sc_oa = dsc("sc_oa", [1024, S])
    c.sc_ob = dsc("sc_ob", [1024, S])
    if "merge" in phases or "peer" not in phases:
        c.sc_x1 = dsc("sc_x1", [S, D])
    else:
        c.sc_x1 = din("sc_x1", [S, D])
    c.y = nc.dram_tensor("y", [S, D], F32, kind="ExternalOutput").ap()

    with ExitStack() as es:
        c.es = es
        p = Prog(nc, es)
        block = es.enter_context(nc.Block())
        c.ident = sb(c, "ident_s", [128, 128])
        c.ident_r = sb(c, "ident_r", [128, 128], F32R)
        c.ones_r = sb(c, "ones_r", [128, 128], F32R)
        c.eps_t = sb(c, "eps_t", [128, 1])
        c.one_t = sb(c, "one_t", [128, 1])
        p.dma("sync", c.ident[:], c.ident_d[:, :], w=["ident"])
        p.dma("sync", c.ident_r[:], r32(c.ident_d[:, :]), w=["ident_r"])
        p.dma("sync", c.ones_r[:], r32(c.ones_d[:, :]), w=["ones_r"])
        p.op("vector", lambda e: e.memset(c.eps_t[:], EPS), w=["eps"])
        p.op("vector", lambda e: e.memset(c.one_t[:], 1.0), w=["one"])
        p.barrier()
        if "inproj" in phases:
            phase_inproj(c, p)
        if "moba" in phases:
            phase_moba(c, p)
        if "gdn" in phases:
            phase_gdn(c, p)
        if "merge" in phases:
            phase_merge(c, p)
        if "peer" in phases:
            phase_peer(c, p)
        p.finish(block)
    return nc


def make_in_maps(inputs, phases=ALL_PHASES):
    f = lambda a: np.ascontiguousarray(np.asarray(a, dtype=np.float32))
    rel_bias = f(inputs["rel_bias"])
    d01, cm, notown, esel = moba_consts(rel_bias)
    shared = {
        "norm1_gain": f(inputs["norm1_gain"]), "w_in": f(inputs["w_in"][0]),
        "ident": np.eye(128, dtype=np.float32), "ones": np.ones((128, 128), np.float32),
        "rel_bias": rel_bias, "q_norm_gain": f(inputs["q_norm_gain"]), "k_norm_gain": f(inputs["k_norm_gain"]),
        "d01": d01, "cm": cm, "notown": notown, "esel": esel, "gdnc": gdn_consts(),
        "conv_wT": f(np.asarray(inputs["conv_w"][0]).T), "a_log": f(inputs["a_log"]), "dt_bias": f(inputs["dt_bias"]),
        "gdn_norm_gain": f(inputs["gdn_norm_gain"]), "w_up_a": f(inputs["w_up_a"][0]), "w_up_b": f(inputs["w_up_b"][0]),
        "w_out": f(inputs["w_out"][0]),
    }
    if "peer" in phases:
        sk = np.asarray(inputs["peer_sub_keys"][0], dtype=np.float32).reshape(16, 128, 128)
        shared.update({
            "norm2_gain": f(inputs["norm2_gain"]), "w_query": f(inputs["peer_w_query"][0]),
            "skT": f(sk.transpose(2, 0, 1)), "peer_u": f(inputs["peer_u"][0]), "peer_v": f(inputs["peer_v"][0]),
            "iota": np.broadcast_to(np.arange(256, dtype=np.float32), (128, 256)).copy(),
        })
    maps = []
    for b in range(8):
        m = dict(shared)
        m["x"] = f(inputs["x"][b])
        maps.append(m)
    return maps


_NC_CACHE = {}


def kernel(**inputs):
    if "nc" not in _NC_CACHE:
        _NC_CACHE["nc"] = build_nc()
    nc = _NC_CACHE["nc"]
    maps = make_in_maps(inputs)
    res = run_bass_kernel_spmd(nc, maps, core_ids=list(range(8)))
    return np.stack([np.asarray(r["y"], dtype=np.float32) for r in res.results], axis=0)
```

```python
import math
from contextlib import ExitStack

import numpy as np
import concourse.bass as bass
import concourse.mybir as mybir
from concourse.bass_utils import run_bass_kernel_spmd

F32 = mybir.dt.float32
F32R = mybir.dt.float32r
U32 = mybir.dt.uint32
I32 = mybir.dt.int32
AF = mybir.ActivationFunctionType
ALU = mybir.AluOpType
AX = mybir.AxisListType

D = 2048
S = 2048
NT = S // 128
IN_TOTAL = 11280
EPS = 1e-6
NEG = -30000.0

COMPUTE = ("tensor", "vector", "scalar", "gpsimd")
ALLENG = ("tensor", "vector", "scalar", "gpsimd", "sync")


import re as _re
_PSUM_KEY = _re.compile(r"^(n\d_tp|gp|aux|scp|op|dp|pp|pa|pb|po|pq)\d+$")


class Op:
    __slots__ = ("eng", "fn", "deps", "needed", "is_dma", "slot", "use", "tok", "idx")


class Prog:
    def __init__(self, nc, es, ndma=8):
        self.nc = nc
        self.ops = {e: [] for e in ALLENG}
        self.last_w = {}
        self.rd_comp = {}
        self.rd_dma = {}
        self.sem = {e: es.enter_context(nc.semaphore("s_" + e)) for e in COMPUTE}
        self.dsem = {}
        self.dlast = {}
        self.dnext = {}
        self.duse = {}
        for q in ("sync", "gpsimd", "scalar"):
            self.dsem[q] = [es.enter_context(nc.semaphore("d_%s%d" % (q, i))) for i in range(ndma)]
            self.dlast[q] = [None] * ndma
            self.duse[q] = [0] * ndma
            self.dnext[q] = 0
        self.pending_barrier = {e: None for e in ALLENG}
        self.all_dma = []

    def _add(self, eng, fn, r, w, is_dma):
        o = Op()
        o.eng = eng
        o.fn = fn
        o.needed = False
        o.is_dma = is_dma
        o.slot = None
        o.use = 0
        deps = set()
        for k in r:
            lw = self.last_w.get(k)
            if lw is not None:
                deps.add(lw)
            if isinstance(k, str) and _PSUM_KEY.match(k):
                for en, ro in self.rd_comp.get(k, {}).items():
                    if en != eng:
                        deps.add(ro)
        for k in w:
            lw = self.last_w.get(k)
            if lw is not None:
                deps.add(lw)
            for ro in self.rd_comp.get(k, {}).values():
                deps.add(ro)
            for ro in self.rd_dma.get(k, ()):
                deps.add(ro)
        if is_dma:
            q = eng
            sl = self.dnext[q]
            self.dnext[q] = (sl + 1) % len(self.dsem[q])
            prev = self.dlast[q][sl]
            if prev is not None:
                deps.add(prev)
            self.duse[q][sl] += 1
            o.slot = sl
            o.use = self.duse[q][sl]
            self.dlast[q][sl] = o
            self.all_dma.append(o)
        pb = self.pending_barrier[eng]
        if pb is not None:
            deps |= pb
            self.pending_barrier[eng] = None
        if eng == "tensor":
            deps = {d for d in deps if not (d.eng == "tensor" and not d.is_dma)}
        o.deps = deps
        for d in deps:
            d.needed = True
        for k in w:
            self.last_w[k] = o
            self.rd_comp[k] = {}
            self.rd_dma[k] = []
        for k in r:
            if is_dma:
                self.rd_dma.setdefault(k, []).append(o)
            else:
                self.rd_comp.setdefault(k, {})[eng] = o
        o.idx = len(self.ops[eng])
        self.ops[eng].append(o)
        return o

    def op(self, eng, fn, r=(), w=()):
        return self._add(eng, fn, tuple(r), tuple(w), False)

    def dma(self, q, out, in_, r=(), w=(), **kw):
        return self._add(q, lambda e: e.dma_start(out=out, in_=in_, **kw), tuple(r), tuple(w), True)

    def dma_fn(self, q, fn, r=(), w=()):
        return self._add(q, fn, tuple(r), tuple(w), True)

    def barrier(self):
        deps = set()
        for e in ALLENG:
            if self.ops[e]:
                last = [o for o in self.ops[e] if not o.is_dma]
                if last:
                    deps.add(last[-1])
        for o in self.all_dma:
            deps.add(o)
        self.all_dma = []
        for e in ALLENG:
            pb = self.pending_barrier[e]
            self.pending_barrier[e] = set(deps) | (pb or set())
        self.last_w = {}
        self.rd_comp = {}
        self.rd_dma = {}

    def finish(self, block):
        self.barrier()
        nc = self.nc
        for e in ALLENG:
            self._add(e, None, (), (), False)
        for e in COMPUTE:
            c = 0
            for o in self.ops[e]:
                if o.is_dma:
                    o.tok = (self.dsem[e][o.slot], 16 * o.use)
                elif o.needed:
                    c += 1
                    o.tok = (self.sem[e], c)
                else:
                    o.tok = None
        for o in self.ops["sync"]:
            if o.is_dma:
                o.tok = (self.dsem["sync"][o.slot], 16 * o.use)
            else:
                o.tok = None

        def emit(engname):
            def body(eng):
                waited = {}
                for o in self.ops[engname]:
                    for d in o.deps:
                        sem, val = d.tok
                        key = id(sem)
                        if waited.get(key, 0) < val:
                            eng.wait_ge(sem, val)
                            waited[key] = val
                    if o.fn is None:
                        continue
                    ins = o.fn(eng)
                    if o.is_dma:
                        ins.then_inc(o.tok[0], 16)
                    elif o.needed:
                        ins.then_inc(o.tok[0], 1)
            return body

        block.tensor(emit("tensor"))
        block.vector(emit("vector"))
        block.scalar(emit("scalar"))
        block.gpsimd(emit("gpsimd"))
        block.sync(emit("sync"))


def r32(ap):
    return ap.bitcast(F32R)


class Ctx:
    pass


_uid = [0]


def _un(name):
    _uid[0] += 1
    return "%s_%d" % (name, _uid[0])


def sb(c, name, shape, dt=F32):
    return c.es.enter_context(c.nc.sbuf_tensor(_un(name), list(shape), dt))


def ps(c, name, shape, dt=F32):
    return c.es.enter_context(c.nc.psum_tensor(_un(name), list(shape), dt))


FM_SRC = [(0, 1024), (1024, 1024), (3072, 3072), (7184, 2048), (9232, 2048)]
TM_SRC = [(2048, 1024), (6144, 1024), (7168, 16)]
N_FM = 9216
N_TM = 2064


def phase_norm_T(c, p, x_ap, gain_ap, hT, pfx, half, h_out=None):
    nc = c.nc
    with ExitStack() as es:
        c2 = Ctx(); c2.nc = nc; c2.es = es
        gbc = sb(c2, pfx + "gbc", [128, D])
        xt = [sb(c2, pfx + "xt%d" % i, [128, D]) for i in range(2)]
        ht = [sb(c2, pfx + "ht%d" % i, [128, D]) for i in range(2)]
        sq = sb(c2, pfx + "sq", [128, D])
        st = [sb(c2, pfx + "st%d" % i, [128, 4]) for i in range(2)]
        tp = [ps(c2, pfx + "tp%d" % i, [128, 512]) for i in range(4)]
        p.dma("sync", gbc[:], gain_ap.partition_broadcast(128), w=[pfx + "gbc"])
        for ti in range(8):
            t = half * 8 + ti
            b = ti % 2
            p.dma("sync", xt[b][:], x_ap[t * 128:(t + 1) * 128, :], w=[pfx + "xt%d" % b])
            p.op("scalar", lambda e, b=b: e.activation(out=sq[:], in_=xt[b][:], func=AF.Square,
                                                       accum_out=st[b][:, 0:1]),
                 r=[pfx + "xt%d" % b], w=[pfx + "sq", pfx + "st%d" % b])
            p.op("scalar", lambda e, b=b: e.activation(out=st[b][:, 1:2], in_=st[b][:, 0:1], func=AF.Sqrt,
                                                       scale=1.0 / D, bias=c.eps_t[:, 0:1]),
                 r=[pfx + "st%d" % b], w=[pfx + "st%d" % b])
            p.op("vector", lambda e, b=b: e.reciprocal(out=st[b][:, 2:3], in_=st[b][:, 1:2]),
                 r=[pfx + "st%d" % b], w=[pfx + "st%d" % b])
            p.op("vector", lambda e, b=b: e.scalar_tensor_tensor(out=ht[b][:], in0=xt[b][:], scalar=st[b][:, 2:3],
                                                                 in1=gbc[:], op0=ALU.mult, op1=ALU.mult),
                 r=[pfx + "xt%d" % b, pfx + "st%d" % b, pfx + "gbc"], w=[pfx + "ht%d" % b])
            if h_out is not None:
                p.dma("sync", h_out[t * 128:(t + 1) * 128, :], ht[b][:], r=[pfx + "ht%d" % b], w=[(pfx + "hout", t)])
            for g4 in range(4):
                pb = tp[g4]
                for j in range(4):
                    kc = g4 * 4 + j
                    p.op("tensor", lambda e, b=b, kc=kc, j=j, pb=pb: e.transpose(
                        out=pb[:, j * 128:(j + 1) * 128], in_=ht[b][:, kc * 128:(kc + 1) * 128], identity=c.ident[:]),
                        r=[pfx + "ht%d" % b], w=[pfx + "tp%d" % g4])
                eng = "scalar" if g4 % 2 == 0 else "vector"
                dst = hT[:, g4 * 4:(g4 + 1) * 4, ti * 128:(ti + 1) * 128]
                src = pb[:].rearrange("p (j t) -> p j t", j=4)
                if eng == "scalar":
                    p.op("scalar", lambda e, dst=dst, src=src: e.copy(out=dst, in_=src),
                         r=[pfx + "tp%d" % g4], w=["hT"])
                else:
                    p.op("vector", lambda e, dst=dst, src=src: e.tensor_copy(out=dst, in_=src),
                         r=[pfx + "tp%d" % g4], w=["hT"])
        p.barrier()


def phase_inproj(c, p):
    nc = c.nc
    for half in range(2):
        with ExitStack() as es:
            c2 = Ctx(); c2.nc = nc; c2.es = es
            hT = sb(c2, "hT", [128, 16, 1024], F32R)
            phase_norm_T(c, p, c.x, c.norm1_gain, hT, "n1_", half)
            with ExitStack() as es2:
                c3 = Ctx(); c3.nc = nc; c3.es = es2
                wb = [sb(c3, "wb%d" % i, [128, 16, 256], F32R) for i in range(2)]
                ob = [sb(c3, "ob%d" % i, [128, 1024]) for i in range(2)]
                pp = [ps(c3, "pp%d" % i, [128, 512]) for i in range(4)]
                gi = 0
                oi = 0
                pi = 0
                t0 = half * 1024
                row = 0
                for (c0, n) in FM_SRC:
                    for g in range(n // 256):
                        col = c0 + g * 256
                        b = gi % 2
                        gi += 1
                        p.dma("sync", wb[b][:], r32(c.w_in[:, col:col + 256]).rearrange("(kc p) n -> p kc n", p=128),
                              w=["wb%d" % b])
                        for cc in range(2):
                            o = oi % 2
                            oi += 1
                            for tg in range(2):
                                pb = pi % 4
                                pi += 1
                                for kc in range(16):
                                    p.op("tensor", lambda e, b=b, cc=cc, tg=tg, kc=kc, pb=pb: e.matmul(
                                        pp[pb][:], lhsT=wb[b][:, kc, cc * 128:(cc + 1) * 128],
                                        rhs=hT[:, kc, tg * 512:(tg + 1) * 512],
                                        start=(kc == 0), stop=(kc == 15)),
                                        r=["wb%d" % b, "hT"], w=["pp%d" % pb])
                                if tg == 0:
                                    p.op("scalar", lambda e, o=o, pb=pb: e.copy(out=ob[o][:, 0:512], in_=pp[pb][:]),
                                         r=["pp%d" % pb], w=["ob%d" % o])
                                else:
                                    p.op("vector", lambda e, o=o, pb=pb: e.tensor_copy(out=ob[o][:, 512:1024], in_=pp[pb][:]),
                                         r=["pp%d" % pb], w=["ob%d" % o])
                            rr = row + g * 256 + cc * 128
                            p.dma("sync", c.sc_fm[rr:rr + 128, t0:t0 + 1024], ob[o][:],
                                  r=["ob%d" % o], w=[("sc_fm", rr // 128)])
                    row += n
                colo = 0
                for (c0, n) in TM_SRC:
                    w = min(n, 256)
                    for g in range(max(1, n // 256)):
                        col = c0 + g * 256
                        b = gi % 2
                        gi += 1
                        p.dma("sync", wb[b][:, :, 0:w], r32(c.w_in[:, col:col + w]).rearrange("(kc p) n -> p kc n", p=128),
                              w=["wb%d" % b])
                        for tq in range(2):
                            o = oi % 2
                            oi += 1
                            for tt in range(4):
                                tl = tq * 4 + tt
                                pb = pi % 4
                                pi += 1
                                for kc in range(16):
                                    p.op("tensor", lambda e, b=b, tl=tl, kc=kc, pb=pb, w=w: e.matmul(
                                        pp[pb][:, 0:w], lhsT=hT[:, kc, tl * 128:(tl + 1) * 128],
                                        rhs=wb[b][:, kc, 0:w],
                                        start=(kc == 0), stop=(kc == 15)),
                                        r=["wb%d" % b, "hT"], w=["pp%d" % pb])
                                if tt % 2 == 0:
                                    p.op("scalar", lambda e, o=o, pb=pb, tt=tt, w=w: e.copy(
                                        out=ob[o][:, tt * 256:tt * 256 + w], in_=pp[pb][:, 0:w]),
                                        r=["pp%d" % pb], w=["ob%d" % o])
                                else:
                                    p.op("vector", lambda e, o=o, pb=pb, tt=tt, w=w: e.tensor_copy(
                                        out=ob[o][:, tt * 256:tt * 256 + w], in_=pp[pb][:, 0:w]),
                                        r=["pp%d" % pb], w=["ob%d" % o])
                            tb = t0 + tq * 512
                            cw = colo + g * 256
                            p.dma("sync",
                                  c.sc_tm[tb:tb + 512, cw:cw + w].rearrange("(tt p) n -> p tt n", p=128),
                                  ob[o][:].rearrange("p (tt n) -> p tt n", n=256)[:, :, 0:w],
                                  r=["ob%d" % o], w=[("sc_tm", tb // 128, cw)])
                    colo += n
                p.barrier()


def t5_bucket_np(n):
    n = np.maximum(n, 0)
    nf = np.maximum(n, 1).astype(np.float32)
    large = 16 + (np.log(nf / np.float32(16)) / np.float32(math.log(8.0)) * np.float32(16)).astype(np.int32)
    large = np.minimum(large, 31)
    return np.where(n < 16, n, large)


def moba_consts(rel_bias):
    k = np.arange(128)[:, None]
    q = np.arange(128)[None, :]
    out = np.zeros((8, 2, 128, 128), np.float32)
    b0 = t5_bucket_np(q - k)
    b1 = t5_bucket_np(q - k + 128)
    for h in range(8):
        out[h, 0] = np.where(q >= k, rel_bias[b0, h], np.float32(NEG))
        out[h, 1] = rel_bias[b1, h]
    cm = np.zeros((128, 16, 8), np.float32)
    notown = np.ones((128, 16, 8), np.float32)
    for t in range(16):
        for n in range(8):
            if n >= t // 2:
                cm[:, t, n] = -1e30
            if n == t // 2:
                notown[:, t, n] = 0.0
    esel = np.zeros((8, 8, 128), np.float32)
    for n in range(8):
        esel[n, n, :] = 1.0
    return out, cm, notown, esel


def phase_moba(c, p):
    nc = c.nc
    with ExitStack() as es:
        c2 = Ctx(); c2.nc = nc; c2.es = es
        cm = sb(c2, "cm", [128, 16, 8])
        notown = sb(c2, "notown", [128, 16, 8])
        esel = sb(c2, "esel", [8, 8, 128], F32R)
        cb = sb(c2, "cb", [128, 8])
        gq = sb(c2, "gq", [128, 2])
        gk = sb(c2, "gk", [128, 1])
        d01 = sb(c2, "d01", [128, 2, 128], F32R)
        d01r = sb(c2, "d01r", [128, 2, 128])
        qr = sb(c2, "qr", [128, S])
        kr = sb(c2, "kr", [128, S])
        sq = sb(c2, "sq", [128, S], F32R)
        qn = sb(c2, "qn", [128, S], F32R)
        kn = sb(c2, "kn", [128, S], F32R)
        vt = sb(c2, "vt", [128, 16, 128], F32R)
        rsb = [sb(c2, "rs%d" % i, [128, 512]) for i in range(2)]
        km = sb(c2, "km", [128, 8], F32R)
        kmf = sb(c2, "kmf", [128, 8])
        gm = sb(c2, "gm", [128, 16, 8])
        cmp_ = sb(c2, "cmp", [128, 16, 8, 8])
        rank = sb(c2, "rank", [128, 16, 8])
        nmk = sb(c2, "nmk", [128, 16, 8])
        negT = sb(c2, "negT", [8, S], F32R)
        pT = [sb(c2, "pT%d" % i, [128, 512], F32R) for i in range(2)]
        rden = sb(c2, "rden", [128, 512])
        oo = [sb(c2, "oo%d" % i, [128, 512]) for i in range(2)]
        aux = [ps(c2, "aux%d" % i, [128, 512]) for i in range(2)]
        scp = [ps(c2, "scp%d" % i, [128, 512]) for i in range(2)]
        op_ = [ps(c2, "op%d" % i, [128, 512]) for i in range(2)]
        dp = [ps(c2, "dp%d" % i, [128, 512]) for i in range(2)]

        p.dma("sync", cm[:], c.cm_d[:, :, :], w=["cm"])
        p.dma("sync", notown[:], c.notown_d[:, :, :], w=["notown"])
        p.dma("sync", esel[:], r32(c.esel_d[:, :, :]), w=["esel"])
        p.dma("sync", cb[:], c.rel_bias[31:32, :].partition_broadcast(128), w=["cb"])
        p.dma("sync", gq[:, 0:1], c.q_norm_gain.rearrange("o d -> d o"), w=["gq"])
        p.dma("sync", gk[:, 0:1], c.k_norm_gain.rearrange("o d -> d o"), w=["gk"])
        p.op("vector", lambda e: e.tensor_scalar(out=gq[:, 1:2], in0=gq[:, 0:1], scalar1=128.0 ** -0.5, scalar2=None,
                                                 op0=ALU.mult), r=["gq"], w=["gq"])
        auxi = 0
        sci = 0
        gi = 0
        for h in range(8):
            p.dma("sync", qr[:], c.sc_fm[h * 128:(h + 1) * 128, :], w=["qr"])
            p.dma("sync", kr[:], c.sc_fm[1024 + h * 128:1024 + (h + 1) * 128, :], w=["kr"])
            p.dma("sync", vt[:], r32(c.sc_tm[:, h * 128:(h + 1) * 128]).rearrange("(t p) d -> p t d", p=128), w=["vt"])
            p.dma("sync", d01r[:], c.d01_d[h].rearrange("a k q -> k a q"), w=["d01r"])
            p.op("vector", lambda e, h=h: e.tensor_scalar(out=d01[:], in0=d01r[:], scalar1=cb[:, h:h + 1], scalar2=None,
                                                          op0=ALU.subtract), r=["d01r", "cb"], w=["d01"])
            for (raw, dst, gcol, rk, wk) in ((qr, qn, gq[:, 1:2], "qr", "qn"), (kr, kn, gk[:, 0:1], "kr", "kn")):
                p.op("scalar", lambda e, raw=raw: e.activation(out=sq[:], in_=raw[:], func=AF.Square), r=[rk], w=["sq"])
                for tg in range(4):
                    a = auxi % 2
                    auxi += 1
                    sl = slice(tg * 512, (tg + 1) * 512)
                    p.op("tensor", lambda e, a=a, sl=sl: e.matmul(aux[a][:], lhsT=c.ones_r[:], rhs=sq[:, sl], start=True, stop=True),
                         r=["sq"], w=["aux%d" % a])
                    rs = rsb[a]
                    p.op("scalar", lambda e, a=a, rs=rs: e.activation(out=rs[:], in_=aux[a][:], func=AF.Sqrt, scale=1.0 / 128,
                                                                      bias=c.eps_t[:, 0:1]), r=["aux%d" % a], w=["rs%d" % a])
                    p.op("vector", lambda e, rs=rs: e.reciprocal(out=rs[:], in_=rs[:]), r=["rs%d" % a], w=["rs%d" % a])
                    p.op("vector", lambda e, raw=raw, dst=dst, gcol=gcol, sl=sl, rs=rs: e.scalar_tensor_tensor(
                        out=dst[:, sl], in0=raw[:, sl], scalar=gcol, in1=rs[:], op0=ALU.mult, op1=ALU.mult),
                        r=[rk, "rs%d" % a, "gq", "gk"], w=[wk])
            p.op("vector", lambda e: e.tensor_reduce(out=kmf[:], in_=kn[:].bitcast(F32).rearrange("p (n j) -> p n j", j=256),
                                                     axis=AX.X, op=ALU.add), r=["kn"], w=["kmf"])
            p.op("vector", lambda e: e.tensor_copy(out=km[:], in_=kmf[:]), r=["kmf"], w=["km"])
            a = auxi % 2
            auxi += 1
            for t in range(16):
                p.op("tensor", lambda e, a=a, t=t: e.matmul(aux[a][:, t * 8:(t + 1) * 8], lhsT=qn[:, t * 128:(t + 1) * 128],
                                                            rhs=km[:], start=True, stop=True),
                     r=["qn", "km"], w=["aux%d" % a])
            p.op("vector", lambda e, a=a: e.tensor_tensor(out=gm[:], in0=aux[a][:, 0:128].rearrange("p (t n) -> p t n", n=8),
                                                          in1=cm[:], op=ALU.add), r=["aux%d" % a, "cm"], w=["gm"])
            p.op("vector", lambda e: e.tensor_tensor(out=cmp_[:], in0=gm[:].unsqueeze(2).to_broadcast([128, 16, 8, 8]),
                                                     in1=gm[:].unsqueeze(3).to_broadcast([128, 16, 8, 8]), op=ALU.is_gt),
                 r=["gm"], w=["cmp"])
            p.op("vector", lambda e: e.tensor_reduce(out=rank[:], in_=cmp_[:], axis=AX.X, op=ALU.add), r=["cmp"], w=["rank"])
            p.op("vector", lambda e: e.tensor_scalar(out=rank[:], in0=rank[:], scalar1=3.0, scalar2=NEG, op0=ALU.is_ge,
                                                     op1=ALU.mult), r=["rank"], w=["rank"])
            p.op("vector", lambda e: e.tensor_tensor(out=nmk[:], in0=rank[:], in1=notown[:], op=ALU.mult),
                 r=["rank", "notown"], w=["nmk"])
            for tg in range(4):
                a = auxi % 2
                auxi += 1
                for j in range(4):
                    t = tg * 4 + j
                    p.op("tensor", lambda e, a=a, t=t, j=j: e.transpose(out=aux[a][0:8, j * 128:(j + 1) * 128], in_=nmk[:, t, :],
                                                                        identity=c.ident[:]),
                         r=["nmk"], w=["aux%d" % a])
                p.op("scalar", lambda e, a=a, tg=tg: e.copy(out=negT[:, tg * 512:(tg + 1) * 512], in_=aux[a][0:8, :]),
                     r=["aux%d" % a], w=["negT"])
            steps = []
            for g in range(4):
                gb = gi % 2
                gi += 1
                nk = 4 * g + 4
                for kt in range(nk):
                    s_ = sci % 2
                    sci += 1
                    jj0 = max(0, kt - 4 * g)
                    c0 = jj0 * 128
                    extra = []
                    if kt >= 4 * g:
                        extra.append((jj0, 0))
                        if jj0 + 1 < 4:
                            extra.append((jj0 + 1, 1))
                    elif kt == 4 * g - 1:
                        extra.append((0, 1))
                    steps.append(dict(g=g, gb=gb, nk=nk, kt=kt, s_=s_, qs=slice(g * 512 + c0, (g + 1) * 512), cs=slice(c0, 512),
                                      n=kt // 2, extra=extra))

            def emit_score(st, h=h):
                s_, kt, qs, cs, n, extra = st["s_"], st["kt"], st["qs"], st["cs"], st["n"], st["extra"]
                p.op("tensor", lambda e: e.matmul(scp[s_][:, cs], lhsT=kn[:, kt * 128:(kt + 1) * 128], rhs=qn[:, qs],
                                                  start=True, stop=False), r=["kn", "qn"], w=["scp%d" % s_])
                p.op("tensor", lambda e: e.matmul(scp[s_][:, cs], lhsT=esel[:, n, :], rhs=negT[:, qs], start=False, stop=(not extra)),
                     r=["esel", "negT"], w=["scp%d" % s_])
                for ei, (jj, which) in enumerate(extra):
                    p.op("tensor", lambda e, jj=jj, which=which, last=(ei == len(extra) - 1): e.matmul(
                        scp[s_][:, jj * 128:(jj + 1) * 128], lhsT=c.ident_r[:], rhs=d01[:, which, :], start=False, stop=last),
                        r=["d01"], w=["scp%d" % s_])
                p.op("scalar", lambda e: e.activation(out=pT[s_][:, cs], in_=scp[s_][:, cs], func=AF.Exp, bias=cb[:, h:h + 1]),
                     r=["scp%d" % s_, "cb"], w=["pT%d" % s_])

            def emit_pv(st, h=h):
                s_, kt, cs, gb, nk, g = st["s_"], st["kt"], st["cs"], st["gb"], st["nk"], st["g"]
                p.op("tensor", lambda e: e.matmul(op_[gb][:, cs], lhsT=vt[:, kt, :], rhs=pT[s_][:, cs], start=(kt == 0), stop=(kt == nk - 1)),
                     r=["vt", "pT%d" % s_], w=["op%d" % gb])
                p.op("tensor", lambda e: e.matmul(dp[gb][:, cs], lhsT=c.ones_r[:], rhs=pT[s_][:, cs], start=(kt == 0), stop=(kt == nk - 1)),
                     r=["pT%d" % s_], w=["dp%d" % gb])
                if kt == nk - 1:
                    p.op("vector", lambda e: e.reciprocal(out=rden[:], in_=dp[gb][:]), r=["dp%d" % gb], w=["rden"])
                    p.op("vector", lambda e: e.tensor_tensor(out=oo[gb][:], in0=op_[gb][:], in1=rden[:], op=ALU.mult),
                         r=["op%d" % gb, "rden"], w=["oo%d" % gb])
                    p.dma("sync", c.sc_oa[h * 128:(h + 1) * 128, g * 512:(g + 1) * 512], oo[gb][:],
                          r=["oo%d" % gb], w=[("sc_oa", h, g)])

            emit_score(steps[0])
            for i_, st in enumerate(steps):
                if i_ + 1 < len(steps):
                    emit_score(steps[i_ + 1])
                emit_pv(st)
        p.barrier()


def gdn_consts():
    k = np.arange(128)[:, None]
    i = np.arange(128)[None, :]
    same = (k // 64) == (i // 64)
    tri = ((k <= i) & same).astype(np.float32)
    blk = same.astype(np.float32)
    half0 = np.broadcast_to((k < 64), (128, 128)).astype(np.float32)
    half1 = np.broadcast_to((k >= 64), (128, 128)).astype(np.float32)
    ustr = ((k > i) & same).astype(np.float32)
    ii = np.arange(128)[:, None]
    jj = np.arange(128)[None, :]
    same2 = (ii // 64) == (jj // 64)
    negm_strict = np.where((ii > jj) & same2, 0.0, NEG).astype(np.float32)
    negm_inclT = np.where((jj >= ii) & same2, 0.0, NEG).astype(np.float32)
    return np.stack([tri, blk, half0, half1, ustr, negm_strict, negm_inclT], 0)


class _Stop(Exception):
    pass


def _chk(n):
    import os
    return int(os.environ.get("GDN_STOP", "99")) == n


def phase_gdn(c, p):
    _phase_gdn(c, p)
    p.barrier()


def _phase_gdn(c, p):
    nc = c.nc
    with ExitStack() as es:
        c2 = Ctx(); c2.nc = nc; c2.es = es
        G = sb(c2, "gc", [128, 7, 128])
        TRI, BLK, H0, H1, USTR, NMS, NMIT = [G[:, i, :] for i in range(7)]
        ba = sb(c2, "ba", [128, 16, 16])
        dtb = sb(c2, "dtb", [128, 8])
        Aex = sb(c2, "Aex", [128, 8])
        gng = sb(c2, "gng", [128, 128])
        beta = sb(c2, "beta", [128, 16, 8])
        nbeta = sb(c2, "nbeta", [128, 16, 8])
        gg = sb(c2, "gg", [128, 16, 8])
        egs = sb(c2, "egs", [128, 16, 8])
        kds = sb(c2, "kds", [128, 16, 8])
        kbs = sb(c2, "kbs", [128, 16, 8])
        egl = sb(c2, "egl", [128, 2, 16, 8])
        tmp8 = sb(c2, "tmp8", [128, 16, 8])
        cw = sb(c2, "cw", [128, 3, 4])
        xp = [sb(c2, "xp%d" % i, [128, 3 + S]) for i in range(3)]
        cv = [sb(c2, "cv%d" % i, [128, S]) for i in range(3)]
        sq = sb(c2, "gsq", [128, S], F32R)
        rs = sb(c2, "grs", [128, 512])
        NGB = 4
        kbg = [sb(c2, "kbg%d" % j, [128, 128]) for j in range(NGB)]
        vbe = [sb(c2, "vbe%d" % j, [128, 128]) for j in range(NGB)]
        Ag = [sb(c2, "Ag%d" % j, [128, 128]) for j in range(NGB)]
        Dec = [sb(c2, "Dec%d" % j, [128, 128]) for j in range(NGB)]
        DecT = [sb(c2, "DecT%d" % j, [128, 128]) for j in range(NGB)]
        Am = [[sb(c2, "Am%d_%d" % (j, i), [128, 128]) for i in range(2)] for j in range(NGB)]
        At = [[sb(c2, "At%d_%d" % (j, i), [128, 128]) for i in range(2)] for j in range(NGB)]
        Rt = [[sb(c2, "Rt%d_%d" % (j, i), [128, 128]) for i in range(2)] for j in range(NGB)]
        u_all = sb(c2, "u_all", [128, 16, 128])
        wT_all = sb(c2, "wT_all", [128, 16, 128])
        aT_all = sb(c2, "aT_all", [128, 16, 128])
        kd_all = sb(c2, "kd_all", [128, 2, 16, 128])
        kds2 = sb(c2, "kds2", [128, 2, 16, 8])
        St = sb(c2, "St", [128, 128])
        vnew = sb(c2, "vnew", [128, 128])
        otmp = sb(c2, "otmp", [128, 128])
        ob = sb(c2, "ob", [128, 16, 128])
        zt = sb(c2, "zt", [128, 16, 128])
        ssq = sb(c2, "ssq", [128, 16])
        junk = sb(c2, "junk", [128, 128])
        obT = sb(c2, "obT", [128, S])
        pool = [ps(c2, "gp%d" % i, [128, 512]) for i in range(8)]
        pc = [0]

        def nxt():
            i = pc[0] % 8
            pc[0] += 1
            return i

        p.dma("sync", G[:], c.gdnc_d.rearrange("a k i -> k a i"), w=["G"])
        p.dma("sync", ba[:], c.sc_tm[:, 2048:2064].rearrange("(t p) n -> p t n", p=128), w=["ba"])
        p.dma("sync", dtb[:], c.dt_bias.partition_broadcast(128), w=["dtb"])
        p.dma("sync", Aex[:], c.a_log.partition_broadcast(128), w=["Aex"])
        p.dma("sync", gng[:], c.gdn_norm_gain.partition_broadcast(128), w=["gng"])
        p.op("scalar", lambda e: e.activation(out=Aex[:], in_=Aex[:], func=AF.Exp), r=["Aex"], w=["Aex"])
        p.op("scalar", lambda e: e.activation(out=beta[:], in_=ba[:, :, 0:8], func=AF.Exp, scale=-1.0), r=["ba"], w=["beta"])
        p.op("vector", lambda e: e.tensor_scalar(out=beta[:], in0=beta[:], scalar1=1.0, scalar2=None, op0=ALU.add), r=["beta"], w=["beta"])
        p.op("vector", lambda e: e.reciprocal(out=beta[:], in_=beta[:]), r=["beta"], w=["beta"])
        p.op("vector", lambda e: e.tensor_scalar(out=nbeta[:], in0=beta[:], scalar1=-1.0, scalar2=None, op0=ALU.mult), r=["beta"], w=["nbeta"])
        p.op("vector", lambda e: e.tensor_tensor(out=gg[:], in0=ba[:, :, 8:16], in1=dtb[:].unsqueeze(1).to_broadcast([128, 16, 8]),
                                                 op=ALU.add), r=["ba", "dtb"], w=["gg"])
        p.op("scalar", lambda e: e.activation(out=gg[:], in_=gg[:], func=AF.Exp), r=["gg"], w=["gg"])
        p.op("scalar", lambda e: e.activation(out=gg[:], in_=gg[:], func=AF.Ln, bias=c.one_t[:, 0:1]), r=["gg"], w=["gg"])
        p.op("vector", lambda e: e.scalar_tensor_tensor(out=gg[:], in0=gg[:], scalar=-1.0, in1=Aex[:].unsqueeze(1).to_broadcast([128, 16, 8]),
                                                        op0=ALU.mult, op1=ALU.mult), r=["gg", "Aex"], w=["gg"])
        ggf = gg[:].rearrange("p t h -> p (t h)")
        i0 = nxt(); i1 = nxt(); i2 = nxt(); i3 = nxt()
        p.op("tensor", lambda e: e.matmul(pool[i0][:, 0:128], lhsT=TRI, rhs=ggf, start=True, stop=True), r=["G", "gg"], w=["gp%d" % i0])
        p.op("tensor", lambda e: e.matmul(pool[i1][:, 0:128], lhsT=BLK, rhs=ggf, start=True, stop=True), r=["G", "gg"], w=["gp%d" % i1])
        p.op("tensor", lambda e: e.matmul(pool[i2][:, 0:128], lhsT=H0, rhs=ggf, start=True, stop=True), r=["G", "gg"], w=["gp%d" % i2])
        p.op("tensor", lambda e: e.matmul(pool[i3][:, 0:128], lhsT=H1, rhs=ggf, start=True, stop=True), r=["G", "gg"], w=["gp%d" % i3])
        v3 = lambda t_: t_.rearrange("p (t h) -> p t h", h=8)
        p.op("scalar", lambda e: e.activation(out=egs[:], in_=v3(pool[i0][:, 0:128]), func=AF.Exp), r=["gp%d" % i0], w=["egs"])
        p.op("vector", lambda e: e.tensor_tensor(out=kbs[:], in0=egs[:], in1=beta[:], op=ALU.mult), r=["egs", "beta"], w=["kbs"])
        p.op("vector", lambda e: e.tensor_copy(out=tmp8[:], in_=v3(pool[i0][:, 0:128])), r=["gp%d" % i0], w=["tmp8"])
        p.op("vector", lambda e: e.tensor_tensor(out=tmp8[:], in0=v3(pool[i1][:, 0:128]), in1=tmp8[:], op=ALU.subtract),
             r=["gp%d" % i1, "tmp8"], w=["tmp8"])
        p.op("scalar", lambda e: e.activation(out=kds[:], in_=tmp8[:], func=AF.Exp), r=["tmp8"], w=["kds"])
        p.op("scalar", lambda e: e.activation(out=egl[:, 0], in_=v3(pool[i2][:, 0:128]), func=AF.Exp), r=["gp%d" % i2], w=["egl"])
        p.op("scalar", lambda e: e.activation(out=egl[:, 1], in_=v3(pool[i3][:, 0:128]), func=AF.Exp), r=["gp%d" % i3], w=["egl"])
        p.op("vector", lambda e: e.tensor_scalar(out=egs[:], in0=egs[:], scalar1=128.0 ** -0.5, scalar2=None, op0=ALU.mult),
             r=["egs", "kbs"], w=["egs"])
        p.op("vector", lambda e: e.tensor_scalar(out=kds2[:, 0], in0=kds[:], scalar1=H0[:, 0:1], scalar2=None, op0=ALU.mult),
             r=["kds", "G"], w=["kds2"])
        p.op("vector", lambda e: e.tensor_scalar(out=kds2[:, 1], in0=kds[:], scalar1=H1[:, 0:1], scalar2=None, op0=ALU.mult),
             r=["kds", "G"], w=["kds2"])
        for i in range(3):
            p.op("vector", lambda e, i=i: e.memset(xp[i][:, 0:3], 0.0), w=["xp%d" % i])
        p.op("vector", lambda e: e.memset(vnew[:], 0.0), w=["vnew"])

        if _chk(0):
            return
        for h in range(8):
            for i in range(3):
                row = 2048 + i * 1024 + h * 128
                p.dma("sync", xp[i][:, 3:3 + S], c.sc_fm[row:row + 128, :], w=["xp%d" % i])
                p.dma("sync", cw[:, i, :], c.conv_wT[i * 1024 + h * 128:i * 1024 + (h + 1) * 128, :], w=["cw"])
                p.op("vector", lambda e, i=i: e.tensor_scalar(out=cv[i][:], in0=xp[i][:, 0:S], scalar1=cw[:, i, 0:1], scalar2=None,
                                                              op0=ALU.mult), r=["xp%d" % i, "cw"], w=["cv%d" % i])
                for tap in range(1, 4):
                    p.op("vector", lambda e, i=i, tap=tap: e.scalar_tensor_tensor(
                        out=cv[i][:], in0=xp[i][:, tap:tap + S], scalar=cw[:, i, tap:tap + 1], in1=cv[i][:],
                        op0=ALU.mult, op1=ALU.add), r=["xp%d" % i, "cw", "cv%d" % i], w=["cv%d" % i])
                p.op("scalar", lambda e, i=i: e.activation(out=cv[i][:], in_=cv[i][:], func=AF.Silu), r=["cv%d" % i], w=["cv%d" % i])
                if i < 2:
                    p.op("scalar", lambda e, i=i: e.activation(out=sq[:], in_=cv[i][:], func=AF.Square), r=["cv%d" % i], w=["gsq"])
                    for tg in range(4):
                        a = nxt()
                        sl = slice(tg * 512, (tg + 1) * 512)
                        p.op("tensor", lambda e, a=a, sl=sl: e.matmul(pool[a][:], lhsT=c.ones_r[:], rhs=sq[:, sl], start=True, stop=True),
                             r=["gsq"], w=["gp%d" % a])
                        p.op("scalar", lambda e, a=a: e.activation(out=rs[:], in_=pool[a][:], func=AF.Sqrt, bias=c.eps_t[:, 0:1]),
                             r=["gp%d" % a], w=["grs"])
                        p.op("vector", lambda e: e.reciprocal(out=rs[:], in_=rs[:]), r=["grs"], w=["grs"])
                        p.op("vector", lambda e, i=i, sl=sl: e.tensor_tensor(out=cv[i][:, sl], in0=cv[i][:, sl], in1=rs[:], op=ALU.mult),
                             r=["cv%d" % i, "grs"], w=["cv%d" % i])
            qn, kn, vn = cv
            if _chk(1):
                return
            p.dma("sync", zt[:], c.sc_tm[:, 1024 + h * 128:1024 + (h + 1) * 128].rearrange("(t p) d -> p t d", p=128), w=["zt"])
            NG = 4
            for t0_ in range(0, 16, NG):
                tl = list(range(t0_, t0_ + NG))
                ak = {}; av = {}; akk = {}; agd = {}; aqk = {}; agt = {}; amt = {}
                for j, t in enumerate(tl):
                    ts = slice(t * 128, (t + 1) * 128)
                    ak[j] = nxt()
                    p.op("tensor", lambda e, a=ak[j], ts=ts: e.transpose(out=pool[a][:, 0:128], in_=kn[:, ts], identity=c.ident[:]),
                         r=["cv1"], w=["gp%d" % ak[j]])
                    p.op("vector", lambda e, a=ak[j], t=t, h=h, j=j: e.tensor_scalar(out=kbg[j][:], in0=pool[a][:, 0:128],
                                                                                   scalar1=kbs[:, t, h:h + 1], scalar2=None, op0=ALU.mult),
                         r=["gp%d" % ak[j], "kbs"], w=["kbg%d" % j])
                    for hf_ in range(2):
                        p.op("scalar", lambda e, a=ak[j], t=t, h=h, hf_=hf_: e.activation(out=kd_all[:, hf_, t, :], in_=pool[a][:, 0:128],
                                                                                        func=AF.Identity, scale=kds2[:, hf_, t, h:h + 1]),
                             r=["gp%d" % ak[j], "kds2"], w=["kd_all"])
                    av[j] = nxt()
                    p.op("tensor", lambda e, a=av[j], ts=ts: e.transpose(out=pool[a][:, 0:128], in_=vn[:, ts], identity=c.ident[:]),
                         r=["cv2"], w=["gp%d" % av[j]])
                    p.op("vector", lambda e, a=av[j], t=t, h=h, j=j: e.tensor_scalar(out=vbe[j][:], in0=pool[a][:, 0:128],
                                                                                   scalar1=beta[:, t, h:h + 1], scalar2=None, op0=ALU.mult),
                         r=["gp%d" % av[j], "beta"], w=["vbe%d" % j])
                    p.op("gpsimd", lambda e, t=t, h=h, j=j: e.tensor_scalar(out=Ag[j][:], in0=USTR, scalar1=gg[:, t, h:h + 1], scalar2=None,
                                                                            op0=ALU.mult), r=["G", "gg"], w=["Ag%d" % j])
                for j, t in enumerate(tl):
                    ts = slice(t * 128, (t + 1) * 128)
                    akk[j] = nxt()
                    p.op("tensor", lambda e, a=akk[j], ts=ts: e.matmul(pool[a][:, 0:128], lhsT=kn[:, ts], rhs=kn[:, ts], start=True, stop=True),
                         r=["cv1"], w=["gp%d" % akk[j]])
                    agd[j] = nxt()
                    p.op("tensor", lambda e, a=agd[j], j=j: e.matmul(pool[a][:, 0:128], lhsT=TRI, rhs=Ag[j][:], start=True, stop=False),
                         r=["G", "Ag%d" % j], w=["gp%d" % agd[j]])
                    p.op("tensor", lambda e, a=agd[j]: e.matmul(pool[a][:, 0:128], lhsT=c.ident[:], rhs=NMS, start=False, stop=True),
                         r=["G"], w=["gp%d" % agd[j]])
                    p.op("scalar", lambda e, a=agd[j], j=j: e.activation(out=Dec[j][:], in_=pool[a][:, 0:128], func=AF.Exp),
                         r=["gp%d" % agd[j]], w=["Dec%d" % j])
                    p.op("vector", lambda e, a=akk[j], t=t, h=h, j=j: e.scalar_tensor_tensor(out=Am[j][0][:], in0=pool[a][:, 0:128],
                                                                                           scalar=nbeta[:, t, h:h + 1], in1=Dec[j][:],
                                                                                           op0=ALU.mult, op1=ALU.mult),
                         r=["gp%d" % akk[j], "nbeta", "Dec%d" % j], w=["Am%d_0" % j])
                for j, t in enumerate(tl):
                    ts = slice(t * 128, (t + 1) * 128)
                    aqk[j] = nxt()
                    p.op("tensor", lambda e, a=aqk[j], ts=ts: e.matmul(pool[a][:, 0:128], lhsT=kn[:, ts], rhs=qn[:, ts], start=True, stop=True),
                         r=["cv1", "cv0"], w=["gp%d" % aqk[j]])
                    agt[j] = nxt()
                    p.op("tensor", lambda e, a=agt[j], j=j: e.matmul(pool[a][:, 0:128], lhsT=Ag[j][:], rhs=TRI, start=True, stop=False),
                         r=["G", "Ag%d" % j], w=["gp%d" % agt[j]])
                    p.op("tensor", lambda e, a=agt[j]: e.matmul(pool[a][:, 0:128], lhsT=c.ident[:], rhs=NMIT, start=False, stop=True),
                         r=["G"], w=["gp%d" % agt[j]])
                    p.op("scalar", lambda e, a=agt[j], j=j: e.activation(out=DecT[j][:], in_=pool[a][:, 0:128], func=AF.Exp),
                         r=["gp%d" % agt[j]], w=["DecT%d" % j])
                    p.op("vector", lambda e, a=aqk[j], t=t, j=j: e.scalar_tensor_tensor(out=aT_all[:, t, :], in0=pool[a][:, 0:128],
                                                                                      scalar=128.0 ** -0.5, in1=DecT[j][:],
                                                                                      op0=ALU.mult, op1=ALU.mult),
                         r=["gp%d" % aqk[j], "DecT%d" % j], w=["aT_all"])
                for j, t in enumerate(tl):
                    amt[j] = nxt()
                    p.op("tensor", lambda e, a=amt[j], j=j: e.transpose(out=pool[a][:, 0:128], in_=Am[j][0][:], identity=c.ident[:]),
                         r=["Am%d_0" % j], w=["gp%d" % amt[j]])
                    p.op("scalar", lambda e, a=amt[j], j=j: e.copy(out=At[j][0][:], in_=pool[a][:, 0:128]),
                         r=["gp%d" % amt[j]], w=["At%d_0" % j])
                    p.op("gpsimd", lambda e, j=j: e.tensor_tensor(out=Rt[j][0][:], in0=At[j][0][:], in1=c.ident[:], op=ALU.add),
                         r=["At%d_0" % j], w=["Rt%d_0" % j])
                cur = 0
                for m in range(1, 6):
                    nx = 1 - cur
                    for j, t in enumerate(tl):
                        a1 = nxt()
                        p.op("tensor", lambda e, a=a1, cur=cur, j=j: e.matmul(pool[a][:, 0:128], lhsT=At[j][cur][:], rhs=Am[j][cur][:],
                                                                              start=True, stop=True),
                             r=["At%d_%d" % (j, cur), "Am%d_%d" % (j, cur)], w=["gp%d" % a1])
                        p.op("scalar", lambda e, a=a1, nx=nx, j=j: e.copy(out=Am[j][nx][:], in_=pool[a][:, 0:128]),
                             r=["gp%d" % a1], w=["Am%d_%d" % (j, nx)])
                        if m < 5:
                            a2 = nxt()
                            p.op("tensor", lambda e, a=a2, cur=cur, j=j: e.matmul(pool[a][:, 0:128], lhsT=Am[j][cur][:], rhs=At[j][cur][:],
                                                                                  start=True, stop=True),
                                 r=["At%d_%d" % (j, cur), "Am%d_%d" % (j, cur)], w=["gp%d" % a2])
                            p.op("vector", lambda e, a=a2, nx=nx, j=j: e.tensor_copy(out=At[j][nx][:], in_=pool[a][:, 0:128]),
                                 r=["gp%d" % a2], w=["At%d_%d" % (j, nx)])
                    for j, t in enumerate(tl):
                        a3 = nxt()
                        p.op("tensor", lambda e, a=a3, cur=cur, nx=nx, j=j: e.matmul(pool[a][:, 0:128], lhsT=Am[j][nx][:], rhs=Rt[j][cur][:],
                                                                                     start=True, stop=True),
                             r=["Am%d_%d" % (j, nx), "Rt%d_%d" % (j, cur)], w=["gp%d" % a3])
                        p.op("vector", lambda e, a=a3, cur=cur, nx=nx, j=j: e.tensor_tensor(out=Rt[j][nx][:], in0=pool[a][:, 0:128],
                                                                                            in1=Rt[j][cur][:], op=ALU.add),
                             r=["gp%d" % a3, "Rt%d_%d" % (j, cur)], w=["Rt%d_%d" % (j, nx)])
                    cur = nx
                for j, t in enumerate(tl):
                    RtF = Rt[j][cur]
                    a_u = nxt()
                    p.op("tensor", lambda e, a=a_u, RtF=RtF, j=j: e.matmul(pool[a][:, 0:128], lhsT=RtF[:], rhs=vbe[j][:], start=True, stop=True),
                         r=["Rt%d_%d" % (j, cur), "vbe%d" % j], w=["gp%d" % a_u])
                    p.op("scalar", lambda e, a=a_u, t=t: e.copy(out=u_all[:, t, :], in_=pool[a][:, 0:128]), r=["gp%d" % a_u], w=["u_all"])
                    a_w = nxt()
                    p.op("tensor", lambda e, a=a_w, RtF=RtF, j=j: e.matmul(pool[a][:, 0:128], lhsT=kbg[j][:], rhs=RtF[:], start=True, stop=True),
                         r=["Rt%d_%d" % (j, cur), "kbg%d" % j], w=["gp%d" % a_w])
                    p.op("vector", lambda e, a=a_w, t=t: e.tensor_copy(out=wT_all[:, t, :], in_=pool[a][:, 0:128]),
                         r=["gp%d" % a_w], w=["wT_all"])
            if _chk(3):
                return
            p.op("vector", lambda e: e.memset(St[:], 0.0), w=["St"])
            for ch in range(32):
                t = ch // 2
                hf = ch % 2
                rows = slice(hf * 64, hf * 64 + 64)
                ts = slice(t * 128, (t + 1) * 128)
                a1 = nxt()
                p.op("tensor", lambda e, a=a1, t=t: e.matmul(pool[a][:, 0:128], lhsT=wT_all[:, t, :], rhs=St[:], start=True, stop=True),
                     r=["wT_all", "St"], w=["gp%d" % a1])
                p.op("vector", lambda e, a=a1, t=t, rows=rows: e.tensor_tensor(out=vnew[rows, :], in0=u_all[rows, t, :],
                                                                               in1=pool[a][rows, 0:128], op=ALU.subtract),
                     r=["gp%d" % a1, "u_all"], w=["vnew"])
                aA = nxt()
                p.op("tensor", lambda e, a=aA, ts=ts: e.matmul(pool[a][:, 0:128], lhsT=qn[:, ts], rhs=St[:], start=True, stop=True),
                     r=["cv0", "St"], w=["gp%d" % aA])
                aB = nxt()
                p.op("tensor", lambda e, a=aB, t=t: e.matmul(pool[a][:, 0:128], lhsT=aT_all[:, t, :], rhs=vnew[:], start=True, stop=True),
                     r=["aT_all", "vnew"], w=["gp%d" % aB])
                aS = nxt()
                p.op("tensor", lambda e, a=aS, t=t, hf=hf: e.matmul(pool[a][:, 0:128], lhsT=kd_all[:, hf, t, :], rhs=vnew[:],
                                                                    start=True, stop=True),
                     r=["kd_all", "vnew"], w=["gp%d" % aS])
                p.op("scalar", lambda e, a=aA, t=t, h=h, rows=rows: e.activation(out=otmp[rows, :], in_=pool[a][rows, 0:128], func=AF.Identity,
                                                                                 scale=egs[rows, t, h:h + 1]),
                     r=["gp%d" % aA, "egs"], w=["otmp"])
                p.op("vector", lambda e, a=aB, t=t, rows=rows: e.tensor_tensor(out=ob[rows, t, :], in0=otmp[rows, :], in1=pool[a][rows, 0:128],
                                                                               op=ALU.add),
                     r=["gp%d" % aB, "otmp"], w=["ob"])
                p.op("vector", lambda e, a=aS, t=t, hf=hf, h=h: e.scalar_tensor_tensor(out=St[:], in0=St[:], scalar=egl[:, hf, t, h:h + 1],
                                                                                       in1=pool[a][:, 0:128], op0=ALU.mult, op1=ALU.add),
                     r=["gp%d" % aS, "St", "egl"], w=["St"])
            if _chk(4):
                return
            for t in range(16):
                p.op("scalar", lambda e, t=t: e.activation(out=junk[:], in_=ob[:, t, :], func=AF.Square, accum_out=ssq[:, t:t + 1]),
                     r=["ob"], w=["junk", "ssq"])
            p.op("scalar", lambda e: e.activation(out=ssq[:], in_=ssq[:], func=AF.Sqrt, scale=1.0 / 128, bias=c.eps_t[:, 0:1]),
                 r=["ssq"], w=["ssq"])
            p.op("vector", lambda e: e.reciprocal(out=ssq[:], in_=ssq[:]), r=["ssq"], w=["ssq"])
            p.op("scalar", lambda e: e.activation(out=zt[:], in_=zt[:], func=AF.Silu), r=["zt"], w=["zt"])
            p.op("vector", lambda e: e.tensor_tensor(out=ob[:], in0=ob[:], in1=ssq[:].unsqueeze(2).to_broadcast([128, 16, 128]), op=ALU.mult),
                 r=["ob", "ssq"], w=["ob"])
            p.op("vector", lambda e: e.tensor_tensor(out=ob[:], in0=ob[:], in1=gng[:].unsqueeze(1).to_broadcast([128, 16, 128]), op=ALU.mult),
                 r=["ob", "gng"], w=["ob"])
            p.op("vector", lambda e: e.tensor_tensor(out=ob[:], in0=ob[:], in1=zt[:], op=ALU.mult), r=["ob", "zt"], w=["ob"])
            for t in range(16):
                a = nxt()
                p.op("tensor", lambda e, a=a, t=t: e.transpose(out=pool[a][:, 0:128], in_=ob[:, t, :], identity=c.ident[:]),
                     r=["ob"], w=["gp%d" % a])
                p.op("scalar", lambda e, a=a, t=t: e.copy(out=obT[:, t * 128:(t + 1) * 128], in_=pool[a][:, 0:128]),
                     r=["gp%d" % a], w=["obT"])
            p.dma("sync", c.sc_ob[h * 128:(h + 1) * 128, :], obT[:], r=["obT"], w=[("sc_ob", h)])
        p.barrier()


def phase_merge(c, p):
    nc = c.nc
    with ExitStack() as es:
        c2 = Ctx(); c2.nc = nc; c2.es = es
        oa = sb(c2, "oa", [128, 8, 512], F32R)
        obb = sb(c2, "obb", [128, 8, 512], F32R)
        wa = [sb(c2, "wa%d" % i, [128, 8, 128], F32R) for i in range(2)]
        wbb = [sb(c2, "wbb%d" % i, [128, 8, 128], F32R) for i in range(2)]
        ga = [sb(c2, "ga%d" % i, [128, 512]) for i in range(2)]
        gb_ = [sb(c2, "gb%d" % i, [128, 512]) for i in range(2)]
        m1 = sb(c2, "m1", [128, 512])
        mT = sb(c2, "mT", [128, 16, 512], F32R)
        wo = [sb(c2, "wo%d" % i, [128, 16, 512], F32R) for i in range(2)]
        xt = [sb(c2, "xt%d" % i, [128, 512]) for i in range(2)]
        pa = [ps(c2, "pa%d" % i, [128, 512]) for i in range(2)]
        pb = [ps(c2, "pb%d" % i, [128, 512]) for i in range(2)]
        po = [ps(c2, "po%d" % i, [128, 512]) for i in range(4)]
        ci = 0
        wi = 0
        oi = 0
        for tg in range(4):
            tsl = slice(tg * 512, (tg + 1) * 512)
            p.dma("sync", oa[:], r32(c.sc_oa[:, tsl]).rearrange("(kc p) t -> p kc t", p=128), w=["oa"])
            p.dma("sync", obb[:], r32(c.sc_ob[:, tsl]).rearrange("(kc p) t -> p kc t", p=128), w=["obb"])
            for cc in range(16):
                b = ci % 2
                ci += 1
                csl = slice(cc * 128, (cc + 1) * 128)
                p.dma("sync", wa[b][:], r32(c.w_up_a[:, csl]).rearrange("(kc p) n -> p kc n", p=128), w=["wa%d" % b])
                p.dma("sync", wbb[b][:], r32(c.w_up_b[:, csl]).rearrange("(kc p) n -> p kc n", p=128), w=["wbb%d" % b])
                p.dma("sync", ga[b][:], c.sc_fm[5120 + cc * 128:5120 + (cc + 1) * 128, tsl], w=["ga%d" % b])
                p.dma("sync", gb_[b][:], c.sc_fm[7168 + cc * 128:7168 + (cc + 1) * 128, tsl], w=["gb%d" % b])
                for kc in range(8):
                    p.op("tensor", lambda e, b=b, kc=kc: e.matmul(pa[b][:], lhsT=wa[b][:, kc, :], rhs=oa[:, kc, :],
                                                                  start=(kc == 0), stop=(kc == 7)),
                         r=["wa%d" % b, "oa"], w=["pa%d" % b])
                for kc in range(8):
                    p.op("tensor", lambda e, b=b, kc=kc: e.matmul(pb[b][:], lhsT=wbb[b][:, kc, :], rhs=obb[:, kc, :],
                                                                  start=(kc == 0), stop=(kc == 7)),
                         r=["wbb%d" % b, "obb"], w=["pb%d" % b])
                p.op("scalar", lambda e, b=b: e.activation(out=ga[b][:], in_=ga[b][:], func=AF.Sigmoid), r=["ga%d" % b], w=["ga%d" % b])
                p.op("scalar", lambda e, b=b: e.activation(out=gb_[b][:], in_=gb_[b][:], func=AF.Sigmoid), r=["gb%d" % b], w=["gb%d" % b])
                p.op("vector", lambda e, b=b: e.tensor_tensor(out=m1[:], in0=pa[b][:], in1=ga[b][:], op=ALU.mult),
                     r=["pa%d" % b, "ga%d" % b], w=["m1"])
                p.op("vector", lambda e, b=b: e.tensor_tensor(out=gb_[b][:], in0=pb[b][:], in1=gb_[b][:], op=ALU.mult),
                     r=["pb%d" % b, "gb%d" % b], w=["gb%d" % b])
                p.op("vector", lambda e, b=b, cc=cc: e.tensor_tensor(out=mT[:, cc, :], in0=m1[:], in1=gb_[b][:], op=ALU.add),
                     r=["m1", "gb%d" % b], w=["mT"])
            for dg in range(4):
                wb_ = wi % 2
                wi += 1
                dsl = slice(dg * 512, (dg + 1) * 512)
                p.dma("sync", wo[wb_][:], r32(c.w_out[:, dsl]).rearrange("(kc p) n -> p kc n", p=128), w=["wo%d" % wb_])
                for tt in range(4):
                    o = oi % 4
                    oi += 1
                    x_ = oi % 2
                    t0 = tg * 512 + tt * 128
                    p.dma("sync", xt[x_][:], c.x[t0:t0 + 128, dsl], w=["xt%d" % x_])
                    for cc in range(16):
                        p.op("tensor", lambda e, o=o, cc=cc, tt=tt, wb_=wb_: e.matmul(
                            po[o][:], lhsT=mT[:, cc, tt * 128:(tt + 1) * 128], rhs=wo[wb_][:, cc, :],
                            start=(cc == 0), stop=(cc == 15)), r=["mT", "wo%d" % wb_], w=["po%d" % o])
                    p.op("vector", lambda e, o=o, x_=x_: e.tensor_tensor(out=xt[x_][:], in0=po[o][:], in1=xt[x_][:], op=ALU.add),
                         r=["po%d" % o, "xt%d" % x_], w=["xt%d" % x_])
                    p.dma("sync", c.sc_x1[t0:t0 + 128, dsl], xt[x_][:], r=["xt%d" % x_], w=[("sc_x1", t0, dg)])
        p.barrier()


def phase_peer(c, p):
    nc = c.nc
    for half in range(2):
        with ExitStack() as es:
            c2 = Ctx(); c2.nc = nc; c2.es = es
            hT = sb(c2, "h2T", [128, 16, 1024], F32R)
            phase_norm_T(c, p, c.sc_x1, c.norm2_gain, hT, "n2_", half, h_out=c.sc_h2)
            with ExitStack() as es2:
                c3 = Ctx(); c3.nc = nc; c3.es = es2
                wq = [sb(c3, "wq%d" % i, [128, 16, 128], F32R) for i in range(2)]
                skT = sb(c3, "skT", [128, 16, 128], F32R)
                qT = [sb(c3, "qT%d" % i, [128, 1024], F32R) for i in range(2)]
                pq = [ps(c3, "pq%d" % i, [128, 512]) for i in range(4)]
                so_t = [sb(c3, "so_t%d" % i, [128, 512]) for i in range(2)]
                pqi = 0
                p.dma("sync", skT[:], r32(c.skT_d[:, :, :]), w=["skT"])
                for ch in range(16):
                    b = ch % 2
                    p.dma("sync", wq[b][:], r32(c.w_query[:, ch * 128:(ch + 1) * 128]).rearrange("(kc p) n -> p kc n", p=128),
                          w=["wq%d" % b])
                    for tg in range(2):
                        a = pqi % 4
                        pqi += 1
                        for kc in range(16):
                            p.op("tensor", lambda e, a=a, b=b, kc=kc, tg=tg: e.matmul(
                                pq[a][:], lhsT=wq[b][:, kc, :], rhs=hT[:, kc, tg * 512:(tg + 1) * 512],
                                start=(kc == 0), stop=(kc == 15)), r=["wq%d" % b, "hT"], w=["pq%d" % a])
                        p.op("scalar", lambda e, a=a, b=b, tg=tg: e.copy(out=qT[b][:, tg * 512:(tg + 1) * 512], in_=pq[a][:]),
                             r=["pq%d" % a], w=["qT%d" % b])
                    for tq in range(2):
                        a = pqi % 4
                        pqi += 1
                        for tt in range(4):
                            tl = tq * 4 + tt
                            p.op("tensor", lambda e, a=a, b=b, tl=tl, tt=tt, ch=ch: e.matmul(
                                pq[a][:, tt * 128:(tt + 1) * 128], lhsT=qT[b][:, tl * 128:(tl + 1) * 128], rhs=skT[:, ch, :],
                                start=True, stop=True), r=["qT%d" % b, "skT"], w=["pq%d" % a])
                        so = "so%d" % (pqi % 2)
                        sot = so_t[pqi % 2]
                        p.op("vector", lambda e, a=a, sot=sot: e.tensor_copy(out=sot[:], in_=pq[a][:]), r=["pq%d" % a], w=[so])
                        tb = half * 1024 + tq * 512
                        p.dma("sync", c.sc_sc[tb:tb + 512, ch * 128:(ch + 1) * 128].rearrange("(tt p) k -> p tt k", p=128),
                              sot[:].rearrange("p (tt k) -> p tt k", k=128), r=[so], w=[("sc_sc", tb, ch)])
                p.barrier()
    with ExitStack() as es:
        c2 = Ctx(); c2.nc = nc; c2.es = es
        sc = sb(c2, "sc", [128, 16, 128])
        wk = sb(c2, "wk", [128, 128])
        stop_ = sb(c2, "stop", [128, 16, 16])
        itop = sb(c2, "itop", [128, 16, 16], U32)
        itf = sb(c2, "itf", [128, 16, 16])
        cand = sb(c2, "cand", [128, 8, 16, 16])
        cidx = sb(c2, "cidx", [128, 8, 16, 16])
        wk2 = sb(c2, "wk2", [128, 256])
        best = sb(c2, "best", [128, 8, 16])
        pos = sb(c2, "pos", [128, 8, 16], U32)
        posf = sb(c2, "posf", [128, 8, 16])
        iota = sb(c2, "iota", [128, 256])
        junk2 = sb(c2, "junk2", [128, 256])
        eidf = sb(c2, "eidf", [128, 128])
        eid = sb(c2, "eid", [128, 128], U32)
        nmx = sb(c2, "nmx", [128, 8])
        gsum = sb(c2, "gsum", [128, 8])
        gate = sb(c2, "gate", [128, 8, 16])
        dots = sb(c2, "dots", [128, 128])
        gact = sb(c2, "gact", [128, 128])
        h2 = sb(c2, "h2", [128, D])
        NB = 6
        gu = [sb(c2, "gu%d" % i, [128, D], F32R) for i in range(NB)]
        diag = [sb(c2, "diag%d" % i, [128, 128], F32R) for i in range(2)]
        po = [ps(c2, "po%d" % i, [128, 512]) for i in range(4)]
        junk = sb(c2, "junkp", [128, D])
        acc = sb(c2, "acc", [128, D])
        x1 = sb(c2, "x1", [128, D])
        p.dma("sync", iota[:], c.iota_d[:, :], w=["iota"])
        gi = 0
        for t in range(16):
            t0 = t * 128
            p.dma("sync", sc[:], c.sc_sc[t0:t0 + 128, :].rearrange("p (c k) -> p c k", k=128), w=["sc"])
            p.dma("sync", h2[:], c.sc_h2[t0:t0 + 128, :], w=["h2"])
            p.dma("sync", x1[:], c.sc_x1[t0:t0 + 128, :], w=["x1"])
            for ch in range(16):
                p.op("vector", lambda e, ch=ch: e.max(out=stop_[:, ch, 0:8], in_=sc[:, ch, :]), r=["sc"], w=["stop"])
                p.op("vector", lambda e, ch=ch: e.max_index(out=itop[:, ch, 0:8], in_max=stop_[:, ch, 0:8], in_values=sc[:, ch, :]),
                     r=["sc", "stop"], w=["itop"])
                p.op("vector", lambda e, ch=ch: e.match_replace(out=wk[:], in_to_replace=stop_[:, ch, 0:8], in_values=sc[:, ch, :],
                                                                imm_value=-1e30), r=["sc", "stop"], w=["wk"])
                p.op("vector", lambda e, ch=ch: e.max(out=stop_[:, ch, 8:16], in_=wk[:]), r=["wk"], w=["stop"])
                p.op("vector", lambda e, ch=ch: e.max_index(out=itop[:, ch, 8:16], in_max=stop_[:, ch, 8:16], in_values=wk[:]),
                     r=["wk", "stop"], w=["itop"])
            p.op("vector", lambda e: e.tensor_copy(out=itf[:], in_=itop[:]), r=["itop"], w=["itf"])
            s4 = stop_[:].rearrange("p (h two) k -> p h two k", two=2)
            i4 = itf[:].rearrange("p (h two) k -> p h two k", two=2)
            p.op("vector", lambda e, s4=s4: e.tensor_tensor(out=cand[:], in0=s4[:, :, 0, :].unsqueeze(3).to_broadcast([128, 8, 16, 16]),
                                                            in1=s4[:, :, 1, :].unsqueeze(2).to_broadcast([128, 8, 16, 16]), op=ALU.add),
                 r=["stop"], w=["cand"])
            for hh in range(8):
                p.op("vector", lambda e, i4=i4, hh=hh: e.scalar_tensor_tensor(
                    out=cidx[:, hh], in0=i4[:, hh, 0, :].unsqueeze(2).to_broadcast([128, 16, 16]), scalar=128.0,
                    in1=i4[:, hh, 1, :].unsqueeze(1).to_broadcast([128, 16, 16]), op0=ALU.mult, op1=ALU.add),
                    r=["itf"], w=["cidx"])
            for hh in range(8):
                cv_ = cand[:, hh].rearrange("p a b -> p (a b)")
                p.op("vector", lambda e, hh=hh, cv_=cv_: e.max(out=best[:, hh, 0:8], in_=cv_), r=["cand"], w=["best"])
                p.op("vector", lambda e, hh=hh, cv_=cv_: e.max_index(out=pos[:, hh, 0:8], in_max=best[:, hh, 0:8], in_values=cv_),
                     r=["cand", "best"], w=["pos"])
                p.op("vector", lambda e, hh=hh, cv_=cv_: e.match_replace(out=wk2[:], in_to_replace=best[:, hh, 0:8], in_values=cv_,
                                                                         imm_value=-1e30), r=["cand", "best"], w=["wk2"])
                p.op("vector", lambda e, hh=hh: e.max(out=best[:, hh, 8:16], in_=wk2[:]), r=["wk2"], w=["best"])
                p.op("vector", lambda e, hh=hh: e.max_index(out=pos[:, hh, 8:16], in_max=best[:, hh, 8:16], in_values=wk2[:]),
                     r=["wk2", "best"], w=["pos"])
            p.op("vector", lambda e: e.tensor_copy(out=posf[:], in_=pos[:]), r=["pos"], w=["posf"])
            for hh in range(8):
                ci_ = cidx[:, hh].rearrange("p a b -> p (a b)")
                for m in range(16):
                    p.op("vector", lambda e, hh=hh, m=m, ci_=ci_: e.scalar_tensor_tensor(
                        out=junk2[:], in0=iota[:], scalar=posf[:, hh, m:m + 1], in1=ci_, op0=ALU.is_equal, op1=ALU.mult,
                        accum_out=eidf[:, hh * 16 + m:hh * 16 + m + 1]), r=["iota", "posf", "cidx"], w=["junk2", "eidf"])
            p.op("vector", lambda e: e.tensor_copy(out=eid[:], in_=eidf[:]), r=["eidf"], w=["eid"])
            p.op("vector", lambda e: e.tensor_scalar(out=nmx[:], in0=best[:, :, 0], scalar1=-1.0, scalar2=None, op0=ALU.mult),
                 r=["best"], w=["nmx"])
            for hh in range(8):
                p.op("scalar", lambda e, hh=hh: e.activation(out=gate[:, hh, :], in_=best[:, hh, :], func=AF.Exp, bias=nmx[:, hh:hh + 1],
                                                             accum_out=gsum[:, hh:hh + 1]), r=["best", "nmx"], w=["gate", "gsum"])
            p.op("vector", lambda e: e.reciprocal(out=gsum[:], in_=gsum[:]), r=["gsum"], w=["gsum"])
            p.op("vector", lambda e: e.tensor_tensor(out=gate[:], in0=gate[:], in1=gsum[:].unsqueeze(2).to_broadcast([128, 8, 16]), op=ALU.mult),
                 r=["gate", "gsum"], w=["gate"])
            for s_ in range(128):
                b = gi % NB
                gi += 1
                p.dma_fn("gpsimd", lambda e, b=b, s_=s_: e.indirect_dma_start(
                    out=gu[b][:], out_offset=None, in_=r32(c.peer_u[:, :]),
                    in_offset=bass.IndirectOffsetOnAxis(ap=eid[:, s_:s_ + 1], axis=0)), r=["eid"], w=["gu%d" % b])
                p.op("vector", lambda e, b=b, s_=s_: e.scalar_tensor_tensor(out=junk[:], in0=gu[b][:].bitcast(F32), scalar=1.0, in1=h2[:],
                                                                            op0=ALU.mult, op1=ALU.mult, accum_out=dots[:, s_:s_ + 1]),
                     r=["gu%d" % b, "h2"], w=["junkp", "dots"])
            p.op("scalar", lambda e: e.activation(out=gact[:], in_=dots[:], func=AF.Gelu), r=["dots"], w=["gact"])
            p.op("vector", lambda e: e.tensor_tensor(out=gact[:], in0=gact[:], in1=gate[:].rearrange("p h k -> p (h k)"), op=ALU.mult),
                 r=["gact", "gate"], w=["gact"])
            for s_ in range(128):
                b = gi % NB
                gi += 1
                db = s_ % 2
                p.dma_fn("gpsimd", lambda e, b=b, s_=s_: e.indirect_dma_start(
                    out=gu[b][:], out_offset=None, in_=r32(c.peer_v[:, :]),
                    in_offset=bass.IndirectOffsetOnAxis(ap=eid[:, s_:s_ + 1], axis=0)), r=["eid"], w=["gu%d" % b])
                p.op("vector", lambda e, db=db, s_=s_: e.tensor_scalar(out=diag[db][:], in0=c.ident[:], scalar1=gact[:, s_:s_ + 1],
                                                                       scalar2=None, op0=ALU.mult),
                     r=["gact"], w=["diag%d" % db])
                for dg in range(4):
                    p.op("tensor", lambda e, b=b, db=db, dg=dg, s_=s_: e.matmul(
                        po[dg][:], lhsT=diag[db][:], rhs=gu[b][:, dg * 512:(dg + 1) * 512], start=(s_ == 0), stop=(s_ == 127)),
                        r=["diag%d" % db, "gu%d" % b], w=["po%d" % dg])
            for dg in range(4):
                dsl = slice(dg * 512, (dg + 1) * 512)
                p.op("vector", lambda e, dg=dg, dsl=dsl: e.tensor_tensor(out=acc[:, dsl], in0=po[dg][:], in1=x1[:, dsl], op=ALU.add),
                     r=["po%d" % dg, "x1"], w=["acc"])
            p.dma("sync", c.y[t0:t0 + 128, :], acc[:], r=["acc"], w=[("y", t)])
        p.barrier()


ALL_PHASES = ("inproj", "moba", "gdn", "merge", "peer")


def build_nc(debug=False, phases=ALL_PHASES):
    nc = bass.Bass("TRN2", target_bir_lowering=False)
    nc.dge_precook = False
    c = Ctx()
    c.nc = nc
    kind_s = "ExternalOutput" if debug else "Internal"

    def din(name, shape, dt=F32):
        return nc.dram_tensor(name, list(shape), dt, kind="ExternalInput").ap()

    def dsc(name, shape):
        return nc.dram_tensor(name, list(shape), F32, kind=kind_s).ap()

    c.x = din("x", [S, D])
    c.norm1_gain = din("norm1_gain", [1, D])
    c.w_in = din("w_in", [D, IN_TOTAL])
    c.ident_d = din("ident", [128, 128])
    c.ones_d = din("ones", [128, 128])
    c.rel_bias = din("rel_bias", [32, 8])
    c.q_norm_gain = din("q_norm_gain", [1, 128])
    c.k_norm_gain = din("k_norm_gain", [1, 128])
    c.d01_d = din("d01", [8, 2, 128, 128])
    c.cm_d = din("cm", [128, 16, 8])
    c.notown_d = din("notown", [128, 16, 8])
    c.esel_d = din("esel", [8, 8, 128])
    c.gdnc_d = din("gdnc", [7, 128, 128])
    c.conv_wT = din("conv_wT", [3072, 4])
    c.a_log = din("a_log", [1, 8])
    c.dt_bias = din("dt_bias", [1, 8])
    c.gdn_norm_gain = din("gdn_norm_gain", [1, 128])
    c.w_up_a = din("w_up_a", [1024, D])
    c.w_up_b = din("w_up_b", [1024, D])
    c.w_out = din("w_out", [D, D])
    if "peer" in phases:
        c.norm2_gain = din("norm2_gain", [1, D])
        c.w_query = din("w_query", [D, D])
        c.skT_d = din("skT", [128, 16, 128])
        c.peer_u = din("peer_u", [16384, D])
        c.peer_v = din("peer_v", [16384, D])
        c.iota_d = din("iota", [128, 256])
        c.sc_h2 = dsc("sc_h2", [S, D])
        c.sc_sc = dsc("sc_sc", [S, D])
    c.sc_fm = dsc("sc_fm", [N_FM, S])
    c.sc_tm = dsc("sc_tm", [S, N_TM])
    c.sc_oa = dsc("sc_oa", [1024, S])
    c.sc_ob = dsc("sc_ob", [1024, S])
    if "merge" in phases or "peer" not in phases:
        c.sc_x1 = dsc("sc_x1", [S, D])
    else:
        c.sc_x1 = din("sc_x1", [S, D])
    c.y = nc.dram_tensor("y", [S, D], F32, kind="ExternalOutput").ap()

    with ExitStack() as es:
        c.es = es
        p = Prog(nc, es)
        block = es.enter_context(nc.Block())
        c.ident = sb(c, "ident_s", [128, 128])
        c.ident_r = sb(c, "ident_r", [128, 128], F32R)
        c.ones_r = sb(c, "ones_r", [128, 128], F32R)
        c.eps_t = sb(c, "eps_t", [128, 1])
        c.one_t = sb(c, "one_t", [128, 1])
        p.dma("sync", c.ident[:], c.ident_d[:, :], w=["ident"])
        p.dma("sync", c.ident_r[:], r32(c.ident_d[:, :]), w=["ident_r"])
        p.dma("sync", c.ones_r[:], r32(c.ones_d[:, :]), w=["ones_r"])
        p.op("vector", lambda e: e.memset(c.eps_t[:], EPS), w=["eps"])
        p.op("vector", lambda e: e.memset(c.one_t[:], 1.0), w=["one"])
        p.barrier()
        if "inproj" in phases:
            phase_inproj(c, p)
        if "moba" in phases:
            phase_moba(c, p)
        if "gdn" in phases:
            phase_gdn(c, p)
        if "merge" in phases:
            phase_merge(c, p)
        if "peer" in phases:
            phase_peer(c, p)
        p.finish(block)
    return nc


def make_in_maps(inputs, phases=ALL_PHASES):
    f = lambda a: np.ascontiguousarray(np.asarray(a, dtype=np.float32))
    rel_bias = f(inputs["rel_bias"])
    d01, cm, notown, esel = moba_consts(rel_bias)
    shared = {
        "norm1_gain": f(inputs["norm1_gain"]), "w_in": f(inputs["w_in"][0]),
        "ident": np.eye(128, dtype=np.float32), "ones": np.ones((128, 128), np.float32),
        "rel_bias": rel_bias, "q_norm_gain": f(inputs["q_norm_gain"]), "k_norm_gain": f(inputs["k_norm_gain"]),
        "d01": d01, "cm": cm, "notown": notown, "esel": esel, "gdnc": gdn_consts(),
        "conv_wT": f(np.asarray(inputs["conv_w"][0]).T), "a_log": f(inputs["a_log"]), "dt_bias": f(inputs["dt_bias"]),
        "gdn_norm_gain": f(inputs["gdn_norm_gain"]), "w_up_a": f(inputs["w_up_a"][0]), "w_up_b": f(inputs["w_up_b"][0]),
        "w_out": f(inputs["w_out"][0]),
    }
    if "peer" in phases:
        sk = np.asarray(inputs["peer_sub_keys"][0], dtype=np.float32).reshape(16, 128, 128)
        shared.update({
            "norm2_gain": f(inputs["norm2_gain"]), "w_query": f(inputs["peer_w_query"][0]),
            "skT": f(sk.transpose(2, 0, 1)), "peer_u": f(inputs["peer_u"][0]), "peer_v": f(inputs["peer_v"][0]),
            "iota": np.broadcast_to(np.arange(256, dtype=np.float32), (128, 256)).copy(),
        })
    maps = []
    for b in range(8):
        m = dict(shared)
        m["x"] = f(inputs["x"][b])
        maps.append(m)
    return maps


_NC_CACHE = {}


def kernel(**inputs):
    if "nc" not in _NC_CACHE:
        _NC_CACHE["nc"] = build_nc()
    nc = _NC_CACHE["nc"]
    maps = make_in_maps(inputs)
    res = run_bass_kernel_spmd(nc, maps, core_ids=list(range(8)))
    return np.stack([np.asarray(r["y"], dtype=np.float32) for r in res.results], axis=0)
```

```python
import math
from contextlib import ExitStack

import numpy as np
import concourse.bass as bass
import concourse.mybir as mybir
from concourse.bass_utils import run_bass_kernel_spmd

F32 = mybir.dt.float32
F32R = mybir.dt.float32r
BF16 = mybir.dt.bfloat16
U32 = mybir.dt.uint32
I32 = mybir.dt.int32
AF = mybir.ActivationFunctionType
ALU = mybir.AluOpType
AX = mybir.AxisListType

D = 2048
S = 2048
NT = S // 128
IN_TOTAL = 11280
EPS = 1e-6
NEG = -30000.0

COMPUTE = ("tensor", "vector", "scalar", "gpsimd")
ALLENG = ("tensor", "vector", "scalar", "gpsimd", "sync")


import re as _re
_PSUM_KEY = _re.compile(r"^(n\d_tp|gp|aux|scp|op|dp|pp|pa|pb|po|pq)\d+$")


class Op:
    __slots__ = ("eng", "fn", "deps", "needed", "is_dma", "slot", "use", "tok", "idx")


class Prog:
    def __init__(self, nc, es, ndma=8):
        self.nc = nc
        self.ops = {e: [] for e in ALLENG}
        self.last_w = {}
        self.rd_comp = {}
        self.rd_dma = {}
        self.sem = {e: es.enter_context(nc.semaphore("s_" + e)) for e in COMPUTE}
        self.dsem = {}
        self.dlast = {}
        self.dnext = {}
        self.duse = {}
        for q in ("sync", "gpsimd", "scalar"):
            self.dsem[q] = [es.enter_context(nc.semaphore("d_%s%d" % (q, i))) for i in range(ndma)]
            self.dlast[q] = [None] * ndma
            self.duse[q] = [0] * ndma
            self.dnext[q] = 0
        self.pending_barrier = {e: None for e in ALLENG}
        self.all_dma = []

    def _add(self, eng, fn, r, w, is_dma):
        o = Op()
        o.eng = eng
        o.fn = fn
        o.needed = False
        o.is_dma = is_dma
        o.slot = None
        o.use = 0
        deps = set()
        for k in r:
            lw = self.last_w.get(k)
            if lw is not None:
                deps.add(lw)
            if isinstance(k, str) and _PSUM_KEY.match(k):
                for en, ro in self.rd_comp.get(k, {}).items():
                    if en != eng:
                        deps.add(ro)
        for k in w:
            lw = self.last_w.get(k)
            if lw is not None:
                deps.add(lw)
            for ro in self.rd_comp.get(k, {}).values():
                deps.add(ro)
            for ro in self.rd_dma.get(k, ()):
                deps.add(ro)
        if is_dma:
            q = eng
            sl = self.dnext[q]
            self.dnext[q] = (sl + 1) % len(self.dsem[q])
            prev = self.dlast[q][sl]
            if prev is not None:
                deps.add(prev)
            self.duse[q][sl] += 1
            o.slot = sl
            o.use = self.duse[q][sl]
            self.dlast[q][sl] = o
            self.all_dma.append(o)
        pb = self.pending_barrier[eng]
        if pb is not None:
            deps |= pb
            self.pending_barrier[eng] = None
        if eng == "tensor":
            deps = {d for d in deps if not (d.eng == "tensor" and not d.is_dma)}
        o.deps = deps
        for d in deps:
            d.needed = True
        for k in w:
            self.last_w[k] = o
            self.rd_comp[k] = {}
            self.rd_dma[k] = []
        for k in r:
            if is_dma:
                self.rd_dma.setdefault(k, []).append(o)
            else:
                self.rd_comp.setdefault(k, {})[eng] = o
        o.idx = len(self.ops[eng])
        self.ops[eng].append(o)
        return o

    def op(self, eng, fn, r=(), w=()):
        return self._add(eng, fn, tuple(r), tuple(w), False)

    def dma(self, q, out, in_, r=(), w=(), **kw):
        return self._add(q, lambda e: e.dma_start(out=out, in_=in_, **kw), tuple(r), tuple(w), True)

    def dma_fn(self, q, fn, r=(), w=()):
        return self._add(q, fn, tuple(r), tuple(w), True)

    def barrier(self):
        deps = set()
        for e in ALLENG:
            if self.ops[e]:
                last = [o for o in self.ops[e] if not o.is_dma]
                if last:
                    deps.add(last[-1])
        for o in self.all_dma:
            deps.add(o)
        self.all_dma = []
        for e in ALLENG:
            pb = self.pending_barrier[e]
            self.pending_barrier[e] = set(deps) | (pb or set())
        self.last_w = {}
        self.rd_comp = {}
        self.rd_dma = {}

    def finish(self, block):
        self.barrier()
        nc = self.nc
        for e in ALLENG:
            self._add(e, None, (), (), False)
        for e in COMPUTE:
            c = 0
            for o in self.ops[e]:
                if o.is_dma:
                    o.tok = (self.dsem[e][o.slot], 16 * o.use)
                elif o.needed:
                    c += 1
                    o.tok = (self.sem[e], c)
                else:
                    o.tok = None
        for o in self.ops["sync"]:
            if o.is_dma:
                o.tok = (self.dsem["sync"][o.slot], 16 * o.use)
            else:
                o.tok = None

        def emit(engname):
            def body(eng):
                waited = {}
                for o in self.ops[engname]:
                    for d in o.deps:
                        sem, val = d.tok
                        key = id(sem)
                        if waited.get(key, 0) < val:
                            eng.wait_ge(sem, val)
                            waited[key] = val
                    if o.fn is None:
                        continue
                    ins = o.fn(eng)
                    if o.is_dma:
                        ins.then_inc(o.tok[0], 16)
                    elif o.needed:
                        ins.then_inc(o.tok[0], 1)
            return body

        block.tensor(emit("tensor"))
        block.vector(emit("vector"))
        block.scalar(emit("scalar"))
        block.gpsimd(emit("gpsimd"))
        block.sync(emit("sync"))


def r32(ap):
    return ap.bitcast(F32R)


class Ctx:
    pass


_uid = [0]


def _un(name):
    _uid[0] += 1
    return "%s_%d" % (name, _uid[0])


def sb(c, name, shape, dt=F32):
    return c.es.enter_context(c.nc.sbuf_tensor(_un(name), list(shape), dt))


def ps(c, name, shape, dt=F32):
    return c.es.enter_context(c.nc.psum_tensor(_un(name), list(shape), dt))


FM_SRC = [(0, 1024), (1024, 1024), (3072, 3072), (7184, 2048), (9232, 2048)]
TM_SRC = [(2048, 1024), (6144, 1024), (7168, 16)]
N_FM = 9216
N_TM = 2064


def phase_norm_T(c, p, x_ap, gain_ap, hT, pfx, half, h_out=None):
    nc = c.nc
    with ExitStack() as es:
        c2 = Ctx(); c2.nc = nc; c2.es = es
        gbc = sb(c2, pfx + "gbc", [128, D])
        xt = [sb(c2, pfx + "xt%d" % i, [128, D]) for i in range(2)]
        ht = [sb(c2, pfx + "ht%d" % i, [128, D]) for i in range(2)]
        sq = sb(c2, pfx + "sq", [128, D])
        st = [sb(c2, pfx + "st%d" % i, [128, 4]) for i in range(2)]
        tp = [ps(c2, pfx + "tp%d" % i, [128, 512]) for i in range(4)]
        p.dma("sync", gbc[:], gain_ap.partition_broadcast(128), w=[pfx + "gbc"])
        for ti in range(8):
            t = half * 8 + ti
            b = ti % 2
            p.dma("sync", xt[b][:], x_ap[t * 128:(t + 1) * 128, :], w=[pfx + "xt%d" % b])
            p.op("scalar", lambda e, b=b: e.activation(out=sq[:], in_=xt[b][:], func=AF.Square,
                                                       accum_out=st[b][:, 0:1]),
                 r=[pfx + "xt%d" % b], w=[pfx + "sq", pfx + "st%d" % b])
            p.op("scalar", lambda e, b=b: e.activation(out=st[b][:, 1:2], in_=st[b][:, 0:1], func=AF.Sqrt,
                                                       scale=1.0 / D, bias=c.eps_t[:, 0:1]),
                 r=[pfx + "st%d" % b], w=[pfx + "st%d" % b])
            p.op("vector", lambda e, b=b: e.reciprocal(out=st[b][:, 2:3], in_=st[b][:, 1:2]),
                 r=[pfx + "st%d" % b], w=[pfx + "st%d" % b])
            p.op("vector", lambda e, b=b: e.scalar_tensor_tensor(out=ht[b][:], in0=xt[b][:], scalar=st[b][:, 2:3],
                                                                 in1=gbc[:], op0=ALU.mult, op1=ALU.mult),
                 r=[pfx + "xt%d" % b, pfx + "st%d" % b, pfx + "gbc"], w=[pfx + "ht%d" % b])
            if h_out is not None:
                p.dma("sync", h_out[t * 128:(t + 1) * 128, :], ht[b][:], r=[pfx + "ht%d" % b], w=[(pfx + "hout", t)])
            for g4 in range(4):
                pb = tp[g4]
                for j in range(4):
                    kc = g4 * 4 + j
                    p.op("tensor", lambda e, b=b, kc=kc, j=j, pb=pb: e.transpose(
                        out=pb[:, j * 128:(j + 1) * 128], in_=ht[b][:, kc * 128:(kc + 1) * 128], identity=c.ident[:]),
                        r=[pfx + "ht%d" % b], w=[pfx + "tp%d" % g4])
                eng = "scalar" if g4 % 2 == 0 else "vector"
                dst = hT[:, g4 * 4:(g4 + 1) * 4, ti * 128:(ti + 1) * 128]
                src = pb[:].rearrange("p (j t) -> p j t", j=4)
                if eng == "scalar":
                    p.op("scalar", lambda e, dst=dst, src=src: e.copy(out=dst, in_=src),
                         r=[pfx + "tp%d" % g4], w=["hT"])
                else:
                    p.op("vector", lambda e, dst=dst, src=src: e.tensor_copy(out=dst, in_=src),
                         r=[pfx + "tp%d" % g4], w=["hT"])
        p.barrier()


def phase_inproj(c, p):
    nc = c.nc
    for half in range(2):
        with ExitStack() as es:
            c2 = Ctx(); c2.nc = nc; c2.es = es
            hT = sb(c2, "hT", [128, 16, 1024], F32R)
            phase_norm_T(c, p, c.x, c.norm1_gain, hT, "n1_", half)
            with ExitStack() as es2:
                c3 = Ctx(); c3.nc = nc; c3.es = es2
                wb = [sb(c3, "wb%d" % i, [128, 16, 256], F32R) for i in range(2)]
                ob = [sb(c3, "ob%d" % i, [128, 1024]) for i in range(2)]
                pp = [ps(c3, "pp%d" % i, [128, 512]) for i in range(4)]
                gi = 0
                oi = 0
                pi = 0
                t0 = half * 1024
                row = 0
                for (c0, n) in FM_SRC:
                    for g in range(n // 256):
                        col = c0 + g * 256
                        b = gi % 2
                        gi += 1
                        p.dma("sync", wb[b][:], r32(c.w_in[:, col:col + 256]).rearrange("(kc p) n -> p kc n", p=128),
                              w=["wb%d" % b])
                        for cc in range(2):
                            o = oi % 2
                            oi += 1
                            for tg in range(2):
                                pb = pi % 4
                                pi += 1
                                for kc in range(16):
                                    p.op("tensor", lambda e, b=b, cc=cc, tg=tg, kc=kc, pb=pb: e.matmul(
                                        pp[pb][:], lhsT=wb[b][:, kc, cc * 128:(cc + 1) * 128],
                                        rhs=hT[:, kc, tg * 512:(tg + 1) * 512],
                                        start=(kc == 0), stop=(kc == 15)),
                                        r=["wb%d" % b, "hT"], w=["pp%d" % pb])
                                if tg == 0:
                                    p.op("scalar", lambda e, o=o, pb=pb: e.copy(out=ob[o][:, 0:512], in_=pp[pb][:]),
                                         r=["pp%d" % pb], w=["ob%d" % o])
                                else:
                                    p.op("vector", lambda e, o=o, pb=pb: e.tensor_copy(out=ob[o][:, 512:1024], in_=pp[pb][:]),
                                         r=["pp%d" % pb], w=["ob%d" % o])
                            rr = row + g * 256 + cc * 128
                            p.dma("sync", c.sc_fm[rr:rr + 128, t0:t0 + 1024], ob[o][:],
                                  r=["ob%d" % o], w=[("sc_fm", rr // 128)])
                    row += n
                colo = 0
                for (c0, n) in TM_SRC:
                    w = min(n, 256)
                    for g in range(max(1, n // 256)):
                        col = c0 + g * 256
                        b = gi % 2
                        gi += 1
                        p.dma("sync", wb[b][:, :, 0:w], r32(c.w_in[:, col:col + w]).rearrange("(kc p) n -> p kc n", p=128),
                              w=["wb%d" % b])
                        for tq in range(2):
                            o = oi % 2
                            oi += 1
                            for tt in range(4):
                                tl = tq * 4 + tt
                                pb = pi % 4
                                pi += 1
                                for kc in range(16):
                                    p.op("tensor", lambda e, b=b, tl=tl, kc=kc, pb=pb, w=w: e.matmul(
                                        pp[pb][:, 0:w], lhsT=hT[:, kc, tl * 128:(tl + 1) * 128],
                                        rhs=wb[b][:, kc, 0:w],
                                        start=(kc == 0), stop=(kc == 15)),
                                        r=["wb%d" % b, "hT"], w=["pp%d" % pb])
                                if tt % 2 == 0:
                                    p.op("scalar", lambda e, o=o, pb=pb, tt=tt, w=w: e.copy(
                                        out=ob[o][:, tt * 256:tt * 256 + w], in_=pp[pb][:, 0:w]),
                                        r=["pp%d" % pb], w=["ob%d" % o])
                                else:
                                    p.op("vector", lambda e, o=o, pb=pb, tt=tt, w=w: e.tensor_copy(
                                        out=ob[o][:, tt * 256:tt * 256 + w], in_=pp[pb][:, 0:w]),
                                        r=["pp%d" % pb], w=["ob%d" % o])
                            tb = t0 + tq * 512
                            cw = colo + g * 256
                            p.dma("sync",
                                  c.sc_tm[tb:tb + 512, cw:cw + w].rearrange("(tt p) n -> p tt n", p=128),
                                  ob[o][:].rearrange("p (tt n) -> p tt n", n=256)[:, :, 0:w],
                                  r=["ob%d" % o], w=[("sc_tm", tb // 128, cw)])
                    colo += n
                p.barrier()


def t5_bucket_np(n):
    n = np.maximum(n, 0)
    nf = np.maximum(n, 1).astype(np.float32)
    large = 16 + (np.log(nf / np.float32(16)) / np.float32(math.log(8.0)) * np.float32(16)).astype(np.int32)
    large = np.minimum(large, 31)
    return np.where(n < 16, n, large)


def moba_consts(rel_bias):
    k = np.arange(128)[:, None]
    q = np.arange(128)[None, :]
    out = np.zeros((8, 2, 128, 128), np.float32)
    b0 = t5_bucket_np(q - k)
    b1 = t5_bucket_np(q - k + 128)
    for h in range(8):
        out[h, 0] = np.where(q >= k, rel_bias[b0, h], np.float32(NEG))
        out[h, 1] = rel_bias[b1, h]
    cm = np.zeros((128, 16, 8), np.float32)
    notown = np.ones((128, 16, 8), np.float32)
    for t in range(16):
        for n in range(8):
            if n >= t // 2:
                cm[:, t, n] = -1e30
            if n == t // 2:
                notown[:, t, n] = 0.0
    esel = np.zeros((8, 8, 128), np.float32)
    for n in range(8):
        esel[n, n, :] = 1.0
    return out, cm, notown, esel


def phase_moba(c, p):
    nc = c.nc
    with ExitStack() as es:
        c2 = Ctx(); c2.nc = nc; c2.es = es
        cm = sb(c2, "cm", [128, 16, 8])
        notown = sb(c2, "notown", [128, 16, 8])
        esel = sb(c2, "esel", [8, 8, 128], F32R)
        cb = sb(c2, "cb", [128, 8])
        gq = sb(c2, "gq", [128, 2])
        gk = sb(c2, "gk", [128, 1])
        d01 = sb(c2, "d01", [128, 2, 128], F32R)
        d01r = sb(c2, "d01r", [128, 2, 128])
        qr = sb(c2, "qr", [128, S])
        kr = sb(c2, "kr", [128, S])
        sq = sb(c2, "sq", [128, S], F32R)
        qn = sb(c2, "qn", [128, S], F32R)
        kn = sb(c2, "kn", [128, S], F32R)
        vt = sb(c2, "vt", [128, 16, 128], F32R)
        rsb = [sb(c2, "rs%d" % i, [128, 512]) for i in range(2)]
        km = sb(c2, "km", [128, 8], F32R)
        kmf = sb(c2, "kmf", [128, 8])
        gm = sb(c2, "gm", [128, 16, 8])
        cmp_ = sb(c2, "cmp", [128, 16, 8, 8])
        rank = sb(c2, "rank", [128, 16, 8])
        nmk = sb(c2, "nmk", [128, 16, 8])
        negT = sb(c2, "negT", [8, S], F32R)
        pT = [sb(c2, "pT%d" % i, [128, 512], F32R) for i in range(2)]
        rden = sb(c2, "rden", [128, 512])
        oo = [sb(c2, "oo%d" % i, [128, 512]) for i in range(2)]
        aux = [ps(c2, "aux%d" % i, [128, 512]) for i in range(2)]
        scp = [ps(c2, "scp%d" % i, [128, 512]) for i in range(2)]
        op_ = [ps(c2, "op%d" % i, [128, 512]) for i in range(2)]
        dp = [ps(c2, "dp%d" % i, [128, 512]) for i in range(2)]

        p.dma("sync", cm[:], c.cm_d[:, :, :], w=["cm"])
        p.dma("sync", notown[:], c.notown_d[:, :, :], w=["notown"])
        p.dma("sync", esel[:], r32(c.esel_d[:, :, :]), w=["esel"])
        p.dma("sync", cb[:], c.rel_bias[31:32, :].partition_broadcast(128), w=["cb"])
        p.dma("sync", gq[:, 0:1], c.q_norm_gain.rearrange("o d -> d o"), w=["gq"])
        p.dma("sync", gk[:, 0:1], c.k_norm_gain.rearrange("o d -> d o"), w=["gk"])
        p.op("vector", lambda e: e.tensor_scalar(out=gq[:, 1:2], in0=gq[:, 0:1], scalar1=128.0 ** -0.5, scalar2=None,
                                                 op0=ALU.mult), r=["gq"], w=["gq"])
        auxi = 0
        sci = 0
        gi = 0
        for h in range(8):
            p.dma("sync", qr[:], c.sc_fm[h * 128:(h + 1) * 128, :], w=["qr"])
            p.dma("sync", kr[:], c.sc_fm[1024 + h * 128:1024 + (h + 1) * 128, :], w=["kr"])
            p.dma("sync", vt[:], r32(c.sc_tm[:, h * 128:(h + 1) * 128]).rearrange("(t p) d -> p t d", p=128), w=["vt"])
            p.dma("sync", d01r[:], c.d01_d[h].rearrange("a k q -> k a q"), w=["d01r"])
            p.op("vector", lambda e, h=h: e.tensor_scalar(out=d01[:], in0=d01r[:], scalar1=cb[:, h:h + 1], scalar2=None,
                                                          op0=ALU.subtract), r=["d01r", "cb"], w=["d01"])
            for (raw, dst, gcol, rk, wk) in ((qr, qn, gq[:, 1:2], "qr", "qn"), (kr, kn, gk[:, 0:1], "kr", "kn")):
                p.op("scalar", lambda e, raw=raw: e.activation(out=sq[:], in_=raw[:], func=AF.Square), r=[rk], w=["sq"])
                for tg in range(4):
                    a = auxi % 2
                    auxi += 1
                    sl = slice(tg * 512, (tg + 1) * 512)
                    p.op("tensor", lambda e, a=a, sl=sl: e.matmul(aux[a][:], lhsT=c.ones_r[:], rhs=sq[:, sl], start=True, stop=True),
                         r=["sq"], w=["aux%d" % a])
                    rs = rsb[a]
                    p.op("scalar", lambda e, a=a, rs=rs: e.activation(out=rs[:], in_=aux[a][:], func=AF.Sqrt, scale=1.0 / 128,
                                                                      bias=c.eps_t[:, 0:1]), r=["aux%d" % a], w=["rs%d" % a])
                    p.op("vector", lambda e, rs=rs: e.reciprocal(out=rs[:], in_=rs[:]), r=["rs%d" % a], w=["rs%d" % a])
                    p.op("vector", lambda e, raw=raw, dst=dst, gcol=gcol, sl=sl, rs=rs: e.scalar_tensor_tensor(
                        out=dst[:, sl], in0=raw[:, sl], scalar=gcol, in1=rs[:], op0=ALU.mult, op1=ALU.mult),
                        r=[rk, "rs%d" % a, "gq", "gk"], w=[wk])
            p.op("vector", lambda e: e.tensor_reduce(out=kmf[:], in_=kn[:].bitcast(F32).rearrange("p (n j) -> p n j", j=256),
                                                     axis=AX.X, op=ALU.add), r=["kn"], w=["kmf"])
            p.op("vector", lambda e: e.tensor_copy(out=km[:], in_=kmf[:]), r=["kmf"], w=["km"])
            a = auxi % 2
            auxi += 1
            for t in range(16):
                p.op("tensor", lambda e, a=a, t=t: e.matmul(aux[a][:, t * 8:(t + 1) * 8], lhsT=qn[:, t * 128:(t + 1) * 128],
                                                            rhs=km[:], start=True, stop=True),
                     r=["qn", "km"], w=["aux%d" % a])
            p.op("vector", lambda e, a=a: e.tensor_tensor(out=gm[:], in0=aux[a][:, 0:128].rearrange("p (t n) -> p t n", n=8),
                                                          in1=cm[:], op=ALU.add), r=["aux%d" % a, "cm"], w=["gm"])
            p.op("vector", lambda e: e.tensor_tensor(out=cmp_[:], in0=gm[:].unsqueeze(2).to_broadcast([128, 16, 8, 8]),
                                                     in1=gm[:].unsqueeze(3).to_broadcast([128, 16, 8, 8]), op=ALU.is_gt),
                 r=["gm"], w=["cmp"])
            p.op("vector", lambda e: e.tensor_reduce(out=rank[:], in_=cmp_[:], axis=AX.X, op=ALU.add), r=["cmp"], w=["rank"])
            p.op("vector", lambda e: e.tensor_scalar(out=rank[:], in0=rank[:], scalar1=3.0, scalar2=NEG, op0=ALU.is_ge,
                                                     op1=ALU.mult), r=["rank"], w=["rank"])
            p.op("vector", lambda e: e.tensor_tensor(out=nmk[:], in0=rank[:], in1=notown[:], op=ALU.mult),
                 r=["rank", "notown"], w=["nmk"])
            for tg in range(4):
                a = auxi % 2
                auxi += 1
                for j in range(4):
                    t = tg * 4 + j
                    p.op("tensor", lambda e, a=a, t=t, j=j: e.transpose(out=aux[a][0:8, j * 128:(j + 1) * 128], in_=nmk[:, t, :],
                                                                        identity=c.ident[:]),
                         r=["nmk"], w=["aux%d" % a])
                p.op("scalar", lambda e, a=a, tg=tg: e.copy(out=negT[:, tg * 512:(tg + 1) * 512], in_=aux[a][0:8, :]),
                     r=["aux%d" % a], w=["negT"])
            steps = []
            for g in range(4):
                gb = gi % 2
                gi += 1
                nk = 4 * g + 4
                for kt in range(nk):
                    s_ = sci % 2
                    sci += 1
                    jj0 = max(0, kt - 4 * g)
                    c0 = jj0 * 128
                    extra = []
                    if kt >= 4 * g:
                        extra.append((jj0, 0))
                        if jj0 + 1 < 4:
                            extra.append((jj0 + 1, 1))
                    elif kt == 4 * g - 1:
                        extra.append((0, 1))
                    steps.append(dict(g=g, gb=gb, nk=nk, kt=kt, s_=s_, qs=slice(g * 512 + c0, (g + 1) * 512), cs=slice(c0, 512),
                                      n=kt // 2, extra=extra))

            def emit_score(st, h=h):
                s_, kt, qs, cs, n, extra = st["s_"], st["kt"], st["qs"], st["cs"], st["n"], st["extra"]
                p.op("tensor", lambda e: e.matmul(scp[s_][:, cs], lhsT=kn[:, kt * 128:(kt + 1) * 128], rhs=qn[:, qs],
                                                  start=True, stop=False), r=["kn", "qn"], w=["scp%d" % s_])
                p.op("tensor", lambda e: e.matmul(scp[s_][:, cs], lhsT=esel[:, n, :], rhs=negT[:, qs], start=False, stop=(not extra)),
                     r=["esel", "negT"], w=["scp%d" % s_])
                for ei, (jj, which) in enumerate(extra):
                    p.op("tensor", lambda e, jj=jj, which=which, last=(ei == len(extra) - 1): e.matmul(
                        scp[s_][:, jj * 128:(jj + 1) * 128], lhsT=c.ident_r[:], rhs=d01[:, which, :], start=False, stop=last),
                        r=["d01"], w=["scp%d" % s_])
                p.op("scalar", lambda e: e.activation(out=pT[s_][:, cs], in_=scp[s_][:, cs], func=AF.Exp, bias=cb[:, h:h + 1]),
                     r=["scp%d" % s_, "cb"], w=["pT%d" % s_])

            def emit_pv(st, h=h):
                s_, kt, cs, gb, nk, g = st["s_"], st["kt"], st["cs"], st["gb"], st["nk"], st["g"]
                p.op("tensor", lambda e: e.matmul(op_[gb][:, cs], lhsT=vt[:, kt, :], rhs=pT[s_][:, cs], start=(kt == 0), stop=(kt == nk - 1)),
                     r=["vt", "pT%d" % s_], w=["op%d" % gb])
                p.op("tensor", lambda e: e.matmul(dp[gb][:, cs], lhsT=c.ones_r[:], rhs=pT[s_][:, cs], start=(kt == 0), stop=(kt == nk - 1)),
                     r=["pT%d" % s_], w=["dp%d" % gb])
                if kt == nk - 1:
                    p.op("vector", lambda e: e.reciprocal(out=rden[:], in_=dp[gb][:]), r=["dp%d" % gb], w=["rden"])
                    p.op("vector", lambda e: e.tensor_tensor(out=oo[gb][:], in0=op_[gb][:], in1=rden[:], op=ALU.mult),
                         r=["op%d" % gb, "rden"], w=["oo%d" % gb])
                    p.dma("sync", c.sc_oa[h * 128:(h + 1) * 128, g * 512:(g + 1) * 512], oo[gb][:],
                          r=["oo%d" % gb], w=[("sc_oa", h, g)])

            emit_score(steps[0])
            for i_, st in enumerate(steps):
                if i_ + 1 < len(steps):
                    emit_score(steps[i_ + 1])
                emit_pv(st)
        p.barrier()


def gdn_consts():
    k = np.arange(128)[:, None]
    i = np.arange(128)[None, :]
    same = (k // 64) == (i // 64)
    tri = ((k <= i) & same).astype(np.float32)
    blk = same.astype(np.float32)
    half0 = np.broadcast_to((k < 64), (128, 128)).astype(np.float32)
    half1 = np.broadcast_to((k >= 64), (128, 128)).astype(np.float32)
    ustr = ((k > i) & same).astype(np.float32)
    ii = np.arange(128)[:, None]
    jj = np.arange(128)[None, :]
    same2 = (ii // 64) == (jj // 64)
    negm_strict = np.where((ii > jj) & same2, 0.0, NEG).astype(np.float32)
    negm_inclT = np.where((jj >= ii) & same2, 0.0, NEG).astype(np.float32)
    return np.stack([tri, blk, half0, half1, ustr, negm_strict, negm_inclT], 0)


class _Stop(Exception):
    pass


def _chk(n):
    import os
    return int(os.environ.get("GDN_STOP", "99")) == n


def phase_gdn(c, p):
    _phase_gdn(c, p)
    p.barrier()


def _phase_gdn(c, p):
    nc = c.nc
    with ExitStack() as es:
        c2 = Ctx(); c2.nc = nc; c2.es = es
        G = sb(c2, "gc", [128, 7, 128])
        TRI, BLK, H0, H1, USTR, NMS, NMIT = [G[:, i, :] for i in range(7)]
        ba = sb(c2, "ba", [128, 16, 16])
        dtb = sb(c2, "dtb", [128, 8])
        Aex = sb(c2, "Aex", [128, 8])
        gng = sb(c2, "gng", [128, 128])
        beta = sb(c2, "beta", [128, 16, 8])
        nbeta = sb(c2, "nbeta", [128, 16, 8])
        gg = sb(c2, "gg", [128, 16, 8])
        egs = sb(c2, "egs", [128, 16, 8])
        kds = sb(c2, "kds", [128, 16, 8])
        kbs = sb(c2, "kbs", [128, 16, 8])
        egl = sb(c2, "egl", [128, 2, 16, 8])
        tmp8 = sb(c2, "tmp8", [128, 16, 8])
        cw = sb(c2, "cw", [128, 3, 4])
        xp = [sb(c2, "xp%d" % i, [128, 3 + S]) for i in range(3)]
        cv = [sb(c2, "cv%d" % i, [128, S]) for i in range(3)]
        sq = sb(c2, "gsq", [128, S], F32R)
        rs = sb(c2, "grs", [128, 512])
        NGB = 4
        kbg = [sb(c2, "kbg%d" % j, [128, 128]) for j in range(NGB)]
        vbe = [sb(c2, "vbe%d" % j, [128, 128]) for j in range(NGB)]
        Ag = [sb(c2, "Ag%d" % j, [128, 128]) for j in range(NGB)]
        Dec = [sb(c2, "Dec%d" % j, [128, 128]) for j in range(NGB)]
        DecT = [sb(c2, "DecT%d" % j, [128, 128]) for j in range(NGB)]
        Am = [[sb(c2, "Am%d_%d" % (j, i), [128, 128]) for i in range(2)] for j in range(NGB)]
        At = [[sb(c2, "At%d_%d" % (j, i), [128, 128]) for i in range(2)] for j in range(NGB)]
        Rt = [[sb(c2, "Rt%d_%d" % (j, i), [128, 128]) for i in range(2)] for j in range(NGB)]
        u_all = sb(c2, "u_all", [128, 16, 128])
        wT_all = sb(c2, "wT_all", [128, 16, 128])
        aT_all = sb(c2, "aT_all", [128, 16, 128])
        kd_all = sb(c2, "kd_all", [128, 2, 16, 128])
        kds2 = sb(c2, "kds2", [128, 2, 16, 8])
        St = sb(c2, "St", [128, 128])
        vnew = sb(c2, "vnew", [128, 128])
        otmp = sb(c2, "otmp", [128, 128])
        ob = sb(c2, "ob", [128, 16, 128])
        zt = sb(c2, "zt", [128, 16, 128])
        ssq = sb(c2, "ssq", [128, 16])
        junk = sb(c2, "junk", [128, 128])
        obT = sb(c2, "obT", [128, S])
        pool = [ps(c2, "gp%d" % i, [128, 512]) for i in range(8)]
        pc = [0]

        def nxt():
            i = pc[0] % 8
            pc[0] += 1
            return i

        p.dma("sync", G[:], c.gdnc_d.rearrange("a k i -> k a i"), w=["G"])
        p.dma("sync", ba[:], c.sc_tm[:, 2048:2064].rearrange("(t p) n -> p t n", p=128), w=["ba"])
        p.dma("sync", dtb[:], c.dt_bias.partition_broadcast(128), w=["dtb"])
        p.dma("sync", Aex[:], c.a_log.partition_broadcast(128), w=["Aex"])
        p.dma("sync", gng[:], c.gdn_norm_gain.partition_broadcast(128), w=["gng"])
        p.op("scalar", lambda e: e.activation(out=Aex[:], in_=Aex[:], func=AF.Exp), r=["Aex"], w=["Aex"])
        p.op("scalar", lambda e: e.activation(out=beta[:], in_=ba[:, :, 0:8], func=AF.Exp, scale=-1.0), r=["ba"], w=["beta"])
        p.op("vector", lambda e: e.tensor_scalar(out=beta[:], in0=beta[:], scalar1=1.0, scalar2=None, op0=ALU.add), r=["beta"], w=["beta"])
        p.op("vector", lambda e: e.reciprocal(out=beta[:], in_=beta[:]), r=["beta"], w=["beta"])
        p.op("vector", lambda e: e.tensor_scalar(out=nbeta[:], in0=beta[:], scalar1=-1.0, scalar2=None, op0=ALU.mult), r=["beta"], w=["nbeta"])
        p.op("vector", lambda e: e.tensor_tensor(out=gg[:], in0=ba[:, :, 8:16], in1=dtb[:].unsqueeze(1).to_broadcast([128, 16, 8]),
                                                 op=ALU.add), r=["ba", "dtb"], w=["gg"])
        p.op("scalar", lambda e: e.activation(out=gg[:], in_=gg[:], func=AF.Exp), r=["gg"], w=["gg"])
        p.op("scalar", lambda e: e.activation(out=gg[:], in_=gg[:], func=AF.Ln, bias=c.one_t[:, 0:1]), r=["gg"], w=["gg"])
        p.op("vector", lambda e: e.scalar_tensor_tensor(out=gg[:], in0=gg[:], scalar=-1.0, in1=Aex[:].unsqueeze(1).to_broadcast([128, 16, 8]),
                                                        op0=ALU.mult, op1=ALU.mult), r=["gg", "Aex"], w=["gg"])
        ggf = gg[:].rearrange("p t h -> p (t h)")
        i0 = nxt(); i1 = nxt(); i2 = nxt(); i3 = nxt()
        p.op("tensor", lambda e: e.matmul(pool[i0][:, 0:128], lhsT=TRI, rhs=ggf, start=True, stop=True), r=["G", "gg"], w=["gp%d" % i0])
        p.op("tensor", lambda e: e.matmul(pool[i1][:, 0:128], lhsT=BLK, rhs=ggf, start=True, stop=True), r=["G", "gg"], w=["gp%d" % i1])
        p.op("tensor", lambda e: e.matmul(pool[i2][:, 0:128], lhsT=H0, rhs=ggf, start=True, stop=True), r=["G", "gg"], w=["gp%d" % i2])
        p.op("tensor", lambda e: e.matmul(pool[i3][:, 0:128], lhsT=H1, rhs=ggf, start=True, stop=True), r=["G", "gg"], w=["gp%d" % i3])
        v3 = lambda t_: t_.rearrange("p (t h) -> p t h", h=8)
        p.op("scalar", lambda e: e.activation(out=egs[:], in_=v3(pool[i0][:, 0:128]), func=AF.Exp), r=["gp%d" % i0], w=["egs"])
        p.op("vector", lambda e: e.tensor_tensor(out=kbs[:], in0=egs[:], in1=beta[:], op=ALU.mult), r=["egs", "beta"], w=["kbs"])
        p.op("vector", lambda e: e.tensor_copy(out=tmp8[:], in_=v3(pool[i0][:, 0:128])), r=["gp%d" % i0], w=["tmp8"])
        p.op("vector", lambda e: e.tensor_tensor(out=tmp8[:], in0=v3(pool[i1][:, 0:128]), in1=tmp8[:], op=ALU.subtract),
             r=["gp%d" % i1, "tmp8"], w=["tmp8"])
        p.op("scalar", lambda e: e.activation(out=kds[:], in_=tmp8[:], func=AF.Exp), r=["tmp8"], w=["kds"])
        p.op("scalar", lambda e: e.activation(out=egl[:, 0], in_=v3(pool[i2][:, 0:128]), func=AF.Exp), r=["gp%d" % i2], w=["egl"])
        p.op("scalar", lambda e: e.activation(out=egl[:, 1], in_=v3(pool[i3][:, 0:128]), func=AF.Exp), r=["gp%d" % i3], w=["egl"])
        p.op("vector", lambda e: e.tensor_scalar(out=egs[:], in0=egs[:], scalar1=128.0 ** -0.5, scalar2=None, op0=ALU.mult),
             r=["egs", "kbs"], w=["egs"])
        p.op("vector", lambda e: e.tensor_scalar(out=kds2[:, 0], in0=kds[:], scalar1=H0[:, 0:1], scalar2=None, op0=ALU.mult),
             r=["kds", "G"], w=["kds2"])
        p.op("vector", lambda e: e.tensor_scalar(out=kds2[:, 1], in0=kds[:], scalar1=H1[:, 0:1], scalar2=None, op0=ALU.mult),
             r=["kds", "G"], w=["kds2"])
        for i in range(3):
            p.op("vector", lambda e, i=i: e.memset(xp[i][:, 0:3], 0.0), w=["xp%d" % i])
        p.op("vector", lambda e: e.memset(vnew[:], 0.0), w=["vnew"])

        if _chk(0):
            return
        for h in range(8):
            for i in range(3):
                row = 2048 + i * 1024 + h * 128
                p.dma("sync", xp[i][:, 3:3 + S], c.sc_fm[row:row + 128, :], w=["xp%d" % i])
                p.dma("sync", cw[:, i, :], c.conv_wT[i * 1024 + h * 128:i * 1024 + (h + 1) * 128, :], w=["cw"])
                p.op("vector", lambda e, i=i: e.tensor_scalar(out=cv[i][:], in0=xp[i][:, 0:S], scalar1=cw[:, i, 0:1], scalar2=None,
                                                              op0=ALU.mult), r=["xp%d" % i, "cw"], w=["cv%d" % i])
                for tap in range(1, 4):
                    p.op("vector", lambda e, i=i, tap=tap: e.scalar_tensor_tensor(
                        out=cv[i][:], in0=xp[i][:, tap:tap + S], scalar=cw[:, i, tap:tap + 1], in1=cv[i][:],
                        op0=ALU.mult, op1=ALU.add), r=["xp%d" % i, "cw", "cv%d" % i], w=["cv%d" % i])
                p.op("scalar", lambda e, i=i: e.activation(out=cv[i][:], in_=cv[i][:], func=AF.Silu), r=["cv%d" % i], w=["cv%d" % i])
                if i < 2:
                    p.op("scalar", lambda e, i=i: e.activation(out=sq[:], in_=cv[i][:], func=AF.Square), r=["cv%d" % i], w=["gsq"])
                    for tg in range(4):
                        a = nxt()
                        sl = slice(tg * 512, (tg + 1) * 512)
                        p.op("tensor", lambda e, a=a, sl=sl: e.matmul(pool[a][:], lhsT=c.ones_r[:], rhs=sq[:, sl], start=True, stop=True),
                             r=["gsq"], w=["gp%d" % a])
                        p.op("scalar", lambda e, a=a: e.activation(out=rs[:], in_=pool[a][:], func=AF.Sqrt, bias=c.eps_t[:, 0:1]),
                             r=["gp%d" % a], w=["grs"])
                        p.op("vector", lambda e: e.reciprocal(out=rs[:], in_=rs[:]), r=["grs"], w=["grs"])
                        p.op("vector", lambda e, i=i, sl=sl: e.tensor_tensor(out=cv[i][:, sl], in0=cv[i][:, sl], in1=rs[:], op=ALU.mult),
                             r=["cv%d" % i, "grs"], w=["cv%d" % i])
            qn, kn, vn = cv
            if _chk(1):
                return
            p.dma("sync", zt[:], c.sc_tm[:, 1024 + h * 128:1024 + (h + 1) * 128].rearrange("(t p) d -> p t d", p=128), w=["zt"])
            NG = 4
            for t0_ in range(0, 16, NG):
                tl = list(range(t0_, t0_ + NG))
                ak = {}; av = {}; akk = {}; agd = {}; aqk = {}; agt = {}; amt = {}
                for j, t in enumerate(tl):
                    ts = slice(t * 128, (t + 1) * 128)
                    ak[j] = nxt()
                    p.op("tensor", lambda e, a=ak[j], ts=ts: e.transpose(out=pool[a][:, 0:128], in_=kn[:, ts], identity=c.ident[:]),
                         r=["cv1"], w=["gp%d" % ak[j]])
                    p.op("vector", lambda e, a=ak[j], t=t, h=h, j=j: e.tensor_scalar(out=kbg[j][:], in0=pool[a][:, 0:128],
                                                                                   scalar1=kbs[:, t, h:h + 1], scalar2=None, op0=ALU.mult),
                         r=["gp%d" % ak[j], "kbs"], w=["kbg%d" % j])
                    for hf_ in range(2):
                        p.op("scalar", lambda e, a=ak[j], t=t, h=h, hf_=hf_: e.activation(out=kd_all[:, hf_, t, :], in_=pool[a][:, 0:128],
                                                                                        func=AF.Identity, scale=kds2[:, hf_, t, h:h + 1]),
                             r=["gp%d" % ak[j], "kds2"], w=["kd_all"])
                    av[j] = nxt()
                    p.op("tensor", lambda e, a=av[j], ts=ts: e.transpose(out=pool[a][:, 0:128], in_=vn[:, ts], identity=c.ident[:]),
                         r=["cv2"], w=["gp%d" % av[j]])
                    p.op("vector", lambda e, a=av[j], t=t, h=h, j=j: e.tensor_scalar(out=vbe[j][:], in0=pool[a][:, 0:128],
                                                                                   scalar1=beta[:, t, h:h + 1], scalar2=None, op0=ALU.mult),
                         r=["gp%d" % av[j], "beta"], w=["vbe%d" % j])
                    p.op("gpsimd", lambda e, t=t, h=h, j=j: e.tensor_scalar(out=Ag[j][:], in0=USTR, scalar1=gg[:, t, h:h + 1], scalar2=None,
                                                                            op0=ALU.mult), r=["G", "gg"], w=["Ag%d" % j])
                for j, t in enumerate(tl):
                    ts = slice(t * 128, (t + 1) * 128)
                    akk[j] = nxt()
                    p.op("tensor", lambda e, a=akk[j], ts=ts: e.matmul(pool[a][:, 0:128], lhsT=kn[:, ts], rhs=kn[:, ts], start=True, stop=True),
                         r=["cv1"], w=["gp%d" % akk[j]])
                    agd[j] = nxt()
                    p.op("tensor", lambda e, a=agd[j], j=j: e.matmul(pool[a][:, 0:128], lhsT=TRI, rhs=Ag[j][:], start=True, stop=False),
                         r=["G", "Ag%d" % j], w=["gp%d" % agd[j]])
                    p.op("tensor", lambda e, a=agd[j]: e.matmul(pool[a][:, 0:128], lhsT=c.ident[:], rhs=NMS, start=False, stop=True),
                         r=["G"], w=["gp%d" % agd[j]])
                    p.op("scalar", lambda e, a=agd[j], j=j: e.activation(out=Dec[j][:], in_=pool[a][:, 0:128], func=AF.Exp),
                         r=["gp%d" % agd[j]], w=["Dec%d" % j])
                    p.op("vector", lambda e, a=akk[j], t=t, h=h, j=j: e.scalar_tensor_tensor(out=Am[j][0][:], in0=pool[a][:, 0:128],
                                                                                           scalar=nbeta[:, t, h:h + 1], in1=Dec[j][:],
                                                                                           op0=ALU.mult, op1=ALU.mult),
                         r=["gp%d" % akk[j], "nbeta", "Dec%d" % j], w=["Am%d_0" % j])
                for j, t in enumerate(tl):
                    ts = slice(t * 128, (t + 1) * 128)
                    aqk[j] = nxt()
                    p.op("tensor", lambda e, a=aqk[j], ts=ts: e.matmul(pool[a][:, 0:128], lhsT=kn[:, ts], rhs=qn[:, ts], start=True, stop=True),
                         r=["cv1", "cv0"], w=["gp%d" % aqk[j]])
                    agt[j] = nxt()
                    p.op("tensor", lambda e, a=agt[j], j=j: e.matmul(pool[a][:, 0:128], lhsT=Ag[j][:], rhs=TRI, start=True, stop=False),
                         r=["G", "Ag%d" % j], w=["gp%d" % agt[j]])
                    p.op("tensor", lambda e, a=agt[j]: e.matmul(pool[a][:, 0:128], lhsT=c.ident[:], rhs=NMIT, start=False, stop=True),
                         r=["G"], w=["gp%d" % agt[j]])
                    p.op("scalar", lambda e, a=agt[j], j=j: e.activation(out=DecT[j][:], in_=pool[a][:, 0:128], func=AF.Exp),
                         r=["gp%d" % agt[j]], w=["DecT%d" % j])
                    p.op("vector", lambda e, a=aqk[j], t=t, j=j: e.scalar_tensor_tensor(out=aT_all[:, t, :], in0=pool[a][:, 0:128],
                                                                                      scalar=128.0 ** -0.5, in1=DecT[j][:],
                                                                                      op0=ALU.mult, op1=ALU.mult),
                         r=["gp%d" % aqk[j], "DecT%d" % j], w=["aT_all"])
                for j, t in enumerate(tl):
                    amt[j] = nxt()
                    p.op("tensor", lambda e, a=amt[j], j=j: e.transpose(out=pool[a][:, 0:128], in_=Am[j][0][:], identity=c.ident[:]),
                         r=["Am%d_0" % j], w=["gp%d" % amt[j]])
                    p.op("scalar", lambda e, a=amt[j], j=j: e.copy(out=At[j][0][:], in_=pool[a][:, 0:128]),
                         r=["gp%d" % amt[j]], w=["At%d_0" % j])
                    p.op("gpsimd", lambda e, j=j: e.tensor_tensor(out=Rt[j][0][:], in0=At[j][0][:], in1=c.ident[:], op=ALU.add),
                         r=["At%d_0" % j], w=["Rt%d_0" % j])
                cur = 0
                for m in range(1, 6):
                    nx = 1 - cur
                    for j, t in enumerate(tl):
                        a1 = nxt()
                        p.op("tensor", lambda e, a=a1, cur=cur, j=j: e.matmul(pool[a][:, 0:128], lhsT=At[j][cur][:], rhs=Am[j][cur][:],
                                                                              start=True, stop=True),
                             r=["At%d_%d" % (j, cur), "Am%d_%d" % (j, cur)], w=["gp%d" % a1])
                        p.op("scalar", lambda e, a=a1, nx=nx, j=j: e.copy(out=Am[j][nx][:], in_=pool[a][:, 0:128]),
                             r=["gp%d" % a1], w=["Am%d_%d" % (j, nx)])
                        if m < 5:
                            a2 = nxt()
                            p.op("tensor", lambda e, a=a2, cur=cur, j=j: e.matmul(pool[a][:, 0:128], lhsT=Am[j][cur][:], rhs=At[j][cur][:],
                                                                                  start=True, stop=True),
                                 r=["At%d_%d" % (j, cur), "Am%d_%d" % (j, cur)], w=["gp%d" % a2])
                            p.op("vector", lambda e, a=a2, nx=nx, j=j: e.tensor_copy(out=At[j][nx][:], in_=pool[a][:, 0:128]),
                                 r=["gp%d" % a2], w=["At%d_%d" % (j, nx)])
                    for j, t in enumerate(tl):
                        a3 = nxt()
                        p.op("tensor", lambda e, a=a3, cur=cur, nx=nx, j=j: e.matmul(pool[a][:, 0:128], lhsT=Am[j][nx][:], rhs=Rt[j][cur][:],
                                                                                     start=True, stop=True),
                             r=["Am%d_%d" % (j, nx), "Rt%d_%d" % (j, cur)], w=["gp%d" % a3])
                        p.op("vector", lambda e, a=a3, cur=cur, nx=nx, j=j: e.tensor_tensor(out=Rt[j][nx][:], in0=pool[a][:, 0:128],
                                                                                            in1=Rt[j][cur][:], op=ALU.add),
                             r=["gp%d" % a3, "Rt%d_%d" % (j, cur)], w=["Rt%d_%d" % (j, nx)])
                    cur = nx
                for j, t in enumerate(tl):
                    RtF = Rt[j][cur]
                    a_u = nxt()
                    p.op("tensor", lambda e, a=a_u, RtF=RtF, j=j: e.matmul(pool[a][:, 0:128], lhsT=RtF[:], rhs=vbe[j][:], start=True, stop=True),
                         r=["Rt%d_%d" % (j, cur), "vbe%d" % j], w=["gp%d" % a_u])
                    p.op("scalar", lambda e, a=a_u, t=t: e.copy(out=u_all[:, t, :], in_=pool[a][:, 0:128]), r=["gp%d" % a_u], w=["u_all"])
                    a_w = nxt()
                    p.op("tensor", lambda e, a=a_w, RtF=RtF, j=j: e.matmul(pool[a][:, 0:128], lhsT=kbg[j][:], rhs=RtF[:], start=True, stop=True),
                         r=["Rt%d_%d" % (j, cur), "kbg%d" % j], w=["gp%d" % a_w])
                    p.op("vector", lambda e, a=a_w, t=t: e.tensor_copy(out=wT_all[:, t, :], in_=pool[a][:, 0:128]),
                         r=["gp%d" % a_w], w=["wT_all"])
            if _chk(3):
                return
            p.op("vector", lambda e: e.memset(St[:], 0.0), w=["St"])
            for ch in range(32):
                t = ch // 2
                hf = ch % 2
                rows = slice(hf * 64, hf * 64 + 64)
                ts = slice(t * 128, (t + 1) * 128)
                a1 = nxt()
                p.op("tensor", lambda e, a=a1, t=t: e.matmul(pool[a][:, 0:128], lhsT=wT_all[:, t, :], rhs=St[:], start=True, stop=True),
                     r=["wT_all", "St"], w=["gp%d" % a1])
                p.op("vector", lambda e, a=a1, t=t, rows=rows: e.tensor_tensor(out=vnew[rows, :], in0=u_all[rows, t, :],
                                                                               in1=pool[a][rows, 0:128], op=ALU.subtract),
                     r=["gp%d" % a1, "u_all"], w=["vnew"])
                aA = nxt()
                p.op("tensor", lambda e, a=aA, ts=ts: e.matmul(pool[a][:, 0:128], lhsT=qn[:, ts], rhs=St[:], start=True, stop=True),
                     r=["cv0", "St"], w=["gp%d" % aA])
                aB = nxt()
                p.op("tensor", lambda e, a=aB, t=t: e.matmul(pool[a][:, 0:128], lhsT=aT_all[:, t, :], rhs=vnew[:], start=True, stop=True),
                     r=["aT_all", "vnew"], w=["gp%d" % aB])
                aS = nxt()
                p.op("tensor", lambda e, a=aS, t=t, hf=hf: e.matmul(pool[a][:, 0:128], lhsT=kd_all[:, hf, t, :], rhs=vnew[:],
                                                                    start=True, stop=True),
                     r=["kd_all", "vnew"], w=["gp%d" % aS])
                p.op("scalar", lambda e, a=aA, t=t, h=h, rows=rows: e.activation(out=otmp[rows, :], in_=pool[a][rows, 0:128], func=AF.Identity,
                                                                                 scale=egs[rows, t, h:h + 1]),
                     r=["gp%d" % aA, "egs"], w=["otmp"])
                p.op("vector", lambda e, a=aB, t=t, rows=rows: e.tensor_tensor(out=ob[rows, t, :], in0=otmp[rows, :], in1=pool[a][rows, 0:128],
                                                                               op=ALU.add),
                     r=["gp%d" % aB, "otmp"], w=["ob"])
                p.op("vector", lambda e, a=aS, t=t, hf=hf, h=h: e.scalar_tensor_tensor(out=St[:], in0=St[:], scalar=egl[:, hf, t, h:h + 1],
                                                                                       in1=pool[a][:, 0:128], op0=ALU.mult, op1=ALU.add),
                     r=["gp%d" % aS, "St", "egl"], w=["St"])
            if _chk(4):
                return
            for t in range(16):
                p.op("scalar", lambda e, t=t: e.activation(out=junk[:], in_=ob[:, t, :], func=AF.Square, accum_out=ssq[:, t:t + 1]),
                     r=["ob"], w=["junk", "ssq"])
            p.op("scalar", lambda e: e.activation(out=ssq[:], in_=ssq[:], func=AF.Sqrt, scale=1.0 / 128, bias=c.eps_t[:, 0:1]),
                 r=["ssq"], w=["ssq"])
            p.op("vector", lambda e: e.reciprocal(out=ssq[:], in_=ssq[:]), r=["ssq"], w=["ssq"])
            p.op("scalar", lambda e: e.activation(out=zt[:], in_=zt[:], func=AF.Silu), r=["zt"], w=["zt"])
            p.op("vector", lambda e: e.tensor_tensor(out=ob[:], in0=ob[:], in1=ssq[:].unsqueeze(2).to_broadcast([128, 16, 128]), op=ALU.mult),
                 r=["ob", "ssq"], w=["ob"])
            p.op("vector", lambda e: e.tensor_tensor(out=ob[:], in0=ob[:], in1=gng[:].unsqueeze(1).to_broadcast([128, 16, 128]), op=ALU.mult),
                 r=["ob", "gng"], w=["ob"])
            p.op("vector", lambda e: e.tensor_tensor(out=ob[:], in0=ob[:], in1=zt[:], op=ALU.mult), r=["ob", "zt"], w=["ob"])
            for t in range(16):
                a = nxt()
                p.op("tensor", lambda e, a=a, t=t: e.transpose(out=pool[a][:, 0:128], in_=ob[:, t, :], identity=c.ident[:]),
                     r=["ob"], w=["gp%d" % a])
                p.op("scalar", lambda e, a=a, t=t: e.copy(out=obT[:, t * 128:(t + 1) * 128], in_=pool[a][:, 0:128]),
                     r=["gp%d" % a], w=["obT"])
            p.dma("sync", c.sc_ob[h * 128:(h + 1) * 128, :], obT[:], r=["obT"], w=[("sc_ob", h)])
        p.barrier()


def phase_merge(c, p):
    nc = c.nc
    with ExitStack() as es:
        c2 = Ctx(); c2.nc = nc; c2.es = es
        oa = sb(c2, "oa", [128, 8, 512], F32R)
        obb = sb(c2, "obb", [128, 8, 512], F32R)
        wa = [sb(c2, "wa%d" % i, [128, 8, 128], F32R) for i in range(2)]
        wbb = [sb(c2, "wbb%d" % i, [128, 8, 128], F32R) for i in range(2)]
        ga = [sb(c2, "ga%d" % i, [128, 512]) for i in range(2)]
        gb_ = [sb(c2, "gb%d" % i, [128, 512]) for i in range(2)]
        m1 = sb(c2, "m1", [128, 512])
        mT = sb(c2, "mT", [128, 16, 512], F32R)
        wo = [sb(c2, "wo%d" % i, [128, 16, 512], F32R) for i in range(2)]
        xt = [sb(c2, "xt%d" % i, [128, 512]) for i in range(2)]
        pa = [ps(c2, "pa%d" % i, [128, 512]) for i in range(2)]
        pb = [ps(c2, "pb%d" % i, [128, 512]) for i in range(2)]
        po = [ps(c2, "po%d" % i, [128, 512]) for i in range(4)]
        ci = 0
        wi = 0
        oi = 0
        for tg in range(4):
            tsl = slice(tg * 512, (tg + 1) * 512)
            p.dma("sync", oa[:], r32(c.sc_oa[:, tsl]).rearrange("(kc p) t -> p kc t", p=128), w=["oa"])
            p.dma("sync", obb[:], r32(c.sc_ob[:, tsl]).rearrange("(kc p) t -> p kc t", p=128), w=["obb"])
            for cc in range(16):
                b = ci % 2
                ci += 1
                csl = slice(cc * 128, (cc + 1) * 128)
                p.dma("sync", wa[b][:], r32(c.w_up_a[:, csl]).rearrange("(kc p) n -> p kc n", p=128), w=["wa%d" % b])
                p.dma("sync", wbb[b][:], r32(c.w_up_b[:, csl]).rearrange("(kc p) n -> p kc n", p=128), w=["wbb%d" % b])
                p.dma("sync", ga[b][:], c.sc_fm[5120 + cc * 128:5120 + (cc + 1) * 128, tsl], w=["ga%d" % b])
                p.dma("sync", gb_[b][:], c.sc_fm[7168 + cc * 128:7168 + (cc + 1) * 128, tsl], w=["gb%d" % b])
                for kc in range(8):
                    p.op("tensor", lambda e, b=b, kc=kc: e.matmul(pa[b][:], lhsT=wa[b][:, kc, :], rhs=oa[:, kc, :],
                                                                  start=(kc == 0), stop=(kc == 7)),
                         r=["wa%d" % b, "oa"], w=["pa%d" % b])
                for kc in range(8):
                    p.op("tensor", lambda e, b=b, kc=kc: e.matmul(pb[b][:], lhsT=wbb[b][:, kc, :], rhs=obb[:, kc, :],
                                                                  start=(kc == 0), stop=(kc == 7)),
                         r=["wbb%d" % b, "obb"], w=["pb%d" % b])
                p.op("scalar", lambda e, b=b: e.activation(out=ga[b][:], in_=ga[b][:], func=AF.Sigmoid), r=["ga%d" % b], w=["ga%d" % b])
                p.op("scalar", lambda e, b=b: e.activation(out=gb_[b][:], in_=gb_[b][:], func=AF.Sigmoid), r=["gb%d" % b], w=["gb%d" % b])
                p.op("vector", lambda e, b=b: e.tensor_tensor(out=m1[:], in0=pa[b][:], in1=ga[b][:], op=ALU.mult),
                     r=["pa%d" % b, "ga%d" % b], w=["m1"])
                p.op("vector", lambda e, b=b: e.tensor_tensor(out=gb_[b][:], in0=pb[b][:], in1=gb_[b][:], op=ALU.mult),
                     r=["pb%d" % b, "gb%d" % b], w=["gb%d" % b])
                p.op("vector", lambda e, b=b, cc=cc: e.tensor_tensor(out=mT[:, cc, :], in0=m1[:], in1=gb_[b][:], op=ALU.add),
                     r=["m1", "gb%d" % b], w=["mT"])
            for dg in range(4):
                wb_ = wi % 2
                wi += 1
                dsl = slice(dg * 512, (dg + 1) * 512)
                p.dma("sync", wo[wb_][:], r32(c.w_out[:, dsl]).rearrange("(kc p) n -> p kc n", p=128), w=["wo%d" % wb_])
                for tt in range(4):
                    o = oi % 4
                    oi += 1
                    x_ = oi % 2
                    t0 = tg * 512 + tt * 128
                    p.dma("sync", xt[x_][:], c.x[t0:t0 + 128, dsl], w=["xt%d" % x_])
                    for cc in range(16):
                        p.op("tensor", lambda e, o=o, cc=cc, tt=tt, wb_=wb_: e.matmul(
                            po[o][:], lhsT=mT[:, cc, tt * 128:(tt + 1) * 128], rhs=wo[wb_][:, cc, :],
                            start=(cc == 0), stop=(cc == 15)), r=["mT", "wo%d" % wb_], w=["po%d" % o])
                    p.op("vector", lambda e, o=o, x_=x_: e.tensor_tensor(out=xt[x_][:], in0=po[o][:], in1=xt[x_][:], op=ALU.add),
                         r=["po%d" % o, "xt%d" % x_], w=["xt%d" % x_])
                    p.dma("sync", c.sc_x1[t0:t0 + 128, dsl], xt[x_][:], r=["xt%d" % x_], w=[("sc_x1", t0, dg)])
        p.barrier()


def phase_peer_cvt(c, p):
    nc = c.nc
    with ExitStack() as es:
        c2 = Ctx(); c2.nc = nc; c2.es = es
        cin = [sb(c2, "cin%d" % i, [128, 8192]) for i in range(2)]
        cout = [sb(c2, "cout%d" % i, [128, 8192], BF16) for i in range(2)]
        k = 0
        for (src, dst) in ((c.peer_u, c.uvb[:, 0:D]), (c.peer_v, c.uvb[:, D:2 * D])):
            for ch in range(32):
                b = k % 2
                k += 1
                rows = slice(ch * 512, (ch + 1) * 512)
                p.dma("sync", cin[b][:], src[rows, :].rearrange("(p r) d -> p (r d)", r=4), w=["cin%d" % b])
                p.op("scalar", lambda e, b=b: e.copy(out=cout[b][:, 0:4096], in_=cin[b][:, 0:4096]), r=["cin%d" % b], w=["coutA%d" % b])
                p.op("vector", lambda e, b=b: e.tensor_copy(out=cout[b][:, 4096:8192], in_=cin[b][:, 4096:8192]),
                     r=["cin%d" % b], w=["coutB%d" % b])
                p.dma("sync", dst[rows, :].rearrange("(p r) d -> p r d", r=4), cout[b][:].rearrange("p (r d) -> p r d", r=4),
                      r=["coutA%d" % b, "coutB%d" % b], w=[("tb", k)])
        p.barrier()


def phase_peer(c, p):
    nc = c.nc
    phase_peer_cvt(c, p)
    for half in range(2):
        with ExitStack() as es:
            c2 = Ctx(); c2.nc = nc; c2.es = es
            hT = sb(c2, "h2T", [128, 16, 1024], F32R)
            phase_norm_T(c, p, c.sc_x1, c.norm2_gain, hT, "n2_", half, h_out=c.sc_h2)
            with ExitStack() as es2:
                c3 = Ctx(); c3.nc = nc; c3.es = es2
                wq = [sb(c3, "wq%d" % i, [128, 16, 128], F32R) for i in range(2)]
                skT = sb(c3, "skT", [128, 16, 128], F32R)
                qT = [sb(c3, "qT%d" % i, [128, 1024], F32R) for i in range(2)]
                pq = [ps(c3, "pq%d" % i, [128, 512]) for i in range(4)]
                so_t = [sb(c3, "so_t%d" % i, [128, 512]) for i in range(2)]
                pqi = 0
                p.dma("sync", skT[:], r32(c.skT_d[:, :, :]), w=["skT"])
                for ch in range(16):
                    b = ch % 2
                    p.dma("sync", wq[b][:], r32(c.w_query[:, ch * 128:(ch + 1) * 128]).rearrange("(kc p) n -> p kc n", p=128),
                          w=["wq%d" % b])
                    for tg in range(2):
                        a = pqi % 4
                        pqi += 1
                        for kc in range(16):
                            p.op("tensor", lambda e, a=a, b=b, kc=kc, tg=tg: e.matmul(
                                pq[a][:], lhsT=wq[b][:, kc, :], rhs=hT[:, kc, tg * 512:(tg + 1) * 512],
                                start=(kc == 0), stop=(kc == 15)), r=["wq%d" % b, "hT"], w=["pq%d" % a])
                        p.op("scalar", lambda e, a=a, b=b, tg=tg: e.copy(out=qT[b][:, tg * 512:(tg + 1) * 512], in_=pq[a][:]),
                             r=["pq%d" % a], w=["qT%d" % b])
                    for tq in range(2):
                        a = pqi % 4
                        pqi += 1
                        for tt in range(4):
                            tl = tq * 4 + tt
                            p.op("tensor", lambda e, a=a, b=b, tl=tl, tt=tt, ch=ch: e.matmul(
                                pq[a][:, tt * 128:(tt + 1) * 128], lhsT=qT[b][:, tl * 128:(tl + 1) * 128], rhs=skT[:, ch, :],
                                start=True, stop=True), r=["qT%d" % b, "skT"], w=["pq%d" % a])
                        so = "so%d" % (pqi % 2)
                        sot = so_t[pqi % 2]
                        p.op("vector", lambda e, a=a, sot=sot: e.tensor_copy(out=sot[:], in_=pq[a][:]), r=["pq%d" % a], w=[so])
                        tb = half * 1024 + tq * 512
                        p.dma("sync", c.sc_sc[tb:tb + 512, ch * 128:(ch + 1) * 128].rearrange("(tt p) k -> p tt k", p=128),
                              sot[:].rearrange("p (tt k) -> p tt k", k=128), r=[so], w=[("sc_sc", tb, ch)])
                p.barrier()
    with ExitStack() as es:
        c2 = Ctx(); c2.nc = nc; c2.es = es
        sc = sb(c2, "sc", [128, 16, 128])
        wk = sb(c2, "wk", [128, 128])
        stop_ = sb(c2, "stop", [128, 16, 16])
        itop = sb(c2, "itop", [128, 16, 16], U32)
        itf = sb(c2, "itf", [128, 16, 16])
        cand = sb(c2, "cand", [128, 8, 16, 16])
        cidx = sb(c2, "cidx", [128, 8, 16, 16])
        wk2 = sb(c2, "wk2", [128, 256])
        best = sb(c2, "best", [128, 8, 16])
        pos = sb(c2, "pos", [128, 8, 16], U32)
        posf = sb(c2, "posf", [128, 8, 16])
        iota = sb(c2, "iota", [128, 256])
        junk2 = sb(c2, "junk2", [128, 256])
        eidf = sb(c2, "eidf", [128, 128])
        eid = sb(c2, "eid", [128, 128], U32)
        nmx = sb(c2, "nmx", [128, 8])
        gsum = sb(c2, "gsum", [128, 8])
        gate = sb(c2, "gate", [128, 8, 16])
        dots = sb(c2, "dots", [128, 128])
        gact = sb(c2, "gact", [128, 128])
        h2 = sb(c2, "h2", [128, D])
        NB = 12
        gu = [sb(c2, "gu%d" % i, [128, 2 * D], BF16) for i in range(NB)]
        gel = sb(c2, "gel", [128, 128])
        diag = [sb(c2, "diag%d" % i, [128, 128], BF16) for i in range(2)]
        po = [ps(c2, "po%d" % i, [128, 512]) for i in range(4)]
        junk = sb(c2, "junkp", [128, D])
        acc = sb(c2, "acc", [128, D])
        x1 = sb(c2, "x1", [128, D])
        p.dma("sync", iota[:], c.iota_d[:, :], w=["iota"])
        gi = 0
        for t in range(16):
            t0 = t * 128
            p.dma("sync", sc[:], c.sc_sc[t0:t0 + 128, :].rearrange("p (c k) -> p c k", k=128), w=["sc"])
            p.dma("sync", h2[:], c.sc_h2[t0:t0 + 128, :], w=["h2"])
            p.dma("sync", x1[:], c.sc_x1[t0:t0 + 128, :], w=["x1"])
            for ch in range(16):
                p.op("vector", lambda e, ch=ch: e.max(out=stop_[:, ch, 0:8], in_=sc[:, ch, :]), r=["sc"], w=["stop"])
                p.op("vector", lambda e, ch=ch: e.max_index(out=itop[:, ch, 0:8], in_max=stop_[:, ch, 0:8], in_values=sc[:, ch, :]),
                     r=["sc", "stop"], w=["itop"])
                p.op("vector", lambda e, ch=ch: e.match_replace(out=wk[:], in_to_replace=stop_[:, ch, 0:8], in_values=sc[:, ch, :],
                                                                imm_value=-1e30), r=["sc", "stop"], w=["wk"])
                p.op("vector", lambda e, ch=ch: e.max(out=stop_[:, ch, 8:16], in_=wk[:]), r=["wk"], w=["stop"])
                p.op("vector", lambda e, ch=ch: e.max_index(out=itop[:, ch, 8:16], in_max=stop_[:, ch, 8:16], in_values=wk[:]),
                     r=["wk", "stop"], w=["itop"])
            p.op("vector", lambda e: e.tensor_copy(out=itf[:], in_=itop[:]), r=["itop"], w=["itf"])
            s4 = stop_[:].rearrange("p (h two) k -> p h two k", two=2)
            i4 = itf[:].rearrange("p (h two) k -> p h two k", two=2)
            p.op("vector", lambda e, s4=s4: e.tensor_tensor(out=cand[:], in0=s4[:, :, 0, :].unsqueeze(3).to_broadcast([128, 8, 16, 16]),
                                                            in1=s4[:, :, 1, :].unsqueeze(2).to_broadcast([128, 8, 16, 16]), op=ALU.add),
                 r=["stop"], w=["cand"])
            for hh in range(8):
                p.op("vector", lambda e, i4=i4, hh=hh: e.scalar_tensor_tensor(
                    out=cidx[:, hh], in0=i4[:, hh, 0, :].unsqueeze(2).to_broadcast([128, 16, 16]), scalar=128.0,
                    in1=i4[:, hh, 1, :].unsqueeze(1).to_broadcast([128, 16, 16]), op0=ALU.mult, op1=ALU.add),
                    r=["itf"], w=["cidx"])
            for hh in range(8):
                cv_ = cand[:, hh].rearrange("p a b -> p (a b)")
                p.op("vector", lambda e, hh=hh, cv_=cv_: e.max(out=best[:, hh, 0:8], in_=cv_), r=["cand"], w=["best"])
                p.op("vector", lambda e, hh=hh, cv_=cv_: e.max_index(out=pos[:, hh, 0:8], in_max=best[:, hh, 0:8], in_values=cv_),
                     r=["cand", "best"], w=["pos"])
                p.op("vector", lambda e, hh=hh, cv_=cv_: e.match_replace(out=wk2[:], in_to_replace=best[:, hh, 0:8], in_values=cv_,
                                                                         imm_value=-1e30), r=["cand", "best"], w=["wk2"])
                p.op("vector", lambda e, hh=hh: e.max(out=best[:, hh, 8:16], in_=wk2[:]), r=["wk2"], w=["best"])
                p.op("vector", lambda e, hh=hh: e.max_index(out=pos[:, hh, 8:16], in_max=best[:, hh, 8:16], in_values=wk2[:]),
                     r=["wk2", "best"], w=["pos"])
            p.op("vector", lambda e: e.tensor_copy(out=posf[:], in_=pos[:]), r=["pos"], w=["posf"])
            for hh in range(8):
                ci_ = cidx[:, hh].rearrange("p a b -> p (a b)")
                for m in range(16):
                    p.op("vector", lambda e, hh=hh, m=m, ci_=ci_: e.scalar_tensor_tensor(
                        out=junk2[:], in0=iota[:], scalar=posf[:, hh, m:m + 1], in1=ci_, op0=ALU.is_equal, op1=ALU.mult,
                        accum_out=eidf[:, hh * 16 + m:hh * 16 + m + 1]), r=["iota", "posf", "cidx"], w=["junk2", "eidf"])
            p.op("vector", lambda e: e.tensor_copy(out=eid[:], in_=eidf[:]), r=["eidf"], w=["eid"])
            p.op("vector", lambda e: e.tensor_scalar(out=nmx[:], in0=best[:, :, 0], scalar1=-1.0, scalar2=None, op0=ALU.mult),
                 r=["best"], w=["nmx"])
            for hh in range(8):
                p.op("scalar", lambda e, hh=hh: e.activation(out=gate[:, hh, :], in_=best[:, hh, :], func=AF.Exp, bias=nmx[:, hh:hh + 1],
                                                             accum_out=gsum[:, hh:hh + 1]), r=["best", "nmx"], w=["gate", "gsum"])
            p.op("vector", lambda e: e.reciprocal(out=gsum[:], in_=gsum[:]), r=["gsum"], w=["gsum"])
            p.op("vector", lambda e: e.tensor_tensor(out=gate[:], in0=gate[:], in1=gsum[:].unsqueeze(2).to_broadcast([128, 8, 16]), op=ALU.mult),
                 r=["gate", "gsum"], w=["gate"])
            gatef = gate[:].rearrange("p h k -> p (h k)")
            pend = []

            def emit_up(args, gatef=gatef):
                b, db, s_ = args
                p.op("vector", lambda e: e.tensor_scalar(out=diag[db][:], in0=c.ident[:], scalar1=gel[:, s_:s_ + 1],
                                                         scalar2=gatef[:, s_:s_ + 1], op0=ALU.mult, op1=ALU.mult),
                     r=[("gel", s_), "gate"], w=["diag%d" % db])
                for dg in range(4):
                    p.op("tensor", lambda e, dg=dg: e.matmul(
                        po[dg][:], lhsT=diag[db][:], rhs=gu[b][:, D + dg * 512:D + (dg + 1) * 512], start=(s_ == 0), stop=(s_ == 127)),
                        r=["diag%d" % db, "gu%d" % b], w=["po%d" % dg])

            for s_ in range(128):
                b = gi % NB
                gi += 1
                db = s_ % 2
                p.dma_fn("gpsimd", lambda e, b=b, s_=s_: e.indirect_dma_start(
                    out=gu[b][:], out_offset=None, in_=c.uvb[:, :],
                    in_offset=bass.IndirectOffsetOnAxis(ap=eid[:, s_:s_ + 1], axis=0)), r=["eid"], w=["gu%d" % b])
                p.op("vector", lambda e, b=b, s_=s_: e.scalar_tensor_tensor(out=junk[:], in0=gu[b][:, 0:D], scalar=1.0, in1=h2[:],
                                                                            op0=ALU.mult, op1=ALU.mult, accum_out=dots[:, s_:s_ + 1]),
                     r=["gu%d" % b, "h2"], w=["junkp", ("dots", s_)])
                p.op("scalar", lambda e, s_=s_: e.activation(out=gel[:, s_:s_ + 1], in_=dots[:, s_:s_ + 1], func=AF.Gelu),
                     r=[("dots", s_)], w=[("gel", s_)])
                pend.append((b, db, s_))
                if len(pend) > 1:
                    emit_up(pend.pop(0))
            while pend:
                emit_up(pend.pop(0))
            for dg in range(4):
                dsl = slice(dg * 512, (dg + 1) * 512)
                p.op("vector", lambda e, dg=dg, dsl=dsl: e.tensor_tensor(out=acc[:, dsl], in0=po[dg][:], in1=x1[:, dsl], op=ALU.add),
                     r=["po%d" % dg, "x1"], w=["acc"])
            p.dma("sync", c.y[t0:t0 + 128, :], acc[:], r=["acc"], w=[("y", t)])
        p.barrier()


ALL_PHASES = ("inproj", "moba", "gdn", "merge", "peer")


def build_nc(debug=False, phases=ALL_PHASES):
    nc = bass.Bass("TRN2", target_bir_lowering=False)
    nc.dge_precook = False
    c = Ctx()
    c.nc = nc
    kind_s = "ExternalOutput" if debug else "Internal"

    def din(name, shape, dt=F32):
        return nc.dram_tensor(name, list(shape), dt, kind="ExternalInput").ap()

    def dsc(name, shape):
        return nc.dram_tensor(name, list(shape), F32, kind=kind_s).ap()

    c.x = din("x", [S, D])
    c.norm1_gain = din("norm1_gain", [1, D])
    c.w_in = din("w_in", [D, IN_TOTAL])
    c.ident_d = din("ident", [128, 128])
    c.ones_d = din("ones", [128, 128])
    c.rel_bias = din("rel_bias", [32, 8])
    c.q_norm_gain = din("q_norm_gain", [1, 128])
    c.k_norm_gain = din("k_norm_gain", [1, 128])
    c.d01_d = din("d01", [8, 2, 128, 128])
    c.cm_d = din("cm", [128, 16, 8])
    c.notown_d = din("notown", [128, 16, 8])
    c.esel_d = din("esel", [8, 8, 128])
    c.gdnc_d = din("gdnc", [7, 128, 128])
    c.conv_wT = din("conv_wT", [3072, 4])
    c.a_log = din("a_log", [1, 8])
    c.dt_bias = din("dt_bias", [1, 8])
    c.gdn_norm_gain = din("gdn_norm_gain", [1, 128])
    c.w_up_a = din("w_up_a", [1024, D])
    c.w_up_b = din("w_up_b", [1024, D])
    c.w_out = din("w_out", [D, D])
    if "peer" in phases:
        c.norm2_gain = din("norm2_gain", [1, D])
        c.w_query = din("w_query", [D, D])
        c.skT_d = din("skT", [128, 16, 128])
        c.peer_u = din("peer_u", [16384, D])
        c.peer_v = din("peer_v", [16384, D])
        c.iota_d = din("iota", [128, 256])
        c.sc_h2 = dsc("sc_h2", [S, D])
        c.uvb = nc.dram_tensor("peer_uvb", [16384, 2 * D], BF16, kind="Internal").ap()
        c.sc_sc = dsc("sc_sc", [S, D])
    c.sc_fm = dsc("sc_fm", [N_FM, S])
    c.sc_tm = dsc("sc_tm", [S, N_TM])
    c.sc_oa = dsc("sc_oa", [1024, S])
    c.sc_ob = dsc("sc_ob", [1024, S])
    if "merge" in phases or "peer" not in phases:
        c.sc_x1 = dsc("sc_x1", [S, D])
    else:
        c.sc_x1 = din("sc_x1", [S, D])
    c.y = nc.dram_tensor("y", [S, D], F32, kind="ExternalOutput").ap()

    with ExitStack() as es:
        c.es = es
        p = Prog(nc, es)
        block = es.enter_context(nc.Block())
        c.ident = sb(c, "ident_s", [128, 128])
        c.ident_r = sb(c, "ident_r", [128, 128], F32R)
        c.ones_r = sb(c, "ones_r", [128, 128], F32R)
        c.eps_t = sb(c, "eps_t", [128, 1])
        c.one_t = sb(c, "one_t", [128, 1])
        p.dma("sync", c.ident[:], c.ident_d[:, :], w=["ident"])
        p.dma("sync", c.ident_r[:], r32(c.ident_d[:, :]), w=["ident_r"])
        p.dma("sync", c.ones_r[:], r32(c.ones_d[:, :]), w=["ones_r"])
        p.op("vector", lambda e: e.memset(c.eps_t[:], EPS), w=["eps"])
        p.op("vector", lambda e: e.memset(c.one_t[:], 1.0), w=["one"])
        p.barrier()
        if "inproj" in phases:
            phase_inproj(c, p)
        if "moba" in phases:
            phase_moba(c, p)
        if "gdn" in phases:
            phase_gdn(c, p)
        if "merge" in phases:
            phase_merge(c, p)
        if "peer" in phases:
            phase_peer(c, p)
        p.finish(block)
    return nc


def make_in_maps(inputs, phases=ALL_PHASES):
    f = lambda a: np.ascontiguousarray(np.asarray(a, dtype=np.float32))
    rel_bias = f(inputs["rel_bias"])
    d01, cm, notown, esel = moba_consts(rel_bias)
    shared = {
        "norm1_gain": f(inputs["norm1_gain"]), "w_in": f(inputs["w_in"][0]),
        "ident": np.eye(128, dtype=np.float32), "ones": np.ones((128, 128), np.float32),
        "rel_bias": rel_bias, "q_norm_gain": f(inputs["q_norm_gain"]), "k_norm_gain": f(inputs["k_norm_gain"]),
        "d01": d01, "cm": cm, "notown": notown, "esel": esel, "gdnc": gdn_consts(),
        "conv_wT": f(np.asarray(inputs["conv_w"][0]).T), "a_log": f(inputs["a_log"]), "dt_bias": f(inputs["dt_bias"]),
        "gdn_norm_gain": f(inputs["gdn_norm_gain"]), "w_up_a": f(inputs["w_up_a"][0]), "w_up_b": f(inputs["w_up_b"][0]),
        "w_out": f(inputs["w_out"][0]),
    }
    if "peer" in phases:
        sk = np.asarray(inputs["peer_sub_keys"][0], dtype=np.float32).reshape(16, 128, 128)
        shared.update({
            "norm2_gain": f(inputs["norm2_gain"]), "w_query": f(inputs["peer_w_query"][0]),
            "skT": f(sk.transpose(2, 0, 1)), "peer_u": f(inputs["peer_u"][0]), "peer_v": f(inputs["peer_v"][0]),
            "iota": np.broadcast_to(np.arange(256, dtype=np.float32), (128, 256)).copy(),
        })
    maps = []
    for b in range(8):
        m = dict(shared)
        m["x"] = f(inputs["x"][b])
        maps.append(m)
    return maps


_NC_CACHE = {}


def kernel(**inputs):
    if "nc" not in _NC_CACHE:
        _NC_CACHE["nc"] = build_nc()
    nc = _NC_CACHE["nc"]
    maps = make_in_maps(inputs)
    res = run_bass_kernel_spmd(nc, maps, core_ids=list(range(8)))
    return np.stack([np.asarray(r["y"], dtype=np.float32) for r in res.results], axis=0)
```

```python
import math
from contextlib import ExitStack

import numpy as np
import concourse.bass as bass
import concourse.mybir as mybir
from concourse.bass_utils import run_bass_kernel_spmd

F32 = mybir.dt.float32
F32R = mybir.dt.float32r
BF16 = mybir.dt.bfloat16
U32 = mybir.dt.uint32
I32 = mybir.dt.int32
AF = mybir.ActivationFunctionType
ALU = mybir.AluOpType
AX = mybir.AxisListType

D = 2048
S = 2048
NT = S // 128
IN_TOTAL = 11280
EPS = 1e-6
NEG = -30000.0

COMPUTE = ("tensor", "vector", "scalar", "gpsimd")
ALLENG = ("tensor", "vector", "scalar", "gpsimd", "sync")


import re as _re
_PSUM_KEY = _re.compile(r"^(n\d_tp|gp|aux|scp|op|dp|pp|pa|pb|po|pq)\d+$")


class Op:
    __slots__ = ("eng", "fn", "deps", "needed", "is_dma", "slot", "use", "tok", "idx")


class Prog:
    def __init__(self, nc, es, ndma=8):
        self.nc = nc
        self.ops = {e: [] for e in ALLENG}
        self.last_w = {}
        self.rd_comp = {}
        self.rd_dma = {}
        self.sem = {e: es.enter_context(nc.semaphore("s_" + e)) for e in COMPUTE}
        self.dsem = {}
        self.dlast = {}
        self.dnext = {}
        self.duse = {}
        for q in ("sync", "gpsimd", "scalar"):
            self.dsem[q] = [es.enter_context(nc.semaphore("d_%s%d" % (q, i))) for i in range(ndma)]
            self.dlast[q] = [None] * ndma
            self.duse[q] = [0] * ndma
            self.dnext[q] = 0
        self.pending_barrier = {e: None for e in ALLENG}
        self.all_dma = []

    def _add(self, eng, fn, r, w, is_dma):
        o = Op()
        o.eng = eng
        o.fn = fn
        o.needed = False
        o.is_dma = is_dma
        o.slot = None
        o.use = 0
        deps = set()
        for k in r:
            lw = self.last_w.get(k)
            if lw is not None:
                deps.add(lw)
            if isinstance(k, str) and _PSUM_KEY.match(k):
                for en, ro in self.rd_comp.get(k, {}).items():
                    if en != eng:
                        deps.add(ro)
        for k in w:
            lw = self.last_w.get(k)
            if lw is not None:
                deps.add(lw)
            for ro in self.rd_comp.get(k, {}).values():
                deps.add(ro)
            for ro in self.rd_dma.get(k, ()):
                deps.add(ro)
        if is_dma:
            q = eng
            sl = self.dnext[q]
            self.dnext[q] = (sl + 1) % len(self.dsem[q])
            prev = self.dlast[q][sl]
            if prev is not None:
                deps.add(prev)
            self.duse[q][sl] += 1
            o.slot = sl
            o.use = self.duse[q][sl]
            self.dlast[q][sl] = o
            self.all_dma.append(o)
        pb = self.pending_barrier[eng]
        if pb is not None:
            deps |= pb
            self.pending_barrier[eng] = None
        if eng == "tensor":
            deps = {d for d in deps if not (d.eng == "tensor" and not d.is_dma)}
        o.deps = deps
        for d in deps:
            d.needed = True
        for k in w:
            self.last_w[k] = o
            self.rd_comp[k] = {}
            self.rd_dma[k] = []
        for k in r:
            if is_dma:
                self.rd_dma.setdefault(k, []).append(o)
            else:
                self.rd_comp.setdefault(k, {})[eng] = o
        o.idx = len(self.ops[eng])
        self.ops[eng].append(o)
        return o

    def op(self, eng, fn, r=(), w=()):
        return self._add(eng, fn, tuple(r), tuple(w), False)

    def dma(self, q, out, in_, r=(), w=(), **kw):
        return self._add(q, lambda e: e.dma_start(out=out, in_=in_, **kw), tuple(r), tuple(w), True)

    def dma_fn(self, q, fn, r=(), w=()):
        return self._add(q, fn, tuple(r), tuple(w), True)

    def barrier(self):
        deps = set()
        for e in ALLENG:
            if self.ops[e]:
                last = [o for o in self.ops[e] if not o.is_dma]
                if last:
                    deps.add(last[-1])
        for o in self.all_dma:
            deps.add(o)
        self.all_dma = []
        for e in ALLENG:
            pb = self.pending_barrier[e]
            self.pending_barrier[e] = set(deps) | (pb or set())
        self.last_w = {}
        self.rd_comp = {}
        self.rd_dma = {}

    def finish(self, block):
        self.barrier()
        nc = self.nc
        for e in ALLENG:
            self._add(e, None, (), (), False)
        for e in COMPUTE:
            c = 0
            for o in self.ops[e]:
                if o.is_dma:
                    o.tok = (self.dsem[e][o.slot], 16 * o.use)
                elif o.needed:
                    c += 1
                    o.tok = (self.sem[e], c)
                else:
                    o.tok = None
        for o in self.ops["sync"]:
            if o.is_dma:
                o.tok = (self.dsem["sync"][o.slot], 16 * o.use)
            else:
                o.tok = None

        def emit(engname):
            def body(eng):
                waited = {}
                for o in self.ops[engname]:
                    for d in o.deps:
                        sem, val = d.tok
                        key = id(sem)
                        if waited.get(key, 0) < val:
                            eng.wait_ge(sem, val)
                            waited[key] = val
                    if o.fn is None:
                        continue
                    ins = o.fn(eng)
                    if o.is_dma:
                        ins.then_inc(o.tok[0], 16)
                    elif o.needed:
                        ins.then_inc(o.tok[0], 1)
            return body

        block.tensor(emit("tensor"))
        block.vector(emit("vector"))
        block.scalar(emit("scalar"))
        block.gpsimd(emit("gpsimd"))
        block.sync(emit("sync"))


def r32(ap):
    return ap.bitcast(F32R)


class Ctx:
    pass


_uid = [0]


def _un(name):
    _uid[0] += 1
    return "%s_%d" % (name, _uid[0])


def sb(c, name, shape, dt=F32):
    return c.es.enter_context(c.nc.sbuf_tensor(_un(name), list(shape), dt))


def ps(c, name, shape, dt=F32):
    return c.es.enter_context(c.nc.psum_tensor(_un(name), list(shape), dt))


FM_SRC = [(0, 1024), (1024, 1024), (3072, 3072), (7184, 2048), (9232, 2048)]
TM_SRC = [(2048, 1024), (6144, 1024), (7168, 16)]
N_FM = 9216
N_TM = 2064


def phase_norm_T(c, p, x_ap, gain_ap, hT, pfx, half, h_out=None):
    nc = c.nc
    with ExitStack() as es:
        c2 = Ctx(); c2.nc = nc; c2.es = es
        gbc = sb(c2, pfx + "gbc", [128, D])
        xt = [sb(c2, pfx + "xt%d" % i, [128, D]) for i in range(2)]
        ht = [sb(c2, pfx + "ht%d" % i, [128, D]) for i in range(2)]
        sq = sb(c2, pfx + "sq", [128, D])
        st = [sb(c2, pfx + "st%d" % i, [128, 4]) for i in range(2)]
        tp = [ps(c2, pfx + "tp%d" % i, [128, 512]) for i in range(4)]
        p.dma("sync", gbc[:], gain_ap.partition_broadcast(128), w=[pfx + "gbc"])
        for ti in range(8):
            t = half * 8 + ti
            b = ti % 2
            p.dma("sync", xt[b][:], x_ap[t * 128:(t + 1) * 128, :], w=[pfx + "xt%d" % b])
            p.op("scalar", lambda e, b=b: e.activation(out=sq[:], in_=xt[b][:], func=AF.Square,
                                                       accum_out=st[b][:, 0:1]),
                 r=[pfx + "xt%d" % b], w=[pfx + "sq", pfx + "st%d" % b])
            p.op("scalar", lambda e, b=b: e.activation(out=st[b][:, 1:2], in_=st[b][:, 0:1], func=AF.Sqrt,
                                                       scale=1.0 / D, bias=c.eps_t[:, 0:1]),
                 r=[pfx + "st%d" % b], w=[pfx + "st%d" % b])
            p.op("vector", lambda e, b=b: e.reciprocal(out=st[b][:, 2:3], in_=st[b][:, 1:2]),
                 r=[pfx + "st%d" % b], w=[pfx + "st%d" % b])
            p.op("vector", lambda e, b=b: e.scalar_tensor_tensor(out=ht[b][:], in0=xt[b][:], scalar=st[b][:, 2:3],
                                                                 in1=gbc[:], op0=ALU.mult, op1=ALU.mult),
                 r=[pfx + "xt%d" % b, pfx + "st%d" % b, pfx + "gbc"], w=[pfx + "ht%d" % b])
            if h_out is not None:
                p.dma("sync", h_out[t * 128:(t + 1) * 128, :], ht[b][:], r=[pfx + "ht%d" % b], w=[(pfx + "hout", t)])
            for g4 in range(4):
                pb = tp[g4]
                for j in range(4):
                    kc = g4 * 4 + j
                    p.op("tensor", lambda e, b=b, kc=kc, j=j, pb=pb: e.transpose(
                        out=pb[:, j * 128:(j + 1) * 128], in_=ht[b][:, kc * 128:(kc + 1) * 128], identity=c.ident[:]),
                        r=[pfx + "ht%d" % b], w=[pfx + "tp%d" % g4])
                eng = "scalar" if g4 % 2 == 0 else "vector"
                dst = hT[:, g4 * 4:(g4 + 1) * 4, ti * 128:(ti + 1) * 128]
                src = pb[:].rearrange("p (j t) -> p j t", j=4)
                if eng == "scalar":
                    p.op("scalar", lambda e, dst=dst, src=src: e.copy(out=dst, in_=src),
                         r=[pfx + "tp%d" % g4], w=["hT"])
                else:
                    p.op("vector", lambda e, dst=dst, src=src: e.tensor_copy(out=dst, in_=src),
                         r=[pfx + "tp%d" % g4], w=["hT"])
        p.barrier()


def phase_inproj(c, p):
    nc = c.nc
    with ExitStack() as es0:
        gen = None
        if getattr(c, "uvb", None) is not None:
            c0 = Ctx(); c0.nc = nc; c0.es = es0
            cin = [sb(c0, "cin%d" % i, [128, 4096]) for i in range(2)]
            cout = [sb(c0, "cout%d" % i, [128, 4096], BF16) for i in range(2)]
            gen = cvt_gen(c, p, cin, cout, 2)
            c.cvt_done = True
        _phase_inproj(c, p, gen)


def _pump(gen, n):
    if gen is None:
        return
    for _ in range(n):
        if next(gen, "end") == "end":
            return


def _phase_inproj(c, p, gen):
    nc = c.nc
    for half in range(2):
        with ExitStack() as es:
            c2 = Ctx(); c2.nc = nc; c2.es = es
            hT = sb(c2, "hT", [128, 16, 1024], F32R)
            phase_norm_T(c, p, c.x, c.norm1_gain, hT, "n1_", half)
            with ExitStack() as es2:
                c3 = Ctx(); c3.nc = nc; c3.es = es2
                wb = [sb(c3, "wb%d" % i, [128, 16, 256], F32R) for i in range(2)]
                ob = [sb(c3, "ob%d" % i, [128, 1024]) for i in range(2)]
                pp = [ps(c3, "pp%d" % i, [128, 512]) for i in range(4)]
                gi = 0
                oi = 0
                pi = 0
                t0 = half * 1024
                row = 0
                for (c0, n) in FM_SRC:
                    for g in range(n // 256):
                        col = c0 + g * 256
                        b = gi % 2
                        gi += 1
                        p.dma("sync", wb[b][:], r32(c.w_in[:, col:col + 256]).rearrange("(kc p) n -> p kc n", p=128),
                              w=["wb%d" % b])
                        _pump(gen, 2)
                        for cc in range(2):
                            o = oi % 2
                            oi += 1
                            for tg in range(2):
                                pb = pi % 4
                                pi += 1
                                for kc in range(16):
                                    p.op("tensor", lambda e, b=b, cc=cc, tg=tg, kc=kc, pb=pb: e.matmul(
                                        pp[pb][:], lhsT=wb[b][:, kc, cc * 128:(cc + 1) * 128],
                                        rhs=hT[:, kc, tg * 512:(tg + 1) * 512],
                                        start=(kc == 0), stop=(kc == 15)),
                                        r=["wb%d" % b, "hT"], w=["pp%d" % pb])
                                if tg == 0:
                                    p.op("scalar", lambda e, o=o, pb=pb: e.copy(out=ob[o][:, 0:512], in_=pp[pb][:]),
                                         r=["pp%d" % pb], w=["ob%d" % o])
                                else:
                                    p.op("vector", lambda e, o=o, pb=pb: e.tensor_copy(out=ob[o][:, 512:1024], in_=pp[pb][:]),
                                         r=["pp%d" % pb], w=["ob%d" % o])
                            rr = row + g * 256 + cc * 128
                            p.dma("sync", c.sc_fm[rr:rr + 128, t0:t0 + 1024], ob[o][:],
                                  r=["ob%d" % o], w=[("sc_fm", rr // 128)])
                    row += n
                colo = 0
                for (c0, n) in TM_SRC:
                    w = min(n, 256)
                    for g in range(max(1, n // 256)):
                        col = c0 + g * 256
                        b = gi % 2
                        gi += 1
                        p.dma("sync", wb[b][:, :, 0:w], r32(c.w_in[:, col:col + w]).rearrange("(kc p) n -> p kc n", p=128),
                              w=["wb%d" % b])
                        _pump(gen, 2)
                        for tq in range(2):
                            o = oi % 2
                            oi += 1
                            for tt in range(4):
                                tl = tq * 4 + tt
                                pb = pi % 4
                                pi += 1
                                for kc in range(16):
                                    p.op("tensor", lambda e, b=b, tl=tl, kc=kc, pb=pb, w=w: e.matmul(
                                        pp[pb][:, 0:w], lhsT=hT[:, kc, tl * 128:(tl + 1) * 128],
                                        rhs=wb[b][:, kc, 0:w],
                                        start=(kc == 0), stop=(kc == 15)),
                                        r=["wb%d" % b, "hT"], w=["pp%d" % pb])
                                if tt % 2 == 0:
                                    p.op("scalar", lambda e, o=o, pb=pb, tt=tt, w=w: e.copy(
                                        out=ob[o][:, tt * 256:tt * 256 + w], in_=pp[pb][:, 0:w]),
                                        r=["pp%d" % pb], w=["ob%d" % o])
                                else:
                                    p.op("vector", lambda e, o=o, pb=pb, tt=tt, w=w: e.tensor_copy(
                                        out=ob[o][:, tt * 256:tt * 256 + w], in_=pp[pb][:, 0:w]),
                                        r=["pp%d" % pb], w=["ob%d" % o])
                            tb = t0 + tq * 512
                            cw = colo + g * 256
                            p.dma("sync",
                                  c.sc_tm[tb:tb + 512, cw:cw + w].rearrange("(tt p) n -> p tt n", p=128),
                                  ob[o][:].rearrange("p (tt n) -> p tt n", n=256)[:, :, 0:w],
                                  r=["ob%d" % o], w=[("sc_tm", tb // 128, cw)])
                    colo += n
                p.barrier()
    if gen is not None:
        _pump(gen, 1000)
        p.barrier()


def t5_bucket_np(n):
    n = np.maximum(n, 0)
    nf = np.maximum(n, 1).astype(np.float32)
    large = 16 + (np.log(nf / np.float32(16)) / np.float32(math.log(8.0)) * np.float32(16)).astype(np.int32)
    large = np.minimum(large, 31)
    return np.where(n < 16, n, large)


def moba_consts(rel_bias):
    k = np.arange(128)[:, None]
    q = np.arange(128)[None, :]
    out = np.zeros((8, 2, 128, 128), np.float32)
    b0 = t5_bucket_np(q - k)
    b1 = t5_bucket_np(q - k + 128)
    for h in range(8):
        out[h, 0] = np.where(q >= k, rel_bias[b0, h], np.float32(NEG))
        out[h, 1] = rel_bias[b1, h]
    cm = np.zeros((128, 16, 8), np.float32)
    notown = np.ones((128, 16, 8), np.float32)
    for t in range(16):
        for n in range(8):
            if n >= t // 2:
                cm[:, t, n] = -1e30
            if n == t // 2:
                notown[:, t, n] = 0.0
    esel = np.zeros((8, 8, 128), np.float32)
    for n in range(8):
        esel[n, n, :] = 1.0
    return out, cm, notown, esel


def phase_moba(c, p):
    nc = c.nc
    with ExitStack() as es:
        c2 = Ctx(); c2.nc = nc; c2.es = es
        cm = sb(c2, "cm", [128, 16, 8])
        notown = sb(c2, "notown", [128, 16, 8])
        esel = sb(c2, "esel", [8, 8, 128], F32R)
        cb = sb(c2, "cb", [128, 8])
        gq = sb(c2, "gq", [128, 2])
        gk = sb(c2, "gk", [128, 1])
        d01 = sb(c2, "d01", [128, 2, 128], F32R)
        d01r = sb(c2, "d01r", [128, 2, 128])
        qr = sb(c2, "qr", [128, S])
        kr = sb(c2, "kr", [128, S])
        sq = sb(c2, "sq", [128, S], F32R)
        qn = sb(c2, "qn", [128, S], F32R)
        kn = sb(c2, "kn", [128, S], F32R)
        vt = sb(c2, "vt", [128, 16, 128], F32R)
        rsb = [sb(c2, "rs%d" % i, [128, 512]) for i in range(2)]
        km = sb(c2, "km", [128, 8], F32R)
        kmf = sb(c2, "kmf", [128, 8])
        gm = sb(c2, "gm", [128, 16, 8])
        cmp_ = sb(c2, "cmp", [128, 16, 8, 8])
        rank = sb(c2, "rank", [128, 16, 8])
        nmk = sb(c2, "nmk", [128, 16, 8])
        negT = sb(c2, "negT", [8, S], F32R)
        pT = [sb(c2, "pT%d" % i, [128, 512], F32R) for i in range(2)]
        rden = sb(c2, "rden", [128, 512])
        oo = [sb(c2, "oo%d" % i, [128, 512]) for i in range(2)]
        aux = [ps(c2, "aux%d" % i, [128, 512]) for i in range(2)]
        scp = [ps(c2, "scp%d" % i, [128, 512]) for i in range(2)]
        op_ = [ps(c2, "op%d" % i, [128, 512]) for i in range(2)]
        dp = [ps(c2, "dp%d" % i, [128, 512]) for i in range(2)]

        p.dma("sync", cm[:], c.cm_d[:, :, :], w=["cm"])
        p.dma("sync", notown[:], c.notown_d[:, :, :], w=["notown"])
        p.dma("sync", esel[:], r32(c.esel_d[:, :, :]), w=["esel"])
        p.dma("sync", cb[:], c.rel_bias[31:32, :].partition_broadcast(128), w=["cb"])
        p.dma("sync", gq[:, 0:1], c.q_norm_gain.rearrange("o d -> d o"), w=["gq"])
        p.dma("sync", gk[:, 0:1], c.k_norm_gain.rearrange("o d -> d o"), w=["gk"])
        p.op("vector", lambda e: e.tensor_scalar(out=gq[:, 1:2], in0=gq[:, 0:1], scalar1=128.0 ** -0.5, scalar2=None,
                                                 op0=ALU.mult), r=["gq"], w=["gq"])
        auxi = 0
        sci = 0
        gi = 0
        for h in range(8):
            p.dma("sync", qr[:], c.sc_fm[h * 128:(h + 1) * 128, :], w=["qr"])
            p.dma("sync", kr[:], c.sc_fm[1024 + h * 128:1024 + (h + 1) * 128, :], w=["kr"])
            p.dma("sync", vt[:], r32(c.sc_tm[:, h * 128:(h + 1) * 128]).rearrange("(t p) d -> p t d", p=128), w=["vt"])
            p.dma("sync", d01r[:], c.d01_d[h].rearrange("a k q -> k a q"), w=["d01r"])
            p.op("vector", lambda e, h=h: e.tensor_scalar(out=d01[:], in0=d01r[:], scalar1=cb[:, h:h + 1], scalar2=None,
                                                          op0=ALU.subtract), r=["d01r", "cb"], w=["d01"])
            for (raw, dst, gcol, rk, wk) in ((qr, qn, gq[:, 1:2], "qr", "qn"), (kr, kn, gk[:, 0:1], "kr", "kn")):
                p.op("scalar", lambda e, raw=raw: e.activation(out=sq[:], in_=raw[:], func=AF.Square), r=[rk], w=["sq"])
                for tg in range(4):
                    a = auxi % 2
                    auxi += 1
                    sl = slice(tg * 512, (tg + 1) * 512)
                    p.op("tensor", lambda e, a=a, sl=sl: e.matmul(aux[a][:], lhsT=c.ones_r[:], rhs=sq[:, sl], start=True, stop=True),
                         r=["sq"], w=["aux%d" % a])
                    rs = rsb[a]
                    p.op("scalar", lambda e, a=a, rs=rs: e.activation(out=rs[:], in_=aux[a][:], func=AF.Sqrt, scale=1.0 / 128,
                                                                      bias=c.eps_t[:, 0:1]), r=["aux%d" % a], w=["rs%d" % a])
                    p.op("vector", lambda e, rs=rs: e.reciprocal(out=rs[:], in_=rs[:]), r=["rs%d" % a], w=["rs%d" % a])
                    p.op("vector", lambda e, raw=raw, dst=dst, gcol=gcol, sl=sl, rs=rs: e.scalar_tensor_tensor(
                        out=dst[:, sl], in0=raw[:, sl], scalar=gcol, in1=rs[:], op0=ALU.mult, op1=ALU.mult),
                        r=[rk, "rs%d" % a, "gq", "gk"], w=[wk])
            p.op("vector", lambda e: e.tensor_reduce(out=kmf[:], in_=kn[:].bitcast(F32).rearrange("p (n j) -> p n j", j=256),
                                                     axis=AX.X, op=ALU.add), r=["kn"], w=["kmf"])
            p.op("vector", lambda e: e.tensor_copy(out=km[:], in_=kmf[:]), r=["kmf"], w=["km"])
            a = auxi % 2
            auxi += 1
            for t in range(16):
                p.op("tensor", lambda e, a=a, t=t: e.matmul(aux[a][:, t * 8:(t + 1) * 8], lhsT=qn[:, t * 128:(t + 1) * 128],
                                                            rhs=km[:], start=True, stop=True),
                     r=["qn", "km"], w=["aux%d" % a])
            p.op("vector", lambda e, a=a: e.tensor_tensor(out=gm[:], in0=aux[a][:, 0:128].rearrange("p (t n) -> p t n", n=8),
                                                          in1=cm[:], op=ALU.add), r=["aux%d" % a, "cm"], w=["gm"])
            p.op("vector", lambda e: e.tensor_tensor(out=cmp_[:], in0=gm[:].unsqueeze(2).to_broadcast([128, 16, 8, 8]),
                                                     in1=gm[:].unsqueeze(3).to_broadcast([128, 16, 8, 8]), op=ALU.is_gt),
                 r=["gm"], w=["cmp"])
            p.op("vector", lambda e: e.tensor_reduce(out=rank[:], in_=cmp_[:], axis=AX.X, op=ALU.add), r=["cmp"], w=["rank"])
            p.op("vector", lambda e: e.tensor_scalar(out=rank[:], in0=rank[:], scalar1=3.0, scalar2=NEG, op0=ALU.is_ge,
                                                     op1=ALU.mult), r=["rank"], w=["rank"])
            p.op("vector", lambda e: e.tensor_tensor(out=nmk[:], in0=rank[:], in1=notown[:], op=ALU.mult),
                 r=["rank", "notown"], w=["nmk"])
            for tg in range(4):
                a = auxi % 2
                auxi += 1
                for j in range(4):
                    t = tg * 4 + j
                    p.op("tensor", lambda e, a=a, t=t, j=j: e.transpose(out=aux[a][0:8, j * 128:(j + 1) * 128], in_=nmk[:, t, :],
                                                                        identity=c.ident[:]),
                         r=["nmk"], w=["aux%d" % a])
                p.op("scalar", lambda e, a=a, tg=tg: e.copy(out=negT[:, tg * 512:(tg + 1) * 512], in_=aux[a][0:8, :]),
                     r=["aux%d" % a], w=["negT"])
            steps = []
            for g in range(4):
                gb = gi % 2
                gi += 1
                nk = 4 * g + 4
                for kt in range(nk):
                    s_ = sci % 2
                    sci += 1
                    jj0 = max(0, kt - 4 * g)
                    c0 = jj0 * 128
                    extra = []
                    if kt >= 4 * g:
                        extra.append((jj0, 0))
                        if jj0 + 1 < 4:
                            extra.append((jj0 + 1, 1))
                    elif kt == 4 * g - 1:
                        extra.append((0, 1))
                    steps.append(dict(g=g, gb=gb, nk=nk, kt=kt, s_=s_, qs=slice(g * 512 + c0, (g + 1) * 512), cs=slice(c0, 512),
                                      n=kt // 2, extra=extra))

            def emit_score(st, h=h):
                s_, kt, qs, cs, n, extra = st["s_"], st["kt"], st["qs"], st["cs"], st["n"], st["extra"]
                p.op("tensor", lambda e: e.matmul(scp[s_][:, cs], lhsT=kn[:, kt * 128:(kt + 1) * 128], rhs=qn[:, qs],
                                                  start=True, stop=False), r=["kn", "qn"], w=["scp%d" % s_])
                p.op("tensor", lambda e: e.matmul(scp[s_][:, cs], lhsT=esel[:, n, :], rhs=negT[:, qs], start=False, stop=(not extra)),
                     r=["esel", "negT"], w=["scp%d" % s_])
                for ei, (jj, which) in enumerate(extra):
                    p.op("tensor", lambda e, jj=jj, which=which, last=(ei == len(extra) - 1): e.matmul(
                        scp[s_][:, jj * 128:(jj + 1) * 128], lhsT=c.ident_r[:], rhs=d01[:, which, :], start=False, stop=last),
                        r=["d01"], w=["scp%d" % s_])
                p.op("scalar", lambda e: e.activation(out=pT[s_][:, cs], in_=scp[s_][:, cs], func=AF.Exp, bias=cb[:, h:h + 1]),
                     r=["scp%d" % s_, "cb"], w=["pT%d" % s_])

            def emit_pv(st, h=h):
                s_, kt, cs, gb, nk, g = st["s_"], st["kt"], st["cs"], st["gb"], st["nk"], st["g"]
                p.op("tensor", lambda e: e.matmul(op_[gb][:, cs], lhsT=vt[:, kt, :], rhs=pT[s_][:, cs], start=(kt == 0), stop=(kt == nk - 1)),
                     r=["vt", "pT%d" % s_], w=["op%d" % gb])
                p.op("tensor", lambda e: e.matmul(dp[gb][:, cs], lhsT=c.ones_r[:], rhs=pT[s_][:, cs], start=(kt == 0), stop=(kt == nk - 1)),
                     r=["pT%d" % s_], w=["dp%d" % gb])
                if kt == nk - 1:
                    p.op("vector", lambda e: e.reciprocal(out=rden[:], in_=dp[gb][:]), r=["dp%d" % gb], w=["rden"])
                    p.op("vector", lambda e: e.tensor_tensor(out=oo[gb][:], in0=op_[gb][:], in1=rden[:], op=ALU.mult),
                         r=["op%d" % gb, "rden"], w=["oo%d" % gb])
                    p.dma("sync", c.sc_oa[h * 128:(h + 1) * 128, g * 512:(g + 1) * 512], oo[gb][:],
                          r=["oo%d" % gb], w=[("sc_oa", h, g)])

            emit_score(steps[0])
            for i_, st in enumerate(steps):
                if i_ + 1 < len(steps):
                    emit_score(steps[i_ + 1])
                emit_pv(st)
        p.barrier()


def gdn_consts():
    k = np.arange(128)[:, None]
    i = np.arange(128)[None, :]
    same = (k // 64) == (i // 64)
    tri = ((k <= i) & same).astype(np.float32)
    blk = same.astype(np.float32)
    half0 = np.broadcast_to((k < 64), (128, 128)).astype(np.float32)
    half1 = np.broadcast_to((k >= 64), (128, 128)).astype(np.float32)
    ustr = ((k > i) & same).astype(np.float32)
    ii = np.arange(128)[:, None]
    jj = np.arange(128)[None, :]
    same2 = (ii // 64) == (jj // 64)
    negm_strict = np.where((ii > jj) & same2, 0.0, NEG).astype(np.float32)
    negm_inclT = np.where((jj >= ii) & same2, 0.0, NEG).astype(np.float32)
    return np.stack([tri, blk, half0, half1, ustr, negm_strict, negm_inclT], 0)


class _Stop(Exception):
    pass


def _chk(n):
    import os
    return int(os.environ.get("GDN_STOP", "99")) == n


def phase_gdn(c, p):
    _phase_gdn(c, p)
    p.barrier()


def _phase_gdn(c, p):
    nc = c.nc
    with ExitStack() as es:
        c2 = Ctx(); c2.nc = nc; c2.es = es
        G = sb(c2, "gc", [128, 7, 128])
        TRI, BLK, H0, H1, USTR, NMS, NMIT = [G[:, i, :] for i in range(7)]
        ba = sb(c2, "ba", [128, 16, 16])
        dtb = sb(c2, "dtb", [128, 8])
        Aex = sb(c2, "Aex", [128, 8])
        gng = sb(c2, "gng", [128, 128])
        beta = sb(c2, "beta", [128, 16, 8])
        nbeta = sb(c2, "nbeta", [128, 16, 8])
        gg = sb(c2, "gg", [128, 16, 8])
        egs = sb(c2, "egs", [128, 16, 8])
        kds = sb(c2, "kds", [128, 16, 8])
        kbs = sb(c2, "kbs", [128, 16, 8])
        egl = sb(c2, "egl", [128, 2, 16, 8])
        tmp8 = sb(c2, "tmp8", [128, 16, 8])
        cw = sb(c2, "cw", [128, 3, 4])
        xp = [sb(c2, "xp%d" % i, [128, 3 + S]) for i in range(3)]
        cv = [sb(c2, "cv%d" % i, [128, S]) for i in range(3)]
        sq = sb(c2, "gsq", [128, S], F32R)
        rs = sb(c2, "grs", [128, 512])
        NGB = 4
        kbg = [sb(c2, "kbg%d" % j, [128, 128]) for j in range(NGB)]
        vbe = [sb(c2, "vbe%d" % j, [128, 128]) for j in range(NGB)]
        Ag = [sb(c2, "Ag%d" % j, [128, 128]) for j in range(NGB)]
        Dec = [sb(c2, "Dec%d" % j, [128, 128]) for j in range(NGB)]
        DecT = [sb(c2, "DecT%d" % j, [128, 128]) for j in range(NGB)]
        Am = [[sb(c2, "Am%d_%d" % (j, i), [128, 128]) for i in range(2)] for j in range(NGB)]
        At = [[sb(c2, "At%d_%d" % (j, i), [128, 128]) for i in range(2)] for j in range(NGB)]
        Rt = [[sb(c2, "Rt%d_%d" % (j, i), [128, 128]) for i in range(2)] for j in range(NGB)]
        u_all = sb(c2, "u_all", [128, 16, 128])
        wT_all = sb(c2, "wT_all", [128, 16, 128])
        aT_all = sb(c2, "aT_all", [128, 16, 128])
        kd_all = sb(c2, "kd_all", [128, 2, 16, 128])
        kds2 = sb(c2, "kds2", [128, 2, 16, 8])
        St = sb(c2, "St", [128, 128])
        vnew = sb(c2, "vnew", [128, 128])
        otmp = sb(c2, "otmp", [128, 128])
        ob = sb(c2, "ob", [128, 16, 128])
        zt = sb(c2, "zt", [128, 16, 128])
        ssq = sb(c2, "ssq", [128, 16])
        junk = sb(c2, "junk", [128, 128])
        obT = sb(c2, "obT", [128, S])
        pool = [ps(c2, "gp%d" % i, [128, 512]) for i in range(8)]
        pc = [0]

        def nxt():
            i = pc[0] % 8
            pc[0] += 1
            return i

        p.dma("sync", G[:], c.gdnc_d.rearrange("a k i -> k a i"), w=["G"])
        p.dma("sync", ba[:], c.sc_tm[:, 2048:2064].rearrange("(t p) n -> p t n", p=128), w=["ba"])
        p.dma("sync", dtb[:], c.dt_bias.partition_broadcast(128), w=["dtb"])
        p.dma("sync", Aex[:], c.a_log.partition_broadcast(128), w=["Aex"])
        p.dma("sync", gng[:], c.gdn_norm_gain.partition_broadcast(128), w=["gng"])
        p.op("scalar", lambda e: e.activation(out=Aex[:], in_=Aex[:], func=AF.Exp), r=["Aex"], w=["Aex"])
        p.op("scalar", lambda e: e.activation(out=beta[:], in_=ba[:, :, 0:8], func=AF.Exp, scale=-1.0), r=["ba"], w=["beta"])
        p.op("vector", lambda e: e.tensor_scalar(out=beta[:], in0=beta[:], scalar1=1.0, scalar2=None, op0=ALU.add), r=["beta"], w=["beta"])
        p.op("vector", lambda e: e.reciprocal(out=beta[:], in_=beta[:]), r=["beta"], w=["beta"])
        p.op("vector", lambda e: e.tensor_scalar(out=nbeta[:], in0=beta[:], scalar1=-1.0, scalar2=None, op0=ALU.mult), r=["beta"], w=["nbeta"])
        p.op("vector", lambda e: e.tensor_tensor(out=gg[:], in0=ba[:, :, 8:16], in1=dtb[:].unsqueeze(1).to_broadcast([128, 16, 8]),
                                                 op=ALU.add), r=["ba", "dtb"], w=["gg"])
        p.op("scalar", lambda e: e.activation(out=gg[:], in_=gg[:], func=AF.Exp), r=["gg"], w=["gg"])
        p.op("scalar", lambda e: e.activation(out=gg[:], in_=gg[:], func=AF.Ln, bias=c.one_t[:, 0:1]), r=["gg"], w=["gg"])
        p.op("vector", lambda e: e.scalar_tensor_tensor(out=gg[:], in0=gg[:], scalar=-1.0, in1=Aex[:].unsqueeze(1).to_broadcast([128, 16, 8]),
                                                        op0=ALU.mult, op1=ALU.mult), r=["gg", "Aex"], w=["gg"])
        ggf = gg[:].rearrange("p t h -> p (t h)")
        i0 = nxt(); i1 = nxt(); i2 = nxt(); i3 = nxt()
        p.op("tensor", lambda e: e.matmul(pool[i0][:, 0:128], lhsT=TRI, rhs=ggf, start=True, stop=True), r=["G", "gg"], w=["gp%d" % i0])
        p.op("tensor", lambda e: e.matmul(pool[i1][:, 0:128], lhsT=BLK, rhs=ggf, start=True, stop=True), r=["G", "gg"], w=["gp%d" % i1])
        p.op("tensor", lambda e: e.matmul(pool[i2][:, 0:128], lhsT=H0, rhs=ggf, start=True, stop=True), r=["G", "gg"], w=["gp%d" % i2])
        p.op("tensor", lambda e: e.matmul(pool[i3][:, 0:128], lhsT=H1, rhs=ggf, start=True, stop=True), r=["G", "gg"], w=["gp%d" % i3])
        v3 = lambda t_: t_.rearrange("p (t h) -> p t h", h=8)
        p.op("scalar", lambda e: e.activation(out=egs[:], in_=v3(pool[i0][:, 0:128]), func=AF.Exp), r=["gp%d" % i0], w=["egs"])
        p.op("vector", lambda e: e.tensor_tensor(out=kbs[:], in0=egs[:], in1=beta[:], op=ALU.mult), r=["egs", "beta"], w=["kbs"])
        p.op("vector", lambda e: e.tensor_copy(out=tmp8[:], in_=v3(pool[i0][:, 0:128])), r=["gp%d" % i0], w=["tmp8"])
        p.op("vector", lambda e: e.tensor_tensor(out=tmp8[:], in0=v3(pool[i1][:, 0:128]), in1=tmp8[:], op=ALU.subtract),
             r=["gp%d" % i1, "tmp8"], w=["tmp8"])
        p.op("scalar", lambda e: e.activation(out=kds[:], in_=tmp8[:], func=AF.Exp), r=["tmp8"], w=["kds"])
        p.op("scalar", lambda e: e.activation(out=egl[:, 0], in_=v3(pool[i2][:, 0:128]), func=AF.Exp), r=["gp%d" % i2], w=["egl"])
        p.op("scalar", lambda e: e.activation(out=egl[:, 1], in_=v3(pool[i3][:, 0:128]), func=AF.Exp), r=["gp%d" % i3], w=["egl"])
        p.op("vector", lambda e: e.tensor_scalar(out=egs[:], in0=egs[:], scalar1=128.0 ** -0.5, scalar2=None, op0=ALU.mult),
             r=["egs", "kbs"], w=["egs"])
        p.op("vector", lambda e: e.tensor_scalar(out=kds2[:, 0], in0=kds[:], scalar1=H0[:, 0:1], scalar2=None, op0=ALU.mult),
             r=["kds", "G"], w=["kds2"])
        p.op("vector", lambda e: e.tensor_scalar(out=kds2[:, 1], in0=kds[:], scalar1=H1[:, 0:1], scalar2=None, op0=ALU.mult),
             r=["kds", "G"], w=["kds2"])
        for i in range(3):
            p.op("vector", lambda e, i=i: e.memset(xp[i][:, 0:3], 0.0), w=["xp%d" % i])
        p.op("vector", lambda e: e.memset(vnew[:], 0.0), w=["vnew"])

        if _chk(0):
            return
        for h in range(8):
            for i in range(3):
                row = 2048 + i * 1024 + h * 128
                p.dma("sync", xp[i][:, 3:3 + S], c.sc_fm[row:row + 128, :], w=["xp%d" % i])
                p.dma("sync", cw[:, i, :], c.conv_wT[i * 1024 + h * 128:i * 1024 + (h + 1) * 128, :], w=["cw"])
                p.op("vector", lambda e, i=i: e.tensor_scalar(out=cv[i][:], in0=xp[i][:, 0:S], scalar1=cw[:, i, 0:1], scalar2=None,
                                                              op0=ALU.mult), r=["xp%d" % i, "cw"], w=["cv%d" % i])
                for tap in range(1, 4):
                    p.op("vector", lambda e, i=i, tap=tap: e.scalar_tensor_tensor(
                        out=cv[i][:], in0=xp[i][:, tap:tap + S], scalar=cw[:, i, tap:tap + 1], in1=cv[i][:],
                        op0=ALU.mult, op1=ALU.add), r=["xp%d" % i, "cw", "cv%d" % i], w=["cv%d" % i])
                p.op("scalar", lambda e, i=i: e.activation(out=cv[i][:], in_=cv[i][:], func=AF.Silu), r=["cv%d" % i], w=["cv%d" % i])
                if i < 2:
                    p.op("scalar", lambda e, i=i: e.activation(out=sq[:], in_=cv[i][:], func=AF.Square), r=["cv%d" % i], w=["gsq"])
                    for tg in range(4):
                        a = nxt()
                        sl = slice(tg * 512, (tg + 1) * 512)
                        p.op("tensor", lambda e, a=a, sl=sl: e.matmul(pool[a][:], lhsT=c.ones_r[:], rhs=sq[:, sl], start=True, stop=True),
                             r=["gsq"], w=["gp%d" % a])
                        p.op("scalar", lambda e, a=a: e.activation(out=rs[:], in_=pool[a][:], func=AF.Sqrt, bias=c.eps_t[:, 0:1]),
                             r=["gp%d" % a], w=["grs"])
                        p.op("vector", lambda e: e.reciprocal(out=rs[:], in_=rs[:]), r=["grs"], w=["grs"])
                        p.op("vector", lambda e, i=i, sl=sl: e.tensor_tensor(out=cv[i][:, sl], in0=cv[i][:, sl], in1=rs[:], op=ALU.mult),
                             r=["cv%d" % i, "grs"], w=["cv%d" % i])
            qn, kn, vn = cv
            if _chk(1):
                return
            p.dma("sync", zt[:], c.sc_tm[:, 1024 + h * 128:1024 + (h + 1) * 128].rearrange("(t p) d -> p t d", p=128), w=["zt"])
            NG = 4
            for t0_ in range(0, 16, NG):
                tl = list(range(t0_, t0_ + NG))
                ak = {}; av = {}; akk = {}; agd = {}; aqk = {}; agt = {}; amt = {}
                for j, t in enumerate(tl):
                    ts = slice(t * 128, (t + 1) * 128)
                    ak[j] = nxt()
                    p.op("tensor", lambda e, a=ak[j], ts=ts: e.transpose(out=pool[a][:, 0:128], in_=kn[:, ts], identity=c.ident[:]),
                         r=["cv1"], w=["gp%d" % ak[j]])
                    p.op("vector", lambda e, a=ak[j], t=t, h=h, j=j: e.tensor_scalar(out=kbg[j][:], in0=pool[a][:, 0:128],
                                                                                   scalar1=kbs[:, t, h:h + 1], scalar2=None, op0=ALU.mult),
                         r=["gp%d" % ak[j], "kbs"], w=["kbg%d" % j])
                    for hf_ in range(2):
                        p.op("scalar", lambda e, a=ak[j], t=t, h=h, hf_=hf_: e.activation(out=kd_all[:, hf_, t, :], in_=pool[a][:, 0:128],
                                                                                        func=AF.Identity, scale=kds2[:, hf_, t, h:h + 1]),
                             r=["gp%d" % ak[j], "kds2"], w=["kd_all"])
                    av[j] = nxt()
                    p.op("tensor", lambda e, a=av[j], ts=ts: e.transpose(out=pool[a][:, 0:128], in_=vn[:, ts], identity=c.ident[:]),
                         r=["cv2"], w=["gp%d" % av[j]])
                    p.op("vector", lambda e, a=av[j], t=t, h=h, j=j: e.tensor_scalar(out=vbe[j][:], in0=pool[a][:, 0:128],
                                                                                   scalar1=beta[:, t, h:h + 1], scalar2=None, op0=ALU.mult),
                         r=["gp%d" % av[j], "beta"], w=["vbe%d" % j])
                    p.op("gpsimd", lambda e, t=t, h=h, j=j: e.tensor_scalar(out=Ag[j][:], in0=USTR, scalar1=gg[:, t, h:h + 1], scalar2=None,
                                                                            op0=ALU.mult), r=["G", "gg"], w=["Ag%d" % j])
                for j, t in enumerate(tl):
                    ts = slice(t * 128, (t + 1) * 128)
                    akk[j] = nxt()
                    p.op("tensor", lambda e, a=akk[j], ts=ts: e.matmul(pool[a][:, 0:128], lhsT=kn[:, ts], rhs=kn[:, ts], start=True, stop=True),
                         r=["cv1"], w=["gp%d" % akk[j]])
                    agd[j] = nxt()
                    p.op("tensor", lambda e, a=agd[j], j=j: e.matmul(pool[a][:, 0:128], lhsT=TRI, rhs=Ag[j][:], start=True, stop=False),
                         r=["G", "Ag%d" % j], w=["gp%d" % agd[j]])
                    p.op("tensor", lambda e, a=agd[j]: e.matmul(pool[a][:, 0:128], lhsT=c.ident[:], rhs=NMS, start=False, stop=True),
                         r=["G"], w=["gp%d" % agd[j]])
                    p.op("scalar", lambda e, a=agd[j], j=j: e.activation(out=Dec[j][:], in_=pool[a][:, 0:128], func=AF.Exp),
                         r=["gp%d" % agd[j]], w=["Dec%d" % j])
                    p.op("vector", lambda e, a=akk[j], t=t, h=h, j=j: e.scalar_tensor_tensor(out=Am[j][0][:], in0=pool[a][:, 0:128],
                                                                                           scalar=nbeta[:, t, h:h + 1], in1=Dec[j][:],
                                                                                           op0=ALU.mult, op1=ALU.mult),
                         r=["gp%d" % akk[j], "nbeta", "Dec%d" % j], w=["Am%d_0" % j])
                for j, t in enumerate(tl):
                    ts = slice(t * 128, (t + 1) * 128)
                    aqk[j] = nxt()
                    p.op("tensor", lambda e, a=aqk[j], ts=ts: e.matmul(pool[a][:, 0:128], lhsT=kn[:, ts], rhs=qn[:, ts], start=True, stop=True),
                         r=["cv1", "cv0"], w=["gp%d" % aqk[j]])
                    agt[j] = nxt()
                    p.op("tensor", lambda e, a=agt[j], j=j: e.matmul(pool[a][:, 0:128], lhsT=Ag[j][:], rhs=TRI, start=True, stop=False),
                         r=["G", "Ag%d" % j], w=["gp%d" % agt[j]])
                    p.op("tensor", lambda e, a=agt[j]: e.matmul(pool[a][:, 0:128], lhsT=c.ident[:], rhs=NMIT, start=False, stop=True),
                         r=["G"], w=["gp%d" % agt[j]])
                    p.op("scalar", lambda e, a=agt[j], j=j: e.activation(out=DecT[j][:], in_=pool[a][:, 0:128], func=AF.Exp),
                         r=["gp%d" % agt[j]], w=["DecT%d" % j])
                    p.op("vector", lambda e, a=aqk[j], t=t, j=j: e.scalar_tensor_tensor(out=aT_all[:, t, :], in0=pool[a][:, 0:128],
                                                                                      scalar=128.0 ** -0.5, in1=DecT[j][:],
                                                                                      op0=ALU.mult, op1=ALU.mult),
                         r=["gp%d" % aqk[j], "DecT%d" % j], w=["aT_all"])
                for j, t in enumerate(tl):
                    amt[j] = nxt()
                    p.op("tensor", lambda e, a=amt[j], j=j: e.transpose(out=pool[a][:, 0:128], in_=Am[j][0][:], identity=c.ident[:]),
                         r=["Am%d_0" % j], w=["gp%d" % amt[j]])
                    p.op("scalar", lambda e, a=amt[j], j=j: e.copy(out=At[j][0][:], in_=pool[a][:, 0:128]),
                         r=["gp%d" % amt[j]], w=["At%d_0" % j])
                    p.op("gpsimd", lambda e, j=j: e.tensor_tensor(out=Rt[j][0][:], in0=At[j][0][:], in1=c.ident[:], op=ALU.add),
                         r=["At%d_0" % j], w=["Rt%d_0" % j])
                cur = 0
                for m in range(1, 6):
                    nx = 1 - cur
                    for j, t in enumerate(tl):
                        a1 = nxt()
                        p.op("tensor", lambda e, a=a1, cur=cur, j=j: e.matmul(pool[a][:, 0:128], lhsT=At[j][cur][:], rhs=Am[j][cur][:],
                                                                              start=True, stop=True),
                             r=["At%d_%d" % (j, cur), "Am%d_%d" % (j, cur)], w=["gp%d" % a1])
                        p.op("scalar", lambda e, a=a1, nx=nx, j=j: e.copy(out=Am[j][nx][:], in_=pool[a][:, 0:128]),
                             r=["gp%d" % a1], w=["Am%d_%d" % (j, nx)])
                        if m < 5:
                            a2 = nxt()
                            p.op("tensor", lambda e, a=a2, cur=cur, j=j: e.matmul(pool[a][:, 0:128], lhsT=Am[j][cur][:], rhs=At[j][cur][:],
                                                                                  start=True, stop=True),
                                 r=["At%d_%d" % (j, cur), "Am%d_%d" % (j, cur)], w=["gp%d" % a2])
                            p.op("vector", lambda e, a=a2, nx=nx, j=j: e.tensor_copy(out=At[j][nx][:], in_=pool[a][:, 0:128]),
                                 r=["gp%d" % a2], w=["At%d_%d" % (j, nx)])
                    for j, t in enumerate(tl):
                        a3 = nxt()
                        p.op("tensor", lambda e, a=a3, cur=cur, nx=nx, j=j: e.matmul(pool[a][:, 0:128], lhsT=Am[j][nx][:], rhs=Rt[j][cur][:],
                                                                                     start=True, stop=True),
                             r=["Am%d_%d" % (j, nx), "Rt%d_%d" % (j, cur)], w=["gp%d" % a3])
                        p.op("vector", lambda e, a=a3, cur=cur, nx=nx, j=j: e.tensor_tensor(out=Rt[j][nx][:], in0=pool[a][:, 0:128],
                                                                                            in1=Rt[j][cur][:], op=ALU.add),
                             r=["gp%d" % a3, "Rt%d_%d" % (j, cur)], w=["Rt%d_%d" % (j, nx)])
                    cur = nx
                for j, t in enumerate(tl):
                    RtF = Rt[j][cur]
                    a_u = nxt()
                    p.op("tensor", lambda e, a=a_u, RtF=RtF, j=j: e.matmul(pool[a][:, 0:128], lhsT=RtF[:], rhs=vbe[j][:], start=True, stop=True),
                         r=["Rt%d_%d" % (j, cur), "vbe%d" % j], w=["gp%d" % a_u])
                    p.op("scalar", lambda e, a=a_u, t=t: e.copy(out=u_all[:, t, :], in_=pool[a][:, 0:128]), r=["gp%d" % a_u], w=["u_all"])
                    a_w = nxt()
                    p.op("tensor", lambda e, a=a_w, RtF=RtF, j=j: e.matmul(pool[a][:, 0:128], lhsT=kbg[j][:], rhs=RtF[:], start=True, stop=True),
                         r=["Rt%d_%d" % (j, cur), "kbg%d" % j], w=["gp%d" % a_w])
                    p.op("vector", lambda e, a=a_w, t=t: e.tensor_copy(out=wT_all[:, t, :], in_=pool[a][:, 0:128]),
                         r=["gp%d" % a_w], w=["wT_all"])
            if _chk(3):
                return
            p.op("vector", lambda e: e.memset(St[:], 0.0), w=["St"])
            for ch in range(32):
                t = ch // 2
                hf = ch % 2
                rows = slice(hf * 64, hf * 64 + 64)
                ts = slice(t * 128, (t + 1) * 128)
                a1 = nxt()
                p.op("tensor", lambda e, a=a1, t=t: e.matmul(pool[a][:, 0:128], lhsT=wT_all[:, t, :], rhs=St[:], start=True, stop=True),
                     r=["wT_all", "St"], w=["gp%d" % a1])
                p.op("vector", lambda e, a=a1, t=t, rows=rows: e.tensor_tensor(out=vnew[rows, :], in0=u_all[rows, t, :],
                                                                               in1=pool[a][rows, 0:128], op=ALU.subtract),
                     r=["gp%d" % a1, "u_all"], w=["vnew"])
                aA = nxt()
                p.op("tensor", lambda e, a=aA, ts=ts: e.matmul(pool[a][:, 0:128], lhsT=qn[:, ts], rhs=St[:], start=True, stop=True),
                     r=["cv0", "St"], w=["gp%d" % aA])
                aB = nxt()
                p.op("tensor", lambda e, a=aB, t=t: e.matmul(pool[a][:, 0:128], lhsT=aT_all[:, t, :], rhs=vnew[:], start=True, stop=True),
                     r=["aT_all", "vnew"], w=["gp%d" % aB])
                aS = nxt()
                p.op("tensor", lambda e, a=aS, t=t, hf=hf: e.matmul(pool[a][:, 0:128], lhsT=kd_all[:, hf, t, :], rhs=vnew[:],
                                                                    start=True, stop=True),
                     r=["kd_all", "vnew"], w=["gp%d" % aS])
                p.op("scalar", lambda e, a=aA, t=t, h=h, rows=rows: e.activation(out=otmp[rows, :], in_=pool[a][rows, 0:128], func=AF.Identity,
                                                                                 scale=egs[rows, t, h:h + 1]),
                     r=["gp%d" % aA, "egs"], w=["otmp"])
                p.op("vector", lambda e, a=aB, t=t, rows=rows: e.tensor_tensor(out=ob[rows, t, :], in0=otmp[rows, :], in1=pool[a][rows, 0:128],
                                                                               op=ALU.add),
                     r=["gp%d" % aB, "otmp"], w=["ob"])
                p.op("vector", lambda e, a=aS, t=t, hf=hf, h=h: e.scalar_tensor_tensor(out=St[:], in0=St[:], scalar=egl[:, hf, t, h:h + 1],
                                                                                       in1=pool[a][:, 0:128], op0=ALU.mult, op1=ALU.add),
                     r=["gp%d" % aS, "St", "egl"], w=["St"])
            if _chk(4):
                return
            for t in range(16):
                p.op("scalar", lambda e, t=t: e.activation(out=junk[:], in_=ob[:, t, :], func=AF.Square, accum_out=ssq[:, t:t + 1]),
                     r=["ob"], w=["junk", "ssq"])
            p.op("scalar", lambda e: e.activation(out=ssq[:], in_=ssq[:], func=AF.Sqrt, scale=1.0 / 128, bias=c.eps_t[:, 0:1]),
                 r=["ssq"], w=["ssq"])
            p.op("vector", lambda e: e.reciprocal(out=ssq[:], in_=ssq[:]), r=["ssq"], w=["ssq"])
            p.op("scalar", lambda e: e.activation(out=zt[:], in_=zt[:], func=AF.Silu), r=["zt"], w=["zt"])
            p.op("vector", lambda e: e.tensor_tensor(out=ob[:], in0=ob[:], in1=ssq[:].unsqueeze(2).to_broadcast([128, 16, 128]), op=ALU.mult),
                 r=["ob", "ssq"], w=["ob"])
            p.op("vector", lambda e: e.tensor_tensor(out=ob[:], in0=ob[:], in1=gng[:].unsqueeze(1).to_broadcast([128, 16, 128]), op=ALU.mult),
                 r=["ob", "gng"], w=["ob"])
            p.op("vector", lambda e: e.tensor_tensor(out=ob[:], in0=ob[:], in1=zt[:], op=ALU.mult), r=["ob", "zt"], w=["ob"])
            for t in range(16):
                a = nxt()
                p.op("tensor", lambda e, a=a, t=t: e.transpose(out=pool[a][:, 0:128], in_=ob[:, t, :], identity=c.ident[:]),
                     r=["ob"], w=["gp%d" % a])
                p.op("scalar", lambda e, a=a, t=t: e.copy(out=obT[:, t * 128:(t + 1) * 128], in_=pool[a][:, 0:128]),
                     r=["gp%d" % a], w=["obT"])
            p.dma("sync", c.sc_ob[h * 128:(h + 1) * 128, :], obT[:], r=["obT"], w=[("sc_ob", h)])
        p.barrier()


def phase_merge(c, p):
    nc = c.nc
    with ExitStack() as es:
        c2 = Ctx(); c2.nc = nc; c2.es = es
        oa = sb(c2, "oa", [128, 8, 512], F32R)
        obb = sb(c2, "obb", [128, 8, 512], F32R)
        wa = [sb(c2, "wa%d" % i, [128, 8, 128], F32R) for i in range(2)]
        wbb = [sb(c2, "wbb%d" % i, [128, 8, 128], F32R) for i in range(2)]
        ga = [sb(c2, "ga%d" % i, [128, 512]) for i in range(2)]
        gb_ = [sb(c2, "gb%d" % i, [128, 512]) for i in range(2)]
        m1 = sb(c2, "m1", [128, 512])
        mT = sb(c2, "mT", [128, 16, 512], F32R)
        wo = [sb(c2, "wo%d" % i, [128, 16, 512], F32R) for i in range(2)]
        xt = [sb(c2, "xt%d" % i, [128, 512]) for i in range(2)]
        pa = [ps(c2, "pa%d" % i, [128, 512]) for i in range(2)]
        pb = [ps(c2, "pb%d" % i, [128, 512]) for i in range(2)]
        po = [ps(c2, "po%d" % i, [128, 512]) for i in range(4)]
        ci = 0
        wi = 0
        oi = 0
        for tg in range(4):
            tsl = slice(tg * 512, (tg + 1) * 512)
            p.dma("sync", oa[:], r32(c.sc_oa[:, tsl]).rearrange("(kc p) t -> p kc t", p=128), w=["oa"])
            p.dma("sync", obb[:], r32(c.sc_ob[:, tsl]).rearrange("(kc p) t -> p kc t", p=128), w=["obb"])
            for cc in range(16):
                b = ci % 2
                ci += 1
                csl = slice(cc * 128, (cc + 1) * 128)
                p.dma("sync", wa[b][:], r32(c.w_up_a[:, csl]).rearrange("(kc p) n -> p kc n", p=128), w=["wa%d" % b])
                p.dma("sync", wbb[b][:], r32(c.w_up_b[:, csl]).rearrange("(kc p) n -> p kc n", p=128), w=["wbb%d" % b])
                p.dma("sync", ga[b][:], c.sc_fm[5120 + cc * 128:5120 + (cc + 1) * 128, tsl], w=["ga%d" % b])
                p.dma("sync", gb_[b][:], c.sc_fm[7168 + cc * 128:7168 + (cc + 1) * 128, tsl], w=["gb%d" % b])
                for kc in range(8):
                    p.op("tensor", lambda e, b=b, kc=kc: e.matmul(pa[b][:], lhsT=wa[b][:, kc, :], rhs=oa[:, kc, :],
                                                                  start=(kc == 0), stop=(kc == 7)),
                         r=["wa%d" % b, "oa"], w=["pa%d" % b])
                for kc in range(8):
                    p.op("tensor", lambda e, b=b, kc=kc: e.matmul(pb[b][:], lhsT=wbb[b][:, kc, :], rhs=obb[:, kc, :],
                                                                  start=(kc == 0), stop=(kc == 7)),
                         r=["wbb%d" % b, "obb"], w=["pb%d" % b])
                p.op("scalar", lambda e, b=b: e.activation(out=ga[b][:], in_=ga[b][:], func=AF.Sigmoid), r=["ga%d" % b], w=["ga%d" % b])
                p.op("scalar", lambda e, b=b: e.activation(out=gb_[b][:], in_=gb_[b][:], func=AF.Sigmoid), r=["gb%d" % b], w=["gb%d" % b])
                p.op("vector", lambda e, b=b: e.tensor_tensor(out=m1[:], in0=pa[b][:], in1=ga[b][:], op=ALU.mult),
                     r=["pa%d" % b, "ga%d" % b], w=["m1"])
                p.op("vector", lambda e, b=b: e.tensor_tensor(out=gb_[b][:], in0=pb[b][:], in1=gb_[b][:], op=ALU.mult),
                     r=["pb%d" % b, "gb%d" % b], w=["gb%d" % b])
                p.op("vector", lambda e, b=b, cc=cc: e.tensor_tensor(out=mT[:, cc, :], in0=m1[:], in1=gb_[b][:], op=ALU.add),
                     r=["m1", "gb%d" % b], w=["mT"])
            for dg in range(4):
                wb_ = wi % 2
                wi += 1
                dsl = slice(dg * 512, (dg + 1) * 512)
                p.dma("sync", wo[wb_][:], r32(c.w_out[:, dsl]).rearrange("(kc p) n -> p kc n", p=128), w=["wo%d" % wb_])
                for tt in range(4):
                    o = oi % 4
                    oi += 1
                    x_ = oi % 2
                    t0 = tg * 512 + tt * 128
                    p.dma("sync", xt[x_][:], c.x[t0:t0 + 128, dsl], w=["xt%d" % x_])
                    for cc in range(16):
                        p.op("tensor", lambda e, o=o, cc=cc, tt=tt, wb_=wb_: e.matmul(
                            po[o][:], lhsT=mT[:, cc, tt * 128:(tt + 1) * 128], rhs=wo[wb_][:, cc, :],
                            start=(cc == 0), stop=(cc == 15)), r=["mT", "wo%d" % wb_], w=["po%d" % o])
                    p.op("vector", lambda e, o=o, x_=x_: e.tensor_tensor(out=xt[x_][:], in0=po[o][:], in1=xt[x_][:], op=ALU.add),
                         r=["po%d" % o, "xt%d" % x_], w=["xt%d" % x_])
                    p.dma("sync", c.sc_x1[t0:t0 + 128, dsl], xt[x_][:], r=["xt%d" % x_], w=[("sc_x1", t0, dg)])
        p.barrier()


def cvt_gen(c, p, cin, cout, rpp):
    k = 0
    nrow = 128 * rpp
    n = rpp * D
    hn = n // 2
    for (src, dst) in ((c.peer_u, c.uvb[:, 0:D]), (c.peer_v, c.uvb[:, D:2 * D])):
        for ch in range(16384 // nrow):
            b = k % 2
            k += 1
            rows = slice(ch * nrow, (ch + 1) * nrow)
            p.dma("sync", cin[b][:], src[rows, :].rearrange("(p r) d -> p (r d)", r=rpp), w=["cin%d" % b])
            p.op("scalar", lambda e, b=b: e.copy(out=cout[b][:, 0:hn], in_=cin[b][:, 0:hn]), r=["cin%d" % b], w=["coutA%d" % b])
            p.op("vector", lambda e, b=b: e.tensor_copy(out=cout[b][:, hn:n], in_=cin[b][:, hn:n]),
                 r=["cin%d" % b], w=["coutB%d" % b])
            p.dma("sync", dst[rows, :].rearrange("(p r) d -> p r d", r=rpp), cout[b][:].rearrange("p (r d) -> p r d", r=rpp),
                  r=["coutA%d" % b, "coutB%d" % b], w=[("tb", k)])
            yield


def phase_peer_cvt(c, p):
    nc = c.nc
    with ExitStack() as es:
        c2 = Ctx(); c2.nc = nc; c2.es = es
        cin = [sb(c2, "cin%d" % i, [128, 8192]) for i in range(2)]
        cout = [sb(c2, "cout%d" % i, [128, 8192], BF16) for i in range(2)]
        for _ in cvt_gen(c, p, cin, cout, 4):
            pass
        p.barrier()


def phase_peer(c, p):
    nc = c.nc
    if not getattr(c, "cvt_done", False):
        phase_peer_cvt(c, p)
    for half in range(2):
        with ExitStack() as es:
            c2 = Ctx(); c2.nc = nc; c2.es = es
            hT = sb(c2, "h2T", [128, 16, 1024], F32R)
            phase_norm_T(c, p, c.sc_x1, c.norm2_gain, hT, "n2_", half, h_out=c.sc_h2)
            with ExitStack() as es2:
                c3 = Ctx(); c3.nc = nc; c3.es = es2
                wq = [sb(c3, "wq%d" % i, [128, 16, 128], F32R) for i in range(2)]
                skT = sb(c3, "skT", [128, 16, 128], F32R)
                qT = [sb(c3, "qT%d" % i, [128, 1024], F32R) for i in range(2)]
                pq = [ps(c3, "pq%d" % i, [128, 512]) for i in range(4)]
                so_t = [sb(c3, "so_t%d" % i, [128, 512]) for i in range(2)]
                pqi = 0
                p.dma("sync", skT[:], r32(c.skT_d[:, :, :]), w=["skT"])
                for ch in range(16):
                    b = ch % 2
                    p.dma("sync", wq[b][:], r32(c.w_query[:, ch * 128:(ch + 1) * 128]).rearrange("(kc p) n -> p kc n", p=128),
                          w=["wq%d" % b])
                    for tg in range(2):
                        a = pqi % 4
                        pqi += 1
                        for kc in range(16):
                            p.op("tensor", lambda e, a=a, b=b, kc=kc, tg=tg: e.matmul(
                                pq[a][:], lhsT=wq[b][:, kc, :], rhs=hT[:, kc, tg * 512:(tg + 1) * 512],
                                start=(kc == 0), stop=(kc == 15)), r=["wq%d" % b, "hT"], w=["pq%d" % a])
                        p.op("scalar", lambda e, a=a, b=b, tg=tg: e.copy(out=qT[b][:, tg * 512:(tg + 1) * 512], in_=pq[a][:]),
                             r=["pq%d" % a], w=["qT%d" % b])
                    for tq in range(2):
                        a = pqi % 4
                        pqi += 1
                        for tt in range(4):
                            tl = tq * 4 + tt
                            p.op("tensor", lambda e, a=a, b=b, tl=tl, tt=tt, ch=ch: e.matmul(
                                pq[a][:, tt * 128:(tt + 1) * 128], lhsT=qT[b][:, tl * 128:(tl + 1) * 128], rhs=skT[:, ch, :],
                                start=True, stop=True), r=["qT%d" % b, "skT"], w=["pq%d" % a])
                        so = "so%d" % (pqi % 2)
                        sot = so_t[pqi % 2]
                        p.op("vector", lambda e, a=a, sot=sot: e.tensor_copy(out=sot[:], in_=pq[a][:]), r=["pq%d" % a], w=[so])
                        tb = half * 1024 + tq * 512
                        p.dma("sync", c.sc_sc[tb:tb + 512, ch * 128:(ch + 1) * 128].rearrange("(tt p) k -> p tt k", p=128),
                              sot[:].rearrange("p (tt k) -> p tt k", k=128), r=[so], w=[("sc_sc", tb, ch)])
                p.barrier()
    with ExitStack() as es:
        c2 = Ctx(); c2.nc = nc; c2.es = es
        sc = sb(c2, "sc", [128, 16, 128])
        wk = sb(c2, "wk", [128, 16, 128])
        stop_ = sb(c2, "stop", [128, 16, 16])
        itop = sb(c2, "itop", [128, 16, 16], U32)
        itf = sb(c2, "itf", [128, 16, 16])
        cand = sb(c2, "cand", [128, 8, 16, 16])
        cidx = sb(c2, "cidx", [128, 8, 16, 16])
        wk2 = sb(c2, "wk2", [128, 8, 256])
        junk2s = [sb(c2, "junk2s%d" % i, [128, 256]) for i in range(4)]
        best = sb(c2, "best", [128, 8, 16])
        pos = sb(c2, "pos", [128, 8, 16], U32)
        posf = sb(c2, "posf", [128, 8, 16])
        iota = sb(c2, "iota", [128, 256])
        junk2 = sb(c2, "junk2", [128, 256])
        eidf = sb(c2, "eidf", [128, 128])
        eid = sb(c2, "eid", [128, 128], U32)
        nmx = sb(c2, "nmx", [128, 8])
        gsum = sb(c2, "gsum", [128, 8])
        gate = sb(c2, "gate", [128, 8, 16])
        dots = sb(c2, "dots", [128, 128])
        gact = sb(c2, "gact", [128, 128])
        h2 = sb(c2, "h2", [128, D])
        NB = 8
        gu = [sb(c2, "gu%d" % i, [128, 2 * D], BF16) for i in range(NB)]
        gel = sb(c2, "gel", [128, 128])
        diag = [sb(c2, "diag%d" % i, [128, 128], BF16) for i in range(2)]
        po = [ps(c2, "po%d" % i, [128, 512]) for i in range(4)]
        junk = sb(c2, "junkp", [128, D])
        acc = sb(c2, "acc", [128, D])
        x1 = sb(c2, "x1", [128, D])
        p.dma("sync", iota[:], c.iota_d[:, :], w=["iota"])
        gi = 0
        for t in range(16):
            t0 = t * 128
            p.dma("sync", sc[:], c.sc_sc[t0:t0 + 128, :].rearrange("p (c k) -> p c k", k=128), w=["sc"])
            p.dma("sync", h2[:], c.sc_h2[t0:t0 + 128, :], w=["h2"])
            p.dma("sync", x1[:], c.sc_x1[t0:t0 + 128, :], w=["x1"])
            CH = range(16)
            HH = range(8)
            for ch in CH:
                p.op("vector", lambda e, ch=ch: e.max(out=stop_[:, ch, 0:8], in_=sc[:, ch, :]), r=["sc"], w=[("stop", ch)])
            for ch in CH:
                p.op("vector", lambda e, ch=ch: e.max_index(out=itop[:, ch, 0:8], in_max=stop_[:, ch, 0:8], in_values=sc[:, ch, :]),
                     r=["sc", ("stop", ch)], w=[("itop", ch)])
            for ch in CH:
                p.op("vector", lambda e, ch=ch: e.match_replace(out=wk[:, ch, :], in_to_replace=stop_[:, ch, 0:8], in_values=sc[:, ch, :],
                                                                imm_value=-1e30), r=["sc", ("stop", ch)], w=[("wk", ch)])
            for ch in CH:
                p.op("vector", lambda e, ch=ch: e.max(out=stop_[:, ch, 8:16], in_=wk[:, ch, :]), r=[("wk", ch)], w=[("stop", ch)])
            for ch in CH:
                p.op("vector", lambda e, ch=ch: e.max_index(out=itop[:, ch, 8:16], in_max=stop_[:, ch, 8:16], in_values=wk[:, ch, :]),
                     r=[("wk", ch), ("stop", ch)], w=[("itop", ch)])
            p.op("vector", lambda e: e.tensor_copy(out=itf[:], in_=itop[:]), r=[("itop", ch) for ch in CH], w=["itf"])
            s4 = stop_[:].rearrange("p (h two) k -> p h two k", two=2)
            i4 = itf[:].rearrange("p (h two) k -> p h two k", two=2)
            p.op("vector", lambda e, s4=s4: e.tensor_tensor(out=cand[:], in0=s4[:, :, 0, :].unsqueeze(3).to_broadcast([128, 8, 16, 16]),
                                                            in1=s4[:, :, 1, :].unsqueeze(2).to_broadcast([128, 8, 16, 16]), op=ALU.add),
                 r=[("stop", ch) for ch in CH], w=["cand"])
            for hh in HH:
                p.op("vector", lambda e, i4=i4, hh=hh: e.scalar_tensor_tensor(
                    out=cidx[:, hh], in0=i4[:, hh, 0, :].unsqueeze(2).to_broadcast([128, 16, 16]), scalar=128.0,
                    in1=i4[:, hh, 1, :].unsqueeze(1).to_broadcast([128, 16, 16]), op0=ALU.mult, op1=ALU.add),
                    r=["itf"], w=[("cidx", hh)])
            cvs = [cand[:, hh].rearrange("p a b -> p (a b)") for hh in HH]
            for hh in HH:
                p.op("vector", lambda e, hh=hh: e.max(out=best[:, hh, 0:8], in_=cvs[hh]), r=["cand"], w=[("best", hh)])
            for hh in HH:
                p.op("vector", lambda e, hh=hh: e.max_index(out=pos[:, hh, 0:8], in_max=best[:, hh, 0:8], in_values=cvs[hh]),
                     r=["cand", ("best", hh)], w=[("pos", hh)])
            for hh in HH:
                p.op("vector", lambda e, hh=hh: e.match_replace(out=wk2[:, hh, :], in_to_replace=best[:, hh, 0:8], in_values=cvs[hh],
                                                                imm_value=-1e30), r=["cand", ("best", hh)], w=[("wk2", hh)])
            for hh in HH:
                p.op("vector", lambda e, hh=hh: e.max(out=best[:, hh, 8:16], in_=wk2[:, hh, :]), r=[("wk2", hh)], w=[("best", hh)])
            for hh in HH:
                p.op("vector", lambda e, hh=hh: e.max_index(out=pos[:, hh, 8:16], in_max=best[:, hh, 8:16], in_values=wk2[:, hh, :]),
                     r=[("wk2", hh), ("best", hh)], w=[("pos", hh)])
            p.op("vector", lambda e: e.tensor_copy(out=posf[:], in_=pos[:]), r=[("pos", hh) for hh in HH], w=["posf"])
            p.op("vector", lambda e: e.tensor_scalar(out=nmx[:], in0=best[:, :, 0], scalar1=-1.0, scalar2=None, op0=ALU.mult),
                 r=[("best", hh) for hh in HH], w=["nmx"])
            for hh in HH:
                p.op("scalar", lambda e, hh=hh: e.activation(out=gate[:, hh, :], in_=best[:, hh, :], func=AF.Exp, bias=nmx[:, hh:hh + 1],
                                                             accum_out=gsum[:, hh:hh + 1]), r=[("best", hh), "nmx"],
                     w=[("gate", hh), ("gsum", hh)])
            for hh in HH:
                ci_ = cidx[:, hh].rearrange("p a b -> p (a b)")
                for m in range(16):
                    jb = junk2s[(hh * 16 + m) % 4]
                    p.op("vector", lambda e, hh=hh, m=m, ci_=ci_, jb=jb: e.scalar_tensor_tensor(
                        out=jb[:], in0=iota[:], scalar=posf[:, hh, m:m + 1], in1=ci_, op0=ALU.is_equal, op1=ALU.mult,
                        accum_out=eidf[:, hh * 16 + m:hh * 16 + m + 1]), r=["iota", "posf", ("cidx", hh)], w=[("eidf", hh * 16 + m)])
            p.op("vector", lambda e: e.tensor_copy(out=eid[:], in_=eidf[:]), r=[("eidf", i) for i in range(128)], w=["eid"])
            p.op("vector", lambda e: e.reciprocal(out=gsum[:], in_=gsum[:]), r=[("gsum", hh) for hh in HH], w=["gsum"])
            p.op("vector", lambda e: e.tensor_tensor(out=gate[:], in0=gate[:], in1=gsum[:].unsqueeze(2).to_broadcast([128, 8, 16]), op=ALU.mult),
                 r=[("gate", hh) for hh in HH] + ["gsum"], w=["gate"])
            gatef = gate[:].rearrange("p h k -> p (h k)")
            pend = []

            def emit_up(args, gatef=gatef):
                b, db, s_ = args
                p.op("vector", lambda e: e.tensor_scalar(out=diag[db][:], in0=c.ident[:], scalar1=gel[:, s_:s_ + 1],
                                                         scalar2=gatef[:, s_:s_ + 1], op0=ALU.mult, op1=ALU.mult),
                     r=[("gel", s_), "gate"], w=["diag%d" % db])
                for dg in range(4):
                    p.op("tensor", lambda e, dg=dg: e.matmul(
                        po[dg][:], lhsT=diag[db][:], rhs=gu[b][:, D + dg * 512:D + (dg + 1) * 512], start=(s_ == 0), stop=(s_ == 127)),
                        r=["diag%d" % db, "gu%d" % b], w=["po%d" % dg])

            for s_ in range(128):
                b = gi % NB
                gi += 1
                db = s_ % 2
                p.dma_fn("gpsimd", lambda e, b=b, s_=s_: e.indirect_dma_start(
                    out=gu[b][:], out_offset=None, in_=c.uvb[:, :],
                    in_offset=bass.IndirectOffsetOnAxis(ap=eid[:, s_:s_ + 1], axis=0)), r=["eid"], w=["gu%d" % b])
                p.op("vector", lambda e, b=b, s_=s_: e.scalar_tensor_tensor(out=junk[:], in0=gu[b][:, 0:D], scalar=1.0, in1=h2[:],
                                                                            op0=ALU.mult, op1=ALU.mult, accum_out=dots[:, s_:s_ + 1]),
                     r=["gu%d" % b, "h2"], w=[("dots", s_)])
                p.op("scalar", lambda e, s_=s_: e.activation(out=gel[:, s_:s_ + 1], in_=dots[:, s_:s_ + 1], func=AF.Gelu),
                     r=[("dots", s_)], w=[("gel", s_)])
                pend.append((b, db, s_))
                if len(pend) > 1:
                    emit_up(pend.pop(0))
            while pend:
                emit_up(pend.pop(0))
            for dg in range(4):
                dsl = slice(dg * 512, (dg + 1) * 512)
                p.op("vector", lambda e, dg=dg, dsl=dsl: e.tensor_tensor(out=acc[:, dsl], in0=po[dg][:], in1=x1[:, dsl], op=ALU.add),
                     r=["po%d" % dg, "x1"], w=["acc"])
            p.dma("sync", c.y[t0:t0 + 128, :], acc[:], r=["acc"], w=[("y", t)])
        p.barrier()


ALL_PHASES = ("inproj", "moba", "gdn", "merge", "peer")


def build_nc(debug=False, phases=ALL_PHASES):
    nc = bass.Bass("TRN2", target_bir_lowering=False)
    nc.dge_precook = False
    c = Ctx()
    c.nc = nc
    kind_s = "ExternalOutput" if debug else "Internal"

    def din(name, shape, dt=F32):
        return nc.dram_tensor(name, list(shape), dt, kind="ExternalInput").ap()

    def dsc(name, shape):
        return nc.dram_tensor(name, list(shape), F32, kind=kind_s).ap()

    c.x = din("x", [S, D])
    c.norm1_gain = din("norm1_gain", [1, D])
    c.w_in = din("w_in", [D, IN_TOTAL])
    c.ident_d = din("ident", [128, 128])
    c.ones_d = din("ones", [128, 128])
    c.rel_bias = din("rel_bias", [32, 8])
    c.q_norm_gain = din("q_norm_gain", [1, 128])
    c.k_norm_gain = din("k_norm_gain", [1, 128])
    c.d01_d = din("d01", [8, 2, 128, 128])
    c.cm_d = din("cm", [128, 16, 8])
    c.notown_d = din("notown", [128, 16, 8])
    c.esel_d = din("esel", [8, 8, 128])
    c.gdnc_d = din("gdnc", [7, 128, 128])
    c.conv_wT = din("conv_wT", [3072, 4])
    c.a_log = din("a_log", [1, 8])
    c.dt_bias = din("dt_bias", [1, 8])
    c.gdn_norm_gain = din("gdn_norm_gain", [1, 128])
    c.w_up_a = din("w_up_a", [1024, D])
    c.w_up_b = din("w_up_b", [1024, D])
    c.w_out = din("w_out", [D, D])
    if "peer" in phases:
        c.norm2_gain = din("norm2_gain", [1, D])
        c.w_query = din("w_query", [D, D])
        c.skT_d = din("skT", [128, 16, 128])
        c.peer_u = din("peer_u", [16384, D])
        c.peer_v = din("peer_v", [16384, D])
        c.iota_d = din("iota", [128, 256])
        c.sc_h2 = dsc("sc_h2", [S, D])
        c.uvb = nc.dram_tensor("peer_uvb", [16384, 2 * D], BF16, kind="Internal").ap()
        c.sc_sc = dsc("sc_sc", [S, D])
    c.sc_fm = dsc("sc_fm", [N_FM, S])
    c.sc_tm = dsc("sc_tm", [S, N_TM])
    c.sc_oa = dsc("sc_oa", [1024, S])
    c.sc_ob = dsc("sc_ob", [1024, S])
    if "merge" in phases or "peer" not in phases:
        c.sc_x1 = dsc("sc_x1", [S, D])
    else:
        c.sc_x1 = din("sc_x1", [S, D])
    c.y = nc.dram_tensor("y", [S, D], F32, kind="ExternalOutput").ap()

    with ExitStack() as es:
        c.es = es
        p = Prog(nc, es)
        block = es.enter_context(nc.Block())
        c.ident = sb(c, "ident_s", [128, 128])
        c.ident_r = sb(c, "ident_r", [128, 128], F32R)
        c.ones_r = sb(c, "ones_r", [128, 128], F32R)
        c.eps_t = sb(c, "eps_t", [128, 1])
        c.one_t = sb(c, "one_t", [128, 1])
        p.dma("sync", c.ident[:], c.ident_d[:, :], w=["ident"])
        p.dma("sync", c.ident_r[:], r32(c.ident_d[:, :]), w=["ident_r"])
        p.dma("sync", c.ones_r[:], r32(c.ones_d[:, :]), w=["ones_r"])
        p.op("vector", lambda e: e.memset(c.eps_t[:], EPS), w=["eps"])
        p.op("vector", lambda e: e.memset(c.one_t[:], 1.0), w=["one"])
        p.barrier()
        if "inproj" in phases:
            phase_inproj(c, p)
        if "moba" in phases:
            phase_moba(c, p)
        if "gdn" in phases:
            phase_gdn(c, p)
        if "merge" in phases:
            phase_merge(c, p)
        if "peer" in phases:
            phase_peer(c, p)
        p.finish(block)
    return nc


def make_in_maps(inputs, phases=ALL_PHASES):
    f = lambda a: np.ascontiguousarray(np.asarray(a, dtype=np.float32))
    rel_bias = f(inputs["rel_bias"])
    d01, cm, notown, esel = moba_consts(rel_bias)
    shared = {
        "norm1_gain": f(inputs["norm1_gain"]), "w_in": f(inputs["w_in"][0]),
        "ident": np.eye(128, dtype=np.float32), "ones": np.ones((128, 128), np.float32),
        "rel_bias": rel_bias, "q_norm_gain": f(inputs["q_norm_gain"]), "k_norm_gain": f(inputs["k_norm_gain"]),
        "d01": d01, "cm": cm, "notown": notown, "esel": esel, "gdnc": gdn_consts(),
        "conv_wT": f(np.asarray(inputs["conv_w"][0]).T), "a_log": f(inputs["a_log"]), "dt_bias": f(inputs["dt_bias"]),
        "gdn_norm_gain": f(inputs["gdn_norm_gain"]), "w_up_a": f(inputs["w_up_a"][0]), "w_up_b": f(inputs["w_up_b"][0]),
        "w_out": f(inputs["w_out"][0]),
    }
    if "peer" in phases:
        sk = np.asarray(inputs["peer_sub_keys"][0], dtype=np.float32).reshape(16, 128, 128)
        shared.update({
            "norm2_gain": f(inputs["norm2_gain"]), "w_query": f(inputs["peer_w_query"][0]),
            "skT": f(sk.transpose(2, 0, 1)), "peer_u": f(inputs["peer_u"][0]), "peer_v": f(inputs["peer_v"][0]),
            "iota": np.broadcast_to(np.arange(256, dtype=np.float32), (128, 256)).copy(),
        })
    maps = []
    for b in range(8):
        m = dict(shared)
        m["x"] = f(inputs["x"][b])
        maps.append(m)
    return maps


_NC_CACHE = {}


def kernel(**inputs):
    if "nc" not in _NC_CACHE:
        _NC_CACHE["nc"] = build_nc()
    nc = _NC_CACHE["nc"]
    maps = make_in_maps(inputs)
    res = run_bass_kernel_spmd(nc, maps, core_ids=list(range(8)))
    return np.stack([np.asarray(r["y"], dtype=np.float32) for r in res.results], axis=0)
```

```python
import math
from contextlib import ExitStack

import numpy as np
import concourse.bass as bass
import concourse.mybir as mybir
from concourse.bass_utils import run_bass_kernel_spmd

F32 = mybir.dt.float32
F32R = mybir.dt.float32r
BF16 = mybir.dt.bfloat16
U32 = mybir.dt.uint32
I32 = mybir.dt.int32
AF = mybir.ActivationFunctionType
ALU = mybir.AluOpType
AX = mybir.AxisListType

D = 2048
S = 2048
NT = S // 128
IN_TOTAL = 11280
EPS = 1e-6
NEG = -30000.0

COMPUTE = ("tensor", "vector", "scalar", "gpsimd")
ALLENG = ("tensor", "vector", "scalar", "gpsimd", "sync")


import re as _re
_PSUM_KEY = _re.compile(r"^(n\d_tp|gp|aux|scp|op|dp|pp|pa|pb|po|pq)\d+$")


class Op:
    __slots__ = ("eng", "fn", "deps", "needed", "is_dma", "slot", "use", "tok", "idx")


class Prog:
    def __init__(self, nc, es, ndma=8):
        self.nc = nc
        self.ops = {e: [] for e in ALLENG}
        self.last_w = {}
        self.rd_comp = {}
        self.rd_dma = {}
        self.sem = {e: es.enter_context(nc.semaphore("s_" + e)) for e in COMPUTE}
        self.dsem = {}
        self.dlast = {}
        self.dnext = {}
        self.duse = {}
        for q in ("sync", "gpsimd", "scalar"):
            self.dsem[q] = [es.enter_context(nc.semaphore("d_%s%d" % (q, i))) for i in range(ndma)]
            self.dlast[q] = [None] * ndma
            self.duse[q] = [0] * ndma
            self.dnext[q] = 0
        self.pending_barrier = {e: None for e in ALLENG}
        self.all_dma = []

    def _add(self, eng, fn, r, w, is_dma):
        o = Op()
        o.eng = eng
        o.fn = fn
        o.needed = False
        o.is_dma = is_dma
        o.slot = None
        o.use = 0
        deps = set()
        for k in r:
            lw = self.last_w.get(k)
            if lw is not None:
                deps.add(lw)
            if isinstance(k, str) and _PSUM_KEY.match(k):
                for en, ro in self.rd_comp.get(k, {}).items():
                    if en != eng:
                        deps.add(ro)
        for k in w:
            lw = self.last_w.get(k)
            if lw is not None:
                deps.add(lw)
            for ro in self.rd_comp.get(k, {}).values():
                deps.add(ro)
            for ro in self.rd_dma.get(k, ()):
                deps.add(ro)
        if is_dma:
            q = eng
            sl = self.dnext[q]
            self.dnext[q] = (sl + 1) % len(self.dsem[q])
            prev = self.dlast[q][sl]
            if prev is not None:
                deps.add(prev)
            self.duse[q][sl] += 1
            o.slot = sl
            o.use = self.duse[q][sl]
            self.dlast[q][sl] = o
            self.all_dma.append(o)
        pb = self.pending_barrier[eng]
        if pb is not None:
            deps |= pb
            self.pending_barrier[eng] = None
        if eng == "tensor":
            deps = {d for d in deps if not (d.eng == "tensor" and not d.is_dma)}
        o.deps = deps
        for d in deps:
            d.needed = True
        for k in w:
            self.last_w[k] = o
            self.rd_comp[k] = {}
            self.rd_dma[k] = []
        for k in r:
            if is_dma:
                self.rd_dma.setdefault(k, []).append(o)
            else:
                self.rd_comp.setdefault(k, {})[eng] = o
        o.idx = len(self.ops[eng])
        self.ops[eng].append(o)
        return o

    def op(self, eng, fn, r=(), w=()):
        return self._add(eng, fn, tuple(r), tuple(w), False)

    def dma(self, q, out, in_, r=(), w=(), **kw):
        return self._add(q, lambda e: e.dma_start(out=out, in_=in_, **kw), tuple(r), tuple(w), True)

    def dma_fn(self, q, fn, r=(), w=()):
        return self._add(q, fn, tuple(r), tuple(w), True)

    def barrier(self):
        deps = set()
        for e in ALLENG:
            if self.ops[e]:
                last = [o for o in self.ops[e] if not o.is_dma]
                if last:
                    deps.add(last[-1])
        for o in self.all_dma:
            deps.add(o)
        self.all_dma = []
        for e in ALLENG:
            pb = self.pending_barrier[e]
            self.pending_barrier[e] = set(deps) | (pb or set())
        self.last_w = {}
        self.rd_comp = {}
        self.rd_dma = {}

    def finish(self, block):
        self.barrier()
        nc = self.nc
        for e in ALLENG:
            self._add(e, None, (), (), False)
        for e in COMPUTE:
            c = 0
            for o in self.ops[e]:
                if o.is_dma:
                    o.tok = (self.dsem[e][o.slot], 16 * o.use)
                elif o.needed:
                    c += 1
                    o.tok = (self.sem[e], c)
                else:
                    o.tok = None
        for o in self.ops["sync"]:
            if o.is_dma:
                o.tok = (self.dsem["sync"][o.slot], 16 * o.use)
            else:
                o.tok = None

        def emit(engname):
            def body(eng):
                waited = {}
                for o in self.ops[engname]:
                    for d in o.deps:
                        sem, val = d.tok
                        key = id(sem)
                        if waited.get(key, 0) < val:
                            eng.wait_ge(sem, val)
                            waited[key] = val
                    if o.fn is None:
                        continue
                    ins = o.fn(eng)
                    if o.is_dma:
                        ins.then_inc(o.tok[0], 16)
                    elif o.needed:
                        ins.then_inc(o.tok[0], 1)
            return body

        block.tensor(emit("tensor"))
        block.vector(emit("vector"))
        block.scalar(emit("scalar"))
        block.gpsimd(emit("gpsimd"))
        block.sync(emit("sync"))


def r32(ap):
    return ap.bitcast(F32R)


class _Rec:
    def __init__(self):
        self.l = []

    def op(self, eng, fn, r=(), w=()):
        self.l.append(("op", eng, (eng, fn), dict(r=r, w=w)))

    def dma(self, q, out, in_, r=(), w=(), **kw):
        self.l.append(("dma", "dma", (q, out, in_), dict(r=r, w=w, **kw)))


class Ctx:
    pass


_uid = [0]


def _un(name):
    _uid[0] += 1
    return "%s_%d" % (name, _uid[0])


def sb(c, name, shape, dt=F32):
    return c.es.enter_context(c.nc.sbuf_tensor(_un(name), list(shape), dt))


def ps(c, name, shape, dt=F32):
    return c.es.enter_context(c.nc.psum_tensor(_un(name), list(shape), dt))


FM_SRC = [(0, 1024), (1024, 1024), (3072, 3072), (7184, 2048), (9232, 2048)]
TM_SRC = [(2048, 1024), (6144, 1024), (7168, 16)]
N_FM = 9216
N_TM = 2064


def phase_norm_T(c, p, x_ap, gain_ap, hT, pfx, half, h_out=None):
    nc = c.nc
    with ExitStack() as es:
        c2 = Ctx(); c2.nc = nc; c2.es = es
        gbc = sb(c2, pfx + "gbc", [128, D])
        xt = [sb(c2, pfx + "xt%d" % i, [128, D]) for i in range(2)]
        ht = [sb(c2, pfx + "ht%d" % i, [128, D]) for i in range(2)]
        sq = sb(c2, pfx + "sq", [128, D])
        st = [sb(c2, pfx + "st%d" % i, [128, 4]) for i in range(2)]
        tp = [ps(c2, pfx + "tp%d" % i, [128, 512]) for i in range(4)]
        p.dma("sync", gbc[:], gain_ap.partition_broadcast(128), w=[pfx + "gbc"])
        for ti in range(8):
            t = half * 8 + ti
            b = ti % 2
            p.dma("sync", xt[b][:], x_ap[t * 128:(t + 1) * 128, :], w=[pfx + "xt%d" % b])
            p.op("scalar", lambda e, b=b: e.activation(out=sq[:], in_=xt[b][:], func=AF.Square,
                                                       accum_out=st[b][:, 0:1]),
                 r=[pfx + "xt%d" % b], w=[pfx + "sq", pfx + "st%d" % b])
            p.op("scalar", lambda e, b=b: e.activation(out=st[b][:, 1:2], in_=st[b][:, 0:1], func=AF.Sqrt,
                                                       scale=1.0 / D, bias=c.eps_t[:, 0:1]),
                 r=[pfx + "st%d" % b], w=[pfx + "st%d" % b])
            p.op("vector", lambda e, b=b: e.reciprocal(out=st[b][:, 2:3], in_=st[b][:, 1:2]),
                 r=[pfx + "st%d" % b], w=[pfx + "st%d" % b])
            p.op("vector", lambda e, b=b: e.scalar_tensor_tensor(out=ht[b][:], in0=xt[b][:], scalar=st[b][:, 2:3],
                                                                 in1=gbc[:], op0=ALU.mult, op1=ALU.mult),
                 r=[pfx + "xt%d" % b, pfx + "st%d" % b, pfx + "gbc"], w=[pfx + "ht%d" % b])
            if h_out is not None:
                p.dma("sync", h_out[t * 128:(t + 1) * 128, :], ht[b][:], r=[pfx + "ht%d" % b], w=[(pfx + "hout", t)])
            for g4 in range(4):
                pb = tp[g4]
                for j in range(4):
                    kc = g4 * 4 + j
                    p.op("tensor", lambda e, b=b, kc=kc, j=j, pb=pb: e.transpose(
                        out=pb[:, j * 128:(j + 1) * 128], in_=ht[b][:, kc * 128:(kc + 1) * 128], identity=c.ident[:]),
                        r=[pfx + "ht%d" % b], w=[pfx + "tp%d" % g4])
                eng = "scalar" if g4 % 2 == 0 else "vector"
                dst = hT[:, g4 * 4:(g4 + 1) * 4, ti * 128:(ti + 1) * 128]
                src = pb[:].rearrange("p (j t) -> p j t", j=4)
                if eng == "scalar":
                    p.op("scalar", lambda e, dst=dst, src=src: e.copy(out=dst, in_=src),
                         r=[pfx + "tp%d" % g4], w=["hT"])
                else:
                    p.op("vector", lambda e, dst=dst, src=src: e.tensor_copy(out=dst, in_=src),
                         r=[pfx + "tp%d" % g4], w=["hT"])
        p.barrier()


def phase_inproj(c, p):
    nc = c.nc
    with ExitStack() as es0:
        gen = None
        if getattr(c, "uvb", None) is not None:
            c0 = Ctx(); c0.nc = nc; c0.es = es0
            cin = [sb(c0, "cin%d" % i, [128, 4096]) for i in range(2)]
            cout = [sb(c0, "cout%d" % i, [128, 4096], BF16) for i in range(2)]
            gen = cvt_gen(c, p, cin, cout, 2)
            c.cvt_done = True
        _phase_inproj(c, p, gen)


def _pump(gen, n):
    if gen is None:
        return
    for _ in range(n):
        if next(gen, "end") == "end":
            return


def _phase_inproj(c, p, gen):
    nc = c.nc
    for half in range(2):
        with ExitStack() as es:
            c2 = Ctx(); c2.nc = nc; c2.es = es
            hT = sb(c2, "hT", [128, 16, 1024], F32R)
            phase_norm_T(c, p, c.x, c.norm1_gain, hT, "n1_", half)
            with ExitStack() as es2:
                c3 = Ctx(); c3.nc = nc; c3.es = es2
                wb = [sb(c3, "wb%d" % i, [128, 16, 256], F32R) for i in range(2)]
                ob = [sb(c3, "ob%d" % i, [128, 1024]) for i in range(2)]
                pp = [ps(c3, "pp%d" % i, [128, 512]) for i in range(4)]
                gi = 0
                oi = 0
                pi = 0
                t0 = half * 1024
                row = 0
                for (c0, n) in FM_SRC:
                    for g in range(n // 256):
                        col = c0 + g * 256
                        b = gi % 2
                        gi += 1
                        p.dma("sync", wb[b][:], r32(c.w_in[:, col:col + 256]).rearrange("(kc p) n -> p kc n", p=128),
                              w=["wb%d" % b])
                        _pump(gen, 2)
                        for cc in range(2):
                            o = oi % 2
                            oi += 1
                            for tg in range(2):
                                pb = pi % 4
                                pi += 1
                                for kc in range(16):
                                    p.op("tensor", lambda e, b=b, cc=cc, tg=tg, kc=kc, pb=pb: e.matmul(
                                        pp[pb][:], lhsT=wb[b][:, kc, cc * 128:(cc + 1) * 128],
                                        rhs=hT[:, kc, tg * 512:(tg + 1) * 512],
                                        start=(kc == 0), stop=(kc == 15)),
                                        r=["wb%d" % b, "hT"], w=["pp%d" % pb])
                                if tg == 0:
                                    p.op("scalar", lambda e, o=o, pb=pb: e.copy(out=ob[o][:, 0:512], in_=pp[pb][:]),
                                         r=["pp%d" % pb], w=["ob%d" % o])
                                else:
                                    p.op("vector", lambda e, o=o, pb=pb: e.tensor_copy(out=ob[o][:, 512:1024], in_=pp[pb][:]),
                                         r=["pp%d" % pb], w=["ob%d" % o])
                            rr = row + g * 256 + cc * 128
                            p.dma("sync", c.sc_fm[rr:rr + 128, t0:t0 + 1024], ob[o][:],
                                  r=["ob%d" % o], w=[("sc_fm", rr // 128)])
                    row += n
                colo = 0
                for (c0, n) in TM_SRC:
                    w = min(n, 256)
                    for g in range(max(1, n // 256)):
                        col = c0 + g * 256
                        b = gi % 2
                        gi += 1
                        p.dma("sync", wb[b][:, :, 0:w], r32(c.w_in[:, col:col + w]).rearrange("(kc p) n -> p kc n", p=128),
                              w=["wb%d" % b])
                        _pump(gen, 2)
                        for tq in range(2):
                            o = oi % 2
                            oi += 1
                            for tt in range(4):
                                tl = tq * 4 + tt
                                pb = pi % 4
                                pi += 1
                                for kc in range(16):
                                    p.op("tensor", lambda e, b=b, tl=tl, kc=kc, pb=pb, w=w: e.matmul(
                                        pp[pb][:, 0:w], lhsT=hT[:, kc, tl * 128:(tl + 1) * 128],
                                        rhs=wb[b][:, kc, 0:w],
                                        start=(kc == 0), stop=(kc == 15)),
                                        r=["wb%d" % b, "hT"], w=["pp%d" % pb])
                                if tt % 2 == 0:
                                    p.op("scalar", lambda e, o=o, pb=pb, tt=tt, w=w: e.copy(
                                        out=ob[o][:, tt * 256:tt * 256 + w], in_=pp[pb][:, 0:w]),
                                        r=["pp%d" % pb], w=["ob%d" % o])
                                else:
                                    p.op("vector", lambda e, o=o, pb=pb, tt=tt, w=w: e.tensor_copy(
                                        out=ob[o][:, tt * 256:tt * 256 + w], in_=pp[pb][:, 0:w]),
                                        r=["pp%d" % pb], w=["ob%d" % o])
                            tb = t0 + tq * 512
                            cw = colo + g * 256
                            p.dma("sync",
                                  c.sc_tm[tb:tb + 512, cw:cw + w].rearrange("(tt p) n -> p tt n", p=128),
                                  ob[o][:].rearrange("p (tt n) -> p tt n", n=256)[:, :, 0:w],
                                  r=["ob%d" % o], w=[("sc_tm", tb // 128, cw)])
                    colo += n
                p.barrier()
    if gen is not None:
        _pump(gen, 1000)
        p.barrier()


def t5_bucket_np(n):
    n = np.maximum(n, 0)
    nf = np.maximum(n, 1).astype(np.float32)
    large = 16 + (np.log(nf / np.float32(16)) / np.float32(math.log(8.0)) * np.float32(16)).astype(np.int32)
    large = np.minimum(large, 31)
    return np.where(n < 16, n, large)


def moba_consts(rel_bias):
    k = np.arange(128)[:, None]
    q = np.arange(128)[None, :]
    out = np.zeros((8, 2, 128, 128), np.float32)
    b0 = t5_bucket_np(q - k)
    b1 = t5_bucket_np(q - k + 128)
    for h in range(8):
        out[h, 0] = np.where(q >= k, rel_bias[b0, h], np.float32(NEG))
        out[h, 1] = rel_bias[b1, h]
    cm = np.zeros((128, 16, 8), np.float32)
    notown = np.ones((128, 16, 8), np.float32)
    for t in range(16):
        for n in range(8):
            if n >= t // 2:
                cm[:, t, n] = -1e30
            if n == t // 2:
                notown[:, t, n] = 0.0
    esel = np.zeros((8, 8, 128), np.float32)
    for n in range(8):
        esel[n, n, :] = 1.0
    return out, cm, notown, esel


def phase_moba(c, p):
    nc = c.nc
    with ExitStack() as es:
        c2 = Ctx(); c2.nc = nc; c2.es = es
        cm = sb(c2, "cm", [128, 16, 8])
        notown = sb(c2, "notown", [128, 16, 8])
        esel = sb(c2, "esel", [8, 8, 128], F32R)
        cb = sb(c2, "cb", [128, 8])
        gq = sb(c2, "gq", [128, 2])
        gk = sb(c2, "gk", [128, 1])
        d01s = [sb(c2, "d01%d" % i, [128, 2, 128], F32R) for i in range(2)]
        d01r = sb(c2, "d01r", [128, 2, 128])
        qr = sb(c2, "qr", [128, S])
        kr = sb(c2, "kr", [128, S])
        sq = sb(c2, "sq", [128, S], F32R)
        qns = [sb(c2, "qn%d" % i, [128, S], F32R) for i in range(2)]
        kns = [sb(c2, "kn%d" % i, [128, S], F32R) for i in range(2)]
        vts = [sb(c2, "vt%d" % i, [128, 16, 128], F32R) for i in range(2)]
        rsb = [sb(c2, "rs%d" % i, [128, 512]) for i in range(2)]
        km = sb(c2, "km", [128, 8], F32R)
        kmf = sb(c2, "kmf", [128, 8])
        gm = sb(c2, "gm", [128, 16, 8])
        cmp_ = sb(c2, "cmp", [128, 16, 8, 8])
        rank = sb(c2, "rank", [128, 16, 8])
        nmk = sb(c2, "nmk", [128, 16, 8])
        negTs = [sb(c2, "negT%d" % i, [8, S], F32R) for i in range(2)]
        pT = [sb(c2, "pT%d" % i, [128, 512], F32R) for i in range(2)]
        rden = sb(c2, "rden", [128, 512])
        oo = [sb(c2, "oo%d" % i, [128, 512]) for i in range(2)]
        aux = [ps(c2, "aux%d" % i, [128, 512]) for i in range(2)]
        scp = [ps(c2, "scp%d" % i, [128, 512]) for i in range(2)]
        op_ = [ps(c2, "op%d" % i, [128, 512]) for i in range(2)]
        dp = [ps(c2, "dp%d" % i, [128, 512]) for i in range(2)]

        p.dma("sync", cm[:], c.cm_d[:, :, :], w=["cm"])
        p.dma("sync", notown[:], c.notown_d[:, :, :], w=["notown"])
        p.dma("sync", esel[:], r32(c.esel_d[:, :, :]), w=["esel"])
        p.dma("sync", cb[:], c.rel_bias[31:32, :].partition_broadcast(128), w=["cb"])
        p.dma("sync", gq[:, 0:1], c.q_norm_gain.rearrange("o d -> d o"), w=["gq"])
        p.dma("sync", gk[:, 0:1], c.k_norm_gain.rearrange("o d -> d o"), w=["gk"])
        p.op("vector", lambda e: e.tensor_scalar(out=gq[:, 1:2], in0=gq[:, 0:1], scalar1=128.0 ** -0.5, scalar2=None,
                                                 op0=ALU.mult), r=["gq"], w=["gq"])
        auxi = 0
        sci = 0
        gi = 0
        def setup(h, P):
            nonlocal auxi, sci, gi
            par = h % 2
            qn = qns[par]
            kn = kns[par]
            vt = vts[par]
            negT = negTs[par]
            d01 = d01s[par]
            kq, kk, kv, kng, kd = "qn%d" % par, "kn%d" % par, "vt%d" % par, "negT%d" % par, "d01_%d" % par
            P.dma("sync", qr[:], c.sc_fm[h * 128:(h + 1) * 128, :], w=["qr"])
            P.dma("sync", kr[:], c.sc_fm[1024 + h * 128:1024 + (h + 1) * 128, :], w=["kr"])
            P.dma("sync", vt[:], r32(c.sc_tm[:, h * 128:(h + 1) * 128]).rearrange("(t p) d -> p t d", p=128), w=[kv])
            P.dma("sync", d01r[:], c.d01_d[h].rearrange("a k q -> k a q"), w=["d01r"])
            P.op("vector", lambda e, h=h: e.tensor_scalar(out=d01[:], in0=d01r[:], scalar1=cb[:, h:h + 1], scalar2=None,
                                                          op0=ALU.subtract), r=["d01r", "cb"], w=[kd])
            for (raw, dst, gcol, rk, wk) in ((qr, qn, gq[:, 1:2], "qr", kq), (kr, kn, gk[:, 0:1], "kr", kk)):
                P.op("scalar", lambda e, raw=raw: e.activation(out=sq[:], in_=raw[:], func=AF.Square), r=[rk], w=["sq"])
                for tg in range(4):
                    a = auxi % 2
                    auxi += 1
                    sl = slice(tg * 512, (tg + 1) * 512)
                    P.op("tensor", lambda e, a=a, sl=sl: e.matmul(aux[a][:], lhsT=c.ones_r[:], rhs=sq[:, sl], start=True, stop=True),
                         r=["sq"], w=["aux%d" % a])
                    rs = rsb[a]
                    P.op("scalar", lambda e, a=a, rs=rs: e.activation(out=rs[:], in_=aux[a][:], func=AF.Sqrt, scale=1.0 / 128,
                                                                      bias=c.eps_t[:, 0:1]), r=["aux%d" % a], w=["rs%d" % a])
                    P.op("vector", lambda e, rs=rs: e.reciprocal(out=rs[:], in_=rs[:]), r=["rs%d" % a], w=["rs%d" % a])
                    P.op("vector", lambda e, raw=raw, dst=dst, gcol=gcol, sl=sl, rs=rs: e.scalar_tensor_tensor(
                        out=dst[:, sl], in0=raw[:, sl], scalar=gcol, in1=rs[:], op0=ALU.mult, op1=ALU.mult),
                        r=[rk, "rs%d" % a, "gq", "gk"], w=[wk])
            P.op("vector", lambda e: e.tensor_reduce(out=kmf[:], in_=kn[:].bitcast(F32).rearrange("p (n j) -> p n j", j=256),
                                                     axis=AX.X, op=ALU.add), r=[kk], w=["kmf"])
            P.op("vector", lambda e: e.tensor_copy(out=km[:], in_=kmf[:]), r=["kmf"], w=["km"])
            a = auxi % 2
            auxi += 1
            for t in range(16):
                P.op("tensor", lambda e, a=a, t=t: e.matmul(aux[a][:, t * 8:(t + 1) * 8], lhsT=qn[:, t * 128:(t + 1) * 128],
                                                            rhs=km[:], start=True, stop=True),
                     r=[kq, "km"], w=["aux%d" % a])
            P.op("vector", lambda e, a=a: e.tensor_tensor(out=gm[:], in0=aux[a][:, 0:128].rearrange("p (t n) -> p t n", n=8),
                                                          in1=cm[:], op=ALU.add), r=["aux%d" % a, "cm"], w=["gm"])
            P.op("vector", lambda e: e.tensor_tensor(out=cmp_[:], in0=gm[:].unsqueeze(2).to_broadcast([128, 16, 8, 8]),
                                                     in1=gm[:].unsqueeze(3).to_broadcast([128, 16, 8, 8]), op=ALU.is_gt),
                 r=["gm"], w=["cmp"])
            P.op("vector", lambda e: e.tensor_reduce(out=rank[:], in_=cmp_[:], axis=AX.X, op=ALU.add), r=["cmp"], w=["rank"])
            P.op("vector", lambda e: e.tensor_scalar(out=rank[:], in0=rank[:], scalar1=3.0, scalar2=NEG, op0=ALU.is_ge,
                                                     op1=ALU.mult), r=["rank"], w=["rank"])
            P.op("vector", lambda e: e.tensor_tensor(out=nmk[:], in0=rank[:], in1=notown[:], op=ALU.mult),
                 r=["rank", "notown"], w=["nmk"])
            for tg in range(4):
                a = auxi % 2
                auxi += 1
                for j in range(4):
                    t = tg * 4 + j
                    P.op("tensor", lambda e, a=a, t=t, j=j: e.transpose(out=aux[a][0:8, j * 128:(j + 1) * 128], in_=nmk[:, t, :],
                                                                        identity=c.ident[:]),
                         r=["nmk"], w=["aux%d" % a])
                P.op("scalar", lambda e, a=a, tg=tg: e.copy(out=negT[:, tg * 512:(tg + 1) * 512], in_=aux[a][0:8, :]),
                     r=["aux%d" % a], w=[kng])

        def main(h, pump):
            nonlocal auxi, sci, gi
            par = h % 2
            qn = qns[par]
            kn = kns[par]
            vt = vts[par]
            negT = negTs[par]
            d01 = d01s[par]
            kq, kk, kv, kng, kd = "qn%d" % par, "kn%d" % par, "vt%d" % par, "negT%d" % par, "d01_%d" % par
            steps = []
            for g in range(4):
                gb = gi % 2
                gi += 1
                nk = 4 * g + 4
                for kt in range(nk):
                    s_ = sci % 2
                    sci += 1
                    jj0 = max(0, kt - 4 * g)
                    c0 = jj0 * 128
                    extra = []
                    if kt >= 4 * g:
                        extra.append((jj0, 0))
                        if jj0 + 1 < 4:
                            extra.append((jj0 + 1, 1))
                    elif kt == 4 * g - 1:
                        extra.append((0, 1))
                    steps.append(dict(g=g, gb=gb, nk=nk, kt=kt, s_=s_, qs=slice(g * 512 + c0, (g + 1) * 512), cs=slice(c0, 512),
                                      n=kt // 2, extra=extra))

            def emit_score(st, h=h):
                s_, kt, qs, cs, n, extra = st["s_"], st["kt"], st["qs"], st["cs"], st["n"], st["extra"]
                p.op("tensor", lambda e: e.matmul(scp[s_][:, cs], lhsT=kn[:, kt * 128:(kt + 1) * 128], rhs=qn[:, qs],
                                                  start=True, stop=False), r=[kk, kq], w=["scp%d" % s_])
                p.op("tensor", lambda e: e.matmul(scp[s_][:, cs], lhsT=esel[:, n, :], rhs=negT[:, qs], start=False, stop=(not extra)),
                     r=["esel", kng], w=["scp%d" % s_])
                for ei, (jj, which) in enumerate(extra):
                    p.op("tensor", lambda e, jj=jj, which=which, last=(ei == len(extra) - 1): e.matmul(
                        scp[s_][:, jj * 128:(jj + 1) * 128], lhsT=c.ident_r[:], rhs=d01[:, which, :], start=False, stop=last),
                        r=[kd], w=["scp%d" % s_])
                p.op("scalar", lambda e: e.activation(out=pT[s_][:, cs], in_=scp[s_][:, cs], func=AF.Exp, bias=cb[:, h:h + 1]),
                     r=["scp%d" % s_, "cb"], w=["pT%d" % s_])

            def emit_pv(st, h=h):
                s_, kt, cs, gb, nk, g = st["s_"], st["kt"], st["cs"], st["gb"], st["nk"], st["g"]
                p.op("tensor", lambda e: e.matmul(op_[gb][:, cs], lhsT=vt[:, kt, :], rhs=pT[s_][:, cs], start=(kt == 0), stop=(kt == nk - 1)),
                     r=[kv, "pT%d" % s_], w=["op%d" % gb])
                p.op("tensor", lambda e: e.matmul(dp[gb][:, cs], lhsT=c.ones_r[:], rhs=pT[s_][:, cs], start=(kt == 0), stop=(kt == nk - 1)),
                     r=["pT%d" % s_], w=["dp%d" % gb])
                if kt == nk - 1:
                    p.op("vector", lambda e: e.reciprocal(out=rden[:], in_=dp[gb][:]), r=["dp%d" % gb], w=["rden"])
                    p.op("vector", lambda e: e.tensor_tensor(out=oo[gb][:], in0=op_[gb][:], in1=rden[:], op=ALU.mult),
                         r=["op%d" % gb, "rden"], w=["oo%d" % gb])
                    p.dma("sync", c.sc_oa[h * 128:(h + 1) * 128, g * 512:(g + 1) * 512], oo[gb][:],
                          r=["oo%d" % gb], w=[("sc_oa", h, g)])

            emit_score(steps[0])
            for i_, st in enumerate(steps):
                if i_ + 1 < len(steps):
                    emit_score(steps[i_ + 1])
                emit_pv(st)
                pump(3)

        setup(0, p)
        for h in range(8):
            rec = _Rec()
            if h + 1 < 8:
                setup(h + 1, rec)
            pend_s = rec.l

            def pump(n, pend_s=pend_s):
                k = 0
                while pend_s and k < n:
                    kind, _, a, kw = pend_s.pop(0)
                    getattr(p, kind)(*a, **kw)
                    k += 1

            main(h, pump)
            pump(100000)
        p.barrier()


def gdn_consts():
    k = np.arange(128)[:, None]
    i = np.arange(128)[None, :]
    same = (k // 64) == (i // 64)
    tri = ((k <= i) & same).astype(np.float32)
    blk = same.astype(np.float32)
    half0 = np.broadcast_to((k < 64), (128, 128)).astype(np.float32)
    half1 = np.broadcast_to((k >= 64), (128, 128)).astype(np.float32)
    ustr = ((k > i) & same).astype(np.float32)
    ii = np.arange(128)[:, None]
    jj = np.arange(128)[None, :]
    same2 = (ii // 64) == (jj // 64)
    negm_strict = np.where((ii > jj) & same2, 0.0, NEG).astype(np.float32)
    negm_inclT = np.where((jj >= ii) & same2, 0.0, NEG).astype(np.float32)
    return np.stack([tri, blk, half0, half1, ustr, negm_strict, negm_inclT], 0)


class _Stop(Exception):
    pass


def _chk(n):
    import os
    return int(os.environ.get("GDN_STOP", "99")) == n


def phase_gdn(c, p):
    _phase_gdn(c, p)
    p.barrier()


def _phase_gdn(c, p):
    nc = c.nc
    with ExitStack() as es:
        c2 = Ctx(); c2.nc = nc; c2.es = es
        G = sb(c2, "gc", [128, 7, 128])
        TRI, BLK, H0, H1, USTR, NMS, NMIT = [G[:, i, :] for i in range(7)]
        ba = sb(c2, "ba", [128, 16, 16])
        dtb = sb(c2, "dtb", [128, 8])
        Aex = sb(c2, "Aex", [128, 8])
        gng = sb(c2, "gng", [128, 128])
        beta = sb(c2, "beta", [128, 16, 8])
        nbeta = sb(c2, "nbeta", [128, 16, 8])
        gg = sb(c2, "gg", [128, 16, 8])
        egs = sb(c2, "egs", [128, 16, 8])
        kds = sb(c2, "kds", [128, 16, 8])
        kbs = sb(c2, "kbs", [128, 16, 8])
        egl = sb(c2, "egl", [128, 2, 16, 8])
        tmp8 = sb(c2, "tmp8", [128, 16, 8])
        cw = sb(c2, "cw", [128, 3, 4])
        xp = [sb(c2, "xp%d" % i, [128, 3 + S]) for i in range(3)]
        cvs = [[sb(c2, "cv%d_%d" % (par, i), [128, S]) for i in range(3)] for par in range(2)]
        sq = sb(c2, "gsq", [128, S], F32R)
        rs = sb(c2, "grs", [128, 512])
        NGB = 4
        kbg = [sb(c2, "kbg%d" % j, [128, 128]) for j in range(NGB)]
        vbe = [sb(c2, "vbe%d" % j, [128, 128]) for j in range(NGB)]
        Ag = [sb(c2, "Ag%d" % j, [128, 128]) for j in range(NGB)]
        Dec = [sb(c2, "Dec%d" % j, [128, 128]) for j in range(NGB)]
        DecT = [sb(c2, "DecT%d" % j, [128, 128]) for j in range(NGB)]
        Am = [[sb(c2, "Am%d_%d" % (j, i), [128, 128]) for i in range(2)] for j in range(NGB)]
        At = [[sb(c2, "At%d_%d" % (j, i), [128, 128]) for i in range(2)] for j in range(NGB)]
        Rt = [[sb(c2, "Rt%d_%d" % (j, i), [128, 128]) for i in range(2)] for j in range(NGB)]
        u_all = sb(c2, "u_all", [128, 16, 128])
        wT_all = sb(c2, "wT_all", [128, 16, 128])
        aT_all = sb(c2, "aT_all", [128, 16, 128])
        kd_all = sb(c2, "kd_all", [128, 2, 16, 128])
        kds2 = sb(c2, "kds2", [128, 2, 16, 8])
        St = sb(c2, "St", [128, 128])
        vnew = sb(c2, "vnew", [128, 128])
        otmp = sb(c2, "otmp", [128, 128])
        ob = sb(c2, "ob", [128, 16, 128])
        zt = sb(c2, "zt", [128, 16, 128])
        ssq = sb(c2, "ssq", [128, 16])
        junk = sb(c2, "junk", [128, 128])
        obT = sb(c2, "obT", [128, S])
        pool = [ps(c2, "gp%d" % i, [128, 512]) for i in range(8)]
        pc = [0]

        def nxt():
            i = pc[0] % 8
            pc[0] += 1
            return i

        p.dma("sync", G[:], c.gdnc_d.rearrange("a k i -> k a i"), w=["G"])
        p.dma("sync", ba[:], c.sc_tm[:, 2048:2064].rearrange("(t p) n -> p t n", p=128), w=["ba"])
        p.dma("sync", dtb[:], c.dt_bias.partition_broadcast(128), w=["dtb"])
        p.dma("sync", Aex[:], c.a_log.partition_broadcast(128), w=["Aex"])
        p.dma("sync", gng[:], c.gdn_norm_gain.partition_broadcast(128), w=["gng"])
        p.op("scalar", lambda e: e.activation(out=Aex[:], in_=Aex[:], func=AF.Exp), r=["Aex"], w=["Aex"])
        p.op("scalar", lambda e: e.activation(out=beta[:], in_=ba[:, :, 0:8], func=AF.Exp, scale=-1.0), r=["ba"], w=["beta"])
        p.op("vector", lambda e: e.tensor_scalar(out=beta[:], in0=beta[:], scalar1=1.0, scalar2=None, op0=ALU.add), r=["beta"], w=["beta"])
        p.op("vector", lambda e: e.reciprocal(out=beta[:], in_=beta[:]), r=["beta"], w=["beta"])
        p.op("vector", lambda e: e.tensor_scalar(out=nbeta[:], in0=beta[:], scalar1=-1.0, scalar2=None, op0=ALU.mult), r=["beta"], w=["nbeta"])
        p.op("vector", lambda e: e.tensor_tensor(out=gg[:], in0=ba[:, :, 8:16], in1=dtb[:].unsqueeze(1).to_broadcast([128, 16, 8]),
                                                 op=ALU.add), r=["ba", "dtb"], w=["gg"])
        p.op("scalar", lambda e: e.activation(out=gg[:], in_=gg[:], func=AF.Exp), r=["gg"], w=["gg"])
        p.op("scalar", lambda e: e.activation(out=gg[:], in_=gg[:], func=AF.Ln, bias=c.one_t[:, 0:1]), r=["gg"], w=["gg"])
        p.op("vector", lambda e: e.scalar_tensor_tensor(out=gg[:], in0=gg[:], scalar=-1.0, in1=Aex[:].unsqueeze(1).to_broadcast([128, 16, 8]),
                                                        op0=ALU.mult, op1=ALU.mult), r=["gg", "Aex"], w=["gg"])
        ggf = gg[:].rearrange("p t h -> p (t h)")
        i0 = nxt(); i1 = nxt(); i2 = nxt(); i3 = nxt()
        p.op("tensor", lambda e: e.matmul(pool[i0][:, 0:128], lhsT=TRI, rhs=ggf, start=True, stop=True), r=["G", "gg"], w=["gp%d" % i0])
        p.op("tensor", lambda e: e.matmul(pool[i1][:, 0:128], lhsT=BLK, rhs=ggf, start=True, stop=True), r=["G", "gg"], w=["gp%d" % i1])
        p.op("tensor", lambda e: e.matmul(pool[i2][:, 0:128], lhsT=H0, rhs=ggf, start=True, stop=True), r=["G", "gg"], w=["gp%d" % i2])
        p.op("tensor", lambda e: e.matmul(pool[i3][:, 0:128], lhsT=H1, rhs=ggf, start=True, stop=True), r=["G", "gg"], w=["gp%d" % i3])
        v3 = lambda t_: t_.rearrange("p (t h) -> p t h", h=8)
        p.op("scalar", lambda e: e.activation(out=egs[:], in_=v3(pool[i0][:, 0:128]), func=AF.Exp), r=["gp%d" % i0], w=["egs"])
        p.op("vector", lambda e: e.tensor_tensor(out=kbs[:], in0=egs[:], in1=beta[:], op=ALU.mult), r=["egs", "beta"], w=["kbs"])
        p.op("vector", lambda e: e.tensor_copy(out=tmp8[:], in_=v3(pool[i0][:, 0:128])), r=["gp%d" % i0], w=["tmp8"])
        p.op("vector", lambda e: e.tensor_tensor(out=tmp8[:], in0=v3(pool[i1][:, 0:128]), in1=tmp8[:], op=ALU.subtract),
             r=["gp%d" % i1, "tmp8"], w=["tmp8"])
        p.op("scalar", lambda e: e.activation(out=kds[:], in_=tmp8[:], func=AF.Exp), r=["tmp8"], w=["kds"])
        p.op("scalar", lambda e: e.activation(out=egl[:, 0], in_=v3(pool[i2][:, 0:128]), func=AF.Exp), r=["gp%d" % i2], w=["egl"])
        p.op("scalar", lambda e: e.activation(out=egl[:, 1], in_=v3(pool[i3][:, 0:128]), func=AF.Exp), r=["gp%d" % i3], w=["egl"])
        p.op("vector", lambda e: e.tensor_scalar(out=egs[:], in0=egs[:], scalar1=128.0 ** -0.5, scalar2=None, op0=ALU.mult),
             r=["egs", "kbs"], w=["egs"])
        p.op("vector", lambda e: e.tensor_scalar(out=kds2[:, 0], in0=kds[:], scalar1=H0[:, 0:1], scalar2=None, op0=ALU.mult),
             r=["kds", "G"], w=["kds2"])
        p.op("vector", lambda e: e.tensor_scalar(out=kds2[:, 1], in0=kds[:], scalar1=H1[:, 0:1], scalar2=None, op0=ALU.mult),
             r=["kds", "G"], w=["kds2"])
        for i in range(3):
            p.op("vector", lambda e, i=i: e.memset(xp[i][:, 0:3], 0.0), w=["xp%d" % i])
        p.op("vector", lambda e: e.memset(vnew[:], 0.0), w=["vnew"])

        def conv(h, P):
            cvp = cvs[h % 2]
            cvk = ["cv%d_%d" % (h % 2, i) for i in range(3)]
            for i in range(3):
                row = 2048 + i * 1024 + h * 128
                P.dma("sync", xp[i][:, 3:3 + S], c.sc_fm[row:row + 128, :], w=["xp%d" % i])
                P.dma("sync", cw[:, i, :], c.conv_wT[i * 1024 + h * 128:i * 1024 + (h + 1) * 128, :], w=["cw"])
                P.op("vector", lambda e, i=i: e.tensor_scalar(out=cvp[i][:], in0=xp[i][:, 0:S], scalar1=cw[:, i, 0:1], scalar2=None,
                                                              op0=ALU.mult), r=["xp%d" % i, "cw"], w=[cvk[i]])
                for tap in range(1, 4):
                    P.op("vector", lambda e, i=i, tap=tap: e.scalar_tensor_tensor(
                        out=cvp[i][:], in0=xp[i][:, tap:tap + S], scalar=cw[:, i, tap:tap + 1], in1=cvp[i][:],
                        op0=ALU.mult, op1=ALU.add), r=["xp%d" % i, "cw", cvk[i]], w=[cvk[i]])
                P.op("scalar", lambda e, i=i: e.activation(out=cvp[i][:], in_=cvp[i][:], func=AF.Silu), r=[cvk[i]], w=[cvk[i]])
                if i < 2:
                    P.op("scalar", lambda e, i=i: e.activation(out=sq[:], in_=cvp[i][:], func=AF.Square), r=[cvk[i]], w=["gsq"])
                    for tg in range(4):
                        a = nxt()
                        sl = slice(tg * 512, (tg + 1) * 512)
                        P.op("tensor", lambda e, a=a, sl=sl: e.matmul(pool[a][:], lhsT=c.ones_r[:], rhs=sq[:, sl], start=True, stop=True),
                             r=["gsq"], w=["gp%d" % a])
                        P.op("scalar", lambda e, a=a: e.activation(out=rs[:], in_=pool[a][:], func=AF.Sqrt, bias=c.eps_t[:, 0:1]),
                             r=["gp%d" % a], w=["grs"])
                        P.op("vector", lambda e: e.reciprocal(out=rs[:], in_=rs[:]), r=["grs"], w=["grs"])
                        P.op("vector", lambda e, i=i, sl=sl: e.tensor_tensor(out=cvp[i][:, sl], in0=cvp[i][:, sl], in1=rs[:], op=ALU.mult),
                             r=[cvk[i], "grs"], w=[cvk[i]])

        def head_rest(h, pump):
            qn, kn, vn = cvs[h % 2]
            cvk = ["cv%d_%d" % (h % 2, i) for i in range(3)]
            p.dma("sync", zt[:], c.sc_tm[:, 1024 + h * 128:1024 + (h + 1) * 128].rearrange("(t p) d -> p t d", p=128), w=["zt"])
            NG = 4
            for t0_ in range(0, 16, NG):
                tl = list(range(t0_, t0_ + NG))
                ak = {}; av = {}; akk = {}; agd = {}; aqk = {}; agt = {}; amt = {}
                for j, t in enumerate(tl):
                    ts = slice(t * 128, (t + 1) * 128)
                    ak[j] = nxt()
                    p.op("tensor", lambda e, a=ak[j], ts=ts: e.transpose(out=pool[a][:, 0:128], in_=kn[:, ts], identity=c.ident[:]),
                         r=[cvk[1]], w=["gp%d" % ak[j]])
                    p.op("vector", lambda e, a=ak[j], t=t, h=h, j=j: e.tensor_scalar(out=kbg[j][:], in0=pool[a][:, 0:128],
                                                                                   scalar1=kbs[:, t, h:h + 1], scalar2=None, op0=ALU.mult),
                         r=["gp%d" % ak[j], "kbs"], w=["kbg%d" % j])
                    for hf_ in range(2):
                        p.op("scalar", lambda e, a=ak[j], t=t, h=h, hf_=hf_: e.activation(out=kd_all[:, hf_, t, :], in_=pool[a][:, 0:128],
                                                                                        func=AF.Identity, scale=kds2[:, hf_, t, h:h + 1]),
                             r=["gp%d" % ak[j], "kds2"], w=["kd_all"])
                    av[j] = nxt()
                    p.op("tensor", lambda e, a=av[j], ts=ts: e.transpose(out=pool[a][:, 0:128], in_=vn[:, ts], identity=c.ident[:]),
                         r=[cvk[2]], w=["gp%d" % av[j]])
                    p.op("vector", lambda e, a=av[j], t=t, h=h, j=j: e.tensor_scalar(out=vbe[j][:], in0=pool[a][:, 0:128],
                                                                                   scalar1=beta[:, t, h:h + 1], scalar2=None, op0=ALU.mult),
                         r=["gp%d" % av[j], "beta"], w=["vbe%d" % j])
                    p.op("gpsimd", lambda e, t=t, h=h, j=j: e.tensor_scalar(out=Ag[j][:], in0=USTR, scalar1=gg[:, t, h:h + 1], scalar2=None,
                                                                            op0=ALU.mult), r=["G", "gg"], w=["Ag%d" % j])
                for j, t in enumerate(tl):
                    ts = slice(t * 128, (t + 1) * 128)
                    akk[j] = nxt()
                    p.op("tensor", lambda e, a=akk[j], ts=ts: e.matmul(pool[a][:, 0:128], lhsT=kn[:, ts], rhs=kn[:, ts], start=True, stop=True),
                         r=[cvk[1]], w=["gp%d" % akk[j]])
                    agd[j] = nxt()
                    p.op("tensor", lambda e, a=agd[j], j=j: e.matmul(pool[a][:, 0:128], lhsT=TRI, rhs=Ag[j][:], start=True, stop=False),
                         r=["G", "Ag%d" % j], w=["gp%d" % agd[j]])
                    p.op("tensor", lambda e, a=agd[j]: e.matmul(pool[a][:, 0:128], lhsT=c.ident[:], rhs=NMS, start=False, stop=True),
                         r=["G"], w=["gp%d" % agd[j]])
                    p.op("scalar", lambda e, a=agd[j], j=j: e.activation(out=Dec[j][:], in_=pool[a][:, 0:128], func=AF.Exp),
                         r=["gp%d" % agd[j]], w=["Dec%d" % j])
                    p.op("vector", lambda e, a=akk[j], t=t, h=h, j=j: e.scalar_tensor_tensor(out=Am[j][0][:], in0=pool[a][:, 0:128],
                                                                                           scalar=nbeta[:, t, h:h + 1], in1=Dec[j][:],
                                                                                           op0=ALU.mult, op1=ALU.mult),
                         r=["gp%d" % akk[j], "nbeta", "Dec%d" % j], w=["Am%d_0" % j])
                for j, t in enumerate(tl):
                    ts = slice(t * 128, (t + 1) * 128)
                    aqk[j] = nxt()
                    p.op("tensor", lambda e, a=aqk[j], ts=ts: e.matmul(pool[a][:, 0:128], lhsT=kn[:, ts], rhs=qn[:, ts], start=True, stop=True),
                         r=[cvk[1], cvk[0]], w=["gp%d" % aqk[j]])
                    agt[j] = nxt()
                    p.op("tensor", lambda e, a=agt[j], j=j: e.matmul(pool[a][:, 0:128], lhsT=Ag[j][:], rhs=TRI, start=True, stop=False),
                         r=["G", "Ag%d" % j], w=["gp%d" % agt[j]])
                    p.op("tensor", lambda e, a=agt[j]: e.matmul(pool[a][:, 0:128], lhsT=c.ident[:], rhs=NMIT, start=False, stop=True),
                         r=["G"], w=["gp%d" % agt[j]])
                    p.op("scalar", lambda e, a=agt[j], j=j: e.activation(out=DecT[j][:], in_=pool[a][:, 0:128], func=AF.Exp),
                         r=["gp%d" % agt[j]], w=["DecT%d" % j])
                    p.op("vector", lambda e, a=aqk[j], t=t, j=j: e.scalar_tensor_tensor(out=aT_all[:, t, :], in0=pool[a][:, 0:128],
                                                                                      scalar=128.0 ** -0.5, in1=DecT[j][:],
                                                                                      op0=ALU.mult, op1=ALU.mult),
                         r=["gp%d" % aqk[j], "DecT%d" % j], w=["aT_all"])
                for j, t in enumerate(tl):
                    amt[j] = nxt()
                    p.op("tensor", lambda e, a=amt[j], j=j: e.transpose(out=pool[a][:, 0:128], in_=Am[j][0][:], identity=c.ident[:]),
                         r=["Am%d_0" % j], w=["gp%d" % amt[j]])
                    p.op("scalar", lambda e, a=amt[j], j=j: e.copy(out=At[j][0][:], in_=pool[a][:, 0:128]),
                         r=["gp%d" % amt[j]], w=["At%d_0" % j])
                    p.op("gpsimd", lambda e, j=j: e.tensor_tensor(out=Rt[j][0][:], in0=At[j][0][:], in1=c.ident[:], op=ALU.add),
                         r=["At%d_0" % j], w=["Rt%d_0" % j])
                cur = 0
                for m in range(1, 6):
                    nx = 1 - cur
                    for j, t in enumerate(tl):
                        a1 = nxt()
                        p.op("tensor", lambda e, a=a1, cur=cur, j=j: e.matmul(pool[a][:, 0:128], lhsT=At[j][cur][:], rhs=Am[j][cur][:],
                                                                              start=True, stop=True),
                             r=["At%d_%d" % (j, cur), "Am%d_%d" % (j, cur)], w=["gp%d" % a1])
                        p.op("scalar", lambda e, a=a1, nx=nx, j=j: e.copy(out=Am[j][nx][:], in_=pool[a][:, 0:128]),
                             r=["gp%d" % a1], w=["Am%d_%d" % (j, nx)])
                        if m < 5:
                            a2 = nxt()
                            p.op("tensor", lambda e, a=a2, cur=cur, j=j: e.matmul(pool[a][:, 0:128], lhsT=Am[j][cur][:], rhs=At[j][cur][:],
                                                                                  start=True, stop=True),
                                 r=["At%d_%d" % (j, cur), "Am%d_%d" % (j, cur)], w=["gp%d" % a2])
                            p.op("vector", lambda e, a=a2, nx=nx, j=j: e.tensor_copy(out=At[j][nx][:], in_=pool[a][:, 0:128]),
                                 r=["gp%d" % a2], w=["At%d_%d" % (j, nx)])
                    for j, t in enumerate(tl):
                        a3 = nxt()
                        p.op("tensor", lambda e, a=a3, cur=cur, nx=nx, j=j: e.matmul(pool[a][:, 0:128], lhsT=Am[j][nx][:], rhs=Rt[j][cur][:],
                                                                                     start=True, stop=True),
                             r=["Am%d_%d" % (j, nx), "Rt%d_%d" % (j, cur)], w=["gp%d" % a3])
                        p.op("vector", lambda e, a=a3, cur=cur, nx=nx, j=j: e.tensor_tensor(out=Rt[j][nx][:], in0=pool[a][:, 0:128],
                                                                                            in1=Rt[j][cur][:], op=ALU.add),
                             r=["gp%d" % a3, "Rt%d_%d" % (j, cur)], w=["Rt%d_%d" % (j, nx)])
                    cur = nx
                for j, t in enumerate(tl):
                    RtF = Rt[j][cur]
                    a_u = nxt()
                    p.op("tensor", lambda e, a=a_u, RtF=RtF, j=j: e.matmul(pool[a][:, 0:128], lhsT=RtF[:], rhs=vbe[j][:], start=True, stop=True),
                         r=["Rt%d_%d" % (j, cur), "vbe%d" % j], w=["gp%d" % a_u])
                    p.op("scalar", lambda e, a=a_u, t=t: e.copy(out=u_all[:, t, :], in_=pool[a][:, 0:128]), r=["gp%d" % a_u], w=["u_all"])
                    a_w = nxt()
                    p.op("tensor", lambda e, a=a_w, RtF=RtF, j=j: e.matmul(pool[a][:, 0:128], lhsT=kbg[j][:], rhs=RtF[:], start=True, stop=True),
                         r=["Rt%d_%d" % (j, cur), "kbg%d" % j], w=["gp%d" % a_w])
                    p.op("vector", lambda e, a=a_w, t=t: e.tensor_copy(out=wT_all[:, t, :], in_=pool[a][:, 0:128]),
                         r=["gp%d" % a_w], w=["wT_all"])
            p.op("vector", lambda e: e.memset(St[:], 0.0), w=["St"])
            for ch in range(32):
                pump(2)
                t = ch // 2
                hf = ch % 2
                rows = slice(hf * 64, hf * 64 + 64)
                ts = slice(t * 128, (t + 1) * 128)
                a1 = nxt()
                p.op("tensor", lambda e, a=a1, t=t: e.matmul(pool[a][:, 0:128], lhsT=wT_all[:, t, :], rhs=St[:], start=True, stop=True),
                     r=["wT_all", "St"], w=["gp%d" % a1])
                p.op("vector", lambda e, a=a1, t=t, rows=rows: e.tensor_tensor(out=vnew[rows, :], in0=u_all[rows, t, :],
                                                                               in1=pool[a][rows, 0:128], op=ALU.subtract),
                     r=["gp%d" % a1, "u_all"], w=["vnew"])
                aA = nxt()
                p.op("tensor", lambda e, a=aA, ts=ts: e.matmul(pool[a][:, 0:128], lhsT=qn[:, ts], rhs=St[:], start=True, stop=True),
                     r=[cvk[0], "St"], w=["gp%d" % aA])
                aB = nxt()
                p.op("tensor", lambda e, a=aB, t=t: e.matmul(pool[a][:, 0:128], lhsT=aT_all[:, t, :], rhs=vnew[:], start=True, stop=True),
                     r=["aT_all", "vnew"], w=["gp%d" % aB])
                aS = nxt()
                p.op("tensor", lambda e, a=aS, t=t, hf=hf: e.matmul(pool[a][:, 0:128], lhsT=kd_all[:, hf, t, :], rhs=vnew[:],
                                                                    start=True, stop=True),
                     r=["kd_all", "vnew"], w=["gp%d" % aS])
                p.op("scalar", lambda e, a=aA, t=t, h=h, rows=rows: e.activation(out=otmp[rows, :], in_=pool[a][rows, 0:128], func=AF.Identity,
                                                                                 scale=egs[rows, t, h:h + 1]),
                     r=["gp%d" % aA, "egs"], w=["otmp"])
                p.op("vector", lambda e, a=aB, t=t, rows=rows: e.tensor_tensor(out=ob[rows, t, :], in0=otmp[rows, :], in1=pool[a][rows, 0:128],
                                                                               op=ALU.add),
                     r=["gp%d" % aB, "otmp"], w=["ob"])
                p.op("vector", lambda e, a=aS, t=t, hf=hf, h=h: e.scalar_tensor_tensor(out=St[:], in0=St[:], scalar=egl[:, hf, t, h:h + 1],
                                                                                       in1=pool[a][:, 0:128], op0=ALU.mult, op1=ALU.add),
                     r=["gp%d" % aS, "St", "egl"], w=["St"])
            for t in range(16):
                p.op("scalar", lambda e, t=t: e.activation(out=junk[:], in_=ob[:, t, :], func=AF.Square, accum_out=ssq[:, t:t + 1]),
                     r=["ob"], w=["junk", "ssq"])
            p.op("scalar", lambda e: e.activation(out=ssq[:], in_=ssq[:], func=AF.Sqrt, scale=1.0 / 128, bias=c.eps_t[:, 0:1]),
                 r=["ssq"], w=["ssq"])
            p.op("vector", lambda e: e.reciprocal(out=ssq[:], in_=ssq[:]), r=["ssq"], w=["ssq"])
            p.op("scalar", lambda e: e.activation(out=zt[:], in_=zt[:], func=AF.Silu), r=["zt"], w=["zt"])
            p.op("vector", lambda e: e.tensor_tensor(out=ob[:], in0=ob[:], in1=ssq[:].unsqueeze(2).to_broadcast([128, 16, 128]), op=ALU.mult),
                 r=["ob", "ssq"], w=["ob"])
            p.op("vector", lambda e: e.tensor_tensor(out=ob[:], in0=ob[:], in1=gng[:].unsqueeze(1).to_broadcast([128, 16, 128]), op=ALU.mult),
                 r=["ob", "gng"], w=["ob"])
            p.op("vector", lambda e: e.tensor_tensor(out=ob[:], in0=ob[:], in1=zt[:], op=ALU.mult), r=["ob", "zt"], w=["ob"])
            for t in range(16):
                a = nxt()
                p.op("tensor", lambda e, a=a, t=t: e.transpose(out=pool[a][:, 0:128], in_=ob[:, t, :], identity=c.ident[:]),
                     r=["ob"], w=["gp%d" % a])
                p.op("scalar", lambda e, a=a, t=t: e.copy(out=obT[:, t * 128:(t + 1) * 128], in_=pool[a][:, 0:128]),
                     r=["gp%d" % a], w=["obT"])
            p.dma("sync", c.sc_ob[h * 128:(h + 1) * 128, :], obT[:], r=["obT"], w=[("sc_ob", h)])

        conv(0, p)
        for h in range(8):
            rec = _Rec()
            if h + 1 < 8:
                conv(h + 1, rec)
            pend_c = rec.l

            def pump(n, pend_c=pend_c):
                k = 0
                while pend_c and k < n:
                    kind, _, a, kw = pend_c.pop(0)
                    getattr(p, kind)(*a, **kw)
                    k += 1

            head_rest(h, pump)
            pump(100000)
        p.barrier()


def phase_merge(c, p):
    nc = c.nc
    with ExitStack() as es:
        c2 = Ctx(); c2.nc = nc; c2.es = es
        oa = sb(c2, "oa", [128, 8, 512], F32R)
        obb = sb(c2, "obb", [128, 8, 512], F32R)
        wa = [sb(c2, "wa%d" % i, [128, 8, 128], F32R) for i in range(2)]
        wbb = [sb(c2, "wbb%d" % i, [128, 8, 128], F32R) for i in range(2)]
        ga = [sb(c2, "ga%d" % i, [128, 512]) for i in range(2)]
        gb_ = [sb(c2, "gb%d" % i, [128, 512]) for i in range(2)]
        m1 = sb(c2, "m1", [128, 512])
        mT = sb(c2, "mT", [128, 16, 512], F32R)
        wo = [sb(c2, "wo%d" % i, [128, 16, 512], F32R) for i in range(2)]
        xt = [sb(c2, "xt%d" % i, [128, 512]) for i in range(2)]
        pa = [ps(c2, "pa%d" % i, [128, 512]) for i in range(2)]
        pb = [ps(c2, "pb%d" % i, [128, 512]) for i in range(2)]
        po = [ps(c2, "po%d" % i, [128, 512]) for i in range(4)]
        ci = 0
        wi = 0
        oi = 0
        for tg in range(4):
            tsl = slice(tg * 512, (tg + 1) * 512)
            p.dma("sync", oa[:], r32(c.sc_oa[:, tsl]).rearrange("(kc p) t -> p kc t", p=128), w=["oa"])
            p.dma("sync", obb[:], r32(c.sc_ob[:, tsl]).rearrange("(kc p) t -> p kc t", p=128), w=["obb"])
            for cc in range(16):
                b = ci % 2
                ci += 1
                csl = slice(cc * 128, (cc + 1) * 128)
                p.dma("sync", wa[b][:], r32(c.w_up_a[:, csl]).rearrange("(kc p) n -> p kc n", p=128), w=["wa%d" % b])
                p.dma("sync", wbb[b][:], r32(c.w_up_b[:, csl]).rearrange("(kc p) n -> p kc n", p=128), w=["wbb%d" % b])
                p.dma("sync", ga[b][:], c.sc_fm[5120 + cc * 128:5120 + (cc + 1) * 128, tsl], w=["ga%d" % b])
                p.dma("sync", gb_[b][:], c.sc_fm[7168 + cc * 128:7168 + (cc + 1) * 128, tsl], w=["gb%d" % b])
                for kc in range(8):
                    p.op("tensor", lambda e, b=b, kc=kc: e.matmul(pa[b][:], lhsT=wa[b][:, kc, :], rhs=oa[:, kc, :],
                                                                  start=(kc == 0), stop=(kc == 7)),
                         r=["wa%d" % b, "oa"], w=["pa%d" % b])
                for kc in range(8):
                    p.op("tensor", lambda e, b=b, kc=kc: e.matmul(pb[b][:], lhsT=wbb[b][:, kc, :], rhs=obb[:, kc, :],
                                                                  start=(kc == 0), stop=(kc == 7)),
                         r=["wbb%d" % b, "obb"], w=["pb%d" % b])
                p.op("scalar", lambda e, b=b: e.activation(out=ga[b][:], in_=ga[b][:], func=AF.Sigmoid), r=["ga%d" % b], w=["ga%d" % b])
                p.op("scalar", lambda e, b=b: e.activation(out=gb_[b][:], in_=gb_[b][:], func=AF.Sigmoid), r=["gb%d" % b], w=["gb%d" % b])
                p.op("vector", lambda e, b=b: e.tensor_tensor(out=m1[:], in0=pa[b][:], in1=ga[b][:], op=ALU.mult),
                     r=["pa%d" % b, "ga%d" % b], w=["m1"])
                p.op("vector", lambda e, b=b: e.tensor_tensor(out=gb_[b][:], in0=pb[b][:], in1=gb_[b][:], op=ALU.mult),
                     r=["pb%d" % b, "gb%d" % b], w=["gb%d" % b])
                p.op("vector", lambda e, b=b, cc=cc: e.tensor_tensor(out=mT[:, cc, :], in0=m1[:], in1=gb_[b][:], op=ALU.add),
                     r=["m1", "gb%d" % b], w=["mT"])
            for dg in range(4):
                wb_ = wi % 2
                wi += 1
                dsl = slice(dg * 512, (dg + 1) * 512)
                p.dma("sync", wo[wb_][:], r32(c.w_out[:, dsl]).rearrange("(kc p) n -> p kc n", p=128), w=["wo%d" % wb_])
                for tt in range(4):
                    o = oi % 4
                    oi += 1
                    x_ = oi % 2
                    t0 = tg * 512 + tt * 128
                    p.dma("sync", xt[x_][:], c.x[t0:t0 + 128, dsl], w=["xt%d" % x_])
                    for cc in range(16):
                        p.op("tensor", lambda e, o=o, cc=cc, tt=tt, wb_=wb_: e.matmul(
                            po[o][:], lhsT=mT[:, cc, tt * 128:(tt + 1) * 128], rhs=wo[wb_][:, cc, :],
                            start=(cc == 0), stop=(cc == 15)), r=["mT", "wo%d" % wb_], w=["po%d" % o])
                    p.op("vector", lambda e, o=o, x_=x_: e.tensor_tensor(out=xt[x_][:], in0=po[o][:], in1=xt[x_][:], op=ALU.add),
                         r=["po%d" % o, "xt%d" % x_], w=["xt%d" % x_])
                    p.dma("sync", c.sc_x1[t0:t0 + 128, dsl], xt[x_][:], r=["xt%d" % x_], w=[("sc_x1", t0, dg)])
        p.barrier()


def cvt_gen(c, p, cin, cout, rpp):
    k = 0
    nrow = 128 * rpp
    n = rpp * D
    hn = n // 2
    for (src, dst) in ((c.peer_u, c.uvb[:, 0:D]), (c.peer_v, c.uvb[:, D:2 * D])):
        for ch in range(16384 // nrow):
            b = k % 2
            k += 1
            rows = slice(ch * nrow, (ch + 1) * nrow)
            p.dma("sync", cin[b][:], src[rows, :].rearrange("(p r) d -> p (r d)", r=rpp), w=["cin%d" % b])
            p.op("scalar", lambda e, b=b: e.copy(out=cout[b][:, 0:hn], in_=cin[b][:, 0:hn]), r=["cin%d" % b], w=["coutA%d" % b])
            p.op("vector", lambda e, b=b: e.tensor_copy(out=cout[b][:, hn:n], in_=cin[b][:, hn:n]),
                 r=["cin%d" % b], w=["coutB%d" % b])
            p.dma("sync", dst[rows, :].rearrange("(p r) d -> p r d", r=rpp), cout[b][:].rearrange("p (r d) -> p r d", r=rpp),
                  r=["coutA%d" % b, "coutB%d" % b], w=[("tb", k)])
            yield


def phase_peer_cvt(c, p):
    nc = c.nc
    with ExitStack() as es:
        c2 = Ctx(); c2.nc = nc; c2.es = es
        cin = [sb(c2, "cin%d" % i, [128, 8192]) for i in range(2)]
        cout = [sb(c2, "cout%d" % i, [128, 8192], BF16) for i in range(2)]
        for _ in cvt_gen(c, p, cin, cout, 4):
            pass
        p.barrier()


def phase_peer(c, p):
    nc = c.nc
    if not getattr(c, "cvt_done", False):
        phase_peer_cvt(c, p)
    for half in range(2):
        with ExitStack() as es:
            c2 = Ctx(); c2.nc = nc; c2.es = es
            hT = sb(c2, "h2T", [128, 16, 1024], F32R)
            phase_norm_T(c, p, c.sc_x1, c.norm2_gain, hT, "n2_", half, h_out=c.sc_h2)
            with ExitStack() as es2:
                c3 = Ctx(); c3.nc = nc; c3.es = es2
                wq = [sb(c3, "wq%d" % i, [128, 16, 128], F32R) for i in range(2)]
                skT = sb(c3, "skT", [128, 16, 128], F32R)
                qT = [sb(c3, "qT%d" % i, [128, 1024], F32R) for i in range(2)]
                pq = [ps(c3, "pq%d" % i, [128, 512]) for i in range(4)]
                so_t = [sb(c3, "so_t%d" % i, [128, 512]) for i in range(2)]
                pqi = 0
                p.dma("sync", skT[:], r32(c.skT_d[:, :, :]), w=["skT"])
                for ch in range(16):
                    b = ch % 2
                    p.dma("sync", wq[b][:], r32(c.w_query[:, ch * 128:(ch + 1) * 128]).rearrange("(kc p) n -> p kc n", p=128),
                          w=["wq%d" % b])
                    for tg in range(2):
                        a = pqi % 4
                        pqi += 1
                        for kc in range(16):
                            p.op("tensor", lambda e, a=a, b=b, kc=kc, tg=tg: e.matmul(
                                pq[a][:], lhsT=wq[b][:, kc, :], rhs=hT[:, kc, tg * 512:(tg + 1) * 512],
                                start=(kc == 0), stop=(kc == 15)), r=["wq%d" % b, "hT"], w=["pq%d" % a])
                        p.op("scalar", lambda e, a=a, b=b, tg=tg: e.copy(out=qT[b][:, tg * 512:(tg + 1) * 512], in_=pq[a][:]),
                             r=["pq%d" % a], w=["qT%d" % b])
                    for tq in range(2):
                        a = pqi % 4
                        pqi += 1
                        for tt in range(4):
                            tl = tq * 4 + tt
                            p.op("tensor", lambda e, a=a, b=b, tl=tl, tt=tt, ch=ch: e.matmul(
                                pq[a][:, tt * 128:(tt + 1) * 128], lhsT=qT[b][:, tl * 128:(tl + 1) * 128], rhs=skT[:, ch, :],
                                start=True, stop=True), r=["qT%d" % b, "skT"], w=["pq%d" % a])
                        so = "so%d" % (pqi % 2)
                        sot = so_t[pqi % 2]
                        p.op("vector", lambda e, a=a, sot=sot: e.tensor_copy(out=sot[:], in_=pq[a][:]), r=["pq%d" % a], w=[so])
                        tb = half * 1024 + tq * 512
                        p.dma("sync", c.sc_sc[tb:tb + 512, ch * 128:(ch + 1) * 128].rearrange("(tt p) k -> p tt k", p=128),
                              sot[:].rearrange("p (tt k) -> p tt k", k=128), r=[so], w=[("sc_sc", tb, ch)])
                p.barrier()
    with ExitStack() as es:
        c2 = Ctx(); c2.nc = nc; c2.es = es
        sc = sb(c2, "sc", [128, 16, 128])
        wk = sb(c2, "wk", [128, 16, 128])
        stop_ = sb(c2, "stop", [128, 16, 16])
        itop = sb(c2, "itop", [128, 16, 16], U32)
        itf = sb(c2, "itf", [128, 16, 16])
        cand = sb(c2, "cand", [128, 8, 16, 16])
        cidx = sb(c2, "cidx", [128, 8, 16, 16])
        wk2 = sb(c2, "wk2", [128, 8, 256])
        junk2s = [sb(c2, "junk2s%d" % i, [128, 256]) for i in range(4)]
        best = sb(c2, "best", [128, 8, 16])
        pos = sb(c2, "pos", [128, 8, 16], U32)
        posf = sb(c2, "posf", [128, 8, 16])
        iota = sb(c2, "iota", [128, 256])
        junk2 = sb(c2, "junk2", [128, 256])
        eidf = sb(c2, "eidf", [128, 128])
        eid = sb(c2, "eid", [128, 128], U32)
        nmx = sb(c2, "nmx", [128, 8])
        gsum = sb(c2, "gsum", [128, 8])
        gate = sb(c2, "gate", [128, 8, 16])
        dots = sb(c2, "dots", [128, 128])
        gact = sb(c2, "gact", [128, 128])
        h2 = sb(c2, "h2", [128, D])
        NB = 8
        gu = [sb(c2, "gu%d" % i, [128, 2 * D], BF16) for i in range(NB)]
        gel = sb(c2, "gel", [128, 128])
        diag = [sb(c2, "diag%d" % i, [128, 128], BF16) for i in range(2)]
        po = [ps(c2, "po%d" % i, [128, 512]) for i in range(4)]
        junk = sb(c2, "junkp", [128, D])
        acc = sb(c2, "acc", [128, D])
        x1 = sb(c2, "x1", [128, D])
        p.dma("sync", iota[:], c.iota_d[:, :], w=["iota"])
        gi = 0
        eids = [eid, sb(c2, "eidB", [128, 128], U32)]
        gates = [gate, sb(c2, "gateB", [128, 8, 16])]
        h2s = [h2, sb(c2, "h2B", [128, D])]

        class _Rec:
            def __init__(self):
                self.l = []

            def op(self, eng, fn, r=(), w=()):
                self.l.append(("op", eng, (eng, fn), dict(r=r, w=w)))

            def dma(self, q, out, in_, r=(), w=(), **kw):
                self.l.append(("dma", "dma", (q, out, in_), dict(r=r, w=w, **kw)))

        def routing(t, P):
            tb = t % 2
            eid = eids[tb]
            gate = gates[tb]
            h2 = h2s[tb]
            t0 = t * 128
            P.dma("sync", sc[:], c.sc_sc[t0:t0 + 128, :].rearrange("p (c k) -> p c k", k=128), w=["sc"])
            P.dma("sync", h2[:], c.sc_h2[t0:t0 + 128, :], w=["h2%d" % tb])
            CH = range(16)
            HH = range(8)
            for ch in CH:
                P.op("vector", lambda e, ch=ch: e.max(out=stop_[:, ch, 0:8], in_=sc[:, ch, :]), r=["sc"], w=[("stop", ch)])
            for ch in CH:
                P.op("vector", lambda e, ch=ch: e.max_index(out=itop[:, ch, 0:8], in_max=stop_[:, ch, 0:8], in_values=sc[:, ch, :]),
                     r=["sc", ("stop", ch)], w=[("itop", ch)])
            for ch in CH:
                P.op("vector", lambda e, ch=ch: e.match_replace(out=wk[:, ch, :], in_to_replace=stop_[:, ch, 0:8], in_values=sc[:, ch, :],
                                                                imm_value=-1e30), r=["sc", ("stop", ch)], w=[("wk", ch)])
            for ch in CH:
                P.op("vector", lambda e, ch=ch: e.max(out=stop_[:, ch, 8:16], in_=wk[:, ch, :]), r=[("wk", ch)], w=[("stop", ch)])
            for ch in CH:
                P.op("vector", lambda e, ch=ch: e.max_index(out=itop[:, ch, 8:16], in_max=stop_[:, ch, 8:16], in_values=wk[:, ch, :]),
                     r=[("wk", ch), ("stop", ch)], w=[("itop", ch)])
            P.op("vector", lambda e: e.tensor_copy(out=itf[:], in_=itop[:]), r=[("itop", ch) for ch in CH], w=["itf"])
            s4 = stop_[:].rearrange("p (h two) k -> p h two k", two=2)
            i4 = itf[:].rearrange("p (h two) k -> p h two k", two=2)
            P.op("vector", lambda e, s4=s4: e.tensor_tensor(out=cand[:], in0=s4[:, :, 0, :].unsqueeze(3).to_broadcast([128, 8, 16, 16]),
                                                            in1=s4[:, :, 1, :].unsqueeze(2).to_broadcast([128, 8, 16, 16]), op=ALU.add),
                 r=[("stop", ch) for ch in CH], w=["cand"])
            for hh in HH:
                P.op("vector", lambda e, i4=i4, hh=hh: e.scalar_tensor_tensor(
                    out=cidx[:, hh], in0=i4[:, hh, 0, :].unsqueeze(2).to_broadcast([128, 16, 16]), scalar=128.0,
                    in1=i4[:, hh, 1, :].unsqueeze(1).to_broadcast([128, 16, 16]), op0=ALU.mult, op1=ALU.add),
                    r=["itf"], w=[("cidx", hh)])
            cvs = [cand[:, hh].rearrange("p a b -> p (a b)") for hh in HH]
            for hh in HH:
                P.op("vector", lambda e, hh=hh: e.max(out=best[:, hh, 0:8], in_=cvs[hh]), r=["cand"], w=[("best", hh)])
            for hh in HH:
                P.op("vector", lambda e, hh=hh: e.max_index(out=pos[:, hh, 0:8], in_max=best[:, hh, 0:8], in_values=cvs[hh]),
                     r=["cand", ("best", hh)], w=[("pos", hh)])
            for hh in HH:
                P.op("vector", lambda e, hh=hh: e.match_replace(out=wk2[:, hh, :], in_to_replace=best[:, hh, 0:8], in_values=cvs[hh],
                                                                imm_value=-1e30), r=["cand", ("best", hh)], w=[("wk2", hh)])
            for hh in HH:
                P.op("vector", lambda e, hh=hh: e.max(out=best[:, hh, 8:16], in_=wk2[:, hh, :]), r=[("wk2", hh)], w=[("best", hh)])
            for hh in HH:
                P.op("vector", lambda e, hh=hh: e.max_index(out=pos[:, hh, 8:16], in_max=best[:, hh, 8:16], in_values=wk2[:, hh, :]),
                     r=[("wk2", hh), ("best", hh)], w=[("pos", hh)])
            P.op("vector", lambda e: e.tensor_copy(out=posf[:], in_=pos[:]), r=[("pos", hh) for hh in HH], w=["posf"])
            P.op("vector", lambda e: e.tensor_scalar(out=nmx[:], in0=best[:, :, 0], scalar1=-1.0, scalar2=None, op0=ALU.mult),
                 r=[("best", hh) for hh in HH], w=["nmx"])
            for hh in HH:
                P.op("scalar", lambda e, hh=hh: e.activation(out=gate[:, hh, :], in_=best[:, hh, :], func=AF.Exp, bias=nmx[:, hh:hh + 1],
                                                             accum_out=gsum[:, hh:hh + 1]), r=[("best", hh), "nmx"],
                     w=[("gate", tb, hh), ("gsum", hh)])
            for hh in HH:
                ci_ = cidx[:, hh].rearrange("p a b -> p (a b)")
                for m in range(16):
                    jb = junk2s[(hh * 16 + m) % 4]
                    P.op("vector", lambda e, hh=hh, m=m, ci_=ci_, jb=jb: e.scalar_tensor_tensor(
                        out=jb[:], in0=iota[:], scalar=posf[:, hh, m:m + 1], in1=ci_, op0=ALU.is_equal, op1=ALU.mult,
                        accum_out=eidf[:, hh * 16 + m:hh * 16 + m + 1]), r=["iota", "posf", ("cidx", hh)], w=[("eidf", hh * 16 + m)])
            P.op("vector", lambda e: e.tensor_copy(out=eid[:], in_=eidf[:]), r=[("eidf", i) for i in range(128)], w=["eid%d" % tb])
            P.op("vector", lambda e: e.reciprocal(out=gsum[:], in_=gsum[:]), r=[("gsum", hh) for hh in HH], w=["gsum"])
            P.op("vector", lambda e: e.tensor_tensor(out=gate[:], in0=gate[:], in1=gsum[:].unsqueeze(2).to_broadcast([128, 8, 16]), op=ALU.mult),
                 r=[("gate", tb, hh) for hh in HH] + ["gsum"], w=["gate%d" % tb])

        routing(0, p)
        for t in range(16):
            t0 = t * 128
            tb = t % 2
            eid = eids[tb]
            gate = gates[tb]
            h2 = h2s[tb]
            p.dma("sync", x1[:], c.sc_x1[t0:t0 + 128, :], w=["x1"])
            rec = _Rec()
            if t + 1 < 16:
                routing(t + 1, rec)
            pendr = rec.l

            def pump(n):
                k = 0
                while pendr and (k < n or pendr[0][1] == "scalar"):
                    kind, _, a, kw = pendr.pop(0)
                    getattr(p, kind)(*a, **kw)
                    k += 1
            gatef = gate[:].rearrange("p h k -> p (h k)")
            pend = []

            def emit_up(args, gatef=gatef):
                b, db, s_ = args
                p.op("vector", lambda e: e.tensor_scalar(out=diag[db][:], in0=c.ident[:], scalar1=gel[:, s_:s_ + 1],
                                                         scalar2=gatef[:, s_:s_ + 1], op0=ALU.mult, op1=ALU.mult),
                     r=[("gel", s_), "gate%d" % tb], w=["diag%d" % db])
                for dg in range(4):
                    p.op("tensor", lambda e, dg=dg: e.matmul(
                        po[dg][:], lhsT=diag[db][:], rhs=gu[b][:, D + dg * 512:D + (dg + 1) * 512], start=(s_ == 0), stop=(s_ == 127)),
                        r=["diag%d" % db, "gu%d" % b], w=["po%d" % dg])

            for s_ in range(128):
                b = gi % NB
                gi += 1
                db = s_ % 2
                p.dma_fn("gpsimd", lambda e, b=b, s_=s_, eid=eid: e.indirect_dma_start(
                    out=gu[b][:], out_offset=None, in_=c.uvb[:, :],
                    in_offset=bass.IndirectOffsetOnAxis(ap=eid[:, s_:s_ + 1], axis=0)), r=["eid%d" % tb], w=["gu%d" % b])
                p.op("vector", lambda e, b=b, s_=s_, h2=h2: e.scalar_tensor_tensor(out=junk[:], in0=gu[b][:, 0:D], scalar=1.0, in1=h2[:],
                                                                            op0=ALU.mult, op1=ALU.mult, accum_out=dots[:, s_:s_ + 1]),
                     r=["gu%d" % b, "h2%d" % tb], w=[("dots", s_)])
                p.op("scalar", lambda e, s_=s_: e.activation(out=gel[:, s_:s_ + 1], in_=dots[:, s_:s_ + 1], func=AF.Gelu),
                     r=[("dots", s_)], w=[("gel", s_)])
                pend.append((b, db, s_))
                if len(pend) > 1:
                    emit_up(pend.pop(0))
                pump(3)
            while pend:
                emit_up(pend.pop(0))
            pump(100000)
            for dg in range(4):
                dsl = slice(dg * 512, (dg + 1) * 512)
                p.op("vector", lambda e, dg=dg, dsl=dsl: e.tensor_tensor(out=acc[:, dsl], in0=po[dg][:], in1=x1[:, dsl], op=ALU.add),
                     r=["po%d" % dg, "x1"], w=["acc"])
            p.dma("sync", c.y[t0:t0 + 128, :], acc[:], r=["acc"], w=[("y", t)])
        p.barrier()


ALL_PHASES = ("inproj", "moba", "gdn", "merge", "peer")


def build_nc(debug=False, phases=ALL_PHASES):
    nc = bass.Bass("TRN2", target_bir_lowering=False)
    nc.dge_precook = False
    c = Ctx()
    c.nc = nc
    kind_s = "ExternalOutput" if debug else "Internal"

    def din(name, shape, dt=F32):
        return nc.dram_tensor(name, list(shape), dt, kind="ExternalInput").ap()

    def dsc(name, shape):
        return nc.dram_tensor(name, list(shape), F32, kind=kind_s).ap()

    c.x = din("x", [S, D])
    c.norm1_gain = din("norm1_gain", [1, D])
    c.w_in = din("w_in", [D, IN_TOTAL])
    c.ident_d = din("ident", [128, 128])
    c.ones_d = din("ones", [128, 128])
    c.rel_bias = din("rel_bias", [32, 8])
    c.q_norm_gain = din("q_norm_gain", [1, 128])
    c.k_norm_gain = din("k_norm_gain", [1, 128])
    c.d01_d = din("d01", [8, 2, 128, 128])
    c.cm_d = din("cm", [128, 16, 8])
    c.notown_d = din("notown", [128, 16, 8])
    c.esel_d = din("esel", [8, 8, 128])
    c.gdnc_d = din("gdnc", [7, 128, 128])
    c.conv_wT = din("conv_wT", [3072, 4])
    c.a_log = din("a_log", [1, 8])
    c.dt_bias = din("dt_bias", [1, 8])
    c.gdn_norm_gain = din("gdn_norm_gain", [1, 128])
    c.w_up_a = din("w_up_a", [1024, D])
    c.w_up_b = din("w_up_b", [1024, D])
    c.w_out = din("w_out", [D, D])
    if "peer" in phases:
        c.norm2_gain = din("norm2_gain", [1, D])
        c.w_query = din("w_query", [D, D])
        c.skT_d = din("skT", [128, 16, 128])
        c.peer_u = din("peer_u", [16384, D])
        c.peer_v = din("peer_v", [16384, D])
        c.iota_d = din("iota", [128, 256])
        c.sc_h2 = dsc("sc_h2", [S, D])
        c.uvb = nc.dram_tensor("peer_uvb", [16384, 2 * D], BF16, kind="Internal").ap()
        c.sc_sc = dsc("sc_sc", [S, D])
    c.sc_fm = dsc("sc_fm", [N_FM, S])
    c.sc_tm = dsc("sc_tm", [S, N_TM])
    c.sc_oa = dsc("sc_oa", [1024, S])
    c.sc_ob = dsc("sc_ob", [1024, S])
    if "merge" in phases or "peer" not in phases:
        c.sc_x1 = dsc("sc_x1", [S, D])
    else:
        c.sc_x1 = din("sc_x1", [S, D])
    c.y = nc.dram_tensor("y", [S, D], F32, kind="ExternalOutput").ap()

    with ExitStack() as es:
        c.es = es
        p = Prog(nc, es)
        block = es.enter_context(nc.Block())
        c.ident = sb(c, "ident_s", [128, 128])
        c.ident_r = sb(c, "ident_r", [128, 128], F32R)
        c.ones_r = sb(c, "ones_r", [128, 128], F32R)
        c.eps_t = sb(c, "eps_t", [128, 1])
        c.one_t = sb(c, "one_t", [128, 1])
        p.dma("sync", c.ident[:], c.ident_d[:, :], w=["ident"])
        p.dma("sync", c.ident_r[:], r32(c.ident_d[:, :]), w=["ident_r"])
        p.dma("sync", c.ones_r[:], r32(c.ones_d[:, :]), w=["ones_r"])
        p.op("vector", lambda e: e.memset(c.eps_t[:], EPS), w=["eps"])
        p.op("vector", lambda e: e.memset(c.one_t[:], 1.0), w=["one"])
        p.barrier()
        if "inproj" in phases:
            phase_inproj(c, p)
        if "moba" in phases:
            phase_moba(c, p)
        if "gdn" in phases:
            phase_gdn(c, p)
        if "merge" in phases:
            phase_merge(c, p)
        if "peer" in phases:
            phase_peer(c, p)
        p.finish(block)
    return nc


def make_in_maps(inputs, phases=ALL_PHASES):
    f = lambda a: np.ascontiguousarray(np.asarray(a, dtype=np.float32))
    rel_bias = f(inputs["rel_bias"])
    d01, cm, notown, esel = moba_consts(rel_bias)
    shared = {
        "norm1_gain": f(inputs["norm1_gain"]), "w_in": f(inputs["w_in"][0]),
        "ident": np.eye(128, dtype=np.float32), "ones": np.ones((128, 128), np.float32),
        "rel_bias": rel_bias, "q_norm_gain": f(inputs["q_norm_gain"]), "k_norm_gain": f(inputs["k_norm_gain"]),
        "d01": d01, "cm": cm, "notown": notown, "esel": esel, "gdnc": gdn_consts(),
        "conv_wT": f(np.asarray(inputs["conv_w"][0]).T), "a_log": f(inputs["a_log"]), "dt_bias": f(inputs["dt_bias"]),
        "gdn_norm_gain": f(inputs["gdn_norm_gain"]), "w_up_a": f(inputs["w_up_a"][0]), "w_up_b": f(inputs["w_up_b"][0]),
        "w_out": f(inputs["w_out"][0]),
    }
    if "peer" in phases:
        sk = np.asarray(inputs["peer_sub_keys"][0], dtype=np.float32).reshape(16, 128, 128)
        shared.update({
            "norm2_gain": f(inputs["norm2_gain"]), "w_query": f(inputs["peer_w_query"][0]),
            "skT": f(sk.transpose(2, 0, 1)), "peer_u": f(inputs["peer_u"][0]), "peer_v": f(inputs["peer_v"][0]),
            "iota": np.broadcast_to(np.arange(256, dtype=np.float32), (128, 256)).copy(),
        })
    maps = []
    for b in range(8):
        m = dict(shared)
        m["x"] = f(inputs["x"][b])
        maps.append(m)
    return maps


_NC_CACHE = {}


def kernel(**inputs):
    if "nc" not in _NC_CACHE:
        _NC_CACHE["nc"] = build_nc()
    nc = _NC_CACHE["nc"]
    maps = make_in_maps(inputs)
    res = run_bass_kernel_spmd(nc, maps, core_ids=list(range(8)))
    return np.stack([np.asarray(r["y"], dtype=np.float32) for r in res.results], axis=0)
```
